# Optimizing a Trainium2 kernel written in Bass

```python
import jax, jax.numpy as jnp
from jax import lax
import numpy as np

D_MODEL = 2048
BATCH = 2
SEQ = 8192
DEPTH = 4

HEAD_DIM = 128
N_HEADS_TOTAL = D_MODEL // HEAD_DIM
N_HEADS_MOBA = N_HEADS_TOTAL // 4
N_HEADS_NSA = N_HEADS_TOTAL // 4
N_HEADS_FOX = N_HEADS_TOTAL - N_HEADS_MOBA - N_HEADS_NSA
MOBA_WIDTH = N_HEADS_MOBA * HEAD_DIM
NSA_WIDTH = N_HEADS_NSA * HEAD_DIM
FOX_WIDTH = N_HEADS_FOX * HEAD_DIM
MIX_WIDTH = MOBA_WIDTH + NSA_WIDTH + FOX_WIDTH
D_FF = 4 * D_MODEL

ROPE_THETA = 500000.0
ROPE_DIMS = HEAD_DIM // 4
Q_BLOCK = 128
MOBA_Q_BLOCK = 64
MOBA_BLOCK = 256
MOBA_TOPK = 3
NSA_CMP_LEN = 32
NSA_CMP_STRIDE = 16
NSA_CMP_HIDDEN = 256
NSA_SEL_BLOCK = 64
NSA_N_SEL = 16
NSA_WINDOW = 512
NSA_N_BRANCH = 3
NSA_N_KV = 6
NORM_EPS = 1e-6
NEG_INF = -1e30

PROJ_SIZES = (3 * MOBA_WIDTH, NSA_WIDTH, NSA_N_KV * HEAD_DIM,
              N_HEADS_NSA * NSA_N_BRANCH, 3 * FOX_WIDTH, N_HEADS_FOX)
PROJ_DIM = sum(PROJ_SIZES)
PROJ_OFFSETS = tuple(int(o) for o in np.cumsum(PROJ_SIZES)[:-1])

kernel_name = "hymba_moba_nsa_fox_sandwich_adaln"


def rms_norm(x, g):
    xf = x.astype(jnp.float32)
    y = xf * lax.rsqrt(jnp.mean(xf * xf, axis=-1, keepdims=True) + NORM_EPS)
    return (y * g.astype(jnp.float32)).astype(x.dtype)


def modulate(h, shift, scale):
    return h * (1.0 + scale) + shift


def rope_partial(x, positions):
    half = ROPE_DIMS // 2
    inv_freq = ROPE_THETA ** (-jnp.arange(half, dtype=jnp.float32) / half)
    ang = positions.astype(jnp.float32)[:, None, :, None] * inv_freq
    cos, sin = jnp.cos(ang), jnp.sin(ang)
    x1 = x[..., :half].astype(jnp.float32)
    x2 = x[..., half:ROPE_DIMS].astype(jnp.float32)
    rot = jnp.concatenate([x1 * cos - x2 * sin, x2 * cos + x1 * sin], axis=-1).astype(x.dtype)
    return jnp.concatenate([rot, x[..., ROPE_DIMS:]], axis=-1)


def masked_softmax(logits, mask):
    logits = jnp.where(mask, logits.astype(jnp.float32), NEG_INF)
    p = jax.nn.softmax(logits, axis=-1)
    return jnp.where(jnp.any(mask, axis=-1, keepdims=True), p, 0.0)


def to_heads(t, n):
    B, S, _ = t.shape
    return t.reshape(B, S, n, HEAD_DIM).transpose(0, 2, 1, 3)


def from_heads(t):
    B, H, S, Dh = t.shape
    return t.transpose(0, 2, 1, 3).reshape(B, S, H * Dh)


def merge_query_blocks(o):
    nq, B, H, qb, Dh = o.shape
    return jnp.moveaxis(o, 0, 2).reshape(B, H, nq * qb, Dh)


def moba_attention(q, k, v):
    B, H, S, Dh = q.shape
    scale = Dh ** -0.5
    nb = -(-S // MOBA_BLOCK)
    pad = nb * MOBA_BLOCK - S
    k_pad = jnp.pad(k, ((0, 0), (0, 0), (0, pad), (0, 0)))
    v_pad = jnp.pad(v, ((0, 0), (0, 0), (0, pad), (0, 0)))
    k_blk = k_pad.reshape(B, H, nb, MOBA_BLOCK, Dh)
    v_blk = v_pad.reshape(B, H, nb, MOBA_BLOCK, Dh)
    k_mean = jnp.mean(k_blk.astype(jnp.float32), axis=3)
    top = min(MOBA_TOPK, max(nb - 1, 1))
    n_sel = top * MOBA_BLOCK
    b_idx = jnp.arange(B)[:, None, None, None]
    h_idx = jnp.arange(H)[None, :, None, None]
    blk_ids = jnp.arange(nb)
    in_blk = jnp.arange(MOBA_BLOCK)

    def block_fn(i):
        t0 = i * MOBA_Q_BLOCK
        t = t0 + jnp.arange(MOBA_Q_BLOCK)
        own = t0 // MOBA_BLOCK
        qc = lax.dynamic_slice_in_dim(q, t0, MOBA_Q_BLOCK, axis=2)
        gate = jnp.einsum('bhqd,bhnd->bhqn', qc.astype(jnp.float32), k_mean)
        gate = jnp.where(blk_ids < own, gate, NEG_INF)
        _, idx = lax.top_k(gate, top)
        k_sel = k_blk[b_idx, h_idx, idx].reshape(B, H, MOBA_Q_BLOCK, n_sel, Dh)
        v_sel = v_blk[b_idx, h_idx, idx].reshape(B, H, MOBA_Q_BLOCK, n_sel, Dh)
        sel_mask = jnp.repeat(idx < own, MOBA_BLOCK, axis=-1)
        k_own = lax.dynamic_slice_in_dim(k_pad, own * MOBA_BLOCK, MOBA_BLOCK, axis=2)
        v_own = lax.dynamic_slice_in_dim(v_pad, own * MOBA_BLOCK, MOBA_BLOCK, axis=2)
        own_mask = (own * MOBA_BLOCK + in_blk)[None, :] <= t[:, None]
        logits = jnp.concatenate([
            jnp.einsum('bhqd,bhqkd->bhqk', qc, k_sel),
            jnp.einsum('bhqd,bhkd->bhqk', qc, k_own)], axis=-1).astype(jnp.float32) * scale
        mask = jnp.concatenate(
            [sel_mask, jnp.broadcast_to(own_mask, (B, H, MOBA_Q_BLOCK, MOBA_BLOCK))], axis=-1)
        p = masked_softmax(logits, mask)
        return (jnp.einsum('bhqk,bhqkd->bhqd', p[..., :n_sel], v_sel)
                + jnp.einsum('bhqk,bhkd->bhqd', p[..., n_sel:], v_own))

    return merge_query_blocks(lax.map(block_fn, jnp.arange(S // MOBA_Q_BLOCK)))


def compress_blocks(t, pe, w1, w2):
    B, S, Dh = t.shape
    n_cmp = (S - NSA_CMP_LEN) // NSA_CMP_STRIDE + 1
    tok = np.arange(n_cmp)[:, None] * NSA_CMP_STRIDE + np.arange(NSA_CMP_LEN)[None, :]
    blocks = t[:, tok] + pe
    flat = blocks.reshape(B, n_cmp, NSA_CMP_LEN * Dh)
    return jax.nn.gelu(flat @ w1) @ w2


def nsa_attention(q, kc, vc, ks, vs, kw, vw, gates, pe_k, pe_v, w1_k, w2_k, w1_v, w2_v):
    B, H, S, Dh = q.shape
    scale = Dh ** -0.5
    pos = jnp.arange(S)
    k_cmp = compress_blocks(kc, pe_k, w1_k, w2_k)
    v_cmp = compress_blocks(vc, pe_v, w1_v, w2_v)
    n_cmp = k_cmp.shape[1]
    cmp_end = jnp.arange(n_cmp) * NSA_CMP_STRIDE + NSA_CMP_LEN - 1
    cmp_mask = cmp_end[None, :] <= pos[:, None]
    p_cmp = masked_softmax(jnp.einsum('bhtd,bcd->bhtc', q, k_cmp) * scale, cmp_mask)
    o_cmp = jnp.einsum('bhtc,bcd->bhtd', p_cmp, v_cmp)
    n_sb = S // NSA_SEL_BLOCK
    c_first = (np.arange(n_cmp) * NSA_CMP_STRIDE) // NSA_SEL_BLOCK
    c_last = (np.arange(n_cmp) * NSA_CMP_STRIDE + NSA_CMP_LEN - 1) // NSA_SEL_BLOCK
    sb = np.arange(n_sb)
    overlap = ((sb[None, :] >= c_first[:, None]) & (sb[None, :] <= c_last[:, None])).astype(np.float32)
    imp = jnp.einsum('bhtc,cn->btn', p_cmp, jnp.asarray(overlap))
    n_sel = min(NSA_N_SEL, n_sb)
    ks_blk = ks.reshape(B, n_sb, NSA_SEL_BLOCK, Dh)
    vs_blk = vs.reshape(B, n_sb, NSA_SEL_BLOCK, Dh)
    kw_pad = jnp.pad(kw, ((0, 0), (NSA_WINDOW, 0), (0, 0)))
    vw_pad = jnp.pad(vw, ((0, 0), (NSA_WINDOW, 0), (0, 0)))
    b_idx = jnp.arange(B)[:, None, None]
    blk_ids = jnp.arange(n_sb)
    in_blk = jnp.arange(NSA_SEL_BLOCK)
    win_off = jnp.arange(NSA_WINDOW + Q_BLOCK)

    def block_fn(i):
        t0 = i * Q_BLOCK
        t = t0 + jnp.arange(Q_BLOCK)
        qc = lax.dynamic_slice_in_dim(q, t0, Q_BLOCK, axis=2)
        imp_c = lax.dynamic_slice_in_dim(imp, t0, Q_BLOCK, axis=1)
        cur = t // NSA_SEL_BLOCK
        forced = ((blk_ids[None, :] == 0) | (blk_ids[None, :] == cur[:, None])
                  | (blk_ids[None, :] == cur[:, None] - 1))
        allowed = blk_ids[None, :] <= cur[:, None]
        score = jnp.where(forced, jnp.inf, jnp.where(allowed, imp_c, -jnp.inf))
        _, idx = lax.top_k(score, n_sel)
        k_sel = ks_blk[b_idx, idx].reshape(B, Q_BLOCK, n_sel * NSA_SEL_BLOCK, Dh)
        v_sel = vs_blk[b_idx, idx].reshape(B, Q_BLOCK, n_sel * NSA_SEL_BLOCK, Dh)
        kpos = (idx[..., None] * NSA_SEL_BLOCK + in_blk).reshape(B, Q_BLOCK, -1)
        sel_mask = (kpos <= t[None, :, None])[:, None]
        p = masked_softmax(jnp.einsum('bhqd,bqkd->bhqk', qc, k_sel) * scale, sel_mask)
        o_sel = jnp.einsum('bhqk,bqkd->bhqd', p, v_sel)
        k_win = lax.dynamic_slice_in_dim(kw_pad, t0, NSA_WINDOW + Q_BLOCK, axis=1)
        v_win = lax.dynamic_slice_in_dim(vw_pad, t0, NSA_WINDOW + Q_BLOCK, axis=1)
        wpos = t0 - NSA_WINDOW + win_off
        win_mask = ((wpos[None, :] <= t[:, None]) & (wpos[None, :] > t[:, None] - NSA_WINDOW)
                    & (wpos[None, :] >= 0))
        p = masked_softmax(jnp.einsum('bhqd,bkd->bhqk', qc, k_win) * scale, win_mask)
        o_win = jnp.einsum('bhqk,bkd->bhqd', p, v_win)
        return o_sel, o_win

    o_sel, o_win = lax.map(block_fn, jnp.arange(S // Q_BLOCK))
    o_sel = merge_query_blocks(o_sel)
    o_win = merge_query_blocks(o_win)
    return gates[..., 0:1] * o_cmp + gates[..., 1:2] * o_sel + gates[..., 2:3] * o_win


def fox_attention(q, k, v, log_f):
    B, H, S, Dh = q.shape
    scale = Dh ** -0.5
    cum = jnp.cumsum(log_f, axis=-1)
    kpos = jnp.arange(S)

    def block_fn(i):
        t0 = i * Q_BLOCK
        t = t0 + jnp.arange(Q_BLOCK)
        qc = lax.dynamic_slice_in_dim(q, t0, Q_BLOCK, axis=2)
        cum_q = lax.dynamic_slice_in_dim(cum, t0, Q_BLOCK, axis=2)
        logits = (jnp.einsum('bhqd,bhkd->bhqk', qc, k).astype(jnp.float32) * scale
                  + (cum_q[..., None] - cum[:, :, None, :]))
        p = masked_softmax(logits, kpos[None, :] <= t[:, None])
        return jnp.einsum('bhqk,bhkd->bhqd', p, v)

    return merge_query_blocks(lax.map(block_fn, jnp.arange(S // Q_BLOCK)))


def hybrid_mixer(h, positions, w_in, b_forget, pe_k, pe_v, w1_k, w2_k, w1_v, w2_v, w_out):
    B, S, _ = h.shape
    proj = h @ w_in
    p_moba, p_nsa_q, p_nsa_kv, p_nsa_g, p_fox, p_fox_f = jnp.split(proj, PROJ_OFFSETS, axis=-1)
    qm, km, vm = (to_heads(t, N_HEADS_MOBA) for t in jnp.split(p_moba, 3, axis=-1))
    o_moba = moba_attention(rope_partial(qm, positions), rope_partial(km, positions), vm)
    qn = rope_partial(to_heads(p_nsa_q, N_HEADS_NSA), positions)
    kc, vc, ks, vs, kw, vw = jnp.split(p_nsa_kv, NSA_N_KV, axis=-1)
    rope_kv = lambda t: rope_partial(t[:, None], positions)[:, 0]
    gates = jax.nn.sigmoid(p_nsa_g.astype(jnp.float32)).reshape(
        B, S, N_HEADS_NSA, NSA_N_BRANCH).transpose(0, 2, 1, 3)
    o_nsa = nsa_attention(qn, rope_kv(kc), vc, rope_kv(ks), vs, rope_kv(kw), vw, gates,
                          pe_k, pe_v, w1_k, w2_k, w1_v, w2_v)
    qf, kf, vf = (to_heads(t, N_HEADS_FOX) for t in jnp.split(p_fox, 3, axis=-1))
    log_f = jax.nn.log_sigmoid((p_fox_f + b_forget).astype(jnp.float32)).transpose(0, 2, 1)
    o_fox = fox_attention(qf, kf, vf, log_f)
    o = jnp.concatenate([from_heads(o_moba), from_heads(o_nsa), from_heads(o_fox)],
                        axis=-1).astype(h.dtype)
    return o @ w_out


def sq_relu_mlp(h, w_up, w_down):
    return jnp.square(jax.nn.relu(h @ w_up)) @ w_down


def setup_inputs(seed: int = 0) -> dict:
    key = jax.random.key(seed)
    ks = jax.random.split(key, 20)
    f32 = jnp.float32

    def dense(k, shape, fan_in):
        return jax.random.normal(k, shape, f32) * fan_in ** -0.5

    def gain(k):
        return 1.0 + 0.05 * jax.random.normal(k, (DEPTH, D_MODEL), f32)

    cmp_in = NSA_CMP_LEN * HEAD_DIM
    return {
        "x": jax.random.normal(ks[0], (BATCH, SEQ, D_MODEL), f32),
        "c": jax.random.normal(ks[1], (BATCH, D_MODEL), f32),
        "positions": jnp.broadcast_to(jnp.arange(SEQ, dtype=jnp.int32), (BATCH, SEQ)),
        "w_mod": dense(ks[2], (DEPTH, D_MODEL, 6 * D_MODEL), D_MODEL),
        "b_mod": 0.02 * jax.random.normal(ks[3], (DEPTH, 6 * D_MODEL), f32),
        "g_pre_mix": gain(ks[4]),
        "g_post_mix": gain(ks[5]),
        "g_pre_mlp": gain(ks[6]),
        "g_post_mlp": gain(ks[7]),
        "w_in": dense(ks[8], (DEPTH, D_MODEL, PROJ_DIM), D_MODEL),
        "b_forget": 3.0 + 0.5 * jax.random.normal(ks[9], (DEPTH, N_HEADS_FOX), f32),
        "cmp_pe_k": 0.02 * jax.random.normal(ks[10], (DEPTH, NSA_CMP_LEN, HEAD_DIM), f32),
        "cmp_pe_v": 0.02 * jax.random.normal(ks[11], (DEPTH, NSA_CMP_LEN, HEAD_DIM), f32),
        "cmp_w1_k": dense(ks[12], (DEPTH, cmp_in, NSA_CMP_HIDDEN), cmp_in),
        "cmp_w2_k": dense(ks[13], (DEPTH, NSA_CMP_HIDDEN, HEAD_DIM), NSA_CMP_HIDDEN),
        "cmp_w1_v": dense(ks[14], (DEPTH, cmp_in, NSA_CMP_HIDDEN), cmp_in),
        "cmp_w2_v": dense(ks[15], (DEPTH, NSA_CMP_HIDDEN, HEAD_DIM), NSA_CMP_HIDDEN),
        "w_out": dense(ks[16], (DEPTH, MIX_WIDTH, D_MODEL), MIX_WIDTH),
        "w_up": dense(ks[17], (DEPTH, D_MODEL, D_FF), D_MODEL),
        "w_down": dense(ks[18], (DEPTH, D_FF, D_MODEL), D_FF),
    }


def reference(x, c, positions, w_mod, b_mod, g_pre_mix, g_post_mix, g_pre_mlp, g_post_mlp,
              w_in, b_forget, cmp_pe_k, cmp_pe_v, cmp_w1_k, cmp_w2_k, cmp_w1_v, cmp_w2_v,
              w_out, w_up, w_down):
    c_act = jax.nn.silu(c)
    for l in range(DEPTH):
        mod = (c_act @ w_mod[l] + b_mod[l])[:, None, :]
        shift_a, scale_a, gate_a, shift_m, scale_m, gate_m = jnp.split(mod, 6, axis=-1)
        h = modulate(rms_norm(x, g_pre_mix[l]), shift_a, scale_a)
        y = hybrid_mixer(h, positions, w_in[l], b_forget[l], cmp_pe_k[l], cmp_pe_v[l],
                         cmp_w1_k[l], cmp_w2_k[l], cmp_w1_v[l], cmp_w2_v[l], w_out[l])
        x = x + gate_a * rms_norm(y, g_post_mix[l])
        h = modulate(rms_norm(x, g_pre_mlp[l]), shift_m, scale_m)
        x = x + gate_m * rms_norm(sq_relu_mlp(h, w_up[l], w_down[l]), g_post_mlp[l])
    return x
```

```python
import numpy as np
from contextlib import ExitStack
import concourse.bass as bass
import concourse.mybir as mybir
from concourse.bass_utils import run_bass_kernel_spmd

F32 = mybir.dt.float32
BF16 = mybir.dt.bfloat16
I32 = mybir.dt.int32
ALU = mybir.AluOpType
AF = mybir.ActivationFunctionType
AX = mybir.AxisListType

D = 2048
KC = 16
HD = 128
PROJ = 5908
DFF = 8192
SCL = 128.0 ** -0.5
EPS = 1e-6
NCH = 201
TINY = 1e-30


class Buf:
    __slots__ = ('t', 'name', 'w', 'r', 'sem', 'sval')

    def __init__(self, t, name):
        self.t = t
        self.name = name
        self.w = None
        self.r = {}
        self.sem = None
        self.sval = 0

    def __getitem__(self, k):
        return self.t[k]


class TR:
    ENG = ('pe', 'act', 'dve', 'pool', 'sp')

    def __init__(self, nc, st):
        self.nc = nc
        self.st = st
        self.h = dict(pe=nc.tensor, act=nc.scalar, dve=nc.vector, pool=nc.gpsimd, sp=nc.sync)
        self.sem = {e: st.enter_context(nc.semaphore("c_" + e)) for e in self.ENG}
        self.cnt = {e: 0 for e in self.ENG}
        self.seen = {e: {} for e in self.ENG}
        self.dsems = []
        self.freesems = []
        self.by_stack = {}
        self.nbuf = 0
        self.ninst = 0

    def buf(self, st, name, shape, dtype, psum=False):
        self.nbuf += 1
        nm = "%s_%d" % (name, self.nbuf)
        if psum:
            t = st.enter_context(self.nc.psum_tensor(nm, shape, dtype))
        else:
            t = st.enter_context(self.nc.sbuf_tensor(nm, shape, dtype))
        b = Buf(t, nm)
        self.by_stack.setdefault(id(st), []).append(b)
        return b

    def _wait(self, eng, rec):
        kind, who, val = rec
        if kind == 'e':
            if who == eng and eng == 'pe':
                return
            key = ('e', who)
            semh = self.sem[who]
        else:
            key = ('d', who.name)
            semh = who.sem
            val = who.sval
        if self.seen[eng].get(key, 0) >= val:
            return
        self.h[eng].wait_ge(semh, val)
        self.ninst += 1
        self.seen[eng][key] = val

    def op(self, eng, fn, reads=(), writes=()):
        recs = []
        for b in reads:
            if b.w is not None:
                recs.append(b.w)
        for b in writes:
            if b.w is not None:
                recs.append(b.w)
            recs.extend(b.r.values())
        for rec in recs:
            self._wait(eng, rec)
        inst = fn(self.h[eng])
        self.cnt[eng] += 1
        self.ninst += 1
        inst.then_inc(self.sem[eng], 1)
        rec = ('e', eng, self.cnt[eng])
        for b in reads:
            b.r[('e', eng)] = rec
        for b in writes:
            b.w = rec
            b.r = {}
        return inst

    def dma(self, eng, out, in_, sb, load):
        recs = []
        if sb.w is not None:
            recs.append(sb.w)
        if load:
            recs.extend(sb.r.values())
        if sb.sem is None:
            if self.freesems:
                sb.sem, sb.sval = self.freesems.pop()
            else:
                sb.sem = self.st.enter_context(self.nc.semaphore("d_" + sb.name))
                sb.sval = 0
            self.dsems.append(sb)
        if sb.sval > 0:
            recs.append(('d', sb, sb.sval))
        for rec in recs:
            self._wait(eng, rec)
        inst = self.h[eng].dma_start(out=out, in_=in_)
        self.ninst += 1
        sb.sval += 16
        inst.then_inc(sb.sem, 16)
        rec = ('d', sb, sb.sval)
        if load:
            sb.w = rec
            sb.r = {}
        else:
            sb.r[('d', sb.name)] = rec
        return inst

    def barrier(self):
        for e in self.ENG:
            for e2 in self.ENG:
                if e2 != e and self.cnt[e2] > 0:
                    self._wait(e, ('e', e2, self.cnt[e2]))
            for b in self.dsems:
                if b.sval > 0:
                    self._wait(e, ('d', b, b.sval))

    def end_phase(self, st):
        self.barrier()
        names = set(b.name for b in self.by_stack.pop(id(st), []))
        keep = []
        for b in self.dsems:
            if b.name in names:
                self.freesems.append((b.sem, b.sval))
            else:
                keep.append(b)
        self.dsems = keep


def const_layout(S):
    n_sb = S // 64
    KW = min(64, n_sb)
    nct = max(1, S // 2048)
    off = {}
    o = 0
    for name, n in [('ident', 128), ('i30k', 128), ('causal', 512), ('w2', 512), ('cmask', 5 * 512),
                    ('am', 32 * 128), ('an', (KW // 2) * 128), ('pmt', 32), ('ov', nct * (n_sb + 1)),
                    ('eg', 12 * 128), ('ones', 128)]:
        off[name] = (o, n)
        o += n
    return off, o


def make_consts(S):
    n_sb = S // 64
    KW = min(64, n_sb)
    nct = max(1, S // 2048)
    ncmp = S // 16 - 1
    off, tot = const_layout(S)
    c = np.zeros((128, tot), np.float32)
    p = np.arange(128)[:, None]

    def put(name, arr):
        o, n = off[name]
        assert arr.shape == (128, n), (name, arr.shape, n)
        c[:, o:o + n] = arr
    put('ident', np.eye(128, dtype=np.float32))
    put('i30k', 30000.0 * np.eye(128, dtype=np.float32))
    f = np.arange(512)[None, :]
    put('causal', np.where(f >= p, 0.0, -1.0).astype(np.float32))
    put('w2', np.where(f - 384 < p, 0.0, -1.0).astype(np.float32))
    cm = [np.where(16 * p - f <= 512 * d - 31, 0.0, -1.0) for d in range(5)]
    put('cmask', np.concatenate(cm, axis=1).astype(np.float32))
    am = np.zeros((128, 32, 128), np.float32)
    for n in range(32):
        am[n, n, :] = 30000.0
    put('am', am.reshape(128, -1))
    an = np.zeros((128, KW // 2, 128), np.float32)
    for r in range(128):
        rr = r % KW
        jj = rr // 2
        if rr % 2 == 0:
            an[r, jj, 0:64] = 30000.0
        else:
            an[r, jj, 64:128] = 30000.0
    put('an', an.reshape(128, -1))
    pm = np.zeros((128, 32), np.float32)
    for i in range(16):
        pm[i + 16, i] = -1.0
        pm[i, i + 16] = 1.0
    put('pmt', pm)
    ov = np.zeros((128, nct, n_sb + 1), np.float32)
    for cc in range(ncmp):
        jc, pp = divmod(cc, 128)
        n0 = cc // 4
        ov[pp, jc, n0] = 1.0
        if cc % 4 == 3 and n0 + 1 < n_sb:
            ov[pp, jc, n0 + 1] = 1.0
        ov[pp, jc, n_sb] = 1.0
    put('ov', ov.reshape(128, -1))
    eg = np.zeros((128, 12, 128), np.float32)
    for r in range(12):
        eg[r, r, :] = 1.0
    put('eg', eg.reshape(128, -1))
    put('ones', np.ones((128, 128), np.float32))
    fc = np.zeros((128, 4), np.float32)
    invf = 500000.0 ** (-(np.arange(16, dtype=np.float64)) / 16.0)
    fc[0:32, 0] = np.tile(invf / (2 * np.pi), 2).astype(np.float32)
    return c, fc


def build_program(S, NL):
    assert S % 2048 == 0
    NB = S // 512
    NT = S // 128
    n_sb = S // 64
    KW = min(64, n_sb)
    nct = S // 2048
    ncmp = S // 16 - 1
    NCP = nct * 128
    assert NCP <= 512
    nmb = S // 256
    coff, ctot = const_layout(S)

    nc = bass.Bass("TRN2", target_bir_lowering=False)

    def din(name, shape, dt=F32):
        return nc.dram_tensor(name, shape, dt, kind="ExternalInput").ap()

    def dscr(name, shape, dt):
        return nc.dram_tensor(name, shape, dt, kind="Internal").ap()

    x_in = din("xT", [128, KC, S])
    pos_in = din("pos", [1, S], I32)
    cT_in = din("cT", [128, KC])
    w_mod = din("w_mod", [NL, D, 6 * D])
    bmodT = din("bmodT", [128, NL, 96])
    gT_in = din("gains", [128, NL, 4, KC])
    w_in = din("w_in", [NL, D, PROJ])
    negb_in = din("negb", [8, NL])
    peT_in = din("peT", [128, NL, 2, 32])
    w1k = din("w1k", [NL, 4096, 256])
    w2k = din("w2k", [NL, 256, 128])
    w1v = din("w1v", [NL, 4096, 256])
    w2v = din("w2v", [NL, 256, 128])
    w_out = din("w_out", [NL, D, D])
    w_up = din("w_up", [NL, D, DFF])
    w_down = din("w_down", [NL, DFF, D])
    cst_in = din("cst", [128, ctot])
    fcst_in = din("fcst", [128, 4])
    y_out = nc.dram_tensor("yT", [128, KC, S], F32, kind="ExternalOutput").ap()

    WC = [dscr("WC%d" % l_, [NCH, 128, KC, 128], BF16) for l_ in range(NL)]
    FM = dscr("FM", [32, 128, S], BF16)
    TM = dscr("TM", [S, 1792], BF16)
    GT = dscr("GT", [12, S], BF16)
    RHO = dscr("RHO", [8, S], BF16)
    COS = dscr("COS", [32, S], F32)
    SIN = dscr("SIN", [32, S], F32)
    OTD = dscr("OTD", [16, 128, S], BF16)
    XS = [dscr("XS0", [128, KC, S], F32), dscr("XS1", [128, KC, S], F32)]

    with ExitStack() as st:
        tr = TR(nc, st)
        op = tr.op

        cstb = tr.buf(st, "cstb", [128, ctot], BF16)
        identf = tr.buf(st, "identf", [128, 128], F32)
        fcst = tr.buf(st, "fcst", [128, 4], F32)
        onecol = tr.buf(st, "onecol", [128, 1], F32)
        der = tr.buf(st, "der", [128, NL, 6, KC], F32)
        negb = tr.buf(st, "negb", [8, NL], F32)
        peb = tr.buf(st, "peb", [128, NL, 2, 32], BF16)
        hT = [tr.buf(st, "hT%d" % i, [128, KC, 512], BF16) for i in range(2)]
        w4k = [tr.buf(st, "w4k%d" % i, [128, KC, 128], BF16) for i in range(4)]
        ps = [tr.buf(st, "ps%d" % i, [128, 512], F32, psum=True) for i in range(7)]
        psb = tr.buf(st, "psb", [128, 1024], BF16, psum=True)
        wslot = [0]

        def cv(name, rows=128, sub=None):
            o, n = coff[name]
            if sub is None:
                return cstb[0:rows, o:o + n]
            return cstb[0:rows, o + sub[0]:o + sub[1]]

        def load_w(l, ci):
            b = w4k[wslot[0] % 4]
            wslot[0] += 1
            tr.dma('sp', b[:], WC[l][ci], b, True)
            return b

        with ExitStack() as ph:
            cstf = tr.buf(ph, "cstf", [128, ctot], F32)
            tr.dma('sp', cstf[:], cst_in, cstf, True)
            tr.dma('sp', fcst[:], fcst_in, fcst, True)
            bfg = tr.buf(ph, "bfg", [8, NL], F32)
            tr.dma('sp', bfg[:], negb_in, bfg, True)
            op('dve', lambda e: e.tensor_scalar(out=negb[:], in0=bfg[:], scalar1=-1.0, scalar2=None, op0=ALU.mult), [bfg], [negb])
            half = ctot // 2
            op('dve', lambda e: e.tensor_copy(out=cstb[:, 0:half], in_=cstf[:, 0:half]), [cstf], [cstb])
            op('pool', lambda e: e.tensor_copy(out=cstb[:, half:ctot], in_=cstf[:, half:ctot]), [cstf], [cstb])
            o_id = coff['ident'][0]
            op('dve', lambda e: e.tensor_copy(out=identf[:], in_=cstf[:, o_id:o_id + 128]), [cstf], [identf])
            op('pool', lambda e: e.memset(onecol[:], 1.0), [], [onecol])
            pef = tr.buf(ph, "pef", [128, NL, 2, 32], F32)
            tr.dma('sp', pef[:], peT_in, pef, True)
            op('dve', lambda e: e.tensor_copy(out=peb[:], in_=pef[:]), [pef], [peb])
            tr.end_phase(ph)
        with ExitStack() as ph:
            gains = tr.buf(ph, "gains", [128, NL, 4, KC], F32)
            tr.dma('sp', gains[:], gT_in, gains, True)
            bmod = tr.buf(ph, "bmod", [128, NL, 96], F32)
            tr.dma('sp', bmod[:], bmodT, bmod, True)
            cT = tr.buf(ph, "cT", [128, KC], F32)
            tr.dma('sp', cT[:], cT_in, cT, True)
            cs = tr.buf(ph, "cs", [128, KC], F32)
            op('act', lambda e: e.activation(out=cs[:], in_=cT[:], func=AF.Silu), [cT], [cs])
            modt = tr.buf(ph, "modt", [128, NL, 96], F32)
            wm = [tr.buf(ph, "wm%d" % i, [128, KC, 128], F32) for i in range(3)]
            for l in range(NL):
                pm_ = ps[l % 2]
                for j in range(96):
                    b = wm[j % 3]
                    tr.dma('sp', b[:], w_mod[l, :, j * 128:(j + 1) * 128].rearrange("(k p) c -> p k c", p=128), b, True)
                    for k in range(KC):
                        op('pe', lambda e: e.matmul(pm_[:, j:j + 1], lhsT=b[:, k, :], rhs=cs[:, k:k + 1],
                                                    start=(k == 0), stop=(k == KC - 1)), [b, cs], [pm_])
                op('dve', lambda e: e.tensor_tensor(out=modt[:, l, :], in0=pm_[:, 0:96], in1=bmod[:, l, :], op=ALU.add),
                   [pm_, bmod], [modt])
                for (di, si, gi) in ((0, 1, 0), (3, 4, 2)):
                    op('dve', lambda e: e.scalar_tensor_tensor(out=der[:, l, di, :], in0=modt[:, l, si * 16:(si + 1) * 16],
                                                               scalar=1.0, in1=gains[:, l, gi, :], op0=ALU.add, op1=ALU.mult),
                       [modt, gains], [der])
                for (di, si) in ((1, 0), (4, 3)):
                    op('dve', lambda e: e.tensor_copy(out=der[:, l, di, :], in_=modt[:, l, si * 16:(si + 1) * 16]), [modt], [der])
                for (di, si, gi) in ((2, 2, 1), (5, 5, 3)):
                    op('dve', lambda e: e.tensor_tensor(out=der[:, l, di, :], in0=modt[:, l, si * 16:(si + 1) * 16],
                                                        in1=gains[:, l, gi, :], op=ALU.mult), [modt, gains], [der])
            tr.end_phase(ph)
        with ExitStack() as ph:
            CW = 2048
            posi = tr.buf(ph, "posi", [32, CW], I32)
            posf = tr.buf(ph, "posf", [32, CW], F32)
            ru = tr.buf(ph, "ru", [32, CW], F32)
            rni = tr.buf(ph, "rni", [32, CW], I32)
            rnf = tr.buf(ph, "rnf", [32, CW], F32)
            rtab = [tr.buf(ph, "rtab%d" % i, [32, CW], F32) for i in range(2)]
            for cidx in range(S // CW):
                sl = slice(cidx * CW, (cidx + 1) * CW)
                tr.dma('sp', posi[:], pos_in[0:1, sl].to_broadcast([32, CW]), posi, True)
                op('dve', lambda e: e.tensor_copy(out=posf[:], in_=posi[:]), [posi], [posf])
                for ti, (tab, addc) in enumerate(((SIN, 0.0), (COS, 0.25))):
                    op('dve', lambda e: e.tensor_scalar(out=ru[:], in0=posf[:], scalar1=fcst[0:32, 0:1], scalar2=addc,
                                                        op0=ALU.mult, op1=ALU.add), [posf, fcst], [ru])
                    op('dve', lambda e: e.tensor_copy(out=rni[:], in_=ru[:]), [ru], [rni])
                    op('dve', lambda e: e.tensor_copy(out=rnf[:], in_=rni[:]), [rni], [rnf])
                    op('dve', lambda e: e.tensor_tensor(out=ru[:], in0=ru[:], in1=rnf[:], op=ALU.subtract), [ru, rnf], [ru])
                    rt = rtab[ti]
                    op('act', lambda e: e.activation(out=rt[:], in_=ru[:], func=AF.Sin, scale=2.0 * np.pi), [ru], [rt])
                    tr.dma('pool', tab[:, sl], rt[:], rt, False)
            tr.end_phase(ph)
        with ExitStack() as ph:
            stg = [tr.buf(ph, "stg%d" % i, [128, KC, 128], F32) for i in range(3)]
            sto = [tr.buf(ph, "sto%d" % i, [128, KC, 128], BF16) for i in range(3)]
            pcount = [0]
            cast_engs = ('dve', 'pool', 'act')

            def prep(l, ci, pieces, nk=KC):
                i = pcount[0]
                pcount[0] += 1
                sg = stg[i % 3]
                so = sto[i % 3]
                wtot = 0
                for (src, co) in pieces:
                    ncols = src.shape[1]
                    tr.dma('sp', sg[:, 0:nk, co:co + ncols], src.rearrange("(k p) c -> p k c", p=128), sg, True)
                    wtot = max(wtot, co + ncols)
                ce = cast_engs[i % 3]
                if ce == 'act':
                    op('act', lambda e: e.copy(out=so[:, 0:nk, 0:wtot], in_=sg[:, 0:nk, 0:wtot]), [sg], [so])
                else:
                    op(ce, lambda e: e.tensor_copy(out=so[:, 0:nk, 0:wtot], in_=sg[:, 0:nk, 0:wtot]), [sg], [so])
                tr.dma('pool', WC[l][ci, :, 0:nk, 0:wtot], so[:, 0:nk, 0:wtot], so, False)

            fm_cols = ([i * 128 for i in range(4)] + [512 + i * 128 for i in range(4)] +
                       [1536 + i * 128 for i in range(4)] + [2048, 2176, 2304, 2560] +
                       [2828 + i * 128 for i in range(8)] + [3852 + i * 128 for i in range(8)])
            v_cols = ([1024 + i * 128 for i in range(4)] + [2432, 2688] + [4876 + i * 128 for i in range(8)])
            for l in range(NL):
                for ci, c0 in enumerate(fm_cols):
                    prep(l, ci, [(w_in[l, :, c0:c0 + 128], 0)])
                for ci, c0 in enumerate(v_cols):
                    prep(l, 32 + ci, [(w_in[l, :, c0:c0 + 128], 0)])
                prep(l, 46, [(w_in[l, :, 2816:2828], 0), (w_in[l, :, 5900:5908], 12)])
                for oc in range(16):
                    prep(l, 47 + oc, [(w_out[l, :, oc * 128:(oc + 1) * 128], 0)])
                for fc_ in range(64):
                    prep(l, 63 + fc_, [(w_up[l, :, fc_ * 128:(fc_ + 1) * 128], 0)])
                for q in range(4):
                    for dc in range(16):
                        prep(l, 127 + q * 16 + dc, [(w_down[l, q * 2048:(q + 1) * 2048, dc * 128:(dc + 1) * 128], 0)])
                for wi, w1 in enumerate((w1k, w1v)):
                    for g in range(2):
                        for hc in range(2):
                            prep(l, 191 + wi * 4 + g * 2 + hc, [(w1[l, g * 2048:(g + 1) * 2048, hc * 128:(hc + 1) * 128], 0)])
                prep(l, 199, [(w2k[l, :, :], 0)], nk=2)
                prep(l, 200, [(w2v[l, :, :], 0)], nk=2)
            tr.end_phase(ph)

        for l in range(NL):
            xin = x_in if l == 0 else XS[(l - 1) % 2]
            xout = y_out if l == NL - 1 else XS[l % 2]
            with ExitStack() as ly:
                kmh = tr.buf(ly, "kmh", [128, 4, 32], BF16)
                kml = tr.buf(ly, "kml", [128, 4, 32], BF16)
                CK = tr.buf(ly, "CK", [128, NT, 8], F32)
                KcmpT = tr.buf(ly, "KcmpT", [128, NCP], BF16)
                Vcmp = tr.buf(ly, "Vcmp", [128, nct, 128], BF16)

                def rms_rstd(ph_bufs, sumps):
                    tmpn, rstd = ph_bufs
                    op('dve', lambda e: e.tensor_scalar(out=tmpn[:], in0=sumps[:], scalar1=1.0 / D, scalar2=EPS,
                                                        op0=ALU.mult, op1=ALU.add), [sumps], [tmpn])
                    op('act', lambda e: e.activation(out=tmpn[:], in_=tmpn[:], func=AF.Sqrt), [tmpn], [tmpn])
                    op('dve', lambda e: e.reciprocal(out=rstd[:], in_=tmpn[:]), [tmpn], [rstd])

                with ExitStack() as ph:
                    xc = [tr.buf(ph, "xc%d" % i, [128, 2, 512], F32) for i in range(3)]
                    sq = [tr.buf(ph, "sq%d" % i, [128, 512], BF16) for i in range(2)]
                    tmpn = tr.buf(ph, "tmpn", [128, 512], F32)
                    rstd = tr.buf(ph, "rstd", [128, 512], F32)
                    tmpx = [tr.buf(ph, "tmpx%d" % i, [128, 512], F32) for i in range(2)]
                    wv = [tr.buf(ph, "wv%d" % i, [128, KC, 512], BF16) for i in range(2)]
                    qk = [tr.buf(ph, "qk%d" % i, [128, 512], BF16) for i in range(3)]
                    ropeA = tr.buf(ph, "ropeA", [32, 512], F32)
                    ropeB = tr.buf(ph, "ropeB", [32, 512], F32)
                    cosb = [tr.buf(ph, "cosb%d" % i, [32, 512], F32) for i in range(2)]
                    sinb = [tr.buf(ph, "sinb%d" % i, [32, 512], F32) for i in range(2)]
                    vout = [tr.buf(ph, "vout%d" % i, [128, 4, 512], BF16) for i in range(2)]
                    gsb = [tr.buf(ph, "gsb%d" % i, [12, 512], BF16) for i in range(2)]
                    fe = tr.buf(ph, "fe", [8, 512], F32)
                    fl = tr.buf(ph, "fl", [8, 512], F32)
                    Cb = [tr.buf(ph, "Cb%d" % i, [8, 512], F32) for i in range(2)]
                    rhob = [tr.buf(ph, "rhob%d" % i, [8, 512], BF16) for i in range(2)]
                    ones8 = tr.buf(ph, "ones8", [8, 512], F32)
                    kmf = tr.buf(ph, "kmf", [128, 4, 32], F32)
                    kmt = tr.buf(ph, "kmt", [128, 4, 32], F32)
                    op('pool', lambda e: e.memset(ones8[:], 1.0), [], [ones8])
                    op('pool', lambda e: e.memset(kmf[:], 0.0), [], [kmf])
                    xcn = [0]
                    qkn = [0]
                    pcn = [0]
                    vgn = [0]
                    roped = set(range(0, 13)) | {14, 15}
                    roped.discard(13)
                    vgroups = [(32, 4, 0), (36, 2, 512), (38, 4, 768), (42, 4, 1280)]
                    for b in range(NB):
                        t0 = b * 512
                        tsl = slice(t0, t0 + 512)
                        h = hT[b % 2]
                        pn = ps[6]
                        for g in range(8):
                            xb = xc[xcn[0] % 3]
                            xcn[0] += 1
                            tr.dma('sp', xb[:], xin[:, 2 * g:2 * g + 2, tsl], xb, True)
                            for c in range(2):
                                s_ = sq[(2 * g + c) % 2]
                                op('act', lambda e: e.activation(out=s_[:], in_=xb[:, c, :], func=AF.Square), [xb], [s_])
                                op('pe', lambda e: e.matmul(pn[:], lhsT=cv('ones'), rhs=s_[:], start=(g == 0 and c == 0),
                                                            stop=(g == 7 and c == 1)), [s_, cstb], [pn])
                        rms_rstd((tmpn, rstd), pn)
                        for g in range(8):
                            xb = xc[xcn[0] % 3]
                            xcn[0] += 1
                            tr.dma('sp', xb[:], xin[:, 2 * g:2 * g + 2, tsl], xb, True)
                            for c in range(2):
                                k = 2 * g + c
                                tx = tmpx[k % 2]
                                op('dve', lambda e: e.tensor_tensor(out=tx[:], in0=xb[:, c, :], in1=rstd[:], op=ALU.mult),
                                   [xb, rstd], [tx])
                                op('pool', lambda e: e.tensor_scalar(out=h[:, k, :], in0=tx[:], scalar1=der[:, l, 0, k:k + 1],
                                                                     scalar2=der[:, l, 1, k:k + 1], op0=ALU.mult, op1=ALU.add),
                                   [tx, der], [h])
                        cb_ = cosb[b % 2]
                        sb_ = sinb[b % 2]
                        tr.dma('sp', cb_[:], COS[:, tsl], cb_, True)
                        tr.dma('sp', sb_[:], SIN[:, tsl], sb_, True)
                        for ci in range(32):
                            w = load_w(l, ci)
                            pp = ps[pcn[0] % 3]
                            pcn[0] += 1
                            for k in range(KC):
                                op('pe', lambda e: e.matmul(pp[:], lhsT=w[:, k, :], rhs=h[:, k, :], start=(k == 0),
                                                            stop=(k == KC - 1)), [w, h], [pp])
                            q_ = qk[qkn[0] % 3]
                            qkn[0] += 1
                            op('act', lambda e: e.copy(out=q_[:], in_=pp[:]), [pp], [q_])
                            if ci in roped:
                                pr = ps[3]
                                op('pe', lambda e: e.matmul(pr[0:32, :], lhsT=cv('pmt'), rhs=q_[:], start=True, stop=True),
                                   [q_, cstb], [pr])
                                op('dve', lambda e: e.tensor_tensor(out=ropeA[:], in0=pr[0:32, :], in1=sb_[:], op=ALU.mult),
                                   [pr, sb_], [ropeA])
                                op('dve', lambda e: e.tensor_tensor(out=ropeB[:], in0=q_[0:32, :], in1=cb_[:], op=ALU.mult),
                                   [q_, cb_], [ropeB])
                                op('dve', lambda e: e.tensor_tensor(out=q_[0:32, :], in0=ropeA[:], in1=ropeB[:], op=ALU.add),
                                   [ropeA, ropeB], [q_])
                            if 4 <= ci < 8:
                                op('dve', lambda e: e.tensor_reduce(out=kmf[:, ci - 4, 2 * b:2 * b + 2],
                                                                    in_=q_[:].rearrange("p (a c) -> p a c", a=2),
                                                                    axis=AX.X, op=ALU.add), [q_], [kmf])
                            tr.dma('pool', FM[ci, :, tsl], q_[:], q_, False)
                        for (c0, nchk, col0) in vgroups:
                            wvb = wv[vgn[0] % 2]
                            vo = vout[vgn[0] % 2]
                            vgn[0] += 1
                            ncols = nchk * 128
                            for i in range(nchk):
                                tr.dma('sp', wvb[:, :, i * 128:(i + 1) * 128], WC[l][c0 + i], wvb, True)
                            for tt in range(4):
                                pp = ps[pcn[0] % 3]
                                pcn[0] += 1
                                for k in range(KC):
                                    op('pe', lambda e: e.matmul(pp[:, 0:ncols], lhsT=h[:, k, tt * 128:(tt + 1) * 128],
                                                                rhs=wvb[:, k, 0:ncols], start=(k == 0), stop=(k == KC - 1)),
                                       [wvb, h], [pp])
                                if tt % 2 == 0:
                                    op('act', lambda e: e.copy(out=vo[:, tt, 0:ncols], in_=pp[:, 0:ncols]), [pp], [vo])
                                else:
                                    op('dve', lambda e: e.tensor_copy(out=vo[:, tt, 0:ncols], in_=pp[:, 0:ncols]), [pp], [vo])
                            tr.dma('pool', TM[t0:t0 + 512, col0:col0 + ncols].rearrange("(t p) c -> p t c", p=128),
                                   vo[:, :, 0:ncols], vo, False)
                        w = load_w(l, 46)
                        pg = ps[4]
                        for k in range(KC):
                            op('pe', lambda e: e.matmul(pg[0:12, :], lhsT=w[:, k, 0:12], rhs=h[:, k, :], start=(k == 0),
                                                        stop=(k == KC - 1)), [w, h], [pg])
                        g_ = gsb[b % 2]
                        op('act', lambda e: e.activation(out=g_[:], in_=pg[0:12, :], func=AF.Sigmoid), [pg], [g_])
                        tr.dma('pool', GT[:, tsl], g_[:], g_, False)
                        pf = ps[5]
                        for k in range(KC):
                            op('pe', lambda e: e.matmul(pf[0:8, :], lhsT=w[:, k, 12:20], rhs=h[:, k, :], start=(k == 0),
                                                        stop=(k == KC - 1)), [w, h], [pf])
                        op('act', lambda e: e.activation(out=fe[:], in_=pf[0:8, :], func=AF.Exp, scale=-1.0,
                                                         bias=negb[:, l:l + 1]), [pf, negb], [fe])
                        op('act', lambda e: e.activation(out=fl[:], in_=fe[:], func=AF.Ln, bias=onecol[0:8, 0:1]),
                           [fe, onecol], [fl])
                        cbuf = Cb[b % 2]
                        cprev = Cb[(b - 1) % 2]
                        if b == 0:
                            op('dve', lambda e: e.tensor_tensor_scan(out=cbuf[:], data0=ones8[:], data1=fl[:], initial=0.0,
                                                                     op0=ALU.mult, op1=ALU.add), [ones8, fl], [cbuf])
                        else:
                            op('dve', lambda e: e.tensor_tensor_scan(out=cbuf[:], data0=ones8[:], data1=fl[:],
                                                                     initial=cprev[:, 511:512], op0=ALU.mult, op1=ALU.add),
                               [ones8, fl, cprev], [cbuf])
                        rb_ = rhob[b % 2]
                        op('dve', lambda e: e.tensor_scalar(out=rb_[:], in0=cbuf[:], scalar1=-1.0 / SCL, scalar2=None,
                                                            op0=ALU.mult), [cbuf], [rb_])
                        tr.dma('pool', RHO[:, tsl], rb_[:], rb_, False)
                        pt_ = ps[3]
                        for i in range(4):
                            op('pe', lambda e: e.transpose(out=pt_[:, 8 * i:8 * i + 8], in_=cbuf[0:8, 128 * i:128 * (i + 1)],
                                                           identity=identf[0:8, 0:8]), [cbuf, identf], [pt_])
                        op('dve', lambda e: e.tensor_copy(out=CK[:, 4 * b:4 * b + 4, :],
                                                          in_=pt_[:, 0:32].rearrange("p (a c) -> p a c", a=4)), [pt_], [CK])
                    op('dve', lambda e: e.tensor_scalar(out=kmf[:], in0=kmf[:], scalar1=1.0 / 256.0, scalar2=None,
                                                        op0=ALU.mult), [kmf], [kmf])
                    op('dve', lambda e: e.tensor_copy(out=kmh[:], in_=kmf[:]), [kmf], [kmh])
                    op('dve', lambda e: e.tensor_copy(out=kmt[:], in_=kmh[:]), [kmh], [kmt])
                    op('dve', lambda e: e.tensor_tensor(out=kmt[:], in0=kmf[:], in1=kmt[:], op=ALU.subtract), [kmf, kmt], [kmt])
                    op('dve', lambda e: e.tensor_copy(out=kml[:], in_=kmt[:]), [kmt], [kml])
                    tr.end_phase(ph)

                with ExitStack() as ph:
                    kcS = tr.buf(ph, "kcS", [128, S], BF16)
                    Z = tr.buf(ph, "Z", [128, 16, S // 16], BF16)
                    W1 = tr.buf(ph, "W1", [128, 32, 256], BF16)
                    W2 = tr.buf(ph, "W2", [128, 2, 128], BF16)
                    bcol = tr.buf(ph, "bcol", [128, 2], F32)
                    xg = tr.buf(ph, "xg", [128, 512], F32)
                    x2 = tr.buf(ph, "x2", [128, 512], F32)
                    sg = tr.buf(ph, "sg", [128, 512], F32)
                    GTt = tr.buf(ph, "GTt", [128, 2, 512], BF16)
                    for wi in range(2):
                        tr.dma('sp', kcS[:], FM[12 + wi], kcS, True)
                        op('pool', lambda e: e.tensor_copy(out=Z[:], in_=kcS[:].rearrange("p (m r) -> p r m", r=16)), [kcS], [Z])
                        for g in range(2):
                            for hc in range(2):
                                tr.dma('sp', W1[:, 16 * g:16 * g + 16, hc * 128:(hc + 1) * 128],
                                       WC[l][191 + wi * 4 + g * 2 + hc], W1, True)
                        tr.dma('sp', W2[:], WC[l][199 + wi, :, 0:2, :], W2, True)
                        op('pool', lambda e: e.memset(GTt[:], 0.0), [], [GTt])
                        for hc in range(2):
                            phd = ps[hc]
                            pbs = ps[2]
                            for tau in range(32):
                                a, r = divmod(tau, 16)
                                op('pe', lambda e: e.matmul(phd[:, 0:ncmp], lhsT=W1[:, tau, hc * 128:(hc + 1) * 128],
                                                            rhs=Z[:, r, a:a + ncmp], start=(tau == 0), stop=(tau == 31)),
                                   [W1, Z], [phd])
                            for tau in range(32):
                                op('pe', lambda e: e.matmul(pbs[:, hc:hc + 1], lhsT=W1[:, tau, hc * 128:(hc + 1) * 128],
                                                            rhs=peb[:, l, wi, tau:tau + 1], start=(tau == 0), stop=(tau == 31)),
                                   [W1, peb], [pbs])
                            op('dve', lambda e: e.tensor_copy(out=bcol[:, hc:hc + 1], in_=pbs[:, hc:hc + 1]), [pbs], [bcol])
                            op('act', lambda e: e.activation(out=xg[:, 0:ncmp], in_=phd[:, 0:ncmp], func=AF.Identity,
                                                             bias=bcol[:, hc:hc + 1]), [phd, bcol], [xg])
                            op('dve', lambda e: e.tensor_tensor(out=x2[:, 0:ncmp], in0=xg[:, 0:ncmp], in1=xg[:, 0:ncmp],
                                                                op=ALU.mult), [xg], [x2])
                            op('dve', lambda e: e.tensor_scalar(out=x2[:, 0:ncmp], in0=x2[:, 0:ncmp], scalar1=0.0713548163,
                                                                scalar2=1.5957691216, op0=ALU.mult, op1=ALU.add), [x2], [x2])
                            op('dve', lambda e: e.tensor_tensor(out=x2[:, 0:ncmp], in0=x2[:, 0:ncmp], in1=xg[:, 0:ncmp],
                                                                op=ALU.mult), [x2, xg], [x2])
                            op('act', lambda e: e.activation(out=sg[:, 0:ncmp], in_=x2[:, 0:ncmp], func=AF.Sigmoid), [x2], [sg])
                            op('dve', lambda e: e.tensor_tensor(out=GTt[:, hc, 0:ncmp], in0=xg[:, 0:ncmp], in1=sg[:, 0:ncmp],
                                                                op=ALU.mult), [xg, sg], [GTt])
                        po = ps[3]
                        if wi == 0:
                            for hc in range(2):
                                op('pe', lambda e: e.matmul(po[:, 0:NCP], lhsT=W2[:, hc, :], rhs=GTt[:, hc, 0:NCP],
                                                            start=(hc == 0), stop=(hc == 1)), [W2, GTt], [po])
                            op('act', lambda e: e.copy(out=KcmpT[:], in_=po[:, 0:NCP]), [po], [KcmpT])
                        else:
                            for ct in range(nct):
                                for hc in range(2):
                                    op('pe', lambda e: e.matmul(po[:, ct * 128:(ct + 1) * 128],
                                                                lhsT=GTt[:, hc, ct * 128:(ct + 1) * 128], rhs=W2[:, hc, :],
                                                                start=(hc == 0), stop=(hc == 1)), [W2, GTt], [po])
                            op('act', lambda e: e.copy(out=Vcmp[:], in_=po[:, 0:NCP].rearrange("p (a c) -> p a c", a=nct)),
                               [po], [Vcmp])
                    tr.end_phase(ph)

                with ExitStack() as ph:
                    QT = [tr.buf(ph, "QT%d" % i, [128, 512], BF16) for i in range(2)]
                    Kc = [tr.buf(ph, "Kc%d" % i, [128, 2048], BF16) for i in range(3)]
                    Vc = [tr.buf(ph, "Vc%d" % i, [128, 16, 128], BF16) for i in range(3)]
                    PT = [tr.buf(ph, "PT%d" % i, [128, 512], BF16) for i in range(4)]
                    ET = tr.buf(ph, "ET", [128, 4, nct, 512], BF16)
                    acc = [tr.buf(ph, "acc%d" % i, [128, 512], F32) for i in range(4)]
                    rec = [tr.buf(ph, "rec%d" % i, [128, 512], F32) for i in range(2)]
                    rgt = [tr.buf(ph, "rgt%d" % i, [128, 512], F32) for i in range(2)]
                    tmc = [tr.buf(ph, "tmc%d" % i, [128, 512], F32) for i in range(2)]
                    rho1 = [tr.buf(ph, "rho1_%d" % i, [1, 512], BF16) for i in range(2)]
                    gsbB = tr.buf(ph, "gsbB", [12, 512], BF16)
                    Gp = [tr.buf(ph, "Gp%d" % i, [128, 32], F32) for i in range(2)]
                    m8 = [tr.buf(ph, "m8_%d" % i, [128, 8], F32) for i in range(2)]
                    m8b = [tr.buf(ph, "m8b_%d" % i, [128, 8], F32) for i in range(2)]
                    Bq = [tr.buf(ph, "Bq%d" % i, [128, 32], BF16) for i in range(2)]
                    BtM = [tr.buf(ph, "BtM%d" % i, [32, 512], BF16) for i in range(2)]
                    imp = [tr.buf(ph, "imp%d" % i, [128, n_sb], F32) for i in range(2)]
                    sc2 = tr.buf(ph, "sc2", [128, n_sb], F32)
                    BqN = [tr.buf(ph, "BqN%d" % i, [128, n_sb], BF16) for i in range(2)]
                    BtN = tr.buf(ph, "BtN", [128, 512], BF16)
                    rd = [tr.buf(ph, "rd%d" % i, [128, 1], F32) for i in range(2)]
                    Ost = [tr.buf(ph, "Ost%d" % i, [128, 512], BF16) for i in range(3)]
                    psL = [ps[0], ps[1]]
                    psN = [ps[2], ps[3]]
                    psD = [ps[4], ps[5]]
                    psX = ps[6]
                    cnt = dict(q=0, kv=0, pt=0, job=0, rho=0, ost=0, u=0, x=0)

                    def load_q(ci, tsl):
                        q_ = QT[cnt['q'] % 2]
                        cnt['q'] += 1
                        tr.dma('sp', q_[:], FM[ci, :, tsl], q_, True)
                        return q_

                    def load_kv(kci, vcol, j0, j1):
                        res = {}
                        ck0, ck1 = j0 // 16, j1 // 16
                        for ck in range(ck0, ck1 + 1):
                            ja = max(j0, ck * 16)
                            jb = min(j1, ck * 16 + 15)
                            kb = Kc[cnt['kv'] % 3]
                            vb = Vc[cnt['kv'] % 3]
                            cnt['kv'] += 1
                            n = jb - ja + 1
                            tr.dma('sp', kb[:, 0:n * 128], FM[kci, :, ja * 128:(jb + 1) * 128], kb, True)
                            tr.dma('sp', vb[:, 0:n, :],
                                   TM[ja * 128:(jb + 1) * 128, vcol:vcol + 128].rearrange("(j p) d -> p j d", p=128), vb, True)
                            for j in range(ja, jb + 1):
                                res[j] = (kb[:, (j - ja) * 128:(j - ja + 1) * 128], kb, vb[:, j - ja, :], vb)
                        return res

                    def attn_job(q_, units, finalize):
                        jn = cnt['job']
                        cnt['job'] += 1
                        pN = psN[jn % 2]
                        pD = psD[jn % 2]
                        n = len(units)
                        pts = [None] * n

                        def emit_S(i):
                            u = units[i]
                            f0, f1 = u.get('f0', 0), u.get('f1', 512)
                            pl = psL[cnt['pt'] % 2]
                            ex = u.get('extras', [])
                            op('pe', lambda e: e.matmul(pl[:, f0:f1], lhsT=u['k'], rhs=q_[:, f0:f1], start=True,
                                                        stop=(len(ex) == 0)), [u['kb'], q_], [pl])
                            for xi, (la, ra, bl) in enumerate(ex):
                                op('pe', lambda e: e.matmul(pl[:, f0:f1], lhsT=la, rhs=ra, start=False,
                                                            stop=(xi == len(ex) - 1)), bl, [pl])
                            if 'pt' in u:
                                pap, pbuf = u['pt']
                            else:
                                pbuf = PT[cnt['pt'] % 4]
                                pap = pbuf[:]
                            cnt['pt'] += 1
                            if u.get('bias') is not None:
                                bap, bbuf = u['bias']
                                op('act', lambda e: e.activation(out=pap[:, f0:f1], in_=pl[:, f0:f1], func=AF.Exp, scale=SCL,
                                                                 bias=bap), [pl, bbuf], [pbuf])
                            else:
                                op('act', lambda e: e.activation(out=pap[:, f0:f1], in_=pl[:, f0:f1], func=AF.Exp, scale=SCL),
                                   [pl], [pbuf])
                            pts[i] = (pap, pbuf)

                        def emit_PV(i):
                            u = units[i]
                            f0, f1 = u.get('f0', 0), u.get('f1', 512)
                            pap, pbuf = pts[i]
                            op('pe', lambda e: e.matmul(pN[:, f0:f1], lhsT=u['v'], rhs=pap[:, f0:f1], start=(i == 0),
                                                        stop=(i == n - 1)), [u['vb'], pbuf], [pN])
                            op('pe', lambda e: e.matmul(pD[:, f0:f1], lhsT=cv('ones'), rhs=pap[:, f0:f1], start=(i == 0),
                                                        stop=(i == n - 1)), [cstb, pbuf], [pD])

                        for i in range(n + 1):
                            if i < n:
                                emit_S(i)
                            if i >= 1:
                                emit_PV(i - 1)
                        finalize(pN, pD)

                    def recip_den(pD):
                        r_ = rec[cnt['x'] % 2]
                        cnt['x'] += 1
                        op('dve', lambda e: e.tensor_scalar(out=r_[:], in0=pD[:], scalar1=TINY, scalar2=None, op0=ALU.max),
                           [pD], [r_])
                        op('dve', lambda e: e.reciprocal(out=r_[:], in_=r_[:]), [r_], [r_])
                        return r_

                    def fin_plain(hc, tsl):
                        def f(pN, pD):
                            r_ = recip_den(pD)
                            o_ = Ost[cnt['ost'] % 3]
                            cnt['ost'] += 1
                            op('dve', lambda e: e.tensor_tensor(out=o_[:], in0=pN[:], in1=r_[:], op=ALU.mult), [pN, r_], [o_])
                            tr.dma('pool', OTD[hc, :, tsl], o_[:], o_, False)
                        return f

                    def fin_nsa(hh, br, tsl):
                        def f(pN, pD):
                            r_ = recip_den(pD)
                            rr = 3 * hh + br
                            op('pe', lambda e: e.matmul(psX[:], lhsT=cv('eg', 12, (rr * 128, (rr + 1) * 128)), rhs=gsbB[:],
                                                        start=True, stop=True), [cstb, gsbB], [psX])
                            g_ = rgt[cnt['x'] % 2]
                            op('dve', lambda e: e.tensor_tensor(out=g_[:], in0=psX[:], in1=r_[:], op=ALU.mult), [psX, r_], [g_])
                            a_ = acc[hh]
                            if br == 0:
                                op('dve', lambda e: e.tensor_tensor(out=a_[:], in0=pN[:], in1=g_[:], op=ALU.mult), [pN, g_], [a_])
                            else:
                                t_ = tmc[cnt['x'] % 2]
                                op('dve', lambda e: e.tensor_tensor(out=t_[:], in0=pN[:], in1=g_[:], op=ALU.mult), [pN, g_], [t_])
                                op('pool', lambda e: e.tensor_tensor(out=a_[:], in0=a_[:], in1=t_[:], op=ALU.add), [a_, t_], [a_])
                            if br == 2:
                                o_ = Ost[cnt['ost'] % 3]
                                cnt['ost'] += 1
                                op('pool', lambda e: e.tensor_copy(out=o_[:], in_=a_[:]), [a_], [o_])
                                tr.dma('pool', OTD[4 + hh, :, tsl], o_[:], o_, False)
                        return f

                    causal = cv('causal')

                    def diag_extra(jd):
                        f0 = 128 * jd
                        return (cv('i30k'), cv('causal', 128, (0, 512 - f0)), [cstb]), f0

                    for b in range(NB):
                        t0 = b * 512
                        tsl = slice(t0, t0 + 512)
                        jlast = 4 * b + 3
                        for hh in range(8):
                            q_ = load_q(16 + hh, tsl)
                            r1 = rho1[cnt['rho'] % 2]
                            cnt['rho'] += 1
                            tr.dma('sp', r1[:], RHO[hh:hh + 1, tsl], r1, True)
                            kv = load_kv(24 + hh, 768 + 128 * hh, 0, jlast)
                            units = []
                            for j in range(jlast + 1):
                                ka, kb, va, vb = kv[j]
                                u = dict(k=ka, kb=kb, v=va, vb=vb, bias=(CK[:, j, hh:hh + 1], CK))
                                f0 = 0
                                ex = []
                                if j >= 4 * b:
                                    dx, f0 = diag_extra(j - 4 * b)
                                    ex.append(dx)
                                ex.insert(0, (cv('ones', 1, (0, 128)), r1[0:1, f0:512], [cstb, r1]))
                                u['extras'] = ex
                                u['f0'] = f0
                                units.append(u)
                            attn_job(q_, units, fin_plain(8 + hh, tsl))
                        for hh in range(4):
                            q_ = load_q(hh, tsl)
                            bt = BtM[hh % 2]
                            for i in range(4):
                                own = 2 * b + i // 2
                                gp = Gp[i % 2]
                                mm = m8[i % 2]
                                bq = Bq[i % 2]
                                op('pe', lambda e: e.matmul(psX[:, 0:32], lhsT=q_[:, 128 * i:128 * (i + 1)], rhs=kmh[:, hh, :],
                                                            start=True, stop=False), [q_, kmh], [psX])
                                op('pe', lambda e: e.matmul(psX[:, 0:32], lhsT=q_[:, 128 * i:128 * (i + 1)], rhs=kml[:, hh, :],
                                                            start=False, stop=True), [q_, kml], [psX])
                                op('pool', lambda e: e.memset(gp[:], -1e30), [], [gp])
                                if own > 0:
                                    op('dve', lambda e: e.tensor_copy(out=gp[:, 0:own], in_=psX[:, 0:own]), [psX], [gp])
                                op('dve', lambda e: e.max(out=mm[:], in_=gp[:]), [gp], [mm])
                                op('dve', lambda e: e.tensor_scalar(out=mm[:, 2:3], in0=mm[:, 2:3], scalar1=-5e29, scalar2=None,
                                                                    op0=ALU.max), [mm], [mm])
                                op('dve', lambda e: e.tensor_scalar(out=bq[:], in0=gp[:], scalar1=mm[:, 2:3], scalar2=1.0,
                                                                    op0=ALU.is_ge, op1=ALU.subtract), [gp, mm], [bq])
                                op('pool', lambda e: e.memset(bq[:, own:own + 1], 0.0), [], [bq])
                                if own + 1 < 32:
                                    op('pool', lambda e: e.memset(bq[:, own + 1:32], -1.0), [], [bq])
                                op('pe', lambda e: e.transpose(out=psb[0:32, 128 * i:128 * (i + 1)], in_=bq[:],
                                                               identity=cv('ident')), [bq, cstb], [psb])
                            op('dve', lambda e: e.tensor_copy(out=bt[:], in_=psb[0:32, 0:512]), [psb], [bt])
                            kv = load_kv(4 + hh, 128 * hh, 0, jlast)
                            units = []
                            for j in range(jlast + 1):
                                ka, kb, va, vb = kv[j]
                                u = dict(k=ka, kb=kb, v=va, vb=vb)
                                f0 = 0
                                ex = []
                                if j >= 4 * b:
                                    dx, f0 = diag_extra(j - 4 * b)
                                    ex.append(dx)
                                nblk = j // 2
                                ex.insert(0, (cv('am', 32, (nblk * 128, (nblk + 1) * 128)), bt[0:32, f0:512], [cstb, bt]))
                                u['extras'] = ex
                                u['f0'] = f0
                                units.append(u)
                            attn_job(q_, units, fin_plain(hh, tsl))
                        tr.dma('sp', gsbB[:], GT[:, tsl], gsbB, True)
                        jcs = [jc for jc in range(nct) if b - 4 * jc >= 0]
                        for hh in range(4):
                            q_ = load_q(8 + hh, tsl)
                            units = []
                            for jc in jcs:
                                dlt = b - 4 * jc
                                u = dict(k=KcmpT[:, jc * 128:(jc + 1) * 128], kb=KcmpT, v=Vcmp[:, jc, :], vb=Vcmp,
                                         pt=(ET[:, hh, jc, :], ET))
                                if dlt <= 4:
                                    u['extras'] = [(cv('i30k'), cv('cmask', 128, (dlt * 512, (dlt + 1) * 512)), [cstb])]
                                units.append(u)
                            attn_job(q_, units, fin_nsa(hh, 0, tsl))
                        for i in range(4):
                            im = imp[i % 2]
                            for hh in range(4):
                                pu = psL[cnt['u'] % 2]
                                cnt['u'] += 1
                                for xi, jc in enumerate(jcs):
                                    op('pe', lambda e: e.matmul(pu[:, 0:n_sb + 1], lhsT=ET[:, hh, jc, 128 * i:128 * (i + 1)],
                                                                rhs=cv('ov', 128, (jc * (n_sb + 1), (jc + 1) * (n_sb + 1))),
                                                                start=(xi == 0), stop=(xi == len(jcs) - 1)), [ET, cstb], [pu])
                                rd_ = rd[hh % 2]
                                op('dve', lambda e: e.tensor_scalar(out=rd_[:], in0=pu[:, n_sb:n_sb + 1], scalar1=TINY,
                                                                    scalar2=None, op0=ALU.max), [pu], [rd_])
                                op('dve', lambda e: e.reciprocal(out=rd_[:], in_=rd_[:]), [rd_], [rd_])
                                if hh == 0:
                                    op('dve', lambda e: e.tensor_scalar(out=im[:], in0=pu[:, 0:n_sb], scalar1=rd_[:, 0:1],
                                                                        scalar2=None, op0=ALU.mult), [pu, rd_], [im])
                                else:
                                    op('dve', lambda e: e.scalar_tensor_tensor(out=im[:], in0=pu[:, 0:n_sb], scalar=rd_[:, 0:1],
                                                                               in1=im[:], op0=ALU.mult, op1=ALU.add),
                                       [pu, rd_, im], [im])
                            for hf in range(2):
                                cur = 8 * b + 2 * i + hf
                                rows = slice(64 * hf, 64 * hf + 64)
                                if cur - 1 >= 0:
                                    op('pool', lambda e: e.memset(im[rows, cur - 1:cur], 2e30), [], [im])
                                op('pool', lambda e: e.memset(im[rows, cur:cur + 1], 1e30), [], [im])
                                if cur + 1 < n_sb:
                                    op('pool', lambda e: e.memset(im[rows, cur + 1:n_sb], -2e30), [], [im])
                            op('pool', lambda e: e.memset(im[:, 0:1], 3e30), [], [im])
                            ma = m8[i % 2]
                            mb = m8b[i % 2]
                            bqn = BqN[i % 2]
                            op('dve', lambda e: e.max(out=ma[:], in_=im[:]), [im], [ma])
                            op('dve', lambda e: e.match_replace(out=sc2[:], in_to_replace=ma[:], in_values=im[:],
                                                                imm_value=-2e30), [ma, im], [sc2])
                            op('dve', lambda e: e.max(out=mb[:], in_=sc2[:]), [sc2], [mb])
                            op('dve', lambda e: e.tensor_scalar(out=mb[:, 7:8], in0=mb[:, 7:8], scalar1=-1e30, scalar2=None,
                                                                op0=ALU.max), [mb], [mb])
                            op('dve', lambda e: e.tensor_scalar(out=bqn[:], in0=im[:], scalar1=mb[:, 7:8], scalar2=1.0,
                                                                op0=ALU.is_ge, op1=ALU.subtract), [im, mb], [bqn])
                            op('pe', lambda e: e.transpose(out=psb[0:n_sb, 128 * i:128 * (i + 1)], in_=bqn[:],
                                                           identity=cv('ident')), [bqn, cstb], [psb])
                        op('dve', lambda e: e.tensor_copy(out=BtN[0:n_sb, :], in_=psb[0:n_sb, 0:512]), [psb], [BtN])
                        for hh in range(4):
                            q_ = load_q(8 + hh, tsl)
                            kv = load_kv(14, 512, 0, jlast)
                            units = []
                            for j in range(jlast + 1):
                                ka, kb, va, vb = kv[j]
                                u = dict(k=ka, kb=kb, v=va, vb=vb)
                                f0 = 0
                                ex = []
                                if j >= 4 * b:
                                    dx, f0 = diag_extra(j - 4 * b)
                                    ex.append(dx)
                                w_ = (2 * j) // KW
                                jj = j % (KW // 2)
                                o_an = coff['an'][0]
                                ex.insert(0, (cstb[w_ * KW:(w_ + 1) * KW, o_an + jj * 128:o_an + (jj + 1) * 128],
                                              BtN[w_ * KW:(w_ + 1) * KW, f0:512], [cstb, BtN]))
                                u['extras'] = ex
                                u['f0'] = f0
                                units.append(u)
                            attn_job(q_, units, fin_nsa(hh, 1, tsl))
                        for hh in range(4):
                            q_ = load_q(8 + hh, tsl)
                            jfirst = max(0, 4 * b - 4)
                            kv = load_kv(15, 640, jfirst, jlast)
                            units = []
                            for j in range(jfirst, jlast + 1):
                                ka, kb, va, vb = kv[j]
                                u = dict(k=ka, kb=kb, v=va, vb=vb)
                                if j >= 4 * b:
                                    dx, f0 = diag_extra(j - 4 * b)
                                    u['extras'] = [dx]
                                    u['f0'] = f0
                                else:
                                    jl = j - (4 * b - 4)
                                    u['f0'] = 0
                                    u['f1'] = 128 * (jl + 1)
                                    u['extras'] = [(cv('i30k'), cv('w2', 128, (384 - 128 * jl, 512)), [cstb])]
                                units.append(u)
                            attn_job(q_, units, fin_nsa(hh, 2, tsl))
                    tr.end_phase(ph)

                with ExitStack() as ph:
                    x1 = tr.buf(ph, "x1", [128, KC, 512], F32)
                    yT = tr.buf(ph, "yT", [128, KC, 512], F32)
                    OTs = tr.buf(ph, "OTs", [128, KC, 512], BF16)
                    sq = [tr.buf(ph, "sq%d" % i, [128, 512], BF16) for i in range(2)]
                    tmpn = tr.buf(ph, "tmpn", [128, 512], F32)
                    rstd = tr.buf(ph, "rstd", [128, 512], F32)
                    tmpx = [tr.buf(ph, "tmpx%d" % i, [128, 512], F32) for i in range(2)]
                    tmpu = [tr.buf(ph, "tmpu%d" % i, [128, 512], F32) for i in range(2)]
                    pcn = [0]
                    h2 = hT[0]
                    aT = hT[1]

                    def gemm16(l_, c0, src, outfn):
                        for oc in range(16):
                            w = load_w(l_, c0 + oc)
                            pp = ps[pcn[0] % 3]
                            pcn[0] += 1
                            for k in range(KC):
                                op('pe', lambda e: e.matmul(pp[:], lhsT=w[:, k, :], rhs=src[:, k, :], start=(k == 0),
                                                            stop=(k == KC - 1)), [w, src], [pp])
                            outfn(oc, pp)

                    def sumsq(src, pn):
                        for k in range(KC):
                            s_ = sq[k % 2]
                            op('act', lambda e: e.activation(out=s_[:], in_=src[:, k, :], func=AF.Square), [src], [s_])
                            op('pe', lambda e: e.matmul(pn[:], lhsT=cv('ones'), rhs=s_[:], start=(k == 0), stop=(k == KC - 1)),
                               [s_, cstb], [pn])

                    def resid(di):
                        for k in range(KC):
                            tx = tmpx[k % 2]
                            op('dve', lambda e: e.tensor_tensor(out=tx[:], in0=yT[:, k, :], in1=rstd[:], op=ALU.mult),
                               [yT, rstd], [tx])
                            op('dve', lambda e: e.scalar_tensor_tensor(out=x1[:, k, :], in0=tx[:], scalar=der[:, l, di, k:k + 1],
                                                                       in1=x1[:, k, :], op0=ALU.mult, op1=ALU.add),
                               [tx, der, x1], [x1])

                    for b in range(NB):
                        t0 = b * 512
                        tsl = slice(t0, t0 + 512)
                        tr.dma('sp', OTs[:], OTD[:, :, tsl].rearrange("c p t -> p c t"), OTs, True)
                        tr.dma('sp', x1[:], xin[:, :, tsl], x1, True)
                        pn = ps[6]

                        def out_c(oc, pp):
                            op('act', lambda e: e.copy(out=yT[:, oc, :], in_=pp[:]), [pp], [yT])
                        gemm16(l, 47, OTs, out_c)
                        sumsq(yT, pn)
                        rms_rstd((tmpn, rstd), pn)
                        resid(2)
                        sumsq(x1, pn)
                        rms_rstd((tmpn, rstd), pn)
                        for k in range(KC):
                            tx = tmpx[k % 2]
                            op('dve', lambda e: e.tensor_tensor(out=tx[:], in0=x1[:, k, :], in1=rstd[:], op=ALU.mult),
                               [x1, rstd], [tx])
                            op('pool', lambda e: e.tensor_scalar(out=h2[:, k, :], in0=tx[:], scalar1=der[:, l, 3, k:k + 1],
                                                                 scalar2=der[:, l, 4, k:k + 1], op0=ALU.mult, op1=ALU.add),
                               [tx, der], [h2])
                        for q in range(4):
                            def out_u(fc_, pp):
                                tu = tmpu[fc_ % 2]
                                op('act', lambda e: e.activation(out=tu[:], in_=pp[:], func=AF.Relu), [pp], [tu])
                                op('dve', lambda e: e.tensor_tensor(out=aT[:, fc_, :], in0=tu[:], in1=tu[:], op=ALU.mult),
                                   [tu], [aT])
                            gemm16(l, 63 + 16 * q, h2, out_u)

                            def out_d(dc, pp):
                                if q == 0:
                                    op('act', lambda e: e.copy(out=yT[:, dc, :], in_=pp[:]), [pp], [yT])
                                else:
                                    op('dve', lambda e: e.tensor_tensor(out=yT[:, dc, :], in0=pp[:], in1=yT[:, dc, :],
                                                                        op=ALU.add), [pp, yT], [yT])
                            gemm16(l, 127 + 16 * q, aT, out_d)
                        sumsq(yT, pn)
                        rms_rstd((tmpn, rstd), pn)
                        resid(5)
                        tr.dma('pool', xout[:, :, tsl], x1[:], x1, False)
                    tr.end_phase(ph)
        tr.barrier()
        ninst = tr.ninst
    return nc, ninst


def fm_layout(v):
    v = np.asarray(v, np.float32)
    lead = v.shape[:-1]
    return np.ascontiguousarray(np.moveaxis(v.reshape(lead + (v.shape[-1] // 128, 128)), -1, 0))


def make_in_maps(inp, S, NL):
    f = lambda a: np.ascontiguousarray(np.asarray(a, np.float32))
    B = inp['x'].shape[0]
    cst, fcst = make_consts(S)
    gains = np.stack([np.asarray(inp[k], np.float32)[:NL] for k in ('g_pre_mix', 'g_post_mix', 'g_pre_mlp', 'g_post_mlp')], axis=1)
    gains = fm_layout(gains)
    bmodT = fm_layout(np.asarray(inp['b_mod'], np.float32)[:NL])
    negb = np.ascontiguousarray(np.asarray(inp['b_forget'], np.float32)[:NL].T)
    peT = np.stack([np.asarray(inp['cmp_pe_k'], np.float32)[:NL], np.asarray(inp['cmp_pe_v'], np.float32)[:NL]], axis=1)
    peT = np.ascontiguousarray(peT.transpose(3, 0, 1, 2))
    shared = dict(
        w_mod=f(inp['w_mod'][:NL]), bmodT=bmodT, gains=gains, w_in=f(inp['w_in'][:NL]), negb=negb, peT=peT,
        w1k=f(inp['cmp_w1_k'][:NL]), w2k=f(inp['cmp_w2_k'][:NL]), w1v=f(inp['cmp_w1_v'][:NL]), w2v=f(inp['cmp_w2_v'][:NL]),
        w_out=f(inp['w_out'][:NL]), w_up=f(inp['w_up'][:NL]), w_down=f(inp['w_down'][:NL]), cst=cst, fcst=fcst)
    maps = []
    for b in range(B):
        xb = np.asarray(inp['x'][b], np.float32)[:S]
        xT = np.ascontiguousarray(xb.T.reshape(KC, 128, S).transpose(1, 0, 2))
        m = dict(shared)
        m['xT'] = xT
        m['pos'] = np.ascontiguousarray(np.asarray(inp['positions'][b], np.int32)[None, :S])
        m['cT'] = fm_layout(np.asarray(inp['c'][b], np.float32))
        maps.append(m)
    return maps


def run(inp, S, NL):
    nc, ninst = build_program(S, NL)
    maps = make_in_maps(inp, S, NL)
    res = run_bass_kernel_spmd(nc, maps, core_ids=list(range(len(maps))))
    outs = []
    for r in res.results:
        yT = np.asarray(r['yT'], np.float32)
        outs.append(yT.transpose(2, 1, 0).reshape(S, D))
    return np.stack(outs, axis=0)


def kernel(**inputs):
    return run(inputs, 8192, 4)
```

```python
import numpy as np
from contextlib import ExitStack
import concourse.bass as bass
import concourse.mybir as mybir
from concourse.bass_utils import run_bass_kernel_spmd

F32 = mybir.dt.float32
BF16 = mybir.dt.bfloat16
I32 = mybir.dt.int32
ALU = mybir.AluOpType
AF = mybir.ActivationFunctionType
AX = mybir.AxisListType

D = 2048
KC = 16
HD = 128
PROJ = 5908
DFF = 8192
SCL = 128.0 ** -0.5
EPS = 1e-6
NCH = 201
TINY = 1e-30


class Buf:
    __slots__ = ('t', 'name', 'w', 'r', 'sem', 'sval')

    def __init__(self, t, name):
        self.t = t
        self.name = name
        self.w = None
        self.r = {}
        self.sem = None
        self.sval = 0

    def __getitem__(self, k):
        return self.t[k]


class TR:
    ENG = ('pe', 'act', 'dve', 'pool', 'sp')

    def __init__(self, nc, st):
        self.nc = nc
        self.st = st
        self.h = dict(pe=nc.tensor, act=nc.scalar, dve=nc.vector, pool=nc.gpsimd, sp=nc.sync)
        self.sem = {e: st.enter_context(nc.semaphore("c_" + e)) for e in self.ENG}
        self.cnt = {e: 0 for e in self.ENG}
        self.seen = {e: {} for e in self.ENG}
        self.dsems = []
        self.freesems = []
        self.by_stack = {}
        self.nbuf = 0
        self.ninst = 0

    def buf(self, st, name, shape, dtype, psum=False):
        self.nbuf += 1
        nm = "%s_%d" % (name, self.nbuf)
        if psum:
            t = st.enter_context(self.nc.psum_tensor(nm, shape, dtype))
        else:
            t = st.enter_context(self.nc.sbuf_tensor(nm, shape, dtype))
        b = Buf(t, nm)
        self.by_stack.setdefault(id(st), []).append(b)
        return b

    def _wait(self, eng, rec):
        kind, who, val = rec
        if kind == 'e':
            if who == eng and eng == 'pe':
                return
            key = ('e', who)
            semh = self.sem[who]
        else:
            key = ('d', who.name)
            semh = who.sem
            val = who.sval
        if self.seen[eng].get(key, 0) >= val:
            return
        self.h[eng].wait_ge(semh, val)
        self.ninst += 1
        self.seen[eng][key] = val

    def op(self, eng, fn, reads=(), writes=()):
        recs = []
        for b in reads:
            if b.w is not None:
                recs.append(b.w)
        for b in writes:
            if b.w is not None:
                recs.append(b.w)
            recs.extend(b.r.values())
        for rec in recs:
            self._wait(eng, rec)
        inst = fn(self.h[eng])
        self.cnt[eng] += 1
        self.ninst += 1
        inst.then_inc(self.sem[eng], 1)
        rec = ('e', eng, self.cnt[eng])
        for b in reads:
            b.r[('e', eng)] = rec
        for b in writes:
            b.w = rec
            b.r = {}
        return inst

    def dma(self, eng, out, in_, sb, load):
        recs = []
        if sb.w is not None:
            recs.append(sb.w)
        if load:
            recs.extend(sb.r.values())
        if sb.sem is None:
            if self.freesems:
                sb.sem, sb.sval = self.freesems.pop()
            else:
                sb.sem = self.st.enter_context(self.nc.semaphore("d_" + sb.name))
                sb.sval = 0
            self.dsems.append(sb)
        if sb.sval > 0:
            recs.append(('d', sb, sb.sval))
        for rec in recs:
            self._wait(eng, rec)
        inst = self.h[eng].dma_start(out=out, in_=in_)
        self.ninst += 1
        sb.sval += 16
        inst.then_inc(sb.sem, 16)
        rec = ('d', sb, sb.sval)
        if load:
            sb.w = rec
            sb.r = {}
        else:
            sb.r[('d', sb.name)] = rec
        return inst

    def barrier(self):
        for e in self.ENG:
            for e2 in self.ENG:
                if e2 != e and self.cnt[e2] > 0:
                    self._wait(e, ('e', e2, self.cnt[e2]))
            for b in self.dsems:
                if b.sval > 0:
                    self._wait(e, ('d', b, b.sval))

    def end_phase(self, st):
        self.barrier()
        names = set(b.name for b in self.by_stack.pop(id(st), []))
        keep = []
        for b in self.dsems:
            if b.name in names:
                self.freesems.append((b.sem, b.sval))
            else:
                keep.append(b)
        self.dsems = keep


def const_layout(S):
    n_sb = S // 64
    KW = min(64, n_sb)
    nct = max(1, S // 2048)
    off = {}
    o = 0
    for name, n in [('ident', 128), ('i30k', 128), ('causal', 512), ('w2', 512), ('cmask', 5 * 512),
                    ('am', 32 * 128), ('an', (S // 128) * 128), ('pmt', 32), ('ov', nct * (n_sb + 1)),
                    ('eg', 12 * 128)]:
        off[name] = (o, n)
        o += n
    return off, o


def make_consts(S):
    n_sb = S // 64
    KW = min(64, n_sb)
    nct = max(1, S // 2048)
    ncmp = S // 16 - 1
    off, tot = const_layout(S)
    c = np.zeros((128, tot), np.float32)
    p = np.arange(128)[:, None]

    def put(name, arr):
        o, n = off[name]
        assert arr.shape == (128, n), (name, arr.shape, n)
        c[:, o:o + n] = arr
    put('ident', np.eye(128, dtype=np.float32))
    put('i30k', 30000.0 * np.eye(128, dtype=np.float32))
    f = np.arange(512)[None, :]
    put('causal', np.where(f >= p, 0.0, -1.0).astype(np.float32))
    put('w2', np.where(f - 384 < p, 0.0, -1.0).astype(np.float32))
    cm = [np.where(16 * p - f <= 512 * d - 31, 0.0, -1.0) for d in range(5)]
    put('cmask', np.concatenate(cm, axis=1).astype(np.float32))
    am = np.zeros((128, 32, 128), np.float32)
    for n in range(32):
        am[n, n, :] = 30000.0
    put('am', am.reshape(128, -1))
    an = np.zeros((128, S // 128, 128), np.float32)
    for j in range(S // 128):
        an[2 * j, j, 0:64] = 30000.0
        an[2 * j + 1, j, 64:128] = 30000.0
    put('an', an.reshape(128, -1))
    pm = np.zeros((128, 32), np.float32)
    for i in range(16):
        pm[i + 16, i] = -1.0
        pm[i, i + 16] = 1.0
    put('pmt', pm)
    ov = np.zeros((128, nct, n_sb + 1), np.float32)
    for cc in range(ncmp):
        jc, pp = divmod(cc, 128)
        n0 = cc // 4
        ov[pp, jc, n0] = 1.0
        if cc % 4 == 3 and n0 + 1 < n_sb:
            ov[pp, jc, n0 + 1] = 1.0
        ov[pp, jc, n_sb] = 1.0
    put('ov', ov.reshape(128, -1))
    eg = np.zeros((128, 12, 128), np.float32)
    for r in range(12):
        eg[r, r, :] = 1.0
    put('eg', eg.reshape(128, -1))
    fc = np.zeros((128, 4), np.float32)
    invf = 500000.0 ** (-(np.arange(16, dtype=np.float64)) / 16.0)
    fc[0:32, 0] = np.tile(invf / (2 * np.pi), 2).astype(np.float32)
    return c, fc


def build_program(S, NL):
    assert S % 2048 == 0
    NB = S // 512
    NT = S // 128
    n_sb = S // 64
    KW = min(64, n_sb)
    nct = S // 2048
    ncmp = S // 16 - 1
    NCP = nct * 128
    assert NCP <= 512
    nmb = S // 256
    coff, ctot = const_layout(S)

    nc = bass.Bass("TRN2", target_bir_lowering=False)

    def din(name, shape, dt=F32):
        return nc.dram_tensor(name, shape, dt, kind="ExternalInput").ap()

    def dscr(name, shape, dt):
        return nc.dram_tensor(name, shape, dt, kind="Internal").ap()

    x_in = din("xT", [128, KC, S])
    pos_in = din("pos", [1, S], I32)
    cT_in = din("cT", [128, KC])
    w_mod = din("w_mod", [NL, D, 6 * D])
    bmodT = din("bmodT", [128, NL, 96])
    gT_in = din("gains", [128, NL, 4, KC])
    w_in = din("w_in", [NL, D, PROJ])
    negb_in = din("negb", [8, NL])
    peT_in = din("peT", [128, NL, 2, 32])
    w1k = din("w1k", [NL, 4096, 256])
    w2k = din("w2k", [NL, 256, 128])
    w1v = din("w1v", [NL, 4096, 256])
    w2v = din("w2v", [NL, 256, 128])
    w_out = din("w_out", [NL, D, D])
    w_up = din("w_up", [NL, D, DFF])
    w_down = din("w_down", [NL, DFF, D])
    cst_in = din("cst", [128, ctot])
    fcst_in = din("fcst", [128, 4])
    y_out = nc.dram_tensor("yT", [128, KC, S], F32, kind="ExternalOutput").ap()

    WC = [dscr("WC%d" % l_, [NCH, 128, KC, 128], BF16) for l_ in range(NL)]
    FM = dscr("FM", [32, 128, S], BF16)
    TM = dscr("TM", [S, 1792], BF16)
    GT = dscr("GT", [12, S], BF16)
    RHO = dscr("RHO", [8, S], BF16)
    COS = dscr("COS", [32, S], F32)
    SIN = dscr("SIN", [32, S], F32)
    OTD = dscr("OTD", [16, 128, S], BF16)
    XS = [dscr("XS0", [128, KC, S], F32), dscr("XS1", [128, KC, S], F32)]
    CSTB = dscr("CSTB", [128, ctot], BF16)

    with ExitStack() as st:
        tr = TR(nc, st)
        op = tr.op

        onesb = tr.buf(st, "onesb", [128, 128], BF16)
        onesf = tr.buf(st, "onesf", [128, 128], F32)
        pmtb = tr.buf(st, "pmtb", [128, 32], BF16)
        identf = tr.buf(st, "identf", [128, 128], F32)
        fcst = tr.buf(st, "fcst", [128, 4], F32)
        onecol = tr.buf(st, "onecol", [128, 1], F32)
        der = tr.buf(st, "der", [128, NL, 6, KC], F32)
        negb = tr.buf(st, "negb", [8, NL], F32)
        peb = tr.buf(st, "peb", [128, NL, 2, 32], BF16)
        w4k = [tr.buf(st, "w4k%d" % i, [128, KC, 128], BF16) for i in range(4)]
        ps = [tr.buf(st, "ps%d" % i, [128, 512], F32, psum=True) for i in range(7)]
        psb = tr.buf(st, "psb", [128, 1024], BF16, psum=True)
        wslot = [0]

        cst_holder = [None]

        def cv(name, rows=128, sub=None):
            cstb = cst_holder[0]
            o, n = coff[name]
            if sub is None:
                return cstb[0:rows, o:o + n]
            return cstb[0:rows, o + sub[0]:o + sub[1]]

        def load_w(l, ci):
            b = w4k[wslot[0] % 4]
            wslot[0] += 1
            tr.dma('sp', b[:], WC[l][ci], b, True)
            return b

        fm_cols = ([i * 128 for i in range(4)] + [512 + i * 128 for i in range(4)] +
                   [1536 + i * 128 for i in range(4)] + [2048, 2176, 2304, 2560] +
                   [2828 + i * 128 for i in range(8)] + [3852 + i * 128 for i in range(8)])
        v_cols = ([1024 + i * 128 for i in range(4)] + [2432, 2688] + [4876 + i * 128 for i in range(8)])

        def prep_list(l):
            items = []
            for ci, c0 in enumerate(fm_cols):
                items.append((ci, [(w_in[l, :, c0:c0 + 128], 0)], KC))
            for ci, c0 in enumerate(v_cols):
                items.append((32 + ci, [(w_in[l, :, c0:c0 + 128], 0)], KC))
            items.append((46, [(w_in[l, :, 2816:2828], 0), (w_in[l, :, 5900:5908], 12)], KC))
            for oc in range(16):
                items.append((47 + oc, [(w_out[l, :, oc * 128:(oc + 1) * 128], 0)], KC))
            for fc_ in range(64):
                items.append((63 + fc_, [(w_up[l, :, fc_ * 128:(fc_ + 1) * 128], 0)], KC))
            for q in range(4):
                for dc in range(16):
                    items.append((127 + q * 16 + dc, [(w_down[l, q * 2048:(q + 1) * 2048, dc * 128:(dc + 1) * 128], 0)], KC))
            for wi, w1 in enumerate((w1k, w1v)):
                for g in range(2):
                    for hc in range(2):
                        items.append((191 + wi * 4 + g * 2 + hc,
                                      [(w1[l, g * 2048:(g + 1) * 2048, hc * 128:(hc + 1) * 128], 0)], KC))
            items.append((199, [(w2k[l, :, :], 0)], 2))
            items.append((200, [(w2v[l, :, :], 0)], 2))
            return items

        def prep_gen(l, stg, sto, cast_engs):
            nb_ = len(stg)
            for i, (ci, pieces, nk) in enumerate(prep_list(l)):
                sg = stg[i % nb_]
                so = sto[i % nb_]
                wtot = 0
                for (src, co) in pieces:
                    ncols = src.shape[1]
                    tr.dma('sp', sg[:, 0:nk, co:co + ncols], src.rearrange("(k p) c -> p k c", p=128), sg, True)
                    wtot = max(wtot, co + ncols)
                ce = cast_engs[i % len(cast_engs)]
                if ce == 'act':
                    op('act', lambda e: e.copy(out=so[:, 0:nk, 0:wtot], in_=sg[:, 0:nk, 0:wtot]), [sg], [so])
                else:
                    op(ce, lambda e: e.tensor_copy(out=so[:, 0:nk, 0:wtot], in_=sg[:, 0:nk, 0:wtot]), [sg], [so])
                tr.dma('pool', WC[l][ci, :, 0:nk, 0:wtot], so[:, 0:nk, 0:wtot], so, False)
                yield

        def advance(gen, n):
            for _ in range(n):
                try:
                    next(gen)
                except StopIteration:
                    return False
            return True

        with ExitStack() as ph:
            cstf = tr.buf(ph, "cstf", [128, ctot], F32)
            tr.dma('sp', cstf[:], cst_in, cstf, True)
            tr.dma('sp', fcst[:], fcst_in, fcst, True)
            bfg = tr.buf(ph, "bfg", [8, NL], F32)
            tr.dma('sp', bfg[:], negb_in, bfg, True)
            op('dve', lambda e: e.tensor_scalar(out=negb[:], in0=bfg[:], scalar1=-1.0, scalar2=None, op0=ALU.mult), [bfg], [negb])
            half = ctot // 2
            cstb = tr.buf(ph, "cstb0", [128, ctot], BF16)
            op('dve', lambda e: e.tensor_copy(out=cstb[:, 0:half], in_=cstf[:, 0:half]), [cstf], [cstb])
            op('pool', lambda e: e.tensor_copy(out=cstb[:, half:ctot], in_=cstf[:, half:ctot]), [cstf], [cstb])
            tr.dma('pool', CSTB, cstb[:], cstb, False)
            o_id = coff['ident'][0]
            op('dve', lambda e: e.tensor_copy(out=identf[:], in_=cstf[:, o_id:o_id + 128]), [cstf], [identf])
            o_pm = coff['pmt'][0]
            op('dve', lambda e: e.tensor_copy(out=pmtb[:], in_=cstf[:, o_pm:o_pm + 32]), [cstf], [pmtb])
            op('pool', lambda e: e.memset(onecol[:], 1.0), [], [onecol])
            op('pool', lambda e: e.memset(onesb[:], 1.0), [], [onesb])
            op('pool', lambda e: e.memset(onesf[:], 1.0), [], [onesf])
            pef = tr.buf(ph, "pef", [128, NL, 2, 32], F32)
            tr.dma('sp', pef[:], peT_in, pef, True)
            op('dve', lambda e: e.tensor_copy(out=peb[:], in_=pef[:]), [pef], [peb])
            tr.end_phase(ph)
        with ExitStack() as ph:
            CW = 2048
            posi = tr.buf(ph, "posi", [32, CW], I32)
            posf = tr.buf(ph, "posf", [32, CW], F32)
            ru = tr.buf(ph, "ru", [32, CW], F32)
            rni = tr.buf(ph, "rni", [32, CW], I32)
            rnf = tr.buf(ph, "rnf", [32, CW], F32)
            rtab = [tr.buf(ph, "rtab%d" % i, [32, CW], F32) for i in range(2)]
            for cidx in range(S // CW):
                sl = slice(cidx * CW, (cidx + 1) * CW)
                tr.dma('sp', posi[:], pos_in[0:1, sl].to_broadcast([32, CW]), posi, True)
                op('dve', lambda e: e.tensor_copy(out=posf[:], in_=posi[:]), [posi], [posf])
                for ti, (tab, addc) in enumerate(((SIN, 0.0), (COS, 0.25))):
                    op('dve', lambda e: e.tensor_scalar(out=ru[:], in0=posf[:], scalar1=fcst[0:32, 0:1], scalar2=addc,
                                                        op0=ALU.mult, op1=ALU.add), [posf, fcst], [ru])
                    op('dve', lambda e: e.tensor_copy(out=rni[:], in_=ru[:]), [ru], [rni])
                    op('dve', lambda e: e.tensor_copy(out=rnf[:], in_=rni[:]), [rni], [rnf])
                    op('dve', lambda e: e.tensor_tensor(out=ru[:], in0=ru[:], in1=rnf[:], op=ALU.subtract), [ru, rnf], [ru])
                    rt = rtab[ti]
                    op('act', lambda e: e.activation(out=rt[:], in_=ru[:], func=AF.Sin, scale=2.0 * np.pi), [ru], [rt])
                    tr.dma('pool', tab[:, sl], rt[:], rt, False)
            tr.end_phase(ph)
        with ExitStack() as ph:
            gains = tr.buf(ph, "gains", [128, NL, 4, KC], F32)
            tr.dma('sp', gains[:], gT_in, gains, True)
            bmod = tr.buf(ph, "bmod", [128, NL, 96], F32)
            tr.dma('sp', bmod[:], bmodT, bmod, True)
            cT = tr.buf(ph, "cT", [128, KC], F32)
            tr.dma('sp', cT[:], cT_in, cT, True)
            cs = tr.buf(ph, "cs", [128, KC], F32)
            op('act', lambda e: e.activation(out=cs[:], in_=cT[:], func=AF.Silu), [cT], [cs])
            modt = tr.buf(ph, "modt", [128, NL, 96], F32)
            wm = [tr.buf(ph, "wm%d" % i, [128, KC, 128], F32) for i in range(3)]
            stg0 = [tr.buf(ph, "stg%d" % i, [128, KC, 128], F32) for i in range(3)]
            sto0 = [tr.buf(ph, "sto%d" % i, [128, KC, 128], BF16) for i in range(3)]
            pg0 = prep_gen(0, stg0, sto0, ('dve', 'pool', 'act'))
            for l in range(NL):
                pm_ = ps[l % 2]
                for j in range(96):
                    b = wm[j % 3]
                    tr.dma('sp', b[:], w_mod[l, :, j * 128:(j + 1) * 128].rearrange("(k p) c -> p k c", p=128), b, True)
                    for k in range(KC):
                        op('pe', lambda e: e.matmul(pm_[:, j:j + 1], lhsT=b[:, k, :], rhs=cs[:, k:k + 1],
                                                    start=(k == 0), stop=(k == KC - 1)), [b, cs], [pm_])
                    advance(pg0, 1)
                op('dve', lambda e: e.tensor_tensor(out=modt[:, l, :], in0=pm_[:, 0:96], in1=bmod[:, l, :], op=ALU.add),
                   [pm_, bmod], [modt])
                for (di, si, gi) in ((0, 1, 0), (3, 4, 2)):
                    op('dve', lambda e: e.scalar_tensor_tensor(out=der[:, l, di, :], in0=modt[:, l, si * 16:(si + 1) * 16],
                                                               scalar=1.0, in1=gains[:, l, gi, :], op0=ALU.add, op1=ALU.mult),
                       [modt, gains], [der])
                for (di, si) in ((1, 0), (4, 3)):
                    op('dve', lambda e: e.tensor_copy(out=der[:, l, di, :], in_=modt[:, l, si * 16:(si + 1) * 16]), [modt], [der])
                for (di, si, gi) in ((2, 2, 1), (5, 5, 3)):
                    op('dve', lambda e: e.tensor_tensor(out=der[:, l, di, :], in0=modt[:, l, si * 16:(si + 1) * 16],
                                                        in1=gains[:, l, gi, :], op=ALU.mult), [modt, gains], [der])
            while advance(pg0, 8):
                pass
            tr.end_phase(ph)
        for l in range(NL):
            xin = x_in if l == 0 else XS[(l - 1) % 2]
            xout = y_out if l == NL - 1 else XS[l % 2]
            with ExitStack() as ly:
                kmh = tr.buf(ly, "kmh", [128, 4, 32], BF16)
                kml = tr.buf(ly, "kml", [128, 4, 32], BF16)
                CK = tr.buf(ly, "CK", [128, NT, 8], F32)
                KcmpT = tr.buf(ly, "KcmpT", [128, NCP], BF16)
                Vcmp = tr.buf(ly, "Vcmp", [128, nct, 128], BF16)

                def rms_rstd(ph_bufs, sumps):
                    tmpn, rstd = ph_bufs
                    op('dve', lambda e: e.tensor_scalar(out=tmpn[:], in0=sumps[:], scalar1=1.0 / D, scalar2=EPS,
                                                        op0=ALU.mult, op1=ALU.add), [sumps], [tmpn])
                    op('act', lambda e: e.activation(out=tmpn[:], in_=tmpn[:], func=AF.Sqrt), [tmpn], [tmpn])
                    op('dve', lambda e: e.reciprocal(out=rstd[:], in_=tmpn[:]), [tmpn], [rstd])

                with ExitStack() as ph:
                    hT = [tr.buf(ph, "hT%d" % i, [128, KC, 512], BF16) for i in range(2)]
                    xc = [tr.buf(ph, "xc%d" % i, [128, 2, 512], F32) for i in range(3)]
                    sq = [tr.buf(ph, "sq%d" % i, [128, 512], BF16) for i in range(2)]
                    tmpn = tr.buf(ph, "tmpn", [128, 512], F32)
                    rstd = tr.buf(ph, "rstd", [128, 512], F32)
                    tmpx = [tr.buf(ph, "tmpx%d" % i, [128, 512], F32) for i in range(2)]
                    wv = [tr.buf(ph, "wv%d" % i, [128, KC, 512], BF16) for i in range(2)]
                    qk = [tr.buf(ph, "qk%d" % i, [128, 512], BF16) for i in range(3)]
                    ropeA = tr.buf(ph, "ropeA", [32, 512], F32)
                    ropeB = tr.buf(ph, "ropeB", [32, 512], F32)
                    cosb = [tr.buf(ph, "cosb%d" % i, [32, 512], F32) for i in range(2)]
                    sinb = [tr.buf(ph, "sinb%d" % i, [32, 512], F32) for i in range(2)]
                    vout = [tr.buf(ph, "vout%d" % i, [128, 4, 512], BF16) for i in range(2)]
                    gsb = [tr.buf(ph, "gsb%d" % i, [12, 512], BF16) for i in range(2)]
                    fe = tr.buf(ph, "fe", [8, 512], F32)
                    fl = tr.buf(ph, "fl", [8, 512], F32)
                    Cb = [tr.buf(ph, "Cb%d" % i, [8, 512], F32) for i in range(2)]
                    rhob = [tr.buf(ph, "rhob%d" % i, [8, 512], BF16) for i in range(2)]
                    ones8 = tr.buf(ph, "ones8", [8, 512], F32)
                    kmf = tr.buf(ph, "kmf", [128, 4, 32], F32)
                    kmt = tr.buf(ph, "kmt", [128, 4, 32], F32)
                    op('pool', lambda e: e.memset(ones8[:], 1.0), [], [ones8])
                    op('pool', lambda e: e.memset(kmf[:], 0.0), [], [kmf])
                    xcn = [0]
                    qkn = [0]
                    pcn = [0]
                    vgn = [0]
                    roped = set(range(0, 13)) | {14, 15}
                    roped.discard(13)
                    vgroups = [(32, 4, 0), (36, 2, 512), (38, 4, 768), (42, 4, 1280)]
                    for b in range(NB):
                        t0 = b * 512
                        tsl = slice(t0, t0 + 512)
                        h = hT[b % 2]
                        pn = ps[6]
                        for g in range(8):
                            xb = xc[xcn[0] % 3]
                            xcn[0] += 1
                            tr.dma('sp', xb[:], xin[:, 2 * g:2 * g + 2, tsl], xb, True)
                            for c in range(2):
                                s_ = sq[(2 * g + c) % 2]
                                op('act', lambda e: e.activation(out=s_[:], in_=xb[:, c, :], func=AF.Square), [xb], [s_])
                                op('pe', lambda e: e.matmul(pn[:], lhsT=onesb[:], rhs=s_[:], start=(g == 0 and c == 0),
                                                            stop=(g == 7 and c == 1)), [s_, onesb], [pn])
                        rms_rstd((tmpn, rstd), pn)
                        for g in range(8):
                            xb = xc[xcn[0] % 3]
                            xcn[0] += 1
                            tr.dma('sp', xb[:], xin[:, 2 * g:2 * g + 2, tsl], xb, True)
                            for c in range(2):
                                k = 2 * g + c
                                tx = tmpx[k % 2]
                                op('dve', lambda e: e.tensor_tensor(out=tx[:], in0=xb[:, c, :], in1=rstd[:], op=ALU.mult),
                                   [xb, rstd], [tx])
                                op('pool', lambda e: e.tensor_scalar(out=h[:, k, :], in0=tx[:], scalar1=der[:, l, 0, k:k + 1],
                                                                     scalar2=der[:, l, 1, k:k + 1], op0=ALU.mult, op1=ALU.add),
                                   [tx, der], [h])
                        cb_ = cosb[b % 2]
                        sb_ = sinb[b % 2]
                        tr.dma('sp', cb_[:], COS[:, tsl], cb_, True)
                        tr.dma('sp', sb_[:], SIN[:, tsl], sb_, True)
                        for ci in range(32):
                            w = load_w(l, ci)
                            pp = ps[pcn[0] % 3]
                            pcn[0] += 1
                            for k in range(KC):
                                op('pe', lambda e: e.matmul(pp[:], lhsT=w[:, k, :], rhs=h[:, k, :], start=(k == 0),
                                                            stop=(k == KC - 1)), [w, h], [pp])
                            q_ = qk[qkn[0] % 3]
                            qkn[0] += 1
                            op('act', lambda e: e.copy(out=q_[:], in_=pp[:]), [pp], [q_])
                            if ci in roped:
                                pr = ps[3]
                                op('pe', lambda e: e.matmul(pr[0:32, :], lhsT=pmtb[:], rhs=q_[:], start=True, stop=True),
                                   [q_, pmtb], [pr])
                                op('dve', lambda e: e.tensor_tensor(out=ropeA[:], in0=pr[0:32, :], in1=sb_[:], op=ALU.mult),
                                   [pr, sb_], [ropeA])
                                op('dve', lambda e: e.tensor_tensor(out=ropeB[:], in0=q_[0:32, :], in1=cb_[:], op=ALU.mult),
                                   [q_, cb_], [ropeB])
                                op('dve', lambda e: e.tensor_tensor(out=q_[0:32, :], in0=ropeA[:], in1=ropeB[:], op=ALU.add),
                                   [ropeA, ropeB], [q_])
                            if 4 <= ci < 8:
                                op('dve', lambda e: e.tensor_reduce(out=kmf[:, ci - 4, 2 * b:2 * b + 2],
                                                                    in_=q_[:].rearrange("p (a c) -> p a c", a=2),
                                                                    axis=AX.X, op=ALU.add), [q_], [kmf])
                            tr.dma('pool', FM[ci, :, tsl], q_[:], q_, False)
                        for (c0, nchk, col0) in vgroups:
                            wvb = wv[vgn[0] % 2]
                            vo = vout[vgn[0] % 2]
                            vgn[0] += 1
                            ncols = nchk * 128
                            for i in range(nchk):
                                tr.dma('sp', wvb[:, :, i * 128:(i + 1) * 128], WC[l][c0 + i], wvb, True)
                            for tt in range(4):
                                pp = ps[pcn[0] % 3]
                                pcn[0] += 1
                                for k in range(KC):
                                    op('pe', lambda e: e.matmul(pp[:, 0:ncols], lhsT=h[:, k, tt * 128:(tt + 1) * 128],
                                                                rhs=wvb[:, k, 0:ncols], start=(k == 0), stop=(k == KC - 1)),
                                       [wvb, h], [pp])
                                if tt % 2 == 0:
                                    op('act', lambda e: e.copy(out=vo[:, tt, 0:ncols], in_=pp[:, 0:ncols]), [pp], [vo])
                                else:
                                    op('dve', lambda e: e.tensor_copy(out=vo[:, tt, 0:ncols], in_=pp[:, 0:ncols]), [pp], [vo])
                            tr.dma('pool', TM[t0:t0 + 512, col0:col0 + ncols].rearrange("(t p) c -> p t c", p=128),
                                   vo[:, :, 0:ncols], vo, False)
                        w = load_w(l, 46)
                        pg = ps[4]
                        for k in range(KC):
                            op('pe', lambda e: e.matmul(pg[0:12, :], lhsT=w[:, k, 0:12], rhs=h[:, k, :], start=(k == 0),
                                                        stop=(k == KC - 1)), [w, h], [pg])
                        g_ = gsb[b % 2]
                        op('act', lambda e: e.activation(out=g_[:], in_=pg[0:12, :], func=AF.Sigmoid), [pg], [g_])
                        tr.dma('pool', GT[:, tsl], g_[:], g_, False)
                        pf = ps[5]
                        for k in range(KC):
                            op('pe', lambda e: e.matmul(pf[0:8, :], lhsT=w[:, k, 12:20], rhs=h[:, k, :], start=(k == 0),
                                                        stop=(k == KC - 1)), [w, h], [pf])
                        op('act', lambda e: e.activation(out=fe[:], in_=pf[0:8, :], func=AF.Exp, scale=-1.0,
                                                         bias=negb[:, l:l + 1]), [pf, negb], [fe])
                        op('act', lambda e: e.activation(out=fl[:], in_=fe[:], func=AF.Ln, bias=onecol[0:8, 0:1]),
                           [fe, onecol], [fl])
                        cbuf = Cb[b % 2]
                        cprev = Cb[(b - 1) % 2]
                        if b == 0:
                            op('dve', lambda e: e.tensor_tensor_scan(out=cbuf[:], data0=ones8[:], data1=fl[:], initial=0.0,
                                                                     op0=ALU.mult, op1=ALU.add), [ones8, fl], [cbuf])
                        else:
                            op('dve', lambda e: e.tensor_tensor_scan(out=cbuf[:], data0=ones8[:], data1=fl[:],
                                                                     initial=cprev[:, 511:512], op0=ALU.mult, op1=ALU.add),
                               [ones8, fl, cprev], [cbuf])
                        rb_ = rhob[b % 2]
                        op('dve', lambda e: e.tensor_scalar(out=rb_[:], in0=cbuf[:], scalar1=-1.0 / (SCL * 128.0), scalar2=None,
                                                            op0=ALU.mult), [cbuf], [rb_])
                        tr.dma('pool', RHO[:, tsl], rb_[:], rb_, False)
                        pt_ = ps[3]
                        for i in range(4):
                            op('pe', lambda e: e.transpose(out=pt_[:, 8 * i:8 * i + 8], in_=cbuf[0:8, 128 * i:128 * (i + 1)],
                                                           identity=identf[0:8, 0:8]), [cbuf, identf], [pt_])
                        op('dve', lambda e: e.tensor_copy(out=CK[:, 4 * b:4 * b + 4, :],
                                                          in_=pt_[:, 0:32].rearrange("p (a c) -> p a c", a=4)), [pt_], [CK])
                    op('dve', lambda e: e.tensor_scalar(out=kmf[:], in0=kmf[:], scalar1=1.0 / 256.0, scalar2=None,
                                                        op0=ALU.mult), [kmf], [kmf])
                    op('dve', lambda e: e.tensor_copy(out=kmh[:], in_=kmf[:]), [kmf], [kmh])
                    op('dve', lambda e: e.tensor_copy(out=kmt[:], in_=kmh[:]), [kmh], [kmt])
                    op('dve', lambda e: e.tensor_tensor(out=kmt[:], in0=kmf[:], in1=kmt[:], op=ALU.subtract), [kmf, kmt], [kmt])
                    op('dve', lambda e: e.tensor_copy(out=kml[:], in_=kmt[:]), [kmt], [kml])
                    tr.end_phase(ph)

                with ExitStack() as ph:
                    kcS = tr.buf(ph, "kcS", [128, S], BF16)
                    Z = tr.buf(ph, "Z", [128, 16, S // 16], BF16)
                    W1 = tr.buf(ph, "W1", [128, 32, 256], BF16)
                    W2 = tr.buf(ph, "W2", [128, 2, 128], BF16)
                    bcol = tr.buf(ph, "bcol", [128, 2], F32)
                    xg = tr.buf(ph, "xg", [128, 512], F32)
                    x2 = tr.buf(ph, "x2", [128, 512], F32)
                    sg = tr.buf(ph, "sg", [128, 512], F32)
                    GTt = tr.buf(ph, "GTt", [128, 2, 512], BF16)
                    for wi in range(2):
                        tr.dma('sp', kcS[:], FM[12 + wi], kcS, True)
                        op('pool', lambda e: e.tensor_copy(out=Z[:], in_=kcS[:].rearrange("p (m r) -> p r m", r=16)), [kcS], [Z])
                        for g in range(2):
                            for hc in range(2):
                                tr.dma('sp', W1[:, 16 * g:16 * g + 16, hc * 128:(hc + 1) * 128],
                                       WC[l][191 + wi * 4 + g * 2 + hc], W1, True)
                        tr.dma('sp', W2[:], WC[l][199 + wi, :, 0:2, :], W2, True)
                        op('pool', lambda e: e.memset(GTt[:], 0.0), [], [GTt])
                        for hc in range(2):
                            phd = ps[hc]
                            pbs = ps[2]
                            for tau in range(32):
                                a, r = divmod(tau, 16)
                                op('pe', lambda e: e.matmul(phd[:, 0:ncmp], lhsT=W1[:, tau, hc * 128:(hc + 1) * 128],
                                                            rhs=Z[:, r, a:a + ncmp], start=(tau == 0), stop=(tau == 31)),
                                   [W1, Z], [phd])
                            for tau in range(32):
                                op('pe', lambda e: e.matmul(pbs[:, hc:hc + 1], lhsT=W1[:, tau, hc * 128:(hc + 1) * 128],
                                                            rhs=peb[:, l, wi, tau:tau + 1], start=(tau == 0), stop=(tau == 31)),
                                   [W1, peb], [pbs])
                            op('dve', lambda e: e.tensor_copy(out=bcol[:, hc:hc + 1], in_=pbs[:, hc:hc + 1]), [pbs], [bcol])
                            op('act', lambda e: e.activation(out=xg[:, 0:ncmp], in_=phd[:, 0:ncmp], func=AF.Identity,
                                                             bias=bcol[:, hc:hc + 1]), [phd, bcol], [xg])
                            op('dve', lambda e: e.tensor_tensor(out=x2[:, 0:ncmp], in0=xg[:, 0:ncmp], in1=xg[:, 0:ncmp],
                                                                op=ALU.mult), [xg], [x2])
                            op('dve', lambda e: e.tensor_scalar(out=x2[:, 0:ncmp], in0=x2[:, 0:ncmp], scalar1=0.0713548163,
                                                                scalar2=1.5957691216, op0=ALU.mult, op1=ALU.add), [x2], [x2])
                            op('dve', lambda e: e.tensor_tensor(out=x2[:, 0:ncmp], in0=x2[:, 0:ncmp], in1=xg[:, 0:ncmp],
                                                                op=ALU.mult), [x2, xg], [x2])
                            op('act', lambda e: e.activation(out=sg[:, 0:ncmp], in_=x2[:, 0:ncmp], func=AF.Sigmoid), [x2], [sg])
                            op('dve', lambda e: e.tensor_tensor(out=GTt[:, hc, 0:ncmp], in0=xg[:, 0:ncmp], in1=sg[:, 0:ncmp],
                                                                op=ALU.mult), [xg, sg], [GTt])
                        po = ps[3]
                        if wi == 0:
                            for hc in range(2):
                                op('pe', lambda e: e.matmul(po[:, 0:NCP], lhsT=W2[:, hc, :], rhs=GTt[:, hc, 0:NCP],
                                                            start=(hc == 0), stop=(hc == 1)), [W2, GTt], [po])
                            op('act', lambda e: e.copy(out=KcmpT[:], in_=po[:, 0:NCP]), [po], [KcmpT])
                        else:
                            for ct in range(nct):
                                for hc in range(2):
                                    op('pe', lambda e: e.matmul(po[:, ct * 128:(ct + 1) * 128],
                                                                lhsT=GTt[:, hc, ct * 128:(ct + 1) * 128], rhs=W2[:, hc, :],
                                                                start=(hc == 0), stop=(hc == 1)), [W2, GTt], [po])
                            op('act', lambda e: e.copy(out=Vcmp[:], in_=po[:, 0:NCP].rearrange("p (a c) -> p a c", a=nct)),
                               [po], [Vcmp])
                    tr.end_phase(ph)

                with ExitStack() as ph:
                    QT = [tr.buf(ph, "QT%d" % i, [128, 512], BF16) for i in range(2)]
                    Kc = [tr.buf(ph, "Kc%d" % i, [128, 2048], BF16) for i in range(3)]
                    Vc = [tr.buf(ph, "Vc%d" % i, [128, 16, 128], BF16) for i in range(3)]
                    PT = [tr.buf(ph, "PT%d" % i, [128, 512], BF16) for i in range(4)]
                    ET = tr.buf(ph, "ET", [128, 4, nct, 512], BF16)
                    acc = [tr.buf(ph, "acc%d" % i, [128, 512], F32) for i in range(4)]
                    rec = [tr.buf(ph, "rec%d" % i, [128, 512], F32) for i in range(2)]
                    rgt = [tr.buf(ph, "rgt%d" % i, [128, 512], F32) for i in range(2)]
                    tmc = [tr.buf(ph, "tmc%d" % i, [128, 512], F32) for i in range(2)]
                    rho1 = [tr.buf(ph, "rho1_%d" % i, [128, 512], BF16) for i in range(2)]
                    cstb = tr.buf(ph, "cstB", [128, ctot], BF16)
                    cst_holder[0] = cstb
                    tr.dma('sp', cstb[:], CSTB, cstb, True)
                    accD = [tr.buf(ph, "accD%d" % i, [128, 512], F32) for i in range(2)]
                    accG = [tr.buf(ph, "accG%d" % i, [128, 512], F32) for i in range(2)]
                    pending = []
                    if l + 1 < NL:
                        stgB = [tr.buf(ph, "stgB%d" % i, [128, KC, 128], F32) for i in range(2)]
                        stoB = [tr.buf(ph, "stoB%d" % i, [128, KC, 128], BF16) for i in range(2)]
                        pgB = prep_gen(l + 1, stgB, stoB, ('dve', 'pool'))
                    else:
                        pgB = iter(())
                    prep_per_job = max(1, -(-NCH // (NB * 28)))
                    gsbB = tr.buf(ph, "gsbB", [12, 512], BF16)
                    Gp = [tr.buf(ph, "Gp%d" % i, [128, 32], F32) for i in range(2)]
                    m8 = [tr.buf(ph, "m8_%d" % i, [128, 8], F32) for i in range(2)]
                    m8b = [tr.buf(ph, "m8b_%d" % i, [128, 8], F32) for i in range(2)]
                    Bq = [tr.buf(ph, "Bq%d" % i, [128, 32], BF16) for i in range(2)]
                    BtM = [tr.buf(ph, "BtM%d" % i, [128, 512], BF16) for i in range(2)]
                    for bt_ in BtM:
                        op('pool', lambda e: e.memset(bt_[:], 0.0), [], [bt_])
                    imp = [tr.buf(ph, "imp%d" % i, [128, n_sb], F32) for i in range(2)]
                    sc2 = tr.buf(ph, "sc2", [128, n_sb], F32)
                    BqN = [tr.buf(ph, "BqN%d" % i, [128, n_sb], BF16) for i in range(2)]
                    BtN = tr.buf(ph, "BtN", [128, 512], BF16)
                    op('pool', lambda e: e.memset(BtN[:], 0.0), [], [BtN])
                    rd = [tr.buf(ph, "rd%d" % i, [128, 1], F32) for i in range(2)]
                    Ost = [tr.buf(ph, "Ost%d" % i, [128, 512], BF16) for i in range(3)]
                    psL = [ps[0], ps[1]]
                    psN = [ps[2], ps[3]]
                    psD = [ps[4], ps[5]]
                    psX = ps[6]
                    cnt = dict(q=0, kv=0, pt=0, job=0, rho=0, ost=0, u=0, x=0)

                    def load_q(ci, tsl):
                        q_ = QT[cnt['q'] % 2]
                        cnt['q'] += 1
                        tr.dma('sp', q_[:], FM[ci, :, tsl], q_, True)
                        return q_

                    def load_kv(kci, vcol, j0, j1):
                        res = {}
                        ck0, ck1 = j0 // 16, j1 // 16
                        for ck in range(ck0, ck1 + 1):
                            ja = max(j0, ck * 16)
                            jb = min(j1, ck * 16 + 15)
                            kb = Kc[cnt['kv'] % 3]
                            vb = Vc[cnt['kv'] % 3]
                            cnt['kv'] += 1
                            n = jb - ja + 1
                            tr.dma('sp', kb[:, 0:n * 128], FM[kci, :, ja * 128:(jb + 1) * 128], kb, True)
                            tr.dma('sp', vb[:, 0:n, :],
                                   TM[ja * 128:(jb + 1) * 128, vcol:vcol + 128].rearrange("(j p) d -> p j d", p=128), vb, True)
                            for j in range(ja, jb + 1):
                                res[j] = (kb[:, (j - ja) * 128:(j - ja + 1) * 128], kb, vb[:, j - ja, :], vb)
                        return res

                    def flush_pending():
                        while pending:
                            pending.pop(0)()

                    def attn_job(q_, units, finalize):
                        jn = cnt['job']
                        cnt['job'] += 1
                        pN = psN[jn % 2]
                        pD = psD[jn % 2]
                        aD = accD[jn % 2]
                        aG = accG[jn % 2]
                        n = len(units)
                        pts = [None] * n
                        op('dve', lambda e: e.memset(aD[:], 0.0), [], [aD])
                        op('pool', lambda e: e.memset(aG[:], 0.0), [], [aG])

                        def emit_S(i):
                            u = units[i]
                            f0, f1 = u.get('f0', 0), u.get('f1', 512)
                            pl = psL[cnt['pt'] % 2]
                            ex = u.get('extras', [])
                            op('pe', lambda e: e.matmul(pl[:, f0:f1], lhsT=u['k'], rhs=q_[:, f0:f1], start=True,
                                                        stop=(len(ex) == 0)), [u['kb'], q_], [pl])
                            for xi, (la, ra, bl) in enumerate(ex):
                                op('pe', lambda e: e.matmul(pl[:, f0:f1], lhsT=la, rhs=ra, start=False,
                                                            stop=(xi == len(ex) - 1)), bl, [pl])
                            if 'pt' in u:
                                pap, pbuf = u['pt']
                            else:
                                pbuf = PT[cnt['pt'] % 4]
                                pap = pbuf[:]
                            cnt['pt'] += 1
                            if u.get('bias') is not None:
                                bap, bbuf = u['bias']
                                op('act', lambda e: e.activation(out=pap[:, f0:f1], in_=pl[:, f0:f1], func=AF.Exp, scale=SCL,
                                                                 bias=bap), [pl, bbuf], [pbuf])
                            else:
                                op('act', lambda e: e.activation(out=pap[:, f0:f1], in_=pl[:, f0:f1], func=AF.Exp, scale=SCL),
                                   [pl], [pbuf])
                            pts[i] = (pap, pbuf)

                        def emit_PV(i):
                            u = units[i]
                            f0, f1 = u.get('f0', 0), u.get('f1', 512)
                            pap, pbuf = pts[i]
                            op('pe', lambda e: e.matmul(pN[:, f0:f1], lhsT=u['v'], rhs=pap[:, f0:f1], start=(i == 0),
                                                        stop=(i == n - 1)), [u['vb'], pbuf], [pN])
                            if i % 2 == 0:
                                op('dve', lambda e: e.tensor_tensor(out=aD[:, f0:f1], in0=aD[:, f0:f1], in1=pap[:, f0:f1],
                                                                    op=ALU.add), [aD, pbuf], [aD])
                            else:
                                op('pool', lambda e: e.tensor_tensor(out=aG[:, f0:f1], in0=aG[:, f0:f1], in1=pap[:, f0:f1],
                                                                     op=ALU.add), [aG, pbuf], [aG])

                        for i in range(n + 1):
                            if i < n:
                                emit_S(i)
                            if i == min(2, n):
                                flush_pending()
                            if i >= 1:
                                emit_PV(i - 1)

                        def fin():
                            op('dve', lambda e: e.tensor_tensor(out=aD[:], in0=aD[:], in1=aG[:], op=ALU.add), [aD, aG], [aD])
                            op('pe', lambda e: e.matmul(pD[:], lhsT=onesf[:], rhs=aD[:], start=True, stop=True),
                               [onesf, aD], [pD])
                            finalize(pN, pD)
                        pending.append(fin)
                        advance(pgB, prep_per_job)

                    def recip_den(pD):
                        r_ = rec[cnt['x'] % 2]
                        cnt['x'] += 1
                        op('dve', lambda e: e.tensor_scalar(out=r_[:], in0=pD[:], scalar1=TINY, scalar2=None, op0=ALU.max),
                           [pD], [r_])
                        op('dve', lambda e: e.reciprocal(out=r_[:], in_=r_[:]), [r_], [r_])
                        return r_

                    def fin_plain(hc, tsl):
                        def f(pN, pD):
                            r_ = recip_den(pD)
                            o_ = Ost[cnt['ost'] % 3]
                            cnt['ost'] += 1
                            op('dve', lambda e: e.tensor_tensor(out=o_[:], in0=pN[:], in1=r_[:], op=ALU.mult), [pN, r_], [o_])
                            tr.dma('pool', OTD[hc, :, tsl], o_[:], o_, False)
                        return f

                    def fin_nsa(hh, br, tsl):
                        def f(pN, pD):
                            r_ = recip_den(pD)
                            rr = 3 * hh + br
                            op('pe', lambda e: e.matmul(psX[:], lhsT=cv('eg', 12, (rr * 128, (rr + 1) * 128)), rhs=gsbB[:],
                                                        start=True, stop=True), [cstb, gsbB], [psX])
                            g_ = rgt[cnt['x'] % 2]
                            op('dve', lambda e: e.tensor_tensor(out=g_[:], in0=psX[:], in1=r_[:], op=ALU.mult), [psX, r_], [g_])
                            a_ = acc[hh]
                            if br == 0:
                                op('dve', lambda e: e.tensor_tensor(out=a_[:], in0=pN[:], in1=g_[:], op=ALU.mult), [pN, g_], [a_])
                            else:
                                t_ = tmc[cnt['x'] % 2]
                                op('dve', lambda e: e.tensor_tensor(out=t_[:], in0=pN[:], in1=g_[:], op=ALU.mult), [pN, g_], [t_])
                                op('pool', lambda e: e.tensor_tensor(out=a_[:], in0=a_[:], in1=t_[:], op=ALU.add), [a_, t_], [a_])
                            if br == 2:
                                o_ = Ost[cnt['ost'] % 3]
                                cnt['ost'] += 1
                                op('pool', lambda e: e.tensor_copy(out=o_[:], in_=a_[:]), [a_], [o_])
                                tr.dma('pool', OTD[4 + hh, :, tsl], o_[:], o_, False)
                        return f

                    causal = cv('causal')

                    def diag_extra(jd):
                        f0 = 128 * jd
                        return (cv('i30k'), cv('causal', 128, (0, 512 - f0)), [cstb]), f0

                    for b in range(NB):
                        t0 = b * 512
                        tsl = slice(t0, t0 + 512)
                        jlast = 4 * b + 3
                        for hh in range(8):
                            q_ = load_q(16 + hh, tsl)
                            r1 = rho1[cnt['rho'] % 2]
                            cnt['rho'] += 1
                            tr.dma('sp', r1[:], RHO[hh:hh + 1, tsl].to_broadcast([128, 512]), r1, True)
                            kv = load_kv(24 + hh, 768 + 128 * hh, 0, jlast)
                            units = []
                            for j in range(jlast + 1):
                                ka, kb, va, vb = kv[j]
                                u = dict(k=ka, kb=kb, v=va, vb=vb, bias=(CK[:, j, hh:hh + 1], CK))
                                f0 = 0
                                ex = []
                                if j >= 4 * b:
                                    dx, f0 = diag_extra(j - 4 * b)
                                    ex.append(dx)
                                ex.insert(0, (onesb[:], r1[:, f0:512], [onesb, r1]))
                                u['extras'] = ex
                                u['f0'] = f0
                                units.append(u)
                            attn_job(q_, units, fin_plain(8 + hh, tsl))
                        for hh in range(4):
                            q_ = load_q(hh, tsl)
                            bt = BtM[hh % 2]
                            for i in range(4):
                                own = 2 * b + i // 2
                                gp = Gp[i % 2]
                                mm = m8[i % 2]
                                bq = Bq[i % 2]
                                op('pe', lambda e: e.matmul(psX[:, 0:32], lhsT=q_[:, 128 * i:128 * (i + 1)], rhs=kmh[:, hh, :],
                                                            start=True, stop=False), [q_, kmh], [psX])
                                op('pe', lambda e: e.matmul(psX[:, 0:32], lhsT=q_[:, 128 * i:128 * (i + 1)], rhs=kml[:, hh, :],
                                                            start=False, stop=True), [q_, kml], [psX])
                                op('pool', lambda e: e.memset(gp[:], -1e30), [], [gp])
                                if own > 0:
                                    op('dve', lambda e: e.tensor_copy(out=gp[:, 0:own], in_=psX[:, 0:own]), [psX], [gp])
                                op('dve', lambda e: e.max(out=mm[:], in_=gp[:]), [gp], [mm])
                                op('dve', lambda e: e.tensor_scalar(out=mm[:, 2:3], in0=mm[:, 2:3], scalar1=-5e29, scalar2=None,
                                                                    op0=ALU.max), [mm], [mm])
                                op('dve', lambda e: e.tensor_scalar(out=bq[:], in0=gp[:], scalar1=mm[:, 2:3], scalar2=1.0,
                                                                    op0=ALU.is_ge, op1=ALU.subtract), [gp, mm], [bq])
                                op('pool', lambda e: e.memset(bq[:, own:own + 1], 0.0), [], [bq])
                                if own + 1 < 32:
                                    op('pool', lambda e: e.memset(bq[:, own + 1:32], -1.0), [], [bq])
                                op('pe', lambda e: e.transpose(out=psb[0:32, 128 * i:128 * (i + 1)], in_=bq[:],
                                                               identity=cv('ident')), [bq, cstb], [psb])
                            op('dve', lambda e: e.tensor_copy(out=bt[0:32, :], in_=psb[0:32, 0:512]), [psb], [bt])
                            kv = load_kv(4 + hh, 128 * hh, 0, jlast)
                            units = []
                            for j in range(jlast + 1):
                                ka, kb, va, vb = kv[j]
                                u = dict(k=ka, kb=kb, v=va, vb=vb)
                                f0 = 0
                                ex = []
                                if j >= 4 * b:
                                    dx, f0 = diag_extra(j - 4 * b)
                                    ex.append(dx)
                                nblk = j // 2
                                ex.insert(0, (cv('am', 128, (nblk * 128, (nblk + 1) * 128)), bt[:, f0:512], [cstb, bt]))
                                u['extras'] = ex
                                u['f0'] = f0
                                units.append(u)
                            attn_job(q_, units, fin_plain(hh, tsl))
                        tr.dma('sp', gsbB[:], GT[:, tsl], gsbB, True)
                        jcs = [jc for jc in range(nct) if b - 4 * jc >= 0]
                        for hh in range(4):
                            q_ = load_q(8 + hh, tsl)
                            units = []
                            for jc in jcs:
                                dlt = b - 4 * jc
                                u = dict(k=KcmpT[:, jc * 128:(jc + 1) * 128], kb=KcmpT, v=Vcmp[:, jc, :], vb=Vcmp,
                                         pt=(ET[:, hh, jc, :], ET))
                                if dlt <= 4:
                                    u['extras'] = [(cv('i30k'), cv('cmask', 128, (dlt * 512, (dlt + 1) * 512)), [cstb])]
                                units.append(u)
                            attn_job(q_, units, fin_nsa(hh, 0, tsl))
                        for i in range(4):
                            im = imp[i % 2]
                            for hh in range(4):
                                pu = psL[cnt['u'] % 2]
                                cnt['u'] += 1
                                for xi, jc in enumerate(jcs):
                                    op('pe', lambda e: e.matmul(pu[:, 0:n_sb + 1], lhsT=ET[:, hh, jc, 128 * i:128 * (i + 1)],
                                                                rhs=cv('ov', 128, (jc * (n_sb + 1), (jc + 1) * (n_sb + 1))),
                                                                start=(xi == 0), stop=(xi == len(jcs) - 1)), [ET, cstb], [pu])
                                rd_ = rd[hh % 2]
                                op('dve', lambda e: e.tensor_scalar(out=rd_[:], in0=pu[:, n_sb:n_sb + 1], scalar1=TINY,
                                                                    scalar2=None, op0=ALU.max), [pu], [rd_])
                                op('dve', lambda e: e.reciprocal(out=rd_[:], in_=rd_[:]), [rd_], [rd_])
                                if hh == 0:
                                    op('dve', lambda e: e.tensor_scalar(out=im[:], in0=pu[:, 0:n_sb], scalar1=rd_[:, 0:1],
                                                                        scalar2=None, op0=ALU.mult), [pu, rd_], [im])
                                else:
                                    op('dve', lambda e: e.scalar_tensor_tensor(out=im[:], in0=pu[:, 0:n_sb], scalar=rd_[:, 0:1],
                                                                               in1=im[:], op0=ALU.mult, op1=ALU.add),
                                       [pu, rd_, im], [im])
                            for hf in range(2):
                                cur = 8 * b + 2 * i + hf
                                rows = slice(64 * hf, 64 * hf + 64)
                                if cur - 1 >= 0:
                                    op('pool', lambda e: e.memset(im[rows, cur - 1:cur], 2e30), [], [im])
                                op('pool', lambda e: e.memset(im[rows, cur:cur + 1], 1e30), [], [im])
                                if cur + 1 < n_sb:
                                    op('pool', lambda e: e.memset(im[rows, cur + 1:n_sb], -2e30), [], [im])
                            op('pool', lambda e: e.memset(im[:, 0:1], 3e30), [], [im])
                            ma = m8[i % 2]
                            mb = m8b[i % 2]
                            bqn = BqN[i % 2]
                            op('dve', lambda e: e.max(out=ma[:], in_=im[:]), [im], [ma])
                            op('dve', lambda e: e.match_replace(out=sc2[:], in_to_replace=ma[:], in_values=im[:],
                                                                imm_value=-2e30), [ma, im], [sc2])
                            op('dve', lambda e: e.max(out=mb[:], in_=sc2[:]), [sc2], [mb])
                            op('dve', lambda e: e.tensor_scalar(out=mb[:, 7:8], in0=mb[:, 7:8], scalar1=-1e30, scalar2=None,
                                                                op0=ALU.max), [mb], [mb])
                            op('dve', lambda e: e.tensor_scalar(out=bqn[:], in0=im[:], scalar1=mb[:, 7:8], scalar2=1.0,
                                                                op0=ALU.is_ge, op1=ALU.subtract), [im, mb], [bqn])
                            op('pe', lambda e: e.transpose(out=psb[0:n_sb, 128 * i:128 * (i + 1)], in_=bqn[:],
                                                           identity=cv('ident')), [bqn, cstb], [psb])
                        op('dve', lambda e: e.tensor_copy(out=BtN[0:n_sb, :], in_=psb[0:n_sb, 0:512]), [psb], [BtN])
                        for hh in range(4):
                            q_ = load_q(8 + hh, tsl)
                            kv = load_kv(14, 512, 0, jlast)
                            units = []
                            for j in range(jlast + 1):
                                ka, kb, va, vb = kv[j]
                                u = dict(k=ka, kb=kb, v=va, vb=vb)
                                f0 = 0
                                ex = []
                                if j >= 4 * b:
                                    dx, f0 = diag_extra(j - 4 * b)
                                    ex.append(dx)
                                ex.insert(0, (cv('an', 128, (j * 128, (j + 1) * 128)), BtN[:, f0:512], [cstb, BtN]))
                                u['extras'] = ex
                                u['f0'] = f0
                                units.append(u)
                            attn_job(q_, units, fin_nsa(hh, 1, tsl))
                        for hh in range(4):
                            q_ = load_q(8 + hh, tsl)
                            jfirst = max(0, 4 * b - 4)
                            kv = load_kv(15, 640, jfirst, jlast)
                            units = []
                            for j in range(jfirst, jlast + 1):
                                ka, kb, va, vb = kv[j]
                                u = dict(k=ka, kb=kb, v=va, vb=vb)
                                if j >= 4 * b:
                                    dx, f0 = diag_extra(j - 4 * b)
                                    u['extras'] = [dx]
                                    u['f0'] = f0
                                else:
                                    jl = j - (4 * b - 4)
                                    u['f0'] = 0
                                    u['f1'] = 128 * (jl + 1)
                                    u['extras'] = [(cv('i30k'), cv('w2', 128, (384 - 128 * jl, 512)), [cstb])]
                                units.append(u)
                            attn_job(q_, units, fin_nsa(hh, 2, tsl))
                    flush_pending()
                    while advance(pgB, 8):
                        pass
                    tr.end_phase(ph)

                with ExitStack() as ph:
                    hT = [tr.buf(ph, "hT%d" % i, [128, KC, 512], BF16) for i in range(2)]
                    x1 = tr.buf(ph, "x1", [128, KC, 512], F32)
                    yT = tr.buf(ph, "yT", [128, KC, 512], F32)
                    OTs = tr.buf(ph, "OTs", [128, KC, 512], BF16)
                    sq = [tr.buf(ph, "sq%d" % i, [128, 512], BF16) for i in range(2)]
                    tmpn = tr.buf(ph, "tmpn", [128, 512], F32)
                    rstd = tr.buf(ph, "rstd", [128, 512], F32)
                    tmpx = [tr.buf(ph, "tmpx%d" % i, [128, 512], F32) for i in range(2)]
                    tmpu = [tr.buf(ph, "tmpu%d" % i, [128, 512], F32) for i in range(2)]
                    pcn = [0]
                    h2 = hT[0]
                    aT = hT[1]

                    def gemm16(l_, c0, src, outfn):
                        for oc in range(16):
                            w = load_w(l_, c0 + oc)
                            pp = ps[pcn[0] % 3]
                            pcn[0] += 1
                            for k in range(KC):
                                op('pe', lambda e: e.matmul(pp[:], lhsT=w[:, k, :], rhs=src[:, k, :], start=(k == 0),
                                                            stop=(k == KC - 1)), [w, src], [pp])
                            outfn(oc, pp)

                    def sumsq(src, pn):
                        for k in range(KC):
                            s_ = sq[k % 2]
                            op('act', lambda e: e.activation(out=s_[:], in_=src[:, k, :], func=AF.Square), [src], [s_])
                            op('pe', lambda e: e.matmul(pn[:], lhsT=onesb[:], rhs=s_[:], start=(k == 0), stop=(k == KC - 1)),
                               [s_, onesb], [pn])

                    def resid(di):
                        for k in range(KC):
                            tx = tmpx[k % 2]
                            op('dve', lambda e: e.tensor_tensor(out=tx[:], in0=yT[:, k, :], in1=rstd[:], op=ALU.mult),
                               [yT, rstd], [tx])
                            op('dve', lambda e: e.scalar_tensor_tensor(out=x1[:, k, :], in0=tx[:], scalar=der[:, l, di, k:k + 1],
                                                                       in1=x1[:, k, :], op0=ALU.mult, op1=ALU.add),
                               [tx, der, x1], [x1])

                    for b in range(NB):
                        t0 = b * 512
                        tsl = slice(t0, t0 + 512)
                        tr.dma('sp', OTs[:], OTD[:, :, tsl].rearrange("c p t -> p c t"), OTs, True)
                        tr.dma('sp', x1[:], xin[:, :, tsl], x1, True)
                        pn = ps[6]

                        def out_c(oc, pp):
                            op('act', lambda e: e.copy(out=yT[:, oc, :], in_=pp[:]), [pp], [yT])
                        gemm16(l, 47, OTs, out_c)
                        sumsq(yT, pn)
                        rms_rstd((tmpn, rstd), pn)
                        resid(2)
                        sumsq(x1, pn)
                        rms_rstd((tmpn, rstd), pn)
                        for k in range(KC):
                            tx = tmpx[k % 2]
                            op('dve', lambda e: e.tensor_tensor(out=tx[:], in0=x1[:, k, :], in1=rstd[:], op=ALU.mult),
                               [x1, rstd], [tx])
                            op('pool', lambda e: e.tensor_scalar(out=h2[:, k, :], in0=tx[:], scalar1=der[:, l, 3, k:k + 1],
                                                                 scalar2=der[:, l, 4, k:k + 1], op0=ALU.mult, op1=ALU.add),
                               [tx, der], [h2])
                        for q in range(4):
                            def out_u(fc_, pp):
                                tu = tmpu[fc_ % 2]
                                op('act', lambda e: e.activation(out=tu[:], in_=pp[:], func=AF.Relu), [pp], [tu])
                                op('dve', lambda e: e.tensor_tensor(out=aT[:, fc_, :], in0=tu[:], in1=tu[:], op=ALU.mult),
                                   [tu], [aT])
                            gemm16(l, 63 + 16 * q, h2, out_u)

                            def out_d(dc, pp):
                                if q == 0:
                                    op('act', lambda e: e.copy(out=yT[:, dc, :], in_=pp[:]), [pp], [yT])
                                else:
                                    op('dve', lambda e: e.tensor_tensor(out=yT[:, dc, :], in0=pp[:], in1=yT[:, dc, :],
                                                                        op=ALU.add), [pp, yT], [yT])
                            gemm16(l, 127 + 16 * q, aT, out_d)
                        sumsq(yT, pn)
                        rms_rstd((tmpn, rstd), pn)
                        resid(5)
                        tr.dma('pool', xout[:, :, tsl], x1[:], x1, False)
                    tr.end_phase(ph)
        tr.barrier()
        ninst = tr.ninst
    return nc, ninst


def fm_layout(v):
    v = np.asarray(v, np.float32)
    lead = v.shape[:-1]
    return np.ascontiguousarray(np.moveaxis(v.reshape(lead + (v.shape[-1] // 128, 128)), -1, 0))


def make_in_maps(inp, S, NL):
    f = lambda a: np.ascontiguousarray(np.asarray(a, np.float32))
    B = inp['x'].shape[0]
    cst, fcst = make_consts(S)
    gains = np.stack([np.asarray(inp[k], np.float32)[:NL] for k in ('g_pre_mix', 'g_post_mix', 'g_pre_mlp', 'g_post_mlp')], axis=1)
    gains = fm_layout(gains)
    bmodT = fm_layout(np.asarray(inp['b_mod'], np.float32)[:NL])
    negb = np.ascontiguousarray(np.asarray(inp['b_forget'], np.float32)[:NL].T)
    peT = np.stack([np.asarray(inp['cmp_pe_k'], np.float32)[:NL], np.asarray(inp['cmp_pe_v'], np.float32)[:NL]], axis=1)
    peT = np.ascontiguousarray(peT.transpose(3, 0, 1, 2))
    shared = dict(
        w_mod=f(inp['w_mod'][:NL]), bmodT=bmodT, gains=gains, w_in=f(inp['w_in'][:NL]), negb=negb, peT=peT,
        w1k=f(inp['cmp_w1_k'][:NL]), w2k=f(inp['cmp_w2_k'][:NL]), w1v=f(inp['cmp_w1_v'][:NL]), w2v=f(inp['cmp_w2_v'][:NL]),
        w_out=f(inp['w_out'][:NL]), w_up=f(inp['w_up'][:NL]), w_down=f(inp['w_down'][:NL]), cst=cst, fcst=fcst)
    maps = []
    for b in range(B):
        xb = np.asarray(inp['x'][b], np.float32)[:S]
        xT = np.ascontiguousarray(xb.T.reshape(KC, 128, S).transpose(1, 0, 2))
        m = dict(shared)
        m['xT'] = xT
        m['pos'] = np.ascontiguousarray(np.asarray(inp['positions'][b], np.int32)[None, :S])
        m['cT'] = fm_layout(np.asarray(inp['c'][b], np.float32))
        maps.append(m)
    return maps


def run(inp, S, NL):
    nc, ninst = build_program(S, NL)
    maps = make_in_maps(inp, S, NL)
    res = run_bass_kernel_spmd(nc, maps, core_ids=list(range(len(maps))))
    outs = []
    for r in res.results:
        yT = np.asarray(r['yT'], np.float32)
        outs.append(yT.transpose(2, 1, 0).reshape(S, D))
    return np.stack(outs, axis=0)


def kernel(**inputs):
    return run(inputs, 8192, 4)
```

```python
import numpy as np
from contextlib import ExitStack
import concourse.bass as bass
import concourse.mybir as mybir
from concourse.bass_utils import run_bass_kernel_spmd

F32 = mybir.dt.float32
BF16 = mybir.dt.bfloat16
I32 = mybir.dt.int32
ALU = mybir.AluOpType
AF = mybir.ActivationFunctionType
AX = mybir.AxisListType

D = 2048
KC = 16
HD = 128
PROJ = 5908
DFF = 8192
SCL = 128.0 ** -0.5
EPS = 1e-6
NCH = 201
TINY = 1e-30


class Buf:
    __slots__ = ('t', 'name', 'w', 'r', 'sem', 'sval')

    def __init__(self, t, name):
        self.t = t
        self.name = name
        self.w = None
        self.r = {}
        self.sem = None
        self.sval = 0

    def __getitem__(self, k):
        return self.t[k]


class TR:
    ENG = ('pe', 'act', 'dve', 'pool', 'sp')

    def __init__(self, nc, st):
        self.nc = nc
        self.st = st
        self.h = dict(pe=nc.tensor, act=nc.scalar, dve=nc.vector, pool=nc.gpsimd, sp=nc.sync)
        self.sem = {e: st.enter_context(nc.semaphore("c_" + e)) for e in self.ENG}
        self.cnt = {e: 0 for e in self.ENG}
        self.seen = {e: {} for e in self.ENG}
        self.dsems = []
        self.freesems = []
        self.by_stack = {}
        self.nbuf = 0
        self.ninst = 0

    def buf(self, st, name, shape, dtype, psum=False):
        self.nbuf += 1
        nm = "%s_%d" % (name, self.nbuf)
        if psum:
            t = st.enter_context(self.nc.psum_tensor(nm, shape, dtype))
        else:
            t = st.enter_context(self.nc.sbuf_tensor(nm, shape, dtype))
        b = Buf(t, nm)
        self.by_stack.setdefault(id(st), []).append(b)
        return b

    def _wait(self, eng, rec):
        kind, who, val = rec
        if kind == 'e':
            if who == eng and eng == 'pe':
                return
            key = ('e', who)
            semh = self.sem[who]
        else:
            key = ('d', who.name)
            semh = who.sem
            val = who.sval
        if self.seen[eng].get(key, 0) >= val:
            return
        self.h[eng].wait_ge(semh, val)
        self.ninst += 1
        self.seen[eng][key] = val

    def op(self, eng, fn, reads=(), writes=()):
        recs = []
        for b in reads:
            if b.w is not None:
                recs.append(b.w)
        for b in writes:
            if b.w is not None:
                recs.append(b.w)
            recs.extend(b.r.values())
        for rec in recs:
            self._wait(eng, rec)
        inst = fn(self.h[eng])
        self.cnt[eng] += 1
        self.ninst += 1
        inst.then_inc(self.sem[eng], 1)
        rec = ('e', eng, self.cnt[eng])
        for b in reads:
            b.r[('e', eng)] = rec
        for b in writes:
            b.w = rec
            b.r = {}
        return inst

    def dma(self, eng, out, in_, sb, load):
        recs = []
        if sb.w is not None:
            recs.append(sb.w)
        if load:
            recs.extend(sb.r.values())
        if sb.sem is None:
            if self.freesems:
                sb.sem, sb.sval = self.freesems.pop()
            else:
                sb.sem = self.st.enter_context(self.nc.semaphore("d_" + sb.name))
                sb.sval = 0
            self.dsems.append(sb)
        if sb.sval > 0:
            recs.append(('d', sb, sb.sval))
        for rec in recs:
            self._wait(eng, rec)
        inst = self.h[eng].dma_start(out=out, in_=in_)
        self.ninst += 1
        sb.sval += 16
        inst.then_inc(sb.sem, 16)
        rec = ('d', sb, sb.sval)
        if load:
            sb.w = rec
            sb.r = {}
        else:
            sb.r[('d', sb.name)] = rec
        return inst

    def barrier(self):
        for e in self.ENG:
            for e2 in self.ENG:
                if e2 != e and self.cnt[e2] > 0:
                    self._wait(e, ('e', e2, self.cnt[e2]))
            for b in self.dsems:
                if b.sval > 0:
                    self._wait(e, ('d', b, b.sval))

    def end_phase(self, st):
        self.barrier()
        names = set(b.name for b in self.by_stack.pop(id(st), []))
        keep = []
        for b in self.dsems:
            if b.name in names:
                self.freesems.append((b.sem, b.sval))
            else:
                keep.append(b)
        self.dsems = keep


def const_layout(S):
    n_sb = S // 64
    KW = min(64, n_sb)
    nct = max(1, S // 2048)
    off = {}
    o = 0
    for name, n in [('ident', 128), ('i30k', 128), ('causal', 512), ('w2', 512), ('cmask', 5 * 512),
                    ('am', 32 * 128), ('an', (S // 128) * 128), ('pmt', 32), ('ov', nct * (n_sb + 1)),
                    ('eg', 12 * 128)]:
        off[name] = (o, n)
        o += n
    return off, o


def make_consts(S):
    n_sb = S // 64
    KW = min(64, n_sb)
    nct = max(1, S // 2048)
    ncmp = S // 16 - 1
    off, tot = const_layout(S)
    c = np.zeros((128, tot), np.float32)
    p = np.arange(128)[:, None]

    def put(name, arr):
        o, n = off[name]
        assert arr.shape == (128, n), (name, arr.shape, n)
        c[:, o:o + n] = arr
    put('ident', np.eye(128, dtype=np.float32))
    put('i30k', 30000.0 * np.eye(128, dtype=np.float32))
    f = np.arange(512)[None, :]
    put('causal', np.where(f >= p, 0.0, -1.0).astype(np.float32))
    put('w2', np.where(f - 384 < p, 0.0, -1.0).astype(np.float32))
    cm = [np.where(16 * p - f <= 512 * d - 31, 0.0, -1.0) for d in range(5)]
    put('cmask', np.concatenate(cm, axis=1).astype(np.float32))
    am = np.zeros((128, 32, 128), np.float32)
    for n in range(32):
        am[n, n, :] = 30000.0
    put('am', am.reshape(128, -1))
    an = np.zeros((128, S // 128, 128), np.float32)
    for j in range(S // 128):
        an[2 * j, j, 0:64] = 30000.0
        an[2 * j + 1, j, 64:128] = 30000.0
    put('an', an.reshape(128, -1))
    pm = np.zeros((128, 32), np.float32)
    for i in range(16):
        pm[i + 16, i] = -1.0
        pm[i, i + 16] = 1.0
    put('pmt', pm)
    ov = np.zeros((128, nct, n_sb + 1), np.float32)
    for cc in range(ncmp):
        jc, pp = divmod(cc, 128)
        n0 = cc // 4
        ov[pp, jc, n0] = 1.0
        if cc % 4 == 3 and n0 + 1 < n_sb:
            ov[pp, jc, n0 + 1] = 1.0
        ov[pp, jc, n_sb] = 1.0
    put('ov', ov.reshape(128, -1))
    eg = np.zeros((128, 12, 128), np.float32)
    for r in range(12):
        eg[r, r, :] = 1.0
    put('eg', eg.reshape(128, -1))
    fc = np.zeros((128, 4), np.float32)
    invf = 500000.0 ** (-(np.arange(16, dtype=np.float64)) / 16.0)
    fc[0:32, 0] = np.tile(invf / (2 * np.pi), 2).astype(np.float32)
    return c, fc


def build_program(S, NL):
    assert S % 2048 == 0
    NB = S // 512
    NT = S // 128
    n_sb = S // 64
    KW = min(64, n_sb)
    nct = S // 2048
    ncmp = S // 16 - 1
    NCP = nct * 128
    assert NCP <= 512
    nmb = S // 256
    coff, ctot = const_layout(S)

    nc = bass.Bass("TRN2", target_bir_lowering=False)

    def din(name, shape, dt=F32):
        return nc.dram_tensor(name, shape, dt, kind="ExternalInput").ap()

    def dscr(name, shape, dt):
        return nc.dram_tensor(name, shape, dt, kind="Internal").ap()

    x_in = din("xT", [128, KC, S])
    pos_in = din("pos", [1, S], I32)
    cT_in = din("cT", [128, KC])
    w_mod = din("w_mod", [NL, D, 6 * D])
    bmodT = din("bmodT", [128, NL, 96])
    gT_in = din("gains", [128, NL, 4, KC])
    w_in = din("w_in", [NL, D, PROJ])
    negb_in = din("negb", [8, NL])
    peT_in = din("peT", [128, NL, 2, 32])
    w1k = din("w1k", [NL, 4096, 256])
    w2k = din("w2k", [NL, 256, 128])
    w1v = din("w1v", [NL, 4096, 256])
    w2v = din("w2v", [NL, 256, 128])
    w_out = din("w_out", [NL, D, D])
    w_up = din("w_up", [NL, D, DFF])
    w_down = din("w_down", [NL, DFF, D])
    cst_in = din("cst", [128, ctot])
    fcst_in = din("fcst", [128, 4])
    y_out = nc.dram_tensor("yT", [128, KC, S], F32, kind="ExternalOutput").ap()

    WC = [dscr("WC%d" % l_, [NCH, 128, KC, 128], BF16) for l_ in range(NL)]
    FM = dscr("FM", [32, 128, S], BF16)
    VT = dscr("VT", [14, 128, S // 128, 128], BF16)
    GT = dscr("GT", [12, S], BF16)
    RHO = dscr("RHO", [8, S], BF16)
    COS = dscr("COS", [32, S], F32)
    SIN = dscr("SIN", [32, S], F32)
    OTD = dscr("OTD", [16, 128, S], BF16)
    XS = [dscr("XS0", [128, KC, S], F32), dscr("XS1", [128, KC, S], F32)]
    CSTB = dscr("CSTB", [128, ctot], BF16)

    with ExitStack() as st:
        tr = TR(nc, st)
        op = tr.op

        onesb = tr.buf(st, "onesb", [128, 128], BF16)
        onesf = tr.buf(st, "onesf", [128, 128], F32)
        pmtb = tr.buf(st, "pmtb", [128, 32], BF16)
        identf = tr.buf(st, "identf", [128, 128], F32)
        fcst = tr.buf(st, "fcst", [128, 4], F32)
        onecol = tr.buf(st, "onecol", [128, 1], F32)
        der = tr.buf(st, "der", [128, NL, 6, KC], F32)
        negb = tr.buf(st, "negb", [8, NL], F32)
        peb = tr.buf(st, "peb", [128, NL, 2, 32], BF16)
        w4k = [tr.buf(st, "w4k%d" % i, [128, KC, 128], BF16) for i in range(4)]
        ps = [tr.buf(st, "ps%d" % i, [128, 512], F32, psum=True) for i in range(7)]
        psb = tr.buf(st, "psb", [128, 1024], BF16, psum=True)
        wslot = [0]

        cst_holder = [None]

        def cv(name, rows=128, sub=None):
            cstb = cst_holder[0]
            o, n = coff[name]
            if sub is None:
                return cstb[0:rows, o:o + n]
            return cstb[0:rows, o + sub[0]:o + sub[1]]

        def load_w(l, ci):
            b = w4k[wslot[0] % 4]
            wslot[0] += 1
            tr.dma('sp', b[:], WC[l][ci], b, True)
            return b

        fm_cols = ([i * 128 for i in range(4)] + [512 + i * 128 for i in range(4)] +
                   [1536 + i * 128 for i in range(4)] + [2048, 2176, 2304, 2560] +
                   [2828 + i * 128 for i in range(8)] + [3852 + i * 128 for i in range(8)])
        v_cols = ([1024 + i * 128 for i in range(4)] + [2432, 2688] + [4876 + i * 128 for i in range(8)])

        def prep_list(l):
            items = []
            for ci, c0 in enumerate(fm_cols):
                items.append((ci, [(w_in[l, :, c0:c0 + 128], 0)], KC))
            for ci, c0 in enumerate(v_cols):
                items.append((32 + ci, [(w_in[l, :, c0:c0 + 128], 0)], KC))
            items.append((46, [(w_in[l, :, 2816:2828], 0), (w_in[l, :, 5900:5908], 12)], KC))
            for oc in range(16):
                items.append((47 + oc, [(w_out[l, :, oc * 128:(oc + 1) * 128], 0)], KC))
            for fc_ in range(64):
                items.append((63 + fc_, [(w_up[l, :, fc_ * 128:(fc_ + 1) * 128], 0)], KC))
            for q in range(4):
                for dc in range(16):
                    items.append((127 + q * 16 + dc, [(w_down[l, q * 2048:(q + 1) * 2048, dc * 128:(dc + 1) * 128], 0)], KC))
            for wi, w1 in enumerate((w1k, w1v)):
                for g in range(2):
                    for hc in range(2):
                        items.append((191 + wi * 4 + g * 2 + hc,
                                      [(w1[l, g * 2048:(g + 1) * 2048, hc * 128:(hc + 1) * 128], 0)], KC))
            items.append((199, [(w2k[l, :, :], 0)], 2))
            items.append((200, [(w2v[l, :, :], 0)], 2))
            return items

        def prep_gen(l, stg, sto, cast_engs):
            nb_ = len(stg)
            for i, (ci, pieces, nk) in enumerate(prep_list(l)):
                sg = stg[i % nb_]
                so = sto[i % nb_]
                wtot = 0
                for (src, co) in pieces:
                    ncols = src.shape[1]
                    tr.dma('sp', sg[:, 0:nk, co:co + ncols], src.rearrange("(k p) c -> p k c", p=128), sg, True)
                    wtot = max(wtot, co + ncols)
                ce = cast_engs[i % len(cast_engs)]
                if ce == 'act':
                    op('act', lambda e: e.copy(out=so[:, 0:nk, 0:wtot], in_=sg[:, 0:nk, 0:wtot]), [sg], [so])
                else:
                    op(ce, lambda e: e.tensor_copy(out=so[:, 0:nk, 0:wtot], in_=sg[:, 0:nk, 0:wtot]), [sg], [so])
                tr.dma('pool', WC[l][ci, :, 0:nk, 0:wtot], so[:, 0:nk, 0:wtot], so, False)
                yield

        def advance(gen, n):
            for _ in range(n):
                try:
                    next(gen)
                except StopIteration:
                    return False
            return True

        with ExitStack() as ph:
            cstf = tr.buf(ph, "cstf", [128, ctot], F32)
            tr.dma('sp', cstf[:], cst_in, cstf, True)
            tr.dma('sp', fcst[:], fcst_in, fcst, True)
            bfg = tr.buf(ph, "bfg", [8, NL], F32)
            tr.dma('sp', bfg[:], negb_in, bfg, True)
            op('dve', lambda e: e.tensor_scalar(out=negb[:], in0=bfg[:], scalar1=-1.0, scalar2=None, op0=ALU.mult), [bfg], [negb])
            half = ctot // 2
            cstb = tr.buf(ph, "cstb0", [128, ctot], BF16)
            op('dve', lambda e: e.tensor_copy(out=cstb[:, 0:half], in_=cstf[:, 0:half]), [cstf], [cstb])
            op('pool', lambda e: e.tensor_copy(out=cstb[:, half:ctot], in_=cstf[:, half:ctot]), [cstf], [cstb])
            tr.dma('pool', CSTB, cstb[:], cstb, False)
            o_id = coff['ident'][0]
            op('dve', lambda e: e.tensor_copy(out=identf[:], in_=cstf[:, o_id:o_id + 128]), [cstf], [identf])
            o_pm = coff['pmt'][0]
            op('dve', lambda e: e.tensor_copy(out=pmtb[:], in_=cstf[:, o_pm:o_pm + 32]), [cstf], [pmtb])
            op('pool', lambda e: e.memset(onecol[:], 1.0), [], [onecol])
            op('pool', lambda e: e.memset(onesb[:], 1.0), [], [onesb])
            op('pool', lambda e: e.memset(onesf[:], 1.0), [], [onesf])
            pef = tr.buf(ph, "pef", [128, NL, 2, 32], F32)
            tr.dma('sp', pef[:], peT_in, pef, True)
            op('dve', lambda e: e.tensor_copy(out=peb[:], in_=pef[:]), [pef], [peb])
            tr.end_phase(ph)
        with ExitStack() as ph:
            CW = 2048
            posi = tr.buf(ph, "posi", [32, CW], I32)
            posf = tr.buf(ph, "posf", [32, CW], F32)
            ru = tr.buf(ph, "ru", [32, CW], F32)
            rni = tr.buf(ph, "rni", [32, CW], I32)
            rnf = tr.buf(ph, "rnf", [32, CW], F32)
            rtab = [tr.buf(ph, "rtab%d" % i, [32, CW], F32) for i in range(2)]
            for cidx in range(S // CW):
                sl = slice(cidx * CW, (cidx + 1) * CW)
                tr.dma('sp', posi[:], pos_in[0:1, sl].to_broadcast([32, CW]), posi, True)
                op('dve', lambda e: e.tensor_copy(out=posf[:], in_=posi[:]), [posi], [posf])
                for ti, (tab, addc) in enumerate(((SIN, 0.0), (COS, 0.25))):
                    op('dve', lambda e: e.tensor_scalar(out=ru[:], in0=posf[:], scalar1=fcst[0:32, 0:1], scalar2=addc,
                                                        op0=ALU.mult, op1=ALU.add), [posf, fcst], [ru])
                    op('dve', lambda e: e.tensor_copy(out=rni[:], in_=ru[:]), [ru], [rni])
                    op('dve', lambda e: e.tensor_copy(out=rnf[:], in_=rni[:]), [rni], [rnf])
                    op('dve', lambda e: e.tensor_tensor(out=ru[:], in0=ru[:], in1=rnf[:], op=ALU.subtract), [ru, rnf], [ru])
                    rt = rtab[ti]
                    op('act', lambda e: e.activation(out=rt[:], in_=ru[:], func=AF.Sin, scale=2.0 * np.pi), [ru], [rt])
                    tr.dma('pool', tab[:, sl], rt[:], rt, False)
            tr.end_phase(ph)
        with ExitStack() as ph:
            gains = tr.buf(ph, "gains", [128, NL, 4, KC], F32)
            tr.dma('sp', gains[:], gT_in, gains, True)
            bmod = tr.buf(ph, "bmod", [128, NL, 96], F32)
            tr.dma('sp', bmod[:], bmodT, bmod, True)
            cT = tr.buf(ph, "cT", [128, KC], F32)
            tr.dma('sp', cT[:], cT_in, cT, True)
            cs = tr.buf(ph, "cs", [128, KC], F32)
            op('act', lambda e: e.activation(out=cs[:], in_=cT[:], func=AF.Silu), [cT], [cs])
            modt = tr.buf(ph, "modt", [128, NL, 96], F32)
            wm = [tr.buf(ph, "wm%d" % i, [128, KC, 128], F32) for i in range(3)]
            stg0 = [tr.buf(ph, "stg%d" % i, [128, KC, 128], F32) for i in range(3)]
            sto0 = [tr.buf(ph, "sto%d" % i, [128, KC, 128], BF16) for i in range(3)]
            pg0 = prep_gen(0, stg0, sto0, ('dve', 'pool', 'act'))
            for l in range(NL):
                pm_ = ps[l % 2]
                for j in range(96):
                    b = wm[j % 3]
                    tr.dma('sp', b[:], w_mod[l, :, j * 128:(j + 1) * 128].rearrange("(k p) c -> p k c", p=128), b, True)
                    for k in range(KC):
                        op('pe', lambda e: e.matmul(pm_[:, j:j + 1], lhsT=b[:, k, :], rhs=cs[:, k:k + 1],
                                                    start=(k == 0), stop=(k == KC - 1)), [b, cs], [pm_])
                    advance(pg0, 1)
                op('dve', lambda e: e.tensor_tensor(out=modt[:, l, :], in0=pm_[:, 0:96], in1=bmod[:, l, :], op=ALU.add),
                   [pm_, bmod], [modt])
                for (di, si, gi) in ((0, 1, 0), (3, 4, 2)):
                    op('dve', lambda e: e.scalar_tensor_tensor(out=der[:, l, di, :], in0=modt[:, l, si * 16:(si + 1) * 16],
                                                               scalar=1.0, in1=gains[:, l, gi, :], op0=ALU.add, op1=ALU.mult),
                       [modt, gains], [der])
                for (di, si) in ((1, 0), (4, 3)):
                    op('dve', lambda e: e.tensor_copy(out=der[:, l, di, :], in_=modt[:, l, si * 16:(si + 1) * 16]), [modt], [der])
                for (di, si, gi) in ((2, 2, 1), (5, 5, 3)):
                    op('dve', lambda e: e.tensor_tensor(out=der[:, l, di, :], in0=modt[:, l, si * 16:(si + 1) * 16],
                                                        in1=gains[:, l, gi, :], op=ALU.mult), [modt, gains], [der])
            while advance(pg0, 8):
                pass
            tr.end_phase(ph)
        for l in range(NL):
            xin = x_in if l == 0 else XS[(l - 1) % 2]
            xout = y_out if l == NL - 1 else XS[l % 2]
            with ExitStack() as ly:
                kmh = tr.buf(ly, "kmh", [128, 4, 32], BF16)
                kml = tr.buf(ly, "kml", [128, 4, 32], BF16)
                CK = tr.buf(ly, "CK", [128, NT, 8], F32)
                KcmpT = tr.buf(ly, "KcmpT", [128, NCP], BF16)
                Vcmp = tr.buf(ly, "Vcmp", [128, nct, 128], BF16)

                def rms_rstd(ph_bufs, sumps):
                    tmpn, rstd = ph_bufs
                    op('dve', lambda e: e.tensor_scalar(out=tmpn[:], in0=sumps[:], scalar1=1.0 / D, scalar2=EPS,
                                                        op0=ALU.mult, op1=ALU.add), [sumps], [tmpn])
                    op('act', lambda e: e.activation(out=tmpn[:], in_=tmpn[:], func=AF.Sqrt), [tmpn], [tmpn])
                    op('dve', lambda e: e.reciprocal(out=rstd[:], in_=tmpn[:]), [tmpn], [rstd])

                with ExitStack() as ph:
                    hT = [tr.buf(ph, "hT%d" % i, [128, KC, 512], BF16) for i in range(2)]
                    xc = [tr.buf(ph, "xc%d" % i, [128, 2, 512], F32) for i in range(3)]
                    sq = [tr.buf(ph, "sq%d" % i, [128, 512], BF16) for i in range(2)]
                    tmpn = tr.buf(ph, "tmpn", [128, 512], F32)
                    rstd = tr.buf(ph, "rstd", [128, 512], F32)
                    tmpx = [tr.buf(ph, "tmpx%d" % i, [128, 512], F32) for i in range(2)]
                    wv = [tr.buf(ph, "wv%d" % i, [128, KC, 512], BF16) for i in range(2)]
                    qk = [tr.buf(ph, "qk%d" % i, [128, 512], BF16) for i in range(3)]
                    ropeA = tr.buf(ph, "ropeA", [32, 512], F32)
                    ropeB = tr.buf(ph, "ropeB", [32, 512], F32)
                    cosb = [tr.buf(ph, "cosb%d" % i, [32, 512], F32) for i in range(2)]
                    sinb = [tr.buf(ph, "sinb%d" % i, [32, 512], F32) for i in range(2)]
                    vout = [tr.buf(ph, "vout%d" % i, [128, 4, 512], BF16) for i in range(2)]
                    gsb = [tr.buf(ph, "gsb%d" % i, [12, 512], BF16) for i in range(2)]
                    fe = tr.buf(ph, "fe", [8, 512], F32)
                    fl = tr.buf(ph, "fl", [8, 512], F32)
                    Cb = [tr.buf(ph, "Cb%d" % i, [8, 512], F32) for i in range(2)]
                    rhob = [tr.buf(ph, "rhob%d" % i, [8, 512], BF16) for i in range(2)]
                    ones8 = tr.buf(ph, "ones8", [8, 512], F32)
                    kmf = tr.buf(ph, "kmf", [128, 4, 32], F32)
                    kmt = tr.buf(ph, "kmt", [128, 4, 32], F32)
                    op('pool', lambda e: e.memset(ones8[:], 1.0), [], [ones8])
                    op('pool', lambda e: e.memset(kmf[:], 0.0), [], [kmf])
                    xcn = [0]
                    qkn = [0]
                    pcn = [0]
                    vgn = [0]
                    roped = set(range(0, 13)) | {14, 15}
                    roped.discard(13)
                    vgroups = [(32, 4, 0), (36, 2, 512), (38, 4, 768), (42, 4, 1280)]
                    for b in range(NB):
                        t0 = b * 512
                        tsl = slice(t0, t0 + 512)
                        h = hT[b % 2]
                        pn = ps[6]
                        for g in range(8):
                            xb = xc[xcn[0] % 3]
                            xcn[0] += 1
                            tr.dma('sp', xb[:], xin[:, 2 * g:2 * g + 2, tsl], xb, True)
                            for c in range(2):
                                s_ = sq[(2 * g + c) % 2]
                                op('act', lambda e: e.activation(out=s_[:], in_=xb[:, c, :], func=AF.Square), [xb], [s_])
                                op('pe', lambda e: e.matmul(pn[:], lhsT=onesb[:], rhs=s_[:], start=(g == 0 and c == 0),
                                                            stop=(g == 7 and c == 1)), [s_, onesb], [pn])
                        rms_rstd((tmpn, rstd), pn)
                        for g in range(8):
                            xb = xc[xcn[0] % 3]
                            xcn[0] += 1
                            tr.dma('sp', xb[:], xin[:, 2 * g:2 * g + 2, tsl], xb, True)
                            for c in range(2):
                                k = 2 * g + c
                                tx = tmpx[k % 2]
                                op('dve', lambda e: e.tensor_tensor(out=tx[:], in0=xb[:, c, :], in1=rstd[:], op=ALU.mult),
                                   [xb, rstd], [tx])
                                op('pool', lambda e: e.tensor_scalar(out=h[:, k, :], in0=tx[:], scalar1=der[:, l, 0, k:k + 1],
                                                                     scalar2=der[:, l, 1, k:k + 1], op0=ALU.mult, op1=ALU.add),
                                   [tx, der], [h])
                        cb_ = cosb[b % 2]
                        sb_ = sinb[b % 2]
                        tr.dma('sp', cb_[:], COS[:, tsl], cb_, True)
                        tr.dma('sp', sb_[:], SIN[:, tsl], sb_, True)
                        for ci in range(32):
                            w = load_w(l, ci)
                            pp = ps[pcn[0] % 3]
                            pcn[0] += 1
                            for k in range(KC):
                                op('pe', lambda e: e.matmul(pp[:], lhsT=w[:, k, :], rhs=h[:, k, :], start=(k == 0),
                                                            stop=(k == KC - 1)), [w, h], [pp])
                            q_ = qk[qkn[0] % 3]
                            qkn[0] += 1
                            op('act', lambda e: e.copy(out=q_[:], in_=pp[:]), [pp], [q_])
                            if ci in roped:
                                pr = ps[3]
                                op('pe', lambda e: e.matmul(pr[0:32, :], lhsT=pmtb[:], rhs=q_[:], start=True, stop=True),
                                   [q_, pmtb], [pr])
                                op('dve', lambda e: e.tensor_tensor(out=ropeA[:], in0=pr[0:32, :], in1=sb_[:], op=ALU.mult),
                                   [pr, sb_], [ropeA])
                                op('dve', lambda e: e.tensor_tensor(out=ropeB[:], in0=q_[0:32, :], in1=cb_[:], op=ALU.mult),
                                   [q_, cb_], [ropeB])
                                op('dve', lambda e: e.tensor_tensor(out=q_[0:32, :], in0=ropeA[:], in1=ropeB[:], op=ALU.add),
                                   [ropeA, ropeB], [q_])
                            if 4 <= ci < 8:
                                op('dve', lambda e: e.tensor_reduce(out=kmf[:, ci - 4, 2 * b:2 * b + 2],
                                                                    in_=q_[:].rearrange("p (a c) -> p a c", a=2),
                                                                    axis=AX.X, op=ALU.add), [q_], [kmf])
                            tr.dma('pool', FM[ci, :, tsl], q_[:], q_, False)
                        for (c0, nchk, col0) in vgroups:
                            wvb = wv[vgn[0] % 2]
                            vo = vout[vgn[0] % 2]
                            vgn[0] += 1
                            ncols = nchk * 128
                            for i in range(nchk):
                                tr.dma('sp', wvb[:, :, i * 128:(i + 1) * 128], WC[l][c0 + i], wvb, True)
                            for tt in range(4):
                                pp = ps[pcn[0] % 3]
                                pcn[0] += 1
                                for k in range(KC):
                                    op('pe', lambda e: e.matmul(pp[:, 0:ncols], lhsT=h[:, k, tt * 128:(tt + 1) * 128],
                                                                rhs=wvb[:, k, 0:ncols], start=(k == 0), stop=(k == KC - 1)),
                                       [wvb, h], [pp])
                                if tt % 2 == 0:
                                    op('act', lambda e: e.copy(out=vo[:, tt, 0:ncols], in_=pp[:, 0:ncols]), [pp], [vo])
                                else:
                                    op('dve', lambda e: e.tensor_copy(out=vo[:, tt, 0:ncols], in_=pp[:, 0:ncols]), [pp], [vo])
                            for i in range(nchk):
                                tr.dma('pool', VT[col0 // 128 + i, :, 4 * b:4 * b + 4, :], vo[:, :, i * 128:(i + 1) * 128], vo, False)
                        w = load_w(l, 46)
                        pg = ps[4]
                        for k in range(KC):
                            op('pe', lambda e: e.matmul(pg[0:12, :], lhsT=w[:, k, 0:12], rhs=h[:, k, :], start=(k == 0),
                                                        stop=(k == KC - 1)), [w, h], [pg])
                        g_ = gsb[b % 2]
                        op('act', lambda e: e.activation(out=g_[:], in_=pg[0:12, :], func=AF.Sigmoid), [pg], [g_])
                        tr.dma('pool', GT[:, tsl], g_[:], g_, False)
                        pf = ps[5]
                        for k in range(KC):
                            op('pe', lambda e: e.matmul(pf[0:8, :], lhsT=w[:, k, 12:20], rhs=h[:, k, :], start=(k == 0),
                                                        stop=(k == KC - 1)), [w, h], [pf])
                        op('act', lambda e: e.activation(out=fe[:], in_=pf[0:8, :], func=AF.Exp, scale=-1.0,
                                                         bias=negb[:, l:l + 1]), [pf, negb], [fe])
                        op('act', lambda e: e.activation(out=fl[:], in_=fe[:], func=AF.Ln, bias=onecol[0:8, 0:1]),
                           [fe, onecol], [fl])
                        cbuf = Cb[b % 2]
                        cprev = Cb[(b - 1) % 2]
                        if b == 0:
                            op('dve', lambda e: e.tensor_tensor_scan(out=cbuf[:], data0=ones8[:], data1=fl[:], initial=0.0,
                                                                     op0=ALU.mult, op1=ALU.add), [ones8, fl], [cbuf])
                        else:
                            op('dve', lambda e: e.tensor_tensor_scan(out=cbuf[:], data0=ones8[:], data1=fl[:],
                                                                     initial=cprev[:, 511:512], op0=ALU.mult, op1=ALU.add),
                               [ones8, fl, cprev], [cbuf])
                        rb_ = rhob[b % 2]
                        op('dve', lambda e: e.tensor_scalar(out=rb_[:], in0=cbuf[:], scalar1=-1.0 / SCL, scalar2=None,
                                                            op0=ALU.mult), [cbuf], [rb_])
                        tr.dma('pool', RHO[:, tsl], rb_[:], rb_, False)
                        pt_ = ps[3]
                        for i in range(4):
                            op('pe', lambda e: e.transpose(out=pt_[:, 8 * i:8 * i + 8], in_=cbuf[0:8, 128 * i:128 * (i + 1)],
                                                           identity=identf[0:8, 0:8]), [cbuf, identf], [pt_])
                        op('dve', lambda e: e.tensor_copy(out=CK[:, 4 * b:4 * b + 4, :],
                                                          in_=pt_[:, 0:32].rearrange("p (a c) -> p a c", a=4)), [pt_], [CK])
                    op('dve', lambda e: e.tensor_scalar(out=kmf[:], in0=kmf[:], scalar1=1.0 / 256.0, scalar2=None,
                                                        op0=ALU.mult), [kmf], [kmf])
                    op('dve', lambda e: e.tensor_copy(out=kmh[:], in_=kmf[:]), [kmf], [kmh])
                    op('dve', lambda e: e.tensor_copy(out=kmt[:], in_=kmh[:]), [kmh], [kmt])
                    op('dve', lambda e: e.tensor_tensor(out=kmt[:], in0=kmf[:], in1=kmt[:], op=ALU.subtract), [kmf, kmt], [kmt])
                    op('dve', lambda e: e.tensor_copy(out=kml[:], in_=kmt[:]), [kmt], [kml])
                    tr.end_phase(ph)

                with ExitStack() as ph:
                    kcS = tr.buf(ph, "kcS", [128, S], BF16)
                    Z = tr.buf(ph, "Z", [128, 16, S // 16], BF16)
                    W1 = tr.buf(ph, "W1", [128, 32, 256], BF16)
                    W2 = tr.buf(ph, "W2", [128, 2, 128], BF16)
                    bcol = tr.buf(ph, "bcol", [128, 2], F32)
                    xg = tr.buf(ph, "xg", [128, 512], F32)
                    x2 = tr.buf(ph, "x2", [128, 512], F32)
                    sg = tr.buf(ph, "sg", [128, 512], F32)
                    GTt = tr.buf(ph, "GTt", [128, 2, 512], BF16)
                    for wi in range(2):
                        tr.dma('sp', kcS[:], FM[12 + wi], kcS, True)
                        op('pool', lambda e: e.tensor_copy(out=Z[:], in_=kcS[:].rearrange("p (m r) -> p r m", r=16)), [kcS], [Z])
                        for g in range(2):
                            for hc in range(2):
                                tr.dma('sp', W1[:, 16 * g:16 * g + 16, hc * 128:(hc + 1) * 128],
                                       WC[l][191 + wi * 4 + g * 2 + hc], W1, True)
                        tr.dma('sp', W2[:], WC[l][199 + wi, :, 0:2, :], W2, True)
                        op('pool', lambda e: e.memset(GTt[:], 0.0), [], [GTt])
                        for hc in range(2):
                            phd = ps[hc]
                            pbs = ps[2]
                            for tau in range(32):
                                a, r = divmod(tau, 16)
                                op('pe', lambda e: e.matmul(phd[:, 0:ncmp], lhsT=W1[:, tau, hc * 128:(hc + 1) * 128],
                                                            rhs=Z[:, r, a:a + ncmp], start=(tau == 0), stop=(tau == 31)),
                                   [W1, Z], [phd])
                            for tau in range(32):
                                op('pe', lambda e: e.matmul(pbs[:, hc:hc + 1], lhsT=W1[:, tau, hc * 128:(hc + 1) * 128],
                                                            rhs=peb[:, l, wi, tau:tau + 1], start=(tau == 0), stop=(tau == 31)),
                                   [W1, peb], [pbs])
                            op('dve', lambda e: e.tensor_copy(out=bcol[:, hc:hc + 1], in_=pbs[:, hc:hc + 1]), [pbs], [bcol])
                            op('act', lambda e: e.activation(out=xg[:, 0:ncmp], in_=phd[:, 0:ncmp], func=AF.Identity,
                                                             bias=bcol[:, hc:hc + 1]), [phd, bcol], [xg])
                            op('dve', lambda e: e.tensor_tensor(out=x2[:, 0:ncmp], in0=xg[:, 0:ncmp], in1=xg[:, 0:ncmp],
                                                                op=ALU.mult), [xg], [x2])
                            op('dve', lambda e: e.tensor_scalar(out=x2[:, 0:ncmp], in0=x2[:, 0:ncmp], scalar1=0.0713548163,
                                                                scalar2=1.5957691216, op0=ALU.mult, op1=ALU.add), [x2], [x2])
                            op('dve', lambda e: e.tensor_tensor(out=x2[:, 0:ncmp], in0=x2[:, 0:ncmp], in1=xg[:, 0:ncmp],
                                                                op=ALU.mult), [x2, xg], [x2])
                            op('act', lambda e: e.activation(out=sg[:, 0:ncmp], in_=x2[:, 0:ncmp], func=AF.Sigmoid), [x2], [sg])
                            op('dve', lambda e: e.tensor_tensor(out=GTt[:, hc, 0:ncmp], in0=xg[:, 0:ncmp], in1=sg[:, 0:ncmp],
                                                                op=ALU.mult), [xg, sg], [GTt])
                        po = ps[3]
                        if wi == 0:
                            for hc in range(2):
                                op('pe', lambda e: e.matmul(po[:, 0:NCP], lhsT=W2[:, hc, :], rhs=GTt[:, hc, 0:NCP],
                                                            start=(hc == 0), stop=(hc == 1)), [W2, GTt], [po])
                            op('act', lambda e: e.copy(out=KcmpT[:], in_=po[:, 0:NCP]), [po], [KcmpT])
                        else:
                            for ct in range(nct):
                                for hc in range(2):
                                    op('pe', lambda e: e.matmul(po[:, ct * 128:(ct + 1) * 128],
                                                                lhsT=GTt[:, hc, ct * 128:(ct + 1) * 128], rhs=W2[:, hc, :],
                                                                start=(hc == 0), stop=(hc == 1)), [W2, GTt], [po])
                            op('act', lambda e: e.copy(out=Vcmp[:], in_=po[:, 0:NCP].rearrange("p (a c) -> p a c", a=nct)),
                               [po], [Vcmp])
                    tr.end_phase(ph)

                with ExitStack() as ph:
                    QT = [tr.buf(ph, "QT%d" % i, [128, 512], BF16) for i in range(2)]
                    Kc = [tr.buf(ph, "Kc%d" % i, [128, 2048], BF16) for i in range(3)]
                    Vc = [tr.buf(ph, "Vc%d" % i, [128, 16, 128], BF16) for i in range(3)]
                    PT = [tr.buf(ph, "PT%d" % i, [128, 512], BF16) for i in range(6)]
                    ET = tr.buf(ph, "ET", [128, 4, nct, 512], BF16)
                    acc = [tr.buf(ph, "acc%d" % i, [128, 512], F32) for i in range(4)]
                    rec = [tr.buf(ph, "rec%d" % i, [128, 512], F32) for i in range(2)]
                    rgt = [tr.buf(ph, "rgt%d" % i, [128, 512], F32) for i in range(2)]
                    tmc = [tr.buf(ph, "tmc%d" % i, [128, 512], F32) for i in range(2)]
                    rho1 = [tr.buf(ph, "rho1_%d" % i, [128, 512], BF16) for i in range(2)]
                    for r_ in rho1:
                        op('pool', lambda e: e.memset(r_[:], 0.0), [], [r_])
                    E0 = tr.buf(ph, "E0", [128, 128], BF16)
                    op('pool', lambda e: e.memset(E0[:], 0.0), [], [E0])
                    op('pool', lambda e: e.memset(E0[0:1, :], 1.0), [], [E0])
                    cstb = tr.buf(ph, "cstB", [128, ctot], BF16)
                    cst_holder[0] = cstb
                    tr.dma('sp', cstb[:], CSTB, cstb, True)
                    accD = [tr.buf(ph, "accD%d" % i, [128, 512], F32) for i in range(2)]
                    accG = [tr.buf(ph, "accG%d" % i, [128, 512], F32) for i in range(2)]
                    pending = []
                    if l + 1 < NL:
                        stgB = [tr.buf(ph, "stgB%d" % i, [128, KC, 128], F32) for i in range(2)]
                        stoB = [tr.buf(ph, "stoB%d" % i, [128, KC, 128], BF16) for i in range(2)]
                        pgB = prep_gen(l + 1, stgB, stoB, ('dve', 'pool'))
                    else:
                        pgB = iter(())
                    prep_per_job = max(1, -(-NCH // (NB * 28)))
                    gsbB = tr.buf(ph, "gsbB", [12, 512], BF16)
                    Gp = [tr.buf(ph, "Gp%d" % i, [128, 32], F32) for i in range(2)]
                    m8 = [tr.buf(ph, "m8_%d" % i, [128, 8], F32) for i in range(2)]
                    m8b = [tr.buf(ph, "m8b_%d" % i, [128, 8], F32) for i in range(2)]
                    Bq = [tr.buf(ph, "Bq%d" % i, [128, 32], BF16) for i in range(2)]
                    BtM = [tr.buf(ph, "BtM%d" % i, [128, 512], BF16) for i in range(2)]
                    for bt_ in BtM:
                        op('pool', lambda e: e.memset(bt_[:], 0.0), [], [bt_])
                    imp = [tr.buf(ph, "imp%d" % i, [128, n_sb], F32) for i in range(2)]
                    sc2 = tr.buf(ph, "sc2", [128, n_sb], F32)
                    BqN = [tr.buf(ph, "BqN%d" % i, [128, n_sb], BF16) for i in range(2)]
                    BtN = tr.buf(ph, "BtN", [128, 512], BF16)
                    op('pool', lambda e: e.memset(BtN[:], 0.0), [], [BtN])
                    rd = [tr.buf(ph, "rd%d" % i, [128, 1], F32) for i in range(2)]
                    Ost = [tr.buf(ph, "Ost%d" % i, [128, 512], BF16) for i in range(3)]
                    psL = [ps[0], ps[1], ps[4]]
                    psN = [ps[2], ps[3]]
                    psD = [ps[5], ps[5]]
                    psX = ps[6]
                    cnt = dict(q=0, kv=0, pt=0, job=0, rho=0, ost=0, u=0, x=0)

                    def load_q(ci, tsl):
                        q_ = QT[cnt['q'] % 2]
                        cnt['q'] += 1
                        tr.dma('sp', q_[:], FM[ci, :, tsl], q_, True)
                        return q_

                    def load_kv(kci, vcol, j0, j1):
                        res = {}
                        ck0, ck1 = j0 // 16, j1 // 16
                        for ck in range(ck0, ck1 + 1):
                            ja = max(j0, ck * 16)
                            jb = min(j1, ck * 16 + 15)
                            kb = Kc[cnt['kv'] % 3]
                            vb = Vc[cnt['kv'] % 3]
                            cnt['kv'] += 1
                            n = jb - ja + 1
                            tr.dma('sp', kb[:, 0:n * 128], FM[kci, :, ja * 128:(jb + 1) * 128], kb, True)
                            tr.dma('sp', vb[:, 0:n, :], VT[vcol // 128, :, ja:jb + 1, :], vb, True)
                            for j in range(ja, jb + 1):
                                res[j] = (kb[:, (j - ja) * 128:(j - ja + 1) * 128], kb, vb[:, j - ja, :], vb)
                        return res

                    def step_pending():
                        while pending:
                            try:
                                next(pending[0])
                                return
                            except StopIteration:
                                pending.pop(0)

                    def flush_pending():
                        while pending:
                            for _ in pending[0]:
                                pass
                            pending.pop(0)

                    def attn_job(q_, units, finalize):
                        jn = cnt['job']
                        cnt['job'] += 1
                        pN = psN[jn % 2]
                        pD = psD[jn % 2]
                        aD = accD[jn % 2]
                        aG = accG[jn % 2]
                        n = len(units)
                        pts = [None] * n
                        op('dve', lambda e: e.memset(aD[:], 0.0), [], [aD])
                        op('pool', lambda e: e.memset(aG[:], 0.0), [], [aG])

                        def emit_S(i):
                            u = units[i]
                            f0, f1 = u.get('f0', 0), u.get('f1', 512)
                            pl = psL[cnt['pt'] % 3]
                            ex = u.get('extras', [])
                            op('pe', lambda e: e.matmul(pl[:, f0:f1], lhsT=u['k'], rhs=q_[:, f0:f1], start=True,
                                                        stop=(len(ex) == 0)), [u['kb'], q_], [pl])
                            for xi, (la, ra, bl) in enumerate(ex):
                                op('pe', lambda e: e.matmul(pl[:, f0:f1], lhsT=la, rhs=ra, start=False,
                                                            stop=(xi == len(ex) - 1)), bl, [pl])
                            if 'pt' in u:
                                pap, pbuf = u['pt']
                            else:
                                pbuf = PT[cnt['pt'] % 6]
                                pap = pbuf[:]
                            cnt['pt'] += 1
                            if u.get('bias') is not None:
                                bap, bbuf = u['bias']
                                op('act', lambda e: e.activation(out=pap[:, f0:f1], in_=pl[:, f0:f1], func=AF.Exp, scale=SCL,
                                                                 bias=bap), [pl, bbuf], [pbuf])
                            else:
                                op('act', lambda e: e.activation(out=pap[:, f0:f1], in_=pl[:, f0:f1], func=AF.Exp, scale=SCL),
                                   [pl], [pbuf])
                            pts[i] = (pap, pbuf)

                        def emit_PV(i):
                            u = units[i]
                            f0, f1 = u.get('f0', 0), u.get('f1', 512)
                            pap, pbuf = pts[i]
                            op('pe', lambda e: e.matmul(pN[:, f0:f1], lhsT=u['v'], rhs=pap[:, f0:f1], start=(i == 0),
                                                        stop=(i == n - 1)), [u['vb'], pbuf], [pN])
                            if i % 3 != 2:
                                op('dve', lambda e: e.tensor_tensor(out=aD[:, f0:f1], in0=aD[:, f0:f1], in1=pap[:, f0:f1],
                                                                    op=ALU.add), [aD, pbuf], [aD])
                            else:
                                op('pool', lambda e: e.tensor_tensor(out=aG[:, f0:f1], in0=aG[:, f0:f1], in1=pap[:, f0:f1],
                                                                     op=ALU.add), [aG, pbuf], [aG])

                        LAG = 2
                        for i in range(n + LAG):
                            if i < n:
                                emit_S(i)
                            if i >= LAG:
                                emit_PV(i - LAG)
                                if i >= LAG + 1:
                                    step_pending()
                        flush_pending()

                        def fin():
                            op('dve', lambda e: e.tensor_tensor(out=aD[:], in0=aD[:], in1=aG[:], op=ALU.add), [aD, aG], [aD])
                            yield
                            op('pe', lambda e: e.matmul(pD[:], lhsT=onesf[:], rhs=aD[:], start=True, stop=True),
                               [onesf, aD], [pD])
                            yield
                            for _ in finalize(pN, pD):
                                yield
                        pending.append(fin())
                        advance(pgB, prep_per_job)

                    def recip_den(pD):
                        r_ = rec[cnt['x'] % 2]
                        cnt['x'] += 1
                        op('dve', lambda e: e.tensor_scalar(out=r_[:], in0=pD[:], scalar1=TINY, scalar2=None, op0=ALU.max),
                           [pD], [r_])
                        yield r_
                        for qq in range(4):
                            op('dve', lambda e: e.reciprocal(out=r_[:, 128 * qq:128 * (qq + 1)], in_=r_[:, 128 * qq:128 * (qq + 1)]),
                               [r_], [r_])
                            yield r_

                    def fin_plain(hc, tsl):
                        def f(pN, pD):
                            r_ = None
                            for r_ in recip_den(pD):
                                yield
                            o_ = Ost[cnt['ost'] % 3]
                            cnt['ost'] += 1
                            op('dve', lambda e: e.tensor_tensor(out=o_[:], in0=pN[:], in1=r_[:], op=ALU.mult), [pN, r_], [o_])
                            tr.dma('pool', OTD[hc, :, tsl], o_[:], o_, False)
                            yield
                        return f

                    def fin_nsa(hh, br, tsl):
                        def f(pN, pD):
                            r_ = None
                            for r_ in recip_den(pD):
                                yield
                            rr = 3 * hh + br
                            op('pe', lambda e: e.matmul(psX[:], lhsT=cv('eg', 12, (rr * 128, (rr + 1) * 128)), rhs=gsbB[:],
                                                        start=True, stop=True), [cstb, gsbB], [psX])
                            g_ = rgt[cnt['x'] % 2]
                            op('dve', lambda e: e.tensor_tensor(out=g_[:], in0=psX[:], in1=r_[:], op=ALU.mult), [psX, r_], [g_])
                            yield
                            a_ = acc[hh]
                            if br == 0:
                                op('dve', lambda e: e.tensor_tensor(out=a_[:], in0=pN[:], in1=g_[:], op=ALU.mult), [pN, g_], [a_])
                            else:
                                t_ = tmc[cnt['x'] % 2]
                                op('dve', lambda e: e.tensor_tensor(out=t_[:], in0=pN[:], in1=g_[:], op=ALU.mult), [pN, g_], [t_])
                                op('pool', lambda e: e.tensor_tensor(out=a_[:], in0=a_[:], in1=t_[:], op=ALU.add), [a_, t_], [a_])
                            yield
                            if br == 2:
                                o_ = Ost[cnt['ost'] % 3]
                                cnt['ost'] += 1
                                op('pool', lambda e: e.tensor_copy(out=o_[:], in_=a_[:]), [a_], [o_])
                                tr.dma('pool', OTD[4 + hh, :, tsl], o_[:], o_, False)
                                yield
                        return f

                    causal = cv('causal')

                    def diag_extra(jd):
                        f0 = 128 * jd
                        return (cv('i30k'), cv('causal', 128, (0, 512 - f0)), [cstb]), f0

                    for b in range(NB):
                        t0 = b * 512
                        tsl = slice(t0, t0 + 512)
                        jlast = 4 * b + 3
                        for hh in range(8):
                            q_ = load_q(16 + hh, tsl)
                            r1 = rho1[cnt['rho'] % 2]
                            cnt['rho'] += 1
                            tr.dma('sp', r1[0:1, :], RHO[hh:hh + 1, tsl], r1, True)
                            kv = load_kv(24 + hh, 768 + 128 * hh, 0, jlast)
                            units = []
                            for j in range(jlast + 1):
                                ka, kb, va, vb = kv[j]
                                u = dict(k=ka, kb=kb, v=va, vb=vb, bias=(CK[:, j, hh:hh + 1], CK))
                                f0 = 0
                                ex = []
                                if j >= 4 * b:
                                    dx, f0 = diag_extra(j - 4 * b)
                                    ex.append(dx)
                                ex.insert(0, (E0[:], r1[:, f0:512], [E0, r1]))
                                u['extras'] = ex
                                u['f0'] = f0
                                units.append(u)
                            attn_job(q_, units, fin_plain(8 + hh, tsl))
                        for hh in range(4):
                            q_ = load_q(hh, tsl)
                            bt = BtM[hh % 2]
                            for i in range(4):
                                own = 2 * b + i // 2
                                gp = Gp[i % 2]
                                mm = m8[i % 2]
                                bq = Bq[i % 2]
                                op('pe', lambda e: e.matmul(psX[:, 0:32], lhsT=q_[:, 128 * i:128 * (i + 1)], rhs=kmh[:, hh, :],
                                                            start=True, stop=False), [q_, kmh], [psX])
                                op('pe', lambda e: e.matmul(psX[:, 0:32], lhsT=q_[:, 128 * i:128 * (i + 1)], rhs=kml[:, hh, :],
                                                            start=False, stop=True), [q_, kml], [psX])
                                op('pool', lambda e: e.memset(gp[:], -1e30), [], [gp])
                                if own > 0:
                                    op('dve', lambda e: e.tensor_copy(out=gp[:, 0:own], in_=psX[:, 0:own]), [psX], [gp])
                                op('dve', lambda e: e.max(out=mm[:], in_=gp[:]), [gp], [mm])
                                op('dve', lambda e: e.tensor_scalar(out=mm[:, 2:3], in0=mm[:, 2:3], scalar1=-5e29, scalar2=None,
                                                                    op0=ALU.max), [mm], [mm])
                                op('dve', lambda e: e.tensor_scalar(out=bq[:], in0=gp[:], scalar1=mm[:, 2:3], scalar2=1.0,
                                                                    op0=ALU.is_ge, op1=ALU.subtract), [gp, mm], [bq])
                                op('pool', lambda e: e.memset(bq[:, own:own + 1], 0.0), [], [bq])
                                if own + 1 < 32:
                                    op('pool', lambda e: e.memset(bq[:, own + 1:32], -1.0), [], [bq])
                                op('pe', lambda e: e.transpose(out=psb[0:32, 128 * i:128 * (i + 1)], in_=bq[:],
                                                               identity=cv('ident')), [bq, cstb], [psb])
                            op('dve', lambda e: e.tensor_copy(out=bt[0:32, :], in_=psb[0:32, 0:512]), [psb], [bt])
                            kv = load_kv(4 + hh, 128 * hh, 0, jlast)
                            units = []
                            for j in range(jlast + 1):
                                ka, kb, va, vb = kv[j]
                                u = dict(k=ka, kb=kb, v=va, vb=vb)
                                f0 = 0
                                ex = []
                                if j >= 4 * b:
                                    dx, f0 = diag_extra(j - 4 * b)
                                    ex.append(dx)
                                nblk = j // 2
                                ex.insert(0, (cv('am', 128, (nblk * 128, (nblk + 1) * 128)), bt[:, f0:512], [cstb, bt]))
                                u['extras'] = ex
                                u['f0'] = f0
                                units.append(u)
                            attn_job(q_, units, fin_plain(hh, tsl))
                        tr.dma('sp', gsbB[:], GT[:, tsl], gsbB, True)
                        jcs = [jc for jc in range(nct) if b - 4 * jc >= 0]
                        for hh in range(4):
                            q_ = load_q(8 + hh, tsl)
                            units = []
                            for jc in jcs:
                                dlt = b - 4 * jc
                                u = dict(k=KcmpT[:, jc * 128:(jc + 1) * 128], kb=KcmpT, v=Vcmp[:, jc, :], vb=Vcmp,
                                         pt=(ET[:, hh, jc, :], ET))
                                if dlt <= 4:
                                    u['extras'] = [(cv('i30k'), cv('cmask', 128, (dlt * 512, (dlt + 1) * 512)), [cstb])]
                                units.append(u)
                            attn_job(q_, units, fin_nsa(hh, 0, tsl))
                        for i in range(4):
                            im = imp[i % 2]
                            for hh in range(4):
                                pu = psL[cnt['u'] % 3]
                                cnt['u'] += 1
                                for xi, jc in enumerate(jcs):
                                    op('pe', lambda e: e.matmul(pu[:, 0:n_sb + 1], lhsT=ET[:, hh, jc, 128 * i:128 * (i + 1)],
                                                                rhs=cv('ov', 128, (jc * (n_sb + 1), (jc + 1) * (n_sb + 1))),
                                                                start=(xi == 0), stop=(xi == len(jcs) - 1)), [ET, cstb], [pu])
                                rd_ = rd[hh % 2]
                                op('dve', lambda e: e.tensor_scalar(out=rd_[:], in0=pu[:, n_sb:n_sb + 1], scalar1=TINY,
                                                                    scalar2=None, op0=ALU.max), [pu], [rd_])
                                op('dve', lambda e: e.reciprocal(out=rd_[:], in_=rd_[:]), [rd_], [rd_])
                                if hh == 0:
                                    op('dve', lambda e: e.tensor_scalar(out=im[:], in0=pu[:, 0:n_sb], scalar1=rd_[:, 0:1],
                                                                        scalar2=None, op0=ALU.mult), [pu, rd_], [im])
                                else:
                                    op('dve', lambda e: e.scalar_tensor_tensor(out=im[:], in0=pu[:, 0:n_sb], scalar=rd_[:, 0:1],
                                                                               in1=im[:], op0=ALU.mult, op1=ALU.add),
                                       [pu, rd_, im], [im])
                            for hf in range(2):
                                cur = 8 * b + 2 * i + hf
                                rows = slice(64 * hf, 64 * hf + 64)
                                if cur - 1 >= 0:
                                    op('pool', lambda e: e.memset(im[rows, cur - 1:cur], 2e30), [], [im])
                                op('pool', lambda e: e.memset(im[rows, cur:cur + 1], 1e30), [], [im])
                                if cur + 1 < n_sb:
                                    op('pool', lambda e: e.memset(im[rows, cur + 1:n_sb], -2e30), [], [im])
                            op('pool', lambda e: e.memset(im[:, 0:1], 3e30), [], [im])
                            ma = m8[i % 2]
                            mb = m8b[i % 2]
                            bqn = BqN[i % 2]
                            op('dve', lambda e: e.max(out=ma[:], in_=im[:]), [im], [ma])
                            op('dve', lambda e: e.match_replace(out=sc2[:], in_to_replace=ma[:], in_values=im[:],
                                                                imm_value=-2e30), [ma, im], [sc2])
                            op('dve', lambda e: e.max(out=mb[:], in_=sc2[:]), [sc2], [mb])
                            op('dve', lambda e: e.tensor_scalar(out=mb[:, 7:8], in0=mb[:, 7:8], scalar1=-1e30, scalar2=None,
                                                                op0=ALU.max), [mb], [mb])
                            op('dve', lambda e: e.tensor_scalar(out=bqn[:], in0=im[:], scalar1=mb[:, 7:8], scalar2=1.0,
                                                                op0=ALU.is_ge, op1=ALU.subtract), [im, mb], [bqn])
                            op('pe', lambda e: e.transpose(out=psb[0:n_sb, 128 * i:128 * (i + 1)], in_=bqn[:],
                                                           identity=cv('ident')), [bqn, cstb], [psb])
                        op('dve', lambda e: e.tensor_copy(out=BtN[0:n_sb, :], in_=psb[0:n_sb, 0:512]), [psb], [BtN])
                        for hh in range(4):
                            q_ = load_q(8 + hh, tsl)
                            kv = load_kv(14, 512, 0, jlast)
                            units = []
                            for j in range(jlast + 1):
                                ka, kb, va, vb = kv[j]
                                u = dict(k=ka, kb=kb, v=va, vb=vb)
                                f0 = 0
                                ex = []
                                if j >= 4 * b:
                                    dx, f0 = diag_extra(j - 4 * b)
                                    ex.append(dx)
                                ex.insert(0, (cv('an', 128, (j * 128, (j + 1) * 128)), BtN[:, f0:512], [cstb, BtN]))
                                u['extras'] = ex
                                u['f0'] = f0
                                units.append(u)
                            attn_job(q_, units, fin_nsa(hh, 1, tsl))
                        for hh in range(4):
                            q_ = load_q(8 + hh, tsl)
                            jfirst = max(0, 4 * b - 4)
                            kv = load_kv(15, 640, jfirst, jlast)
                            units = []
                            for j in range(jfirst, jlast + 1):
                                ka, kb, va, vb = kv[j]
                                u = dict(k=ka, kb=kb, v=va, vb=vb)
                                if j >= 4 * b:
                                    dx, f0 = diag_extra(j - 4 * b)
                                    u['extras'] = [dx]
                                    u['f0'] = f0
                                else:
                                    jl = j - (4 * b - 4)
                                    u['f0'] = 0
                                    u['f1'] = 128 * (jl + 1)
                                    u['extras'] = [(cv('i30k'), cv('w2', 128, (384 - 128 * jl, 512)), [cstb])]
                                units.append(u)
                            attn_job(q_, units, fin_nsa(hh, 2, tsl))
                    flush_pending()
                    while advance(pgB, 8):
                        pass
                    tr.end_phase(ph)

                with ExitStack() as ph:
                    hT = [tr.buf(ph, "hT%d" % i, [128, KC, 512], BF16) for i in range(2)]
                    x1 = tr.buf(ph, "x1", [128, KC, 512], F32)
                    yT = tr.buf(ph, "yT", [128, KC, 512], F32)
                    OTs = tr.buf(ph, "OTs", [128, KC, 512], BF16)
                    sq = [tr.buf(ph, "sq%d" % i, [128, 512], BF16) for i in range(2)]
                    tmpn = tr.buf(ph, "tmpn", [128, 512], F32)
                    rstd = tr.buf(ph, "rstd", [128, 512], F32)
                    tmpx = [tr.buf(ph, "tmpx%d" % i, [128, 512], F32) for i in range(2)]
                    tmpu = [tr.buf(ph, "tmpu%d" % i, [128, 512], F32) for i in range(2)]
                    pcn = [0]
                    h2 = hT[0]
                    aT = hT[1]

                    def gemm16(l_, c0, src, outfn):
                        for oc in range(16):
                            w = load_w(l_, c0 + oc)
                            pp = ps[pcn[0] % 3]
                            pcn[0] += 1
                            for k in range(KC):
                                op('pe', lambda e: e.matmul(pp[:], lhsT=w[:, k, :], rhs=src[:, k, :], start=(k == 0),
                                                            stop=(k == KC - 1)), [w, src], [pp])
                            outfn(oc, pp)

                    def sumsq(src, pn):
                        for k in range(KC):
                            s_ = sq[k % 2]
                            op('act', lambda e: e.activation(out=s_[:], in_=src[:, k, :], func=AF.Square), [src], [s_])
                            op('pe', lambda e: e.matmul(pn[:], lhsT=onesb[:], rhs=s_[:], start=(k == 0), stop=(k == KC - 1)),
                               [s_, onesb], [pn])

                    def resid(di):
                        for k in range(KC):
                            tx = tmpx[k % 2]
                            op('dve', lambda e: e.tensor_tensor(out=tx[:], in0=yT[:, k, :], in1=rstd[:], op=ALU.mult),
                               [yT, rstd], [tx])
                            op('dve', lambda e: e.scalar_tensor_tensor(out=x1[:, k, :], in0=tx[:], scalar=der[:, l, di, k:k + 1],
                                                                       in1=x1[:, k, :], op0=ALU.mult, op1=ALU.add),
                               [tx, der, x1], [x1])

                    for b in range(NB):
                        t0 = b * 512
                        tsl = slice(t0, t0 + 512)
                        tr.dma('sp', OTs[:], OTD[:, :, tsl].rearrange("c p t -> p c t"), OTs, True)
                        tr.dma('sp', x1[:], xin[:, :, tsl], x1, True)
                        pn = ps[6]

                        def out_c(oc, pp):
                            op('act', lambda e: e.copy(out=yT[:, oc, :], in_=pp[:]), [pp], [yT])
                        gemm16(l, 47, OTs, out_c)
                        sumsq(yT, pn)
                        rms_rstd((tmpn, rstd), pn)
                        resid(2)
                        sumsq(x1, pn)
                        rms_rstd((tmpn, rstd), pn)
                        for k in range(KC):
                            tx = tmpx[k % 2]
                            op('dve', lambda e: e.tensor_tensor(out=tx[:], in0=x1[:, k, :], in1=rstd[:], op=ALU.mult),
                               [x1, rstd], [tx])
                            op('pool', lambda e: e.tensor_scalar(out=h2[:, k, :], in0=tx[:], scalar1=der[:, l, 3, k:k + 1],
                                                                 scalar2=der[:, l, 4, k:k + 1], op0=ALU.mult, op1=ALU.add),
                               [tx, der], [h2])
                        for q in range(4):
                            def out_u(fc_, pp):
                                tu = tmpu[fc_ % 2]
                                op('act', lambda e: e.activation(out=tu[:], in_=pp[:], func=AF.Relu), [pp], [tu])
                                op('dve', lambda e: e.tensor_tensor(out=aT[:, fc_, :], in0=tu[:], in1=tu[:], op=ALU.mult),
                                   [tu], [aT])
                            gemm16(l, 63 + 16 * q, h2, out_u)

                            def out_d(dc, pp):
                                if q == 0:
                                    op('act', lambda e: e.copy(out=yT[:, dc, :], in_=pp[:]), [pp], [yT])
                                else:
                                    op('dve', lambda e: e.tensor_tensor(out=yT[:, dc, :], in0=pp[:], in1=yT[:, dc, :],
                                                                        op=ALU.add), [pp, yT], [yT])
                            gemm16(l, 127 + 16 * q, aT, out_d)
                        sumsq(yT, pn)
                        rms_rstd((tmpn, rstd), pn)
                        resid(5)
                        tr.dma('pool', xout[:, :, tsl], x1[:], x1, False)
                    tr.end_phase(ph)
        tr.barrier()
        ninst = tr.ninst
    return nc, ninst


def fm_layout(v):
    v = np.asarray(v, np.float32)
    lead = v.shape[:-1]
    return np.ascontiguousarray(np.moveaxis(v.reshape(lead + (v.shape[-1] // 128, 128)), -1, 0))


def make_in_maps(inp, S, NL):
    f = lambda a: np.ascontiguousarray(np.asarray(a, np.float32))
    B = inp['x'].shape[0]
    cst, fcst = make_consts(S)
    gains = np.stack([np.asarray(inp[k], np.float32)[:NL] for k in ('g_pre_mix', 'g_post_mix', 'g_pre_mlp', 'g_post_mlp')], axis=1)
    gains = fm_layout(gains)
    bmodT = fm_layout(np.asarray(inp['b_mod'], np.float32)[:NL])
    negb = np.ascontiguousarray(np.asarray(inp['b_forget'], np.float32)[:NL].T)
    peT = np.stack([np.asarray(inp['cmp_pe_k'], np.float32)[:NL], np.asarray(inp['cmp_pe_v'], np.float32)[:NL]], axis=1)
    peT = np.ascontiguousarray(peT.transpose(3, 0, 1, 2))
    shared = dict(
        w_mod=f(inp['w_mod'][:NL]), bmodT=bmodT, gains=gains, w_in=f(inp['w_in'][:NL]), negb=negb, peT=peT,
        w1k=f(inp['cmp_w1_k'][:NL]), w2k=f(inp['cmp_w2_k'][:NL]), w1v=f(inp['cmp_w1_v'][:NL]), w2v=f(inp['cmp_w2_v'][:NL]),
        w_out=f(inp['w_out'][:NL]), w_up=f(inp['w_up'][:NL]), w_down=f(inp['w_down'][:NL]), cst=cst, fcst=fcst)
    maps = []
    for b in range(B):
        xb = np.asarray(inp['x'][b], np.float32)[:S]
        xT = np.ascontiguousarray(xb.T.reshape(KC, 128, S).transpose(1, 0, 2))
        m = dict(shared)
        m['xT'] = xT
        m['pos'] = np.ascontiguousarray(np.asarray(inp['positions'][b], np.int32)[None, :S])
        m['cT'] = fm_layout(np.asarray(inp['c'][b], np.float32))
        maps.append(m)
    return maps


def run(inp, S, NL):
    nc, ninst = build_program(S, NL)
    maps = make_in_maps(inp, S, NL)
    res = run_bass_kernel_spmd(nc, maps, core_ids=list(range(len(maps))))
    outs = []
    for r in res.results:
        yT = np.asarray(r['yT'], np.float32)
        outs.append(yT.transpose(2, 1, 0).reshape(S, D))
    return np.stack(outs, axis=0)


def kernel(**inputs):
    return run(inputs, 8192, 4)
```

```python
import numpy as np
from contextlib import ExitStack
import concourse.bass as bass
import concourse.mybir as mybir
from concourse.bass_utils import run_bass_kernel_spmd

F32 = mybir.dt.float32
BF16 = mybir.dt.bfloat16
I32 = mybir.dt.int32
ALU = mybir.AluOpType
AF = mybir.ActivationFunctionType
AX = mybir.AxisListType

D = 2048
KC = 16
HD = 128
PROJ = 5908
DFF = 8192
SCL = 128.0 ** -0.5
EPS = 1e-6
NCH = 201
TINY = 1e-30


class Buf:
    __slots__ = ('t', 'name', 'w', 'r', 'sem', 'sval')

    def __init__(self, t, name):
        self.t = t
        self.name = name
        self.w = None
        self.r = {}
        self.sem = None
        self.sval = 0

    def __getitem__(self, k):
        return self.t[k]


class TR:
    ENG = ('pe', 'act', 'dve', 'pool', 'sp')

    def __init__(self, nc, st):
        self.nc = nc
        self.st = st
        self.h = dict(pe=nc.tensor, act=nc.scalar, dve=nc.vector, pool=nc.gpsimd, sp=nc.sync)
        self.sem = {e: st.enter_context(nc.semaphore("c_" + e)) for e in self.ENG}
        self.cnt = {e: 0 for e in self.ENG}
        self.seen = {e: {} for e in self.ENG}
        self.dsems = []
        self.freesems = []
        self.by_stack = {}
        self.nbuf = 0
        self.ninst = 0

    def buf(self, st, name, shape, dtype, psum=False):
        self.nbuf += 1
        nm = "%s_%d" % (name, self.nbuf)
        if psum:
            t = st.enter_context(self.nc.psum_tensor(nm, shape, dtype))
        else:
            t = st.enter_context(self.nc.sbuf_tensor(nm, shape, dtype))
        b = Buf(t, nm)
        self.by_stack.setdefault(id(st), []).append(b)
        return b

    def _wait(self, eng, rec):
        kind, who, val = rec
        if kind == 'e':
            if who == eng and eng == 'pe':
                return
            key = ('e', who)
            semh = self.sem[who]
        else:
            key = ('d', who.name)
            semh = who.sem
            val = who.sval
        if self.seen[eng].get(key, 0) >= val:
            return
        self.h[eng].wait_ge(semh, val)
        self.ninst += 1
        self.seen[eng][key] = val

    def op(self, eng, fn, reads=(), writes=()):
        recs = []
        for b in reads:
            if b.w is not None:
                recs.append(b.w)
        for b in writes:
            if b.w is not None:
                recs.append(b.w)
            recs.extend(b.r.values())
        for rec in recs:
            self._wait(eng, rec)
        inst = fn(self.h[eng])
        self.cnt[eng] += 1
        self.ninst += 1
        inst.then_inc(self.sem[eng], 1)
        rec = ('e', eng, self.cnt[eng])
        for b in reads:
            b.r[('e', eng)] = rec
        for b in writes:
            b.w = rec
            b.r = {}
        return inst

    def dma(self, eng, out, in_, sb, load):
        recs = []
        if sb.w is not None:
            recs.append(sb.w)
        if load:
            recs.extend(sb.r.values())
        if sb.sem is None:
            if self.freesems:
                sb.sem, sb.sval = self.freesems.pop()
            else:
                sb.sem = self.st.enter_context(self.nc.semaphore("d_" + sb.name))
                sb.sval = 0
            self.dsems.append(sb)
        if sb.sval > 0:
            recs.append(('d', sb, sb.sval))
        for rec in recs:
            self._wait(eng, rec)
        inst = self.h[eng].dma_start(out=out, in_=in_)
        self.ninst += 1
        sb.sval += 16
        inst.then_inc(sb.sem, 16)
        rec = ('d', sb, sb.sval)
        if load:
            sb.w = rec
            sb.r = {}
        else:
            sb.r[('d', sb.name)] = rec
        return inst

    def barrier(self):
        for e in self.ENG:
            for e2 in self.ENG:
                if e2 != e and self.cnt[e2] > 0:
                    self._wait(e, ('e', e2, self.cnt[e2]))
            for b in self.dsems:
                if b.sval > 0:
                    self._wait(e, ('d', b, b.sval))

    def end_phase(self, st):
        self.barrier()
        names = set(b.name for b in self.by_stack.pop(id(st), []))
        keep = []
        for b in self.dsems:
            if b.name in names:
                self.freesems.append((b.sem, b.sval))
            else:
                keep.append(b)
        self.dsems = keep


def const_layout(S):
    n_sb = S // 64
    KW = min(64, n_sb)
    nct = max(1, S // 2048)
    off = {}
    o = 0
    for name, n in [('ident', 128), ('i30k', 128), ('causal', 512), ('w2', 512), ('cmask', 5 * 512),
                    ('am', 32 * 128), ('an', (S // 128) * 128), ('pmt', 32), ('ov', nct * (n_sb + 1)),
                    ('eg', 12 * 128)]:
        off[name] = (o, n)
        o += n
    return off, o


def make_consts(S):
    n_sb = S // 64
    KW = min(64, n_sb)
    nct = max(1, S // 2048)
    ncmp = S // 16 - 1
    off, tot = const_layout(S)
    c = np.zeros((128, tot), np.float32)
    p = np.arange(128)[:, None]

    def put(name, arr):
        o, n = off[name]
        assert arr.shape == (128, n), (name, arr.shape, n)
        c[:, o:o + n] = arr
    put('ident', np.eye(128, dtype=np.float32))
    put('i30k', 30000.0 * np.eye(128, dtype=np.float32))
    f = np.arange(512)[None, :]
    put('causal', np.where(f >= p, 0.0, -1.0).astype(np.float32))
    put('w2', np.where(f - 384 < p, 0.0, -1.0).astype(np.float32))
    cm = [np.where(16 * p - f <= 512 * d - 31, 0.0, -1.0) for d in range(5)]
    put('cmask', np.concatenate(cm, axis=1).astype(np.float32))
    am = np.zeros((128, 32, 128), np.float32)
    for n in range(32):
        am[n, n, :] = 30000.0
    put('am', am.reshape(128, -1))
    an = np.zeros((128, S // 128, 128), np.float32)
    for j in range(S // 128):
        an[2 * j, j, 0:64] = 30000.0
        an[2 * j + 1, j, 64:128] = 30000.0
    put('an', an.reshape(128, -1))
    pm = np.zeros((128, 32), np.float32)
    for i in range(16):
        pm[i + 16, i] = -1.0
        pm[i, i + 16] = 1.0
    put('pmt', pm)
    ov = np.zeros((128, nct, n_sb + 1), np.float32)
    for cc in range(ncmp):
        jc, pp = divmod(cc, 128)
        n0 = cc // 4
        ov[pp, jc, n0] = 1.0
        if cc % 4 == 3 and n0 + 1 < n_sb:
            ov[pp, jc, n0 + 1] = 1.0
        ov[pp, jc, n_sb] = 1.0
    put('ov', ov.reshape(128, -1))
    eg = np.zeros((128, 12, 128), np.float32)
    for r in range(12):
        eg[r, r, :] = 1.0
    put('eg', eg.reshape(128, -1))
    fc = np.zeros((128, 4), np.float32)
    invf = 500000.0 ** (-(np.arange(16, dtype=np.float64)) / 16.0)
    fc[0:32, 0] = np.tile(invf / (2 * np.pi), 2).astype(np.float32)
    return c, fc


def build_program(S, NL):
    assert S % 2048 == 0
    NB = S // 512
    NT = S // 128
    n_sb = S // 64
    KW = min(64, n_sb)
    nct = S // 2048
    ncmp = S // 16 - 1
    NCP = nct * 128
    assert NCP <= 512
    nmb = S // 256
    coff, ctot = const_layout(S)

    nc = bass.Bass("TRN2", target_bir_lowering=False)

    def din(name, shape, dt=F32):
        return nc.dram_tensor(name, shape, dt, kind="ExternalInput").ap()

    def dscr(name, shape, dt):
        return nc.dram_tensor(name, shape, dt, kind="Internal").ap()

    x_in = din("xT", [128, KC, S])
    pos_in = din("pos", [1, S], I32)
    cT_in = din("cT", [128, KC])
    w_mod = din("w_mod", [NL, D, 6 * D])
    bmodT = din("bmodT", [128, NL, 96])
    gT_in = din("gains", [128, NL, 4, KC])
    w_in = din("w_in", [NL, D, PROJ])
    negb_in = din("negb", [8, NL])
    peT_in = din("peT", [128, NL, 2, 32])
    w1k = din("w1k", [NL, 4096, 256])
    w2k = din("w2k", [NL, 256, 128])
    w1v = din("w1v", [NL, 4096, 256])
    w2v = din("w2v", [NL, 256, 128])
    w_out = din("w_out", [NL, D, D])
    w_up = din("w_up", [NL, D, DFF])
    w_down = din("w_down", [NL, DFF, D])
    cst_in = din("cst", [128, ctot])
    fcst_in = din("fcst", [128, 4])
    y_out = nc.dram_tensor("yT", [128, KC, S], F32, kind="ExternalOutput").ap()

    WC = [dscr("WC%d" % l_, [NCH, 128, KC, 128], BF16) for l_ in range(NL)]
    FM = dscr("FM", [32, 128, S], BF16)
    VT = dscr("VT", [14, 128, S // 128, 128], BF16)
    GT = dscr("GT", [12, S], BF16)
    RHO = dscr("RHO", [8, S], BF16)
    COS = dscr("COS", [32, S], F32)
    SIN = dscr("SIN", [32, S], F32)
    OTD = dscr("OTD", [16, 128, S], BF16)
    XS = [dscr("XS0", [128, KC, S], F32), dscr("XS1", [128, KC, S], F32)]
    CSTB = dscr("CSTB", [128, ctot], BF16)

    with ExitStack() as st:
        tr = TR(nc, st)
        op = tr.op

        onesb = tr.buf(st, "onesb", [128, 128], BF16)
        onesf = tr.buf(st, "onesf", [128, 128], F32)
        pmtb = tr.buf(st, "pmtb", [128, 32], BF16)
        identf = tr.buf(st, "identf", [128, 128], F32)
        fcst = tr.buf(st, "fcst", [128, 4], F32)
        onecol = tr.buf(st, "onecol", [128, 1], F32)
        der = tr.buf(st, "der", [128, NL, 6, KC], F32)
        negb = tr.buf(st, "negb", [8, NL], F32)
        peb = tr.buf(st, "peb", [128, NL, 2, 32], BF16)
        w4k = [tr.buf(st, "w4k%d" % i, [128, KC, 128], BF16) for i in range(4)]
        ps = [tr.buf(st, "ps%d" % i, [128, 512], F32, psum=True) for i in range(7)]
        psb = tr.buf(st, "psb", [128, 1024], BF16, psum=True)
        wslot = [0]

        cst_holder = [None]

        def cv(name, rows=128, sub=None):
            cstb = cst_holder[0]
            o, n = coff[name]
            if sub is None:
                return cstb[0:rows, o:o + n]
            return cstb[0:rows, o + sub[0]:o + sub[1]]

        def load_w(l, ci):
            b = w4k[wslot[0] % 4]
            wslot[0] += 1
            tr.dma('sp', b[:], WC[l][ci], b, True)
            return b

        fm_cols = ([i * 128 for i in range(4)] + [512 + i * 128 for i in range(4)] +
                   [1536 + i * 128 for i in range(4)] + [2048, 2176, 2304, 2560] +
                   [2828 + i * 128 for i in range(8)] + [3852 + i * 128 for i in range(8)])
        v_cols = ([1024 + i * 128 for i in range(4)] + [2432, 2688] + [4876 + i * 128 for i in range(8)])

        def prep_list(l):
            items = []
            for ci, c0 in enumerate(fm_cols):
                items.append((ci, [(w_in[l, :, c0:c0 + 128], 0)], KC))
            for ci, c0 in enumerate(v_cols):
                items.append((32 + ci, [(w_in[l, :, c0:c0 + 128], 0)], KC))
            items.append((46, [(w_in[l, :, 2816:2828], 0), (w_in[l, :, 5900:5908], 12)], KC))
            for oc in range(16):
                items.append((47 + oc, [(w_out[l, :, oc * 128:(oc + 1) * 128], 0)], KC))
            for fc_ in range(64):
                items.append((63 + fc_, [(w_up[l, :, fc_ * 128:(fc_ + 1) * 128], 0)], KC))
            for q in range(4):
                for dc in range(16):
                    items.append((127 + q * 16 + dc, [(w_down[l, q * 2048:(q + 1) * 2048, dc * 128:(dc + 1) * 128], 0)], KC))
            for wi, w1 in enumerate((w1k, w1v)):
                for g in range(2):
                    for hc in range(2):
                        items.append((191 + wi * 4 + g * 2 + hc,
                                      [(w1[l, g * 2048:(g + 1) * 2048, hc * 128:(hc + 1) * 128], 0)], KC))
            items.append((199, [(w2k[l, :, :], 0)], 2))
            items.append((200, [(w2v[l, :, :], 0)], 2))
            return items

        def prep_gen(l, stg, sto, cast_engs):
            nb_ = len(stg)
            for i, (ci, pieces, nk) in enumerate(prep_list(l)):
                sg = stg[i % nb_]
                so = sto[i % nb_]
                wtot = 0
                for (src, co) in pieces:
                    ncols = src.shape[1]
                    tr.dma('sp', sg[:, 0:nk, co:co + ncols], src.rearrange("(k p) c -> p k c", p=128), sg, True)
                    wtot = max(wtot, co + ncols)
                ce = cast_engs[i % len(cast_engs)]
                if ce == 'act':
                    op('act', lambda e: e.copy(out=so[:, 0:nk, 0:wtot], in_=sg[:, 0:nk, 0:wtot]), [sg], [so])
                else:
                    op(ce, lambda e: e.tensor_copy(out=so[:, 0:nk, 0:wtot], in_=sg[:, 0:nk, 0:wtot]), [sg], [so])
                tr.dma('pool', WC[l][ci, :, 0:nk, 0:wtot], so[:, 0:nk, 0:wtot], so, False)
                yield

        def advance(gen, n):
            for _ in range(n):
                try:
                    next(gen)
                except StopIteration:
                    return False
            return True

        with ExitStack() as ph:
            cstf = tr.buf(ph, "cstf", [128, ctot], F32)
            tr.dma('sp', cstf[:], cst_in, cstf, True)
            tr.dma('sp', fcst[:], fcst_in, fcst, True)
            bfg = tr.buf(ph, "bfg", [8, NL], F32)
            tr.dma('sp', bfg[:], negb_in, bfg, True)
            op('dve', lambda e: e.tensor_scalar(out=negb[:], in0=bfg[:], scalar1=-1.0, scalar2=None, op0=ALU.mult), [bfg], [negb])
            half = ctot // 2
            cstb = tr.buf(ph, "cstb0", [128, ctot], BF16)
            op('dve', lambda e: e.tensor_copy(out=cstb[:, 0:half], in_=cstf[:, 0:half]), [cstf], [cstb])
            op('pool', lambda e: e.tensor_copy(out=cstb[:, half:ctot], in_=cstf[:, half:ctot]), [cstf], [cstb])
            tr.dma('pool', CSTB, cstb[:], cstb, False)
            o_id = coff['ident'][0]
            op('dve', lambda e: e.tensor_copy(out=identf[:], in_=cstf[:, o_id:o_id + 128]), [cstf], [identf])
            o_pm = coff['pmt'][0]
            op('dve', lambda e: e.tensor_copy(out=pmtb[:], in_=cstf[:, o_pm:o_pm + 32]), [cstf], [pmtb])
            op('pool', lambda e: e.memset(onecol[:], 1.0), [], [onecol])
            op('pool', lambda e: e.memset(onesb[:], 1.0), [], [onesb])
            op('pool', lambda e: e.memset(onesf[:], 1.0), [], [onesf])
            pef = tr.buf(ph, "pef", [128, NL, 2, 32], F32)
            tr.dma('sp', pef[:], peT_in, pef, True)
            op('dve', lambda e: e.tensor_copy(out=peb[:], in_=pef[:]), [pef], [peb])
            tr.end_phase(ph)
        with ExitStack() as ph:
            CW = 2048
            posi = tr.buf(ph, "posi", [32, CW], I32)
            posf = tr.buf(ph, "posf", [32, CW], F32)
            ru = tr.buf(ph, "ru", [32, CW], F32)
            rni = tr.buf(ph, "rni", [32, CW], I32)
            rnf = tr.buf(ph, "rnf", [32, CW], F32)
            rtab = [tr.buf(ph, "rtab%d" % i, [32, CW], F32) for i in range(2)]
            for cidx in range(S // CW):
                sl = slice(cidx * CW, (cidx + 1) * CW)
                tr.dma('sp', posi[:], pos_in[0:1, sl].to_broadcast([32, CW]), posi, True)
                op('dve', lambda e: e.tensor_copy(out=posf[:], in_=posi[:]), [posi], [posf])
                for ti, (tab, addc) in enumerate(((SIN, 0.0), (COS, 0.25))):
                    op('dve', lambda e: e.tensor_scalar(out=ru[:], in0=posf[:], scalar1=fcst[0:32, 0:1], scalar2=addc,
                                                        op0=ALU.mult, op1=ALU.add), [posf, fcst], [ru])
                    op('dve', lambda e: e.tensor_copy(out=rni[:], in_=ru[:]), [ru], [rni])
                    op('dve', lambda e: e.tensor_copy(out=rnf[:], in_=rni[:]), [rni], [rnf])
                    op('dve', lambda e: e.tensor_tensor(out=ru[:], in0=ru[:], in1=rnf[:], op=ALU.subtract), [ru, rnf], [ru])
                    rt = rtab[ti]
                    op('act', lambda e: e.activation(out=rt[:], in_=ru[:], func=AF.Sin, scale=2.0 * np.pi), [ru], [rt])
                    tr.dma('pool', tab[:, sl], rt[:], rt, False)
            tr.end_phase(ph)
        with ExitStack() as ph:
            gains = tr.buf(ph, "gains", [128, NL, 4, KC], F32)
            tr.dma('sp', gains[:], gT_in, gains, True)
            bmod = tr.buf(ph, "bmod", [128, NL, 96], F32)
            tr.dma('sp', bmod[:], bmodT, bmod, True)
            cT = tr.buf(ph, "cT", [128, KC], F32)
            tr.dma('sp', cT[:], cT_in, cT, True)
            cs = tr.buf(ph, "cs", [128, KC], F32)
            op('act', lambda e: e.activation(out=cs[:], in_=cT[:], func=AF.Silu), [cT], [cs])
            modt = tr.buf(ph, "modt", [128, NL, 96], F32)
            wm = [tr.buf(ph, "wm%d" % i, [128, KC, 128], F32) for i in range(3)]
            stg0 = [tr.buf(ph, "stg%d" % i, [128, KC, 128], F32) for i in range(3)]
            sto0 = [tr.buf(ph, "sto%d" % i, [128, KC, 128], BF16) for i in range(3)]
            pg0 = prep_gen(0, stg0, sto0, ('dve', 'pool', 'act'))
            for l in range(NL):
                pm_ = ps[l % 2]
                for j in range(96):
                    b = wm[j % 3]
                    tr.dma('sp', b[:], w_mod[l, :, j * 128:(j + 1) * 128].rearrange("(k p) c -> p k c", p=128), b, True)
                    for k in range(KC):
                        op('pe', lambda e: e.matmul(pm_[:, j:j + 1], lhsT=b[:, k, :], rhs=cs[:, k:k + 1],
                                                    start=(k == 0), stop=(k == KC - 1)), [b, cs], [pm_])
                    advance(pg0, 1)
                op('dve', lambda e: e.tensor_tensor(out=modt[:, l, :], in0=pm_[:, 0:96], in1=bmod[:, l, :], op=ALU.add),
                   [pm_, bmod], [modt])
                for (di, si, gi) in ((0, 1, 0), (3, 4, 2)):
                    op('dve', lambda e: e.scalar_tensor_tensor(out=der[:, l, di, :], in0=modt[:, l, si * 16:(si + 1) * 16],
                                                               scalar=1.0, in1=gains[:, l, gi, :], op0=ALU.add, op1=ALU.mult),
                       [modt, gains], [der])
                for (di, si) in ((1, 0), (4, 3)):
                    op('dve', lambda e: e.tensor_copy(out=der[:, l, di, :], in_=modt[:, l, si * 16:(si + 1) * 16]), [modt], [der])
                for (di, si, gi) in ((2, 2, 1), (5, 5, 3)):
                    op('dve', lambda e: e.tensor_tensor(out=der[:, l, di, :], in0=modt[:, l, si * 16:(si + 1) * 16],
                                                        in1=gains[:, l, gi, :], op=ALU.mult), [modt, gains], [der])
            while advance(pg0, 8):
                pass
            tr.end_phase(ph)
        for l in range(NL):
            xin = x_in if l == 0 else XS[(l - 1) % 2]
            xout = y_out if l == NL - 1 else XS[l % 2]
            with ExitStack() as ly:
                kmh = tr.buf(ly, "kmh", [128, 4, 32], BF16)
                kml = tr.buf(ly, "kml", [128, 4, 32], BF16)
                CK = tr.buf(ly, "CK", [128, NT, 8], F32)
                KcmpT = tr.buf(ly, "KcmpT", [128, NCP], BF16)
                Vcmp = tr.buf(ly, "Vcmp", [128, nct, 128], BF16)

                def rms_rstd(ph_bufs, sumps):
                    tmpn, rstd = ph_bufs
                    op('dve', lambda e: e.tensor_scalar(out=tmpn[:], in0=sumps[:], scalar1=1.0 / D, scalar2=EPS,
                                                        op0=ALU.mult, op1=ALU.add), [sumps], [tmpn])
                    op('act', lambda e: e.activation(out=tmpn[:], in_=tmpn[:], func=AF.Sqrt), [tmpn], [tmpn])
                    op('dve', lambda e: e.reciprocal(out=rstd[:], in_=tmpn[:]), [tmpn], [rstd])

                with ExitStack() as ph:
                    hT = [tr.buf(ph, "hT%d" % i, [128, KC, 512], BF16) for i in range(2)]
                    xc = [tr.buf(ph, "xc%d" % i, [128, 2, 512], F32) for i in range(3)]
                    sq = [tr.buf(ph, "sq%d" % i, [128, 512], BF16) for i in range(2)]
                    tmpn = tr.buf(ph, "tmpn", [128, 512], F32)
                    rstd = tr.buf(ph, "rstd", [128, 512], F32)
                    tmpx = [tr.buf(ph, "tmpx%d" % i, [128, 512], F32) for i in range(2)]
                    wv = [tr.buf(ph, "wv%d" % i, [128, KC, 512], BF16) for i in range(2)]
                    qk = [tr.buf(ph, "qk%d" % i, [128, 512], BF16) for i in range(3)]
                    ropeA = tr.buf(ph, "ropeA", [32, 512], F32)
                    ropeB = tr.buf(ph, "ropeB", [32, 512], F32)
                    cosb = [tr.buf(ph, "cosb%d" % i, [32, 512], F32) for i in range(2)]
                    sinb = [tr.buf(ph, "sinb%d" % i, [32, 512], F32) for i in range(2)]
                    vout = [tr.buf(ph, "vout%d" % i, [128, 4, 512], BF16) for i in range(2)]
                    gsb = [tr.buf(ph, "gsb%d" % i, [12, 512], BF16) for i in range(2)]
                    fe = tr.buf(ph, "fe", [8, 512], F32)
                    fl = tr.buf(ph, "fl", [8, 512], F32)
                    Cb = [tr.buf(ph, "Cb%d" % i, [8, 512], F32) for i in range(2)]
                    rhob = [tr.buf(ph, "rhob%d" % i, [8, 512], BF16) for i in range(2)]
                    ones8 = tr.buf(ph, "ones8", [8, 512], F32)
                    kmf = tr.buf(ph, "kmf", [128, 4, 32], F32)
                    kmt = tr.buf(ph, "kmt", [128, 4, 32], F32)
                    op('pool', lambda e: e.memset(ones8[:], 1.0), [], [ones8])
                    op('pool', lambda e: e.memset(kmf[:], 0.0), [], [kmf])
                    xcn = [0]
                    qkn = [0]
                    pcn = [0]
                    vgn = [0]
                    roped = set(range(0, 13)) | {14, 15}
                    roped.discard(13)
                    vgroups = [(32, 4, 0), (36, 2, 512), (38, 4, 768), (42, 4, 1280)]
                    for b in range(NB):
                        t0 = b * 512
                        tsl = slice(t0, t0 + 512)
                        h = hT[b % 2]
                        pn = ps[6]
                        for g in range(8):
                            xb = xc[xcn[0] % 3]
                            xcn[0] += 1
                            tr.dma('sp', xb[:], xin[:, 2 * g:2 * g + 2, tsl], xb, True)
                            for c in range(2):
                                s_ = sq[(2 * g + c) % 2]
                                op('act', lambda e: e.activation(out=s_[:], in_=xb[:, c, :], func=AF.Square), [xb], [s_])
                                op('pe', lambda e: e.matmul(pn[:], lhsT=onesb[:], rhs=s_[:], start=(g == 0 and c == 0),
                                                            stop=(g == 7 and c == 1)), [s_, onesb], [pn])
                        rms_rstd((tmpn, rstd), pn)
                        for g in range(8):
                            xb = xc[xcn[0] % 3]
                            xcn[0] += 1
                            tr.dma('sp', xb[:], xin[:, 2 * g:2 * g + 2, tsl], xb, True)
                            for c in range(2):
                                k = 2 * g + c
                                tx = tmpx[k % 2]
                                op('dve', lambda e: e.tensor_tensor(out=tx[:], in0=xb[:, c, :], in1=rstd[:], op=ALU.mult),
                                   [xb, rstd], [tx])
                                op('pool', lambda e: e.tensor_scalar(out=h[:, k, :], in0=tx[:], scalar1=der[:, l, 0, k:k + 1],
                                                                     scalar2=der[:, l, 1, k:k + 1], op0=ALU.mult, op1=ALU.add),
                                   [tx, der], [h])
                        cb_ = cosb[b % 2]
                        sb_ = sinb[b % 2]
                        tr.dma('sp', cb_[:], COS[:, tsl], cb_, True)
                        tr.dma('sp', sb_[:], SIN[:, tsl], sb_, True)
                        for ci in range(32):
                            w = load_w(l, ci)
                            pp = ps[pcn[0] % 3]
                            pcn[0] += 1
                            for k in range(KC):
                                op('pe', lambda e: e.matmul(pp[:], lhsT=w[:, k, :], rhs=h[:, k, :], start=(k == 0),
                                                            stop=(k == KC - 1)), [w, h], [pp])
                            q_ = qk[qkn[0] % 3]
                            qkn[0] += 1
                            op('act', lambda e: e.copy(out=q_[:], in_=pp[:]), [pp], [q_])
                            if ci in roped:
                                pr = ps[3]
                                op('pe', lambda e: e.matmul(pr[0:32, :], lhsT=pmtb[:], rhs=q_[:], start=True, stop=True),
                                   [q_, pmtb], [pr])
                                op('dve', lambda e: e.tensor_tensor(out=ropeA[:], in0=pr[0:32, :], in1=sb_[:], op=ALU.mult),
                                   [pr, sb_], [ropeA])
                                op('dve', lambda e: e.tensor_tensor(out=ropeB[:], in0=q_[0:32, :], in1=cb_[:], op=ALU.mult),
                                   [q_, cb_], [ropeB])
                                op('dve', lambda e: e.tensor_tensor(out=q_[0:32, :], in0=ropeA[:], in1=ropeB[:], op=ALU.add),
                                   [ropeA, ropeB], [q_])
                            if 4 <= ci < 8:
                                op('dve', lambda e: e.tensor_reduce(out=kmf[:, ci - 4, 2 * b:2 * b + 2],
                                                                    in_=q_[:].rearrange("p (a c) -> p a c", a=2),
                                                                    axis=AX.X, op=ALU.add), [q_], [kmf])
                            tr.dma('pool', FM[ci, :, tsl], q_[:], q_, False)
                        for (c0, nchk, col0) in vgroups:
                            wvb = wv[vgn[0] % 2]
                            vo = vout[vgn[0] % 2]
                            vgn[0] += 1
                            ncols = nchk * 128
                            for i in range(nchk):
                                tr.dma('sp', wvb[:, :, i * 128:(i + 1) * 128], WC[l][c0 + i], wvb, True)
                            for tt in range(4):
                                pp = ps[pcn[0] % 3]
                                pcn[0] += 1
                                for k in range(KC):
                                    op('pe', lambda e: e.matmul(pp[:, 0:ncols], lhsT=h[:, k, tt * 128:(tt + 1) * 128],
                                                                rhs=wvb[:, k, 0:ncols], start=(k == 0), stop=(k == KC - 1)),
                                       [wvb, h], [pp])
                                if tt % 2 == 0:
                                    op('act', lambda e: e.copy(out=vo[:, tt, 0:ncols], in_=pp[:, 0:ncols]), [pp], [vo])
                                else:
                                    op('dve', lambda e: e.tensor_copy(out=vo[:, tt, 0:ncols], in_=pp[:, 0:ncols]), [pp], [vo])
                            for i in range(nchk):
                                tr.dma('pool', VT[col0 // 128 + i, :, 4 * b:4 * b + 4, :], vo[:, :, i * 128:(i + 1) * 128], vo, False)
                        w = load_w(l, 46)
                        pg = ps[4]
                        for k in range(KC):
                            op('pe', lambda e: e.matmul(pg[0:12, :], lhsT=w[:, k, 0:12], rhs=h[:, k, :], start=(k == 0),
                                                        stop=(k == KC - 1)), [w, h], [pg])
                        g_ = gsb[b % 2]
                        op('act', lambda e: e.activation(out=g_[:], in_=pg[0:12, :], func=AF.Sigmoid), [pg], [g_])
                        tr.dma('pool', GT[:, tsl], g_[:], g_, False)
                        pf = ps[5]
                        for k in range(KC):
                            op('pe', lambda e: e.matmul(pf[0:8, :], lhsT=w[:, k, 12:20], rhs=h[:, k, :], start=(k == 0),
                                                        stop=(k == KC - 1)), [w, h], [pf])
                        op('act', lambda e: e.activation(out=fe[:], in_=pf[0:8, :], func=AF.Exp, scale=-1.0,
                                                         bias=negb[:, l:l + 1]), [pf, negb], [fe])
                        op('act', lambda e: e.activation(out=fl[:], in_=fe[:], func=AF.Ln, bias=onecol[0:8, 0:1]),
                           [fe, onecol], [fl])
                        cbuf = Cb[b % 2]
                        cprev = Cb[(b - 1) % 2]
                        if b == 0:
                            op('dve', lambda e: e.tensor_tensor_scan(out=cbuf[:], data0=ones8[:], data1=fl[:], initial=0.0,
                                                                     op0=ALU.mult, op1=ALU.add), [ones8, fl], [cbuf])
                        else:
                            op('dve', lambda e: e.tensor_tensor_scan(out=cbuf[:], data0=ones8[:], data1=fl[:],
                                                                     initial=cprev[:, 511:512], op0=ALU.mult, op1=ALU.add),
                               [ones8, fl, cprev], [cbuf])
                        rb_ = rhob[b % 2]
                        op('dve', lambda e: e.tensor_scalar(out=rb_[:], in0=cbuf[:], scalar1=-1.0 / SCL, scalar2=None,
                                                            op0=ALU.mult), [cbuf], [rb_])
                        tr.dma('pool', RHO[:, tsl], rb_[:], rb_, False)
                        pt_ = ps[3]
                        for i in range(4):
                            op('pe', lambda e: e.transpose(out=pt_[:, 8 * i:8 * i + 8], in_=cbuf[0:8, 128 * i:128 * (i + 1)],
                                                           identity=identf[0:8, 0:8]), [cbuf, identf], [pt_])
                        op('dve', lambda e: e.tensor_copy(out=CK[:, 4 * b:4 * b + 4, :],
                                                          in_=pt_[:, 0:32].rearrange("p (a c) -> p a c", a=4)), [pt_], [CK])
                    op('dve', lambda e: e.tensor_scalar(out=kmf[:], in0=kmf[:], scalar1=1.0 / 256.0, scalar2=None,
                                                        op0=ALU.mult), [kmf], [kmf])
                    op('dve', lambda e: e.tensor_copy(out=kmh[:], in_=kmf[:]), [kmf], [kmh])
                    op('dve', lambda e: e.tensor_copy(out=kmt[:], in_=kmh[:]), [kmh], [kmt])
                    op('dve', lambda e: e.tensor_tensor(out=kmt[:], in0=kmf[:], in1=kmt[:], op=ALU.subtract), [kmf, kmt], [kmt])
                    op('dve', lambda e: e.tensor_copy(out=kml[:], in_=kmt[:]), [kmt], [kml])
                    tr.end_phase(ph)

                with ExitStack() as ph:
                    kcS = tr.buf(ph, "kcS", [128, S], BF16)
                    Z = tr.buf(ph, "Z", [128, 16, S // 16], BF16)
                    W1 = tr.buf(ph, "W1", [128, 32, 256], BF16)
                    W2 = tr.buf(ph, "W2", [128, 2, 128], BF16)
                    bcol = tr.buf(ph, "bcol", [128, 2], F32)
                    xg = tr.buf(ph, "xg", [128, 512], F32)
                    x2 = tr.buf(ph, "x2", [128, 512], F32)
                    sg = tr.buf(ph, "sg", [128, 512], F32)
                    GTt = tr.buf(ph, "GTt", [128, 2, 512], BF16)
                    for wi in range(2):
                        tr.dma('sp', kcS[:], FM[12 + wi], kcS, True)
                        op('pool', lambda e: e.tensor_copy(out=Z[:], in_=kcS[:].rearrange("p (m r) -> p r m", r=16)), [kcS], [Z])
                        for g in range(2):
                            for hc in range(2):
                                tr.dma('sp', W1[:, 16 * g:16 * g + 16, hc * 128:(hc + 1) * 128],
                                       WC[l][191 + wi * 4 + g * 2 + hc], W1, True)
                        tr.dma('sp', W2[:], WC[l][199 + wi, :, 0:2, :], W2, True)
                        op('pool', lambda e: e.memset(GTt[:], 0.0), [], [GTt])
                        for hc in range(2):
                            phd = ps[hc]
                            pbs = ps[2]
                            for tau in range(32):
                                a, r = divmod(tau, 16)
                                op('pe', lambda e: e.matmul(phd[:, 0:ncmp], lhsT=W1[:, tau, hc * 128:(hc + 1) * 128],
                                                            rhs=Z[:, r, a:a + ncmp], start=(tau == 0), stop=(tau == 31)),
                                   [W1, Z], [phd])
                            for tau in range(32):
                                op('pe', lambda e: e.matmul(pbs[:, hc:hc + 1], lhsT=W1[:, tau, hc * 128:(hc + 1) * 128],
                                                            rhs=peb[:, l, wi, tau:tau + 1], start=(tau == 0), stop=(tau == 31)),
                                   [W1, peb], [pbs])
                            op('dve', lambda e: e.tensor_copy(out=bcol[:, hc:hc + 1], in_=pbs[:, hc:hc + 1]), [pbs], [bcol])
                            op('act', lambda e: e.activation(out=xg[:, 0:ncmp], in_=phd[:, 0:ncmp], func=AF.Identity,
                                                             bias=bcol[:, hc:hc + 1]), [phd, bcol], [xg])
                            op('dve', lambda e: e.tensor_tensor(out=x2[:, 0:ncmp], in0=xg[:, 0:ncmp], in1=xg[:, 0:ncmp],
                                                                op=ALU.mult), [xg], [x2])
                            op('dve', lambda e: e.tensor_scalar(out=x2[:, 0:ncmp], in0=x2[:, 0:ncmp], scalar1=0.0713548163,
                                                                scalar2=1.5957691216, op0=ALU.mult, op1=ALU.add), [x2], [x2])
                            op('dve', lambda e: e.tensor_tensor(out=x2[:, 0:ncmp], in0=x2[:, 0:ncmp], in1=xg[:, 0:ncmp],
                                                                op=ALU.mult), [x2, xg], [x2])
                            op('act', lambda e: e.activation(out=sg[:, 0:ncmp], in_=x2[:, 0:ncmp], func=AF.Sigmoid), [x2], [sg])
                            op('dve', lambda e: e.tensor_tensor(out=GTt[:, hc, 0:ncmp], in0=xg[:, 0:ncmp], in1=sg[:, 0:ncmp],
                                                                op=ALU.mult), [xg, sg], [GTt])
                        po = ps[3]
                        if wi == 0:
                            for hc in range(2):
                                op('pe', lambda e: e.matmul(po[:, 0:NCP], lhsT=W2[:, hc, :], rhs=GTt[:, hc, 0:NCP],
                                                            start=(hc == 0), stop=(hc == 1)), [W2, GTt], [po])
                            op('act', lambda e: e.copy(out=KcmpT[:], in_=po[:, 0:NCP]), [po], [KcmpT])
                        else:
                            for ct in range(nct):
                                for hc in range(2):
                                    op('pe', lambda e: e.matmul(po[:, ct * 128:(ct + 1) * 128],
                                                                lhsT=GTt[:, hc, ct * 128:(ct + 1) * 128], rhs=W2[:, hc, :],
                                                                start=(hc == 0), stop=(hc == 1)), [W2, GTt], [po])
                            op('act', lambda e: e.copy(out=Vcmp[:], in_=po[:, 0:NCP].rearrange("p (a c) -> p a c", a=nct)),
                               [po], [Vcmp])
                    tr.end_phase(ph)

                with ExitStack() as ph:
                    QT = [tr.buf(ph, "QT%d" % i, [128, 512], BF16) for i in range(2)]
                    Kc = [tr.buf(ph, "Kc%d" % i, [128, 2048], BF16) for i in range(3)]
                    Vc = [tr.buf(ph, "Vc%d" % i, [128, 16, 128], BF16) for i in range(3)]
                    PT = [tr.buf(ph, "PT%d" % i, [128, 512], BF16) for i in range(6)]
                    ET = tr.buf(ph, "ET", [128, 4, nct, 512], BF16)
                    acc = [tr.buf(ph, "acc%d" % i, [128, 512], F32) for i in range(4)]
                    rec = [tr.buf(ph, "rec%d" % i, [128, 512], F32) for i in range(2)]
                    rgt = [tr.buf(ph, "rgt%d" % i, [128, 512], F32) for i in range(2)]
                    tmc = [tr.buf(ph, "tmc%d" % i, [128, 512], F32) for i in range(2)]
                    rho1 = [tr.buf(ph, "rho1_%d" % i, [128, 512], BF16) for i in range(2)]
                    for r_ in rho1:
                        op('pool', lambda e: e.memset(r_[:], 0.0), [], [r_])
                    tinycol = tr.buf(ph, "tinycol", [128, 1], F32)
                    op('pool', lambda e: e.memset(tinycol[:], TINY), [], [tinycol])
                    E0 = tr.buf(ph, "E0", [128, 128], BF16)
                    op('pool', lambda e: e.memset(E0[:], 0.0), [], [E0])
                    op('pool', lambda e: e.memset(E0[0:1, :], 1.0), [], [E0])
                    cstb = tr.buf(ph, "cstB", [128, ctot], BF16)
                    cst_holder[0] = cstb
                    tr.dma('sp', cstb[:], CSTB, cstb, True)
                    accD = [tr.buf(ph, "accD%d" % i, [128, 512], F32) for i in range(2)]
                    accG = [tr.buf(ph, "accG%d" % i, [128, 512], F32) for i in range(2)]
                    pending = []
                    if l + 1 < NL:
                        stgB = [tr.buf(ph, "stgB%d" % i, [128, KC, 128], F32) for i in range(2)]
                        stoB = [tr.buf(ph, "stoB%d" % i, [128, KC, 128], BF16) for i in range(2)]
                        pgB = prep_gen(l + 1, stgB, stoB, ('dve', 'pool'))
                    else:
                        pgB = iter(())
                    prep_per_job = max(1, -(-NCH // (NB * 28)))
                    gsbB = tr.buf(ph, "gsbB", [12, 512], BF16)
                    Gp = [tr.buf(ph, "Gp%d" % i, [128, 32], F32) for i in range(2)]
                    m8 = [tr.buf(ph, "m8_%d" % i, [128, 8], F32) for i in range(2)]
                    m8b = [tr.buf(ph, "m8b_%d" % i, [128, 8], F32) for i in range(2)]
                    Bq = [tr.buf(ph, "Bq%d" % i, [128, 32], BF16) for i in range(2)]
                    BtM = [tr.buf(ph, "BtM%d" % i, [128, 512], BF16) for i in range(2)]
                    for bt_ in BtM:
                        op('pool', lambda e: e.memset(bt_[:], 0.0), [], [bt_])
                    imp = [tr.buf(ph, "imp%d" % i, [128, n_sb], F32) for i in range(2)]
                    sc2 = tr.buf(ph, "sc2", [128, n_sb], F32)
                    BqN = [tr.buf(ph, "BqN%d" % i, [128, n_sb], BF16) for i in range(2)]
                    BtN = tr.buf(ph, "BtN", [128, 512], BF16)
                    op('pool', lambda e: e.memset(BtN[:], 0.0), [], [BtN])
                    rd = [tr.buf(ph, "rd%d" % i, [128, 1], F32) for i in range(2)]
                    Ost = [tr.buf(ph, "Ost%d" % i, [128, 512], BF16) for i in range(3)]
                    psL = [ps[0], ps[1], ps[4]]
                    psN = [ps[2], ps[3]]
                    psD = [ps[5], ps[5]]
                    psX = ps[6]
                    cnt = dict(q=0, kv=0, pt=0, job=0, rho=0, ost=0, u=0, x=0)

                    def load_q(ci, tsl):
                        q_ = QT[cnt['q'] % 2]
                        cnt['q'] += 1
                        tr.dma('sp', q_[:], FM[ci, :, tsl], q_, True)
                        return q_

                    def load_kv(kci, vcol, j0, j1):
                        res = {}
                        ck0, ck1 = j0 // 16, j1 // 16
                        for ck in range(ck0, ck1 + 1):
                            ja = max(j0, ck * 16)
                            jb = min(j1, ck * 16 + 15)
                            kb = Kc[cnt['kv'] % 3]
                            vb = Vc[cnt['kv'] % 3]
                            cnt['kv'] += 1
                            n = jb - ja + 1
                            tr.dma('sp', kb[:, 0:n * 128], FM[kci, :, ja * 128:(jb + 1) * 128], kb, True)
                            tr.dma('sp', vb[:, 0:n, :], VT[vcol // 128, :, ja:jb + 1, :], vb, True)
                            for j in range(ja, jb + 1):
                                res[j] = (kb[:, (j - ja) * 128:(j - ja + 1) * 128], kb, vb[:, j - ja, :], vb)
                        return res

                    def step_pending():
                        while pending:
                            try:
                                next(pending[0])
                                return
                            except StopIteration:
                                pending.pop(0)

                    def flush_pending():
                        while pending:
                            for _ in pending[0]:
                                pass
                            pending.pop(0)

                    def attn_job(q_, units, finalize):
                        jn = cnt['job']
                        cnt['job'] += 1
                        pN = psN[jn % 2]
                        pD = psD[jn % 2]
                        aD = accD[jn % 2]
                        aG = accG[jn % 2]
                        n = len(units)
                        pts = [None] * n
                        op('pool', lambda e: e.memset(aD[:], 0.0), [], [aD])
                        op('pool', lambda e: e.memset(aG[:], 0.0), [], [aG])

                        def emit_S(i):
                            u = units[i]
                            f0, f1 = u.get('f0', 0), u.get('f1', 512)
                            pl = psL[cnt['pt'] % 3]
                            ex = u.get('extras', [])
                            op('pe', lambda e: e.matmul(pl[:, f0:f1], lhsT=u['k'], rhs=q_[:, f0:f1], start=True,
                                                        stop=(len(ex) == 0)), [u['kb'], q_], [pl])
                            for xi, (la, ra, bl) in enumerate(ex):
                                op('pe', lambda e: e.matmul(pl[:, f0:f1], lhsT=la, rhs=ra, start=False,
                                                            stop=(xi == len(ex) - 1)), bl, [pl])
                            if 'pt' in u:
                                pap, pbuf = u['pt']
                            else:
                                pbuf = PT[cnt['pt'] % 6]
                                pap = pbuf[:]
                            cnt['pt'] += 1
                            if u.get('bias') is not None:
                                bap, bbuf = u['bias']
                                op('act', lambda e: e.activation(out=pap[:, f0:f1], in_=pl[:, f0:f1], func=AF.Exp, scale=SCL,
                                                                 bias=bap), [pl, bbuf], [pbuf])
                            else:
                                op('act', lambda e: e.activation(out=pap[:, f0:f1], in_=pl[:, f0:f1], func=AF.Exp, scale=SCL),
                                   [pl], [pbuf])
                            pts[i] = (pap, pbuf)

                        def emit_PV(i):
                            u = units[i]
                            f0, f1 = u.get('f0', 0), u.get('f1', 512)
                            pap, pbuf = pts[i]
                            op('pe', lambda e: e.matmul(pN[:, f0:f1], lhsT=u['v'], rhs=pap[:, f0:f1], start=(i == 0),
                                                        stop=(i == n - 1)), [u['vb'], pbuf], [pN])
                            if i % 2 == 0:
                                op('dve', lambda e: e.tensor_tensor(out=aD[:, f0:f1], in0=aD[:, f0:f1], in1=pap[:, f0:f1],
                                                                    op=ALU.add), [aD, pbuf], [aD])
                            else:
                                op('pool', lambda e: e.tensor_tensor(out=aG[:, f0:f1], in0=aG[:, f0:f1], in1=pap[:, f0:f1],
                                                                     op=ALU.add), [aG, pbuf], [aG])

                        LAG = 2
                        for i in range(n + LAG):
                            if i < n:
                                emit_S(i)
                            if i >= LAG:
                                emit_PV(i - LAG)
                                if i >= LAG + 1:
                                    step_pending()
                        flush_pending()

                        def fin():
                            op('dve', lambda e: e.tensor_tensor(out=aD[:], in0=aD[:], in1=aG[:], op=ALU.add), [aD, aG], [aD])
                            yield
                            op('pe', lambda e: e.matmul(pD[:], lhsT=onesf[:], rhs=aD[:], start=True, stop=True),
                               [onesf, aD], [pD])
                            yield
                            for _ in finalize(pN, pD):
                                yield
                        pending.append(fin())
                        advance(pgB, prep_per_job)

                    def recip_den(pD):
                        r_ = rec[cnt['x'] % 2]
                        cnt['x'] += 1
                        op('act', lambda e: e.activation(out=r_[:], in_=pD[:], func=AF.Ln, bias=tinycol[:, 0:1]),
                           [pD, tinycol], [r_])
                        yield r_
                        op('act', lambda e: e.activation(out=r_[:], in_=r_[:], func=AF.Exp, scale=-1.0), [r_], [r_])
                        yield r_

                    def fin_plain(hc, tsl):
                        def f(pN, pD):
                            r_ = None
                            for r_ in recip_den(pD):
                                yield
                            o_ = Ost[cnt['ost'] % 3]
                            cnt['ost'] += 1
                            op('dve', lambda e: e.tensor_tensor(out=o_[:], in0=pN[:], in1=r_[:], op=ALU.mult), [pN, r_], [o_])
                            tr.dma('pool', OTD[hc, :, tsl], o_[:], o_, False)
                            yield
                        return f

                    def fin_nsa(hh, br, tsl):
                        def f(pN, pD):
                            r_ = None
                            for r_ in recip_den(pD):
                                yield
                            rr = 3 * hh + br
                            op('pe', lambda e: e.matmul(psX[:], lhsT=cv('eg', 12, (rr * 128, (rr + 1) * 128)), rhs=gsbB[:],
                                                        start=True, stop=True), [cstb, gsbB], [psX])
                            g_ = rgt[cnt['x'] % 2]
                            op('dve', lambda e: e.tensor_tensor(out=g_[:], in0=psX[:], in1=r_[:], op=ALU.mult), [psX, r_], [g_])
                            yield
                            a_ = acc[hh]
                            if br == 0:
                                op('dve', lambda e: e.tensor_tensor(out=a_[:], in0=pN[:], in1=g_[:], op=ALU.mult), [pN, g_], [a_])
                            else:
                                t_ = tmc[cnt['x'] % 2]
                                op('dve', lambda e: e.tensor_tensor(out=t_[:], in0=pN[:], in1=g_[:], op=ALU.mult), [pN, g_], [t_])
                                op('pool', lambda e: e.tensor_tensor(out=a_[:], in0=a_[:], in1=t_[:], op=ALU.add), [a_, t_], [a_])
                            yield
                            if br == 2:
                                o_ = Ost[cnt['ost'] % 3]
                                cnt['ost'] += 1
                                op('pool', lambda e: e.tensor_copy(out=o_[:], in_=a_[:]), [a_], [o_])
                                tr.dma('pool', OTD[4 + hh, :, tsl], o_[:], o_, False)
                                yield
                        return f

                    causal = cv('causal')

                    def diag_extra(jd):
                        f0 = 128 * jd
                        return (cv('i30k'), cv('causal', 128, (0, 512 - f0)), [cstb]), f0

                    for b in range(NB):
                        t0 = b * 512
                        tsl = slice(t0, t0 + 512)
                        jlast = 4 * b + 3
                        for hh in range(8):
                            q_ = load_q(16 + hh, tsl)
                            r1 = rho1[cnt['rho'] % 2]
                            cnt['rho'] += 1
                            tr.dma('sp', r1[0:1, :], RHO[hh:hh + 1, tsl], r1, True)
                            kv = load_kv(24 + hh, 768 + 128 * hh, 0, jlast)
                            units = []
                            for j in range(jlast + 1):
                                ka, kb, va, vb = kv[j]
                                u = dict(k=ka, kb=kb, v=va, vb=vb, bias=(CK[:, j, hh:hh + 1], CK))
                                f0 = 0
                                ex = []
                                if j >= 4 * b:
                                    dx, f0 = diag_extra(j - 4 * b)
                                    ex.append(dx)
                                ex.insert(0, (E0[:], r1[:, f0:512], [E0, r1]))
                                u['extras'] = ex
                                u['f0'] = f0
                                units.append(u)
                            attn_job(q_, units, fin_plain(8 + hh, tsl))
                        for hh in range(4):
                            q_ = load_q(hh, tsl)
                            bt = BtM[hh % 2]
                            for i in range(4):
                                own = 2 * b + i // 2
                                gp = Gp[i % 2]
                                mm = m8[i % 2]
                                bq = Bq[i % 2]
                                op('pe', lambda e: e.matmul(psX[:, 0:32], lhsT=q_[:, 128 * i:128 * (i + 1)], rhs=kmh[:, hh, :],
                                                            start=True, stop=False), [q_, kmh], [psX])
                                op('pe', lambda e: e.matmul(psX[:, 0:32], lhsT=q_[:, 128 * i:128 * (i + 1)], rhs=kml[:, hh, :],
                                                            start=False, stop=True), [q_, kml], [psX])
                                op('pool', lambda e: e.memset(gp[:], -1e30), [], [gp])
                                if own > 0:
                                    op('dve', lambda e: e.tensor_copy(out=gp[:, 0:own], in_=psX[:, 0:own]), [psX], [gp])
                                op('dve', lambda e: e.max(out=mm[:], in_=gp[:]), [gp], [mm])
                                op('dve', lambda e: e.tensor_scalar(out=mm[:, 2:3], in0=mm[:, 2:3], scalar1=-5e29, scalar2=None,
                                                                    op0=ALU.max), [mm], [mm])
                                op('dve', lambda e: e.tensor_scalar(out=bq[:], in0=gp[:], scalar1=mm[:, 2:3], scalar2=1.0,
                                                                    op0=ALU.is_ge, op1=ALU.subtract), [gp, mm], [bq])
                                op('pool', lambda e: e.memset(bq[:, own:own + 1], 0.0), [], [bq])
                                if own + 1 < 32:
                                    op('pool', lambda e: e.memset(bq[:, own + 1:32], -1.0), [], [bq])
                                op('pe', lambda e: e.transpose(out=psb[0:32, 128 * i:128 * (i + 1)], in_=bq[:],
                                                               identity=cv('ident')), [bq, cstb], [psb])
                            op('dve', lambda e: e.tensor_copy(out=bt[0:32, :], in_=psb[0:32, 0:512]), [psb], [bt])
                            kv = load_kv(4 + hh, 128 * hh, 0, jlast)
                            units = []
                            for j in range(jlast + 1):
                                ka, kb, va, vb = kv[j]
                                u = dict(k=ka, kb=kb, v=va, vb=vb)
                                f0 = 0
                                ex = []
                                if j >= 4 * b:
                                    dx, f0 = diag_extra(j - 4 * b)
                                    ex.append(dx)
                                nblk = j // 2
                                ex.insert(0, (cv('am', 128, (nblk * 128, (nblk + 1) * 128)), bt[:, f0:512], [cstb, bt]))
                                u['extras'] = ex
                                u['f0'] = f0
                                units.append(u)
                            attn_job(q_, units, fin_plain(hh, tsl))
                        tr.dma('sp', gsbB[:], GT[:, tsl], gsbB, True)
                        jcs = [jc for jc in range(nct) if b - 4 * jc >= 0]
                        for hh in range(4):
                            q_ = load_q(8 + hh, tsl)
                            units = []
                            for jc in jcs:
                                dlt = b - 4 * jc
                                u = dict(k=KcmpT[:, jc * 128:(jc + 1) * 128], kb=KcmpT, v=Vcmp[:, jc, :], vb=Vcmp,
                                         pt=(ET[:, hh, jc, :], ET))
                                if dlt <= 4:
                                    u['extras'] = [(cv('i30k'), cv('cmask', 128, (dlt * 512, (dlt + 1) * 512)), [cstb])]
                                units.append(u)
                            attn_job(q_, units, fin_nsa(hh, 0, tsl))
                        for i in range(4):
                            im = imp[i % 2]
                            for hh in range(4):
                                pu = psL[cnt['u'] % 3]
                                cnt['u'] += 1
                                for xi, jc in enumerate(jcs):
                                    op('pe', lambda e: e.matmul(pu[:, 0:n_sb + 1], lhsT=ET[:, hh, jc, 128 * i:128 * (i + 1)],
                                                                rhs=cv('ov', 128, (jc * (n_sb + 1), (jc + 1) * (n_sb + 1))),
                                                                start=(xi == 0), stop=(xi == len(jcs) - 1)), [ET, cstb], [pu])
                                rd_ = rd[hh % 2]
                                op('dve', lambda e: e.tensor_scalar(out=rd_[:], in0=pu[:, n_sb:n_sb + 1], scalar1=TINY,
                                                                    scalar2=None, op0=ALU.max), [pu], [rd_])
                                op('dve', lambda e: e.reciprocal(out=rd_[:], in_=rd_[:]), [rd_], [rd_])
                                if hh == 0:
                                    op('dve', lambda e: e.tensor_scalar(out=im[:], in0=pu[:, 0:n_sb], scalar1=rd_[:, 0:1],
                                                                        scalar2=None, op0=ALU.mult), [pu, rd_], [im])
                                else:
                                    op('dve', lambda e: e.scalar_tensor_tensor(out=im[:], in0=pu[:, 0:n_sb], scalar=rd_[:, 0:1],
                                                                               in1=im[:], op0=ALU.mult, op1=ALU.add),
                                       [pu, rd_, im], [im])
                            for hf in range(2):
                                cur = 8 * b + 2 * i + hf
                                rows = slice(64 * hf, 64 * hf + 64)
                                if cur - 1 >= 0:
                                    op('pool', lambda e: e.memset(im[rows, cur - 1:cur], 2e30), [], [im])
                                op('pool', lambda e: e.memset(im[rows, cur:cur + 1], 1e30), [], [im])
                                if cur + 1 < n_sb:
                                    op('pool', lambda e: e.memset(im[rows, cur + 1:n_sb], -2e30), [], [im])
                            op('pool', lambda e: e.memset(im[:, 0:1], 3e30), [], [im])
                            ma = m8[i % 2]
                            mb = m8b[i % 2]
                            bqn = BqN[i % 2]
                            op('dve', lambda e: e.max(out=ma[:], in_=im[:]), [im], [ma])
                            op('dve', lambda e: e.match_replace(out=sc2[:], in_to_replace=ma[:], in_values=im[:],
                                                                imm_value=-2e30), [ma, im], [sc2])
                            op('dve', lambda e: e.max(out=mb[:], in_=sc2[:]), [sc2], [mb])
                            op('dve', lambda e: e.tensor_scalar(out=mb[:, 7:8], in0=mb[:, 7:8], scalar1=-1e30, scalar2=None,
                                                                op0=ALU.max), [mb], [mb])
                            op('dve', lambda e: e.tensor_scalar(out=bqn[:], in0=im[:], scalar1=mb[:, 7:8], scalar2=1.0,
                                                                op0=ALU.is_ge, op1=ALU.subtract), [im, mb], [bqn])
                            op('pe', lambda e: e.transpose(out=psb[0:n_sb, 128 * i:128 * (i + 1)], in_=bqn[:],
                                                           identity=cv('ident')), [bqn, cstb], [psb])
                        op('dve', lambda e: e.tensor_copy(out=BtN[0:n_sb, :], in_=psb[0:n_sb, 0:512]), [psb], [BtN])
                        for hh in range(4):
                            q_ = load_q(8 + hh, tsl)
                            kv = load_kv(14, 512, 0, jlast)
                            units = []
                            for j in range(jlast + 1):
                                ka, kb, va, vb = kv[j]
                                u = dict(k=ka, kb=kb, v=va, vb=vb)
                                f0 = 0
                                ex = []
                                if j >= 4 * b:
                                    dx, f0 = diag_extra(j - 4 * b)
                                    ex.append(dx)
                                ex.insert(0, (cv('an', 128, (j * 128, (j + 1) * 128)), BtN[:, f0:512], [cstb, BtN]))
                                u['extras'] = ex
                                u['f0'] = f0
                                units.append(u)
                            attn_job(q_, units, fin_nsa(hh, 1, tsl))
                        for hh in range(4):
                            q_ = load_q(8 + hh, tsl)
                            jfirst = max(0, 4 * b - 4)
                            kv = load_kv(15, 640, jfirst, jlast)
                            units = []
                            for j in range(jfirst, jlast + 1):
                                ka, kb, va, vb = kv[j]
                                u = dict(k=ka, kb=kb, v=va, vb=vb)
                                if j >= 4 * b:
                                    dx, f0 = diag_extra(j - 4 * b)
                                    u['extras'] = [dx]
                                    u['f0'] = f0
                                else:
                                    jl = j - (4 * b - 4)
                                    u['f0'] = 0
                                    u['f1'] = 128 * (jl + 1)
                                    u['extras'] = [(cv('i30k'), cv('w2', 128, (384 - 128 * jl, 512)), [cstb])]
                                units.append(u)
                            attn_job(q_, units, fin_nsa(hh, 2, tsl))
                    flush_pending()
                    while advance(pgB, 8):
                        pass
                    tr.end_phase(ph)

                with ExitStack() as ph:
                    hT = [tr.buf(ph, "hT%d" % i, [128, KC, 512], BF16) for i in range(2)]
                    x1 = tr.buf(ph, "x1", [128, KC, 512], F32)
                    yT = tr.buf(ph, "yT", [128, KC, 512], F32)
                    OTs = tr.buf(ph, "OTs", [128, KC, 512], BF16)
                    sq = [tr.buf(ph, "sq%d" % i, [128, 512], BF16) for i in range(2)]
                    tmpn = tr.buf(ph, "tmpn", [128, 512], F32)
                    rstd = tr.buf(ph, "rstd", [128, 512], F32)
                    tmpx = [tr.buf(ph, "tmpx%d" % i, [128, 512], F32) for i in range(2)]
                    tmpu = [tr.buf(ph, "tmpu%d" % i, [128, 512], F32) for i in range(2)]
                    pcn = [0]
                    h2 = hT[0]
                    aT = hT[1]

                    def gemm16(l_, c0, src, outfn):
                        for oc in range(16):
                            w = load_w(l_, c0 + oc)
                            pp = ps[pcn[0] % 3]
                            pcn[0] += 1
                            for k in range(KC):
                                op('pe', lambda e: e.matmul(pp[:], lhsT=w[:, k, :], rhs=src[:, k, :], start=(k == 0),
                                                            stop=(k == KC - 1)), [w, src], [pp])
                            outfn(oc, pp)

                    def sumsq(src, pn):
                        for k in range(KC):
                            s_ = sq[k % 2]
                            op('act', lambda e: e.activation(out=s_[:], in_=src[:, k, :], func=AF.Square), [src], [s_])
                            op('pe', lambda e: e.matmul(pn[:], lhsT=onesb[:], rhs=s_[:], start=(k == 0), stop=(k == KC - 1)),
                               [s_, onesb], [pn])

                    def resid(di):
                        for k in range(KC):
                            tx = tmpx[k % 2]
                            op('dve', lambda e: e.tensor_tensor(out=tx[:], in0=yT[:, k, :], in1=rstd[:], op=ALU.mult),
                               [yT, rstd], [tx])
                            op('dve', lambda e: e.scalar_tensor_tensor(out=x1[:, k, :], in0=tx[:], scalar=der[:, l, di, k:k + 1],
                                                                       in1=x1[:, k, :], op0=ALU.mult, op1=ALU.add),
                               [tx, der, x1], [x1])

                    for b in range(NB):
                        t0 = b * 512
                        tsl = slice(t0, t0 + 512)
                        tr.dma('sp', OTs[:], OTD[:, :, tsl].rearrange("c p t -> p c t"), OTs, True)
                        tr.dma('sp', x1[:], xin[:, :, tsl], x1, True)
                        pn = ps[6]

                        def out_c(oc, pp):
                            op('act', lambda e: e.copy(out=yT[:, oc, :], in_=pp[:]), [pp], [yT])
                        gemm16(l, 47, OTs, out_c)
                        sumsq(yT, pn)
                        rms_rstd((tmpn, rstd), pn)
                        resid(2)
                        sumsq(x1, pn)
                        rms_rstd((tmpn, rstd), pn)
                        for k in range(KC):
                            tx = tmpx[k % 2]
                            op('dve', lambda e: e.tensor_tensor(out=tx[:], in0=x1[:, k, :], in1=rstd[:], op=ALU.mult),
                               [x1, rstd], [tx])
                            op('pool', lambda e: e.tensor_scalar(out=h2[:, k, :], in0=tx[:], scalar1=der[:, l, 3, k:k + 1],
                                                                 scalar2=der[:, l, 4, k:k + 1], op0=ALU.mult, op1=ALU.add),
                               [tx, der], [h2])
                        for q in range(4):
                            def out_u(fc_, pp):
                                tu = tmpu[fc_ % 2]
                                op('act', lambda e: e.activation(out=tu[:], in_=pp[:], func=AF.Relu), [pp], [tu])
                                op('dve', lambda e: e.tensor_tensor(out=aT[:, fc_, :], in0=tu[:], in1=tu[:], op=ALU.mult),
                                   [tu], [aT])
                            gemm16(l, 63 + 16 * q, h2, out_u)

                            def out_d(dc, pp):
                                if q == 0:
                                    op('act', lambda e: e.copy(out=yT[:, dc, :], in_=pp[:]), [pp], [yT])
                                else:
                                    op('dve', lambda e: e.tensor_tensor(out=yT[:, dc, :], in0=pp[:], in1=yT[:, dc, :],
                                                                        op=ALU.add), [pp, yT], [yT])
                            gemm16(l, 127 + 16 * q, aT, out_d)
                        sumsq(yT, pn)
                        rms_rstd((tmpn, rstd), pn)
                        resid(5)
                        tr.dma('pool', xout[:, :, tsl], x1[:], x1, False)
                    tr.end_phase(ph)
        tr.barrier()
        ninst = tr.ninst
    return nc, ninst


def fm_layout(v):
    v = np.asarray(v, np.float32)
    lead = v.shape[:-1]
    return np.ascontiguousarray(np.moveaxis(v.reshape(lead + (v.shape[-1] // 128, 128)), -1, 0))


def make_in_maps(inp, S, NL):
    f = lambda a: np.ascontiguousarray(np.asarray(a, np.float32))
    B = inp['x'].shape[0]
    cst, fcst = make_consts(S)
    gains = np.stack([np.asarray(inp[k], np.float32)[:NL] for k in ('g_pre_mix', 'g_post_mix', 'g_pre_mlp', 'g_post_mlp')], axis=1)
    gains = fm_layout(gains)
    bmodT = fm_layout(np.asarray(inp['b_mod'], np.float32)[:NL])
    negb = np.ascontiguousarray(np.asarray(inp['b_forget'], np.float32)[:NL].T)
    peT = np.stack([np.asarray(inp['cmp_pe_k'], np.float32)[:NL], np.asarray(inp['cmp_pe_v'], np.float32)[:NL]], axis=1)
    peT = np.ascontiguousarray(peT.transpose(3, 0, 1, 2))
    shared = dict(
        w_mod=f(inp['w_mod'][:NL]), bmodT=bmodT, gains=gains, w_in=f(inp['w_in'][:NL]), negb=negb, peT=peT,
        w1k=f(inp['cmp_w1_k'][:NL]), w2k=f(inp['cmp_w2_k'][:NL]), w1v=f(inp['cmp_w1_v'][:NL]), w2v=f(inp['cmp_w2_v'][:NL]),
        w_out=f(inp['w_out'][:NL]), w_up=f(inp['w_up'][:NL]), w_down=f(inp['w_down'][:NL]), cst=cst, fcst=fcst)
    maps = []
    for b in range(B):
        xb = np.asarray(inp['x'][b], np.float32)[:S]
        xT = np.ascontiguousarray(xb.T.reshape(KC, 128, S).transpose(1, 0, 2))
        m = dict(shared)
        m['xT'] = xT
        m['pos'] = np.ascontiguousarray(np.asarray(inp['positions'][b], np.int32)[None, :S])
        m['cT'] = fm_layout(np.asarray(inp['c'][b], np.float32))
        maps.append(m)
    return maps


def run(inp, S, NL):
    nc, ninst = build_program(S, NL)
    maps = make_in_maps(inp, S, NL)
    res = run_bass_kernel_spmd(nc, maps, core_ids=list(range(len(maps))))
    outs = []
    for r in res.results:
        yT = np.asarray(r['yT'], np.float32)
        outs.append(yT.transpose(2, 1, 0).reshape(S, D))
    return np.stack(outs, axis=0)


def kernel(**inputs):
    return run(inputs, 8192, 4)
```

```python
import numpy as np
from contextlib import ExitStack
import concourse.bass as bass
import concourse.mybir as mybir
from concourse.bass_utils import run_bass_kernel_spmd

F32 = mybir.dt.float32
BF16 = mybir.dt.bfloat16
I32 = mybir.dt.int32
ALU = mybir.AluOpType
AF = mybir.ActivationFunctionType
AX = mybir.AxisListType

D = 2048
KC = 16
HD = 128
PROJ = 5908
DFF = 8192
SCL = 128.0 ** -0.5
EPS = 1e-6
NCH = 201
TINY = 1e-30


class Buf:
    __slots__ = ('t', 'name', 'w', 'r', 'sem', 'sval')

    def __init__(self, t, name):
        self.t = t
        self.name = name
        self.w = None
        self.r = {}
        self.sem = None
        self.sval = 0

    def __getitem__(self, k):
        return self.t[k]


class TR:
    ENG = ('pe', 'act', 'dve', 'pool', 'sp')

    def __init__(self, nc, st):
        self.nc = nc
        self.st = st
        self.h = dict(pe=nc.tensor, act=nc.scalar, dve=nc.vector, pool=nc.gpsimd, sp=nc.sync)
        self.sem = {e: st.enter_context(nc.semaphore("c_" + e)) for e in self.ENG}
        self.cnt = {e: 0 for e in self.ENG}
        self.seen = {e: {} for e in self.ENG}
        self.dsems = []
        self.freesems = []
        self.by_stack = {}
        self.nbuf = 0
        self.ninst = 0

    def buf(self, st, name, shape, dtype, psum=False):
        self.nbuf += 1
        nm = "%s_%d" % (name, self.nbuf)
        if psum:
            t = st.enter_context(self.nc.psum_tensor(nm, shape, dtype))
        else:
            t = st.enter_context(self.nc.sbuf_tensor(nm, shape, dtype))
        b = Buf(t, nm)
        self.by_stack.setdefault(id(st), []).append(b)
        return b

    def _wait(self, eng, rec):
        kind, who, val = rec
        if kind == 'e':
            if who == eng and eng == 'pe':
                return
            key = ('e', who)
            semh = self.sem[who]
        else:
            key = ('d', who.name)
            semh = who.sem
            val = who.sval
        if self.seen[eng].get(key, 0) >= val:
            return
        self.h[eng].wait_ge(semh, val)
        self.ninst += 1
        self.seen[eng][key] = val

    def op(self, eng, fn, reads=(), writes=()):
        recs = []
        for b in reads:
            if b.w is not None:
                recs.append(b.w)
        for b in writes:
            if b.w is not None:
                recs.append(b.w)
            recs.extend(b.r.values())
        for rec in recs:
            self._wait(eng, rec)
        inst = fn(self.h[eng])
        self.cnt[eng] += 1
        self.ninst += 1
        inst.then_inc(self.sem[eng], 1)
        rec = ('e', eng, self.cnt[eng])
        for b in reads:
            b.r[('e', eng)] = rec
        for b in writes:
            b.w = rec
            b.r = {}
        return inst

    def dma(self, eng, out, in_, sb, load):
        recs = []
        if sb.w is not None:
            recs.append(sb.w)
        if load:
            recs.extend(sb.r.values())
        if sb.sem is None:
            if self.freesems:
                sb.sem, sb.sval = self.freesems.pop()
            else:
                sb.sem = self.st.enter_context(self.nc.semaphore("d_" + sb.name))
                sb.sval = 0
            self.dsems.append(sb)
        if sb.sval > 0:
            recs.append(('d', sb, sb.sval))
        for rec in recs:
            self._wait(eng, rec)
        inst = self.h[eng].dma_start(out=out, in_=in_)
        self.ninst += 1
        sb.sval += 16
        inst.then_inc(sb.sem, 16)
        rec = ('d', sb, sb.sval)
        if load:
            sb.w = rec
            sb.r = {}
        else:
            sb.r[('d', sb.name)] = rec
        return inst

    def barrier(self):
        for e in self.ENG:
            for e2 in self.ENG:
                if e2 != e and self.cnt[e2] > 0:
                    self._wait(e, ('e', e2, self.cnt[e2]))
            for b in self.dsems:
                if b.sval > 0:
                    self._wait(e, ('d', b, b.sval))

    def end_phase(self, st):
        self.barrier()
        names = set(b.name for b in self.by_stack.pop(id(st), []))
        keep = []
        for b in self.dsems:
            if b.name in names:
                self.freesems.append((b.sem, b.sval))
            else:
                keep.append(b)
        self.dsems = keep


def const_layout(S):
    n_sb = S // 64
    KW = min(64, n_sb)
    nct = max(1, S // 2048)
    off = {}
    o = 0
    for name, n in [('ident', 128), ('i30k', 128), ('causal', 512), ('w2', 512), ('cmask', 5 * 512),
                    ('am', 32 * 128), ('an', (S // 128) * 128), ('pmt', 32), ('ov', nct * (n_sb + 1)),
                    ('eg', 12 * 128)]:
        off[name] = (o, n)
        o += n
    return off, o


def make_consts(S):
    n_sb = S // 64
    KW = min(64, n_sb)
    nct = max(1, S // 2048)
    ncmp = S // 16 - 1
    off, tot = const_layout(S)
    c = np.zeros((128, tot), np.float32)
    p = np.arange(128)[:, None]

    def put(name, arr):
        o, n = off[name]
        assert arr.shape == (128, n), (name, arr.shape, n)
        c[:, o:o + n] = arr
    put('ident', np.eye(128, dtype=np.float32))
    put('i30k', 30000.0 * np.eye(128, dtype=np.float32))
    f = np.arange(512)[None, :]
    put('causal', np.where(f >= p, 0.0, -1.0).astype(np.float32))
    put('w2', np.where(f - 384 < p, 0.0, -1.0).astype(np.float32))
    cm = [np.where(16 * p - f <= 512 * d - 31, 0.0, -1.0) for d in range(5)]
    put('cmask', np.concatenate(cm, axis=1).astype(np.float32))
    am = np.zeros((128, 32, 128), np.float32)
    for n in range(32):
        am[n, n, :] = 30000.0
    put('am', am.reshape(128, -1))
    an = np.zeros((128, S // 128, 128), np.float32)
    for j in range(S // 128):
        an[2 * j, j, 0:64] = 30000.0
        an[2 * j + 1, j, 64:128] = 30000.0
    put('an', an.reshape(128, -1))
    pm = np.zeros((128, 32), np.float32)
    for i in range(16):
        pm[i + 16, i] = -1.0
        pm[i, i + 16] = 1.0
    put('pmt', pm)
    ov = np.zeros((128, nct, n_sb + 1), np.float32)
    for cc in range(ncmp):
        jc, pp = divmod(cc, 128)
        n0 = cc // 4
        ov[pp, jc, n0] = 1.0
        if cc % 4 == 3 and n0 + 1 < n_sb:
            ov[pp, jc, n0 + 1] = 1.0
        ov[pp, jc, n_sb] = 1.0
    put('ov', ov.reshape(128, -1))
    eg = np.zeros((128, 12, 128), np.float32)
    for r in range(12):
        eg[r, r, :] = 1.0
    put('eg', eg.reshape(128, -1))
    fc = np.zeros((128, 4), np.float32)
    invf = 500000.0 ** (-(np.arange(16, dtype=np.float64)) / 16.0)
    fc[0:32, 0] = np.tile(invf / (2 * np.pi), 2).astype(np.float32)
    return c, fc


def build_program(S, NL):
    assert S % 2048 == 0
    NB = S // 512
    NT = S // 128
    n_sb = S // 64
    KW = min(64, n_sb)
    nct = S // 2048
    ncmp = S // 16 - 1
    NCP = nct * 128
    assert NCP <= 512
    nmb = S // 256
    coff, ctot = const_layout(S)

    nc = bass.Bass("TRN2", target_bir_lowering=False)

    def din(name, shape, dt=F32):
        return nc.dram_tensor(name, shape, dt, kind="ExternalInput").ap()

    def dscr(name, shape, dt):
        return nc.dram_tensor(name, shape, dt, kind="Internal").ap()

    x_in = din("xT", [128, KC, S])
    pos_in = din("pos", [1, S], I32)
    cT_in = din("cT", [128, KC])
    w_mod = din("w_mod", [NL, D, 6 * D])
    bmodT = din("bmodT", [128, NL, 96])
    gT_in = din("gains", [128, NL, 4, KC])
    w_in = din("w_in", [NL, D, PROJ])
    negb_in = din("negb", [8, NL])
    peT_in = din("peT", [128, NL, 2, 32])
    w1k = din("w1k", [NL, 4096, 256])
    w2k = din("w2k", [NL, 256, 128])
    w1v = din("w1v", [NL, 4096, 256])
    w2v = din("w2v", [NL, 256, 128])
    w_out = din("w_out", [NL, D, D])
    w_up = din("w_up", [NL, D, DFF])
    w_down = din("w_down", [NL, DFF, D])
    cst_in = din("cst", [128, ctot])
    fcst_in = din("fcst", [128, 4])
    y_out = nc.dram_tensor("yT", [128, KC, S], F32, kind="ExternalOutput").ap()

    WC = [dscr("WC%d" % l_, [NCH, 128, KC, 128], BF16) for l_ in range(NL)]
    FM = dscr("FM", [32, 128, S], BF16)
    VT = dscr("VT", [14, 128, S // 128, 128], BF16)
    GT = dscr("GT", [12, S], BF16)
    RHO = dscr("RHO", [8, S], BF16)
    COS = dscr("COS", [32, S], F32)
    SIN = dscr("SIN", [32, S], F32)
    OTD = dscr("OTD", [16, 128, S], BF16)
    XS = [dscr("XS0", [128, KC, S], F32), dscr("XS1", [128, KC, S], F32)]
    CSTB = dscr("CSTB", [128, ctot], BF16)

    with ExitStack() as st:
        tr = TR(nc, st)
        op = tr.op

        onesb = tr.buf(st, "onesb", [128, 128], BF16)
        onesf = tr.buf(st, "onesf", [128, 128], F32)
        pmtb = tr.buf(st, "pmtb", [128, 32], BF16)
        identf = tr.buf(st, "identf", [128, 128], F32)
        fcst = tr.buf(st, "fcst", [128, 4], F32)
        onecol = tr.buf(st, "onecol", [128, 1], F32)
        der = tr.buf(st, "der", [128, NL, 6, KC], F32)
        negb = tr.buf(st, "negb", [8, NL], F32)
        peb = tr.buf(st, "peb", [128, NL, 2, 32], BF16)
        w4k = [tr.buf(st, "w4k%d" % i, [128, KC, 128], BF16) for i in range(4)]
        ps = [tr.buf(st, "ps%d" % i, [128, 512], F32, psum=True) for i in range(7)]
        psb = tr.buf(st, "psb", [128, 1024], BF16, psum=True)
        wslot = [0]

        cst_holder = [None]

        def cv(name, rows=128, sub=None):
            cstb = cst_holder[0]
            o, n = coff[name]
            if sub is None:
                return cstb[0:rows, o:o + n]
            return cstb[0:rows, o + sub[0]:o + sub[1]]

        def load_w(l, ci):
            b = w4k[wslot[0] % 4]
            wslot[0] += 1
            tr.dma('sp', b[:], WC[l][ci], b, True)
            return b

        fm_cols = ([i * 128 for i in range(4)] + [512 + i * 128 for i in range(4)] +
                   [1536 + i * 128 for i in range(4)] + [2048, 2176, 2304, 2560] +
                   [2828 + i * 128 for i in range(8)] + [3852 + i * 128 for i in range(8)])
        v_cols = ([1024 + i * 128 for i in range(4)] + [2432, 2688] + [4876 + i * 128 for i in range(8)])

        def prep_list(l):
            items = []
            for ci, c0 in enumerate(fm_cols):
                items.append((ci, [(w_in[l, :, c0:c0 + 128], 0)], KC))
            for ci, c0 in enumerate(v_cols):
                items.append((32 + ci, [(w_in[l, :, c0:c0 + 128], 0)], KC))
            items.append((46, [(w_in[l, :, 2816:2828], 0), (w_in[l, :, 5900:5908], 12)], KC))
            for oc in range(16):
                items.append((47 + oc, [(w_out[l, :, oc * 128:(oc + 1) * 128], 0)], KC))
            for fc_ in range(64):
                items.append((63 + fc_, [(w_up[l, :, fc_ * 128:(fc_ + 1) * 128], 0)], KC))
            for q in range(4):
                for dc in range(16):
                    items.append((127 + q * 16 + dc, [(w_down[l, q * 2048:(q + 1) * 2048, dc * 128:(dc + 1) * 128], 0)], KC))
            for wi, w1 in enumerate((w1k, w1v)):
                for g in range(2):
                    for hc in range(2):
                        items.append((191 + wi * 4 + g * 2 + hc,
                                      [(w1[l, g * 2048:(g + 1) * 2048, hc * 128:(hc + 1) * 128], 0)], KC))
            items.append((199, [(w2k[l, :, :], 0)], 2))
            items.append((200, [(w2v[l, :, :], 0)], 2))
            return items

        def prep_gen(l, stg, sto, cast_engs, kh=KC):
            nb_ = len(stg)
            steps = []
            for (ci, pieces, nk) in prep_list(l):
                for k0 in range(0, nk, kh):
                    steps.append((ci, pieces, k0, min(nk, k0 + kh)))

            def load(i):
                ci, pieces, k0, k1 = steps[i]
                sg = stg[i % nb_]
                for (src, co) in pieces:
                    ncols = src.shape[1]
                    tr.dma('sp', sg[:, 0:k1 - k0, co:co + ncols], src.rearrange("(k p) c -> p k c", p=128)[:, k0:k1, :], sg, True)

            load(0)
            for i, (ci, pieces, k0, k1) in enumerate(steps):
                if i + 1 < len(steps):
                    load(i + 1)
                sg = stg[i % nb_]
                so = sto[i % nb_]
                nk_ = k1 - k0
                wtot = max(co + src.shape[1] for (src, co) in pieces)
                ce = cast_engs[i % len(cast_engs)]
                if ce == 'act':
                    op('act', lambda e: e.copy(out=so[:, 0:nk_, 0:wtot], in_=sg[:, 0:nk_, 0:wtot]), [sg], [so])
                else:
                    op(ce, lambda e: e.tensor_copy(out=so[:, 0:nk_, 0:wtot], in_=sg[:, 0:nk_, 0:wtot]), [sg], [so])
                tr.dma('pool', WC[l][ci, :, k0:k1, 0:wtot], so[:, 0:nk_, 0:wtot], so, False)
                yield

        def advance(gen, n):
            for _ in range(n):
                try:
                    next(gen)
                except StopIteration:
                    return False
            return True

        with ExitStack() as ph:
            cstf = tr.buf(ph, "cstf", [128, ctot], F32)
            tr.dma('sp', cstf[:], cst_in, cstf, True)
            tr.dma('sp', fcst[:], fcst_in, fcst, True)
            bfg = tr.buf(ph, "bfg", [8, NL], F32)
            tr.dma('sp', bfg[:], negb_in, bfg, True)
            op('dve', lambda e: e.tensor_scalar(out=negb[:], in0=bfg[:], scalar1=-1.0, scalar2=None, op0=ALU.mult), [bfg], [negb])
            half = ctot // 2
            cstb = tr.buf(ph, "cstb0", [128, ctot], BF16)
            op('dve', lambda e: e.tensor_copy(out=cstb[:, 0:half], in_=cstf[:, 0:half]), [cstf], [cstb])
            op('pool', lambda e: e.tensor_copy(out=cstb[:, half:ctot], in_=cstf[:, half:ctot]), [cstf], [cstb])
            tr.dma('pool', CSTB, cstb[:], cstb, False)
            o_id = coff['ident'][0]
            op('dve', lambda e: e.tensor_copy(out=identf[:], in_=cstf[:, o_id:o_id + 128]), [cstf], [identf])
            o_pm = coff['pmt'][0]
            op('dve', lambda e: e.tensor_copy(out=pmtb[:], in_=cstf[:, o_pm:o_pm + 32]), [cstf], [pmtb])
            op('pool', lambda e: e.memset(onecol[:], 1.0), [], [onecol])
            op('pool', lambda e: e.memset(onesb[:], 1.0), [], [onesb])
            op('pool', lambda e: e.memset(onesf[:], 1.0), [], [onesf])
            pef = tr.buf(ph, "pef", [128, NL, 2, 32], F32)
            tr.dma('sp', pef[:], peT_in, pef, True)
            op('dve', lambda e: e.tensor_copy(out=peb[:], in_=pef[:]), [pef], [peb])
            tr.end_phase(ph)
        with ExitStack() as ph:
            CW = 2048
            posi = tr.buf(ph, "posi", [32, CW], I32)
            posf = tr.buf(ph, "posf", [32, CW], F32)
            ru = tr.buf(ph, "ru", [32, CW], F32)
            rni = tr.buf(ph, "rni", [32, CW], I32)
            rnf = tr.buf(ph, "rnf", [32, CW], F32)
            rtab = [tr.buf(ph, "rtab%d" % i, [32, CW], F32) for i in range(2)]
            for cidx in range(S // CW):
                sl = slice(cidx * CW, (cidx + 1) * CW)
                tr.dma('sp', posi[:], pos_in[0:1, sl].to_broadcast([32, CW]), posi, True)
                op('dve', lambda e: e.tensor_copy(out=posf[:], in_=posi[:]), [posi], [posf])
                for ti, (tab, addc) in enumerate(((SIN, 0.0), (COS, 0.25))):
                    op('dve', lambda e: e.tensor_scalar(out=ru[:], in0=posf[:], scalar1=fcst[0:32, 0:1], scalar2=addc,
                                                        op0=ALU.mult, op1=ALU.add), [posf, fcst], [ru])
                    op('dve', lambda e: e.tensor_copy(out=rni[:], in_=ru[:]), [ru], [rni])
                    op('dve', lambda e: e.tensor_copy(out=rnf[:], in_=rni[:]), [rni], [rnf])
                    op('dve', lambda e: e.tensor_tensor(out=ru[:], in0=ru[:], in1=rnf[:], op=ALU.subtract), [ru, rnf], [ru])
                    rt = rtab[ti]
                    op('act', lambda e: e.activation(out=rt[:], in_=ru[:], func=AF.Sin, scale=2.0 * np.pi), [ru], [rt])
                    tr.dma('pool', tab[:, sl], rt[:], rt, False)
            tr.end_phase(ph)
        with ExitStack() as ph:
            gains = tr.buf(ph, "gains", [128, NL, 4, KC], F32)
            tr.dma('sp', gains[:], gT_in, gains, True)
            bmod = tr.buf(ph, "bmod", [128, NL, 96], F32)
            tr.dma('sp', bmod[:], bmodT, bmod, True)
            cT = tr.buf(ph, "cT", [128, KC], F32)
            tr.dma('sp', cT[:], cT_in, cT, True)
            cs = tr.buf(ph, "cs", [128, KC], F32)
            op('act', lambda e: e.activation(out=cs[:], in_=cT[:], func=AF.Silu), [cT], [cs])
            modt = tr.buf(ph, "modt", [128, NL, 96], F32)
            wm = [tr.buf(ph, "wm%d" % i, [128, KC, 128], F32) for i in range(3)]
            stg0 = [tr.buf(ph, "stg%d" % i, [128, KC, 128], F32) for i in range(3)]
            sto0 = [tr.buf(ph, "sto%d" % i, [128, KC, 128], BF16) for i in range(3)]
            pg0 = prep_gen(0, stg0, sto0, ('dve', 'pool', 'act'))
            for l in range(NL):
                pm_ = ps[l % 2]
                for j in range(96):
                    b = wm[j % 3]
                    tr.dma('sp', b[:], w_mod[l, :, j * 128:(j + 1) * 128].rearrange("(k p) c -> p k c", p=128), b, True)
                    for k in range(KC):
                        op('pe', lambda e: e.matmul(pm_[:, j:j + 1], lhsT=b[:, k, :], rhs=cs[:, k:k + 1],
                                                    start=(k == 0), stop=(k == KC - 1)), [b, cs], [pm_])
                    advance(pg0, 1)
                op('dve', lambda e: e.tensor_tensor(out=modt[:, l, :], in0=pm_[:, 0:96], in1=bmod[:, l, :], op=ALU.add),
                   [pm_, bmod], [modt])
                for (di, si, gi) in ((0, 1, 0), (3, 4, 2)):
                    op('dve', lambda e: e.scalar_tensor_tensor(out=der[:, l, di, :], in0=modt[:, l, si * 16:(si + 1) * 16],
                                                               scalar=1.0, in1=gains[:, l, gi, :], op0=ALU.add, op1=ALU.mult),
                       [modt, gains], [der])
                for (di, si) in ((1, 0), (4, 3)):
                    op('dve', lambda e: e.tensor_copy(out=der[:, l, di, :], in_=modt[:, l, si * 16:(si + 1) * 16]), [modt], [der])
                for (di, si, gi) in ((2, 2, 1), (5, 5, 3)):
                    op('dve', lambda e: e.tensor_tensor(out=der[:, l, di, :], in0=modt[:, l, si * 16:(si + 1) * 16],
                                                        in1=gains[:, l, gi, :], op=ALU.mult), [modt, gains], [der])
            while advance(pg0, 8):
                pass
            tr.end_phase(ph)
        for l in range(NL):
            xin = x_in if l == 0 else XS[(l - 1) % 2]
            xout = y_out if l == NL - 1 else XS[l % 2]
            with ExitStack() as ly:
                kmh = tr.buf(ly, "kmh", [128, 4, 32], BF16)
                kml = tr.buf(ly, "kml", [128, 4, 32], BF16)
                CK = tr.buf(ly, "CK", [128, NT, 8], F32)
                KcmpT = tr.buf(ly, "KcmpT", [128, NCP], BF16)
                Vcmp = tr.buf(ly, "Vcmp", [128, nct, 128], BF16)

                def rms_rstd(ph_bufs, sumps):
                    tmpn, rstd = ph_bufs
                    op('dve', lambda e: e.tensor_scalar(out=tmpn[:], in0=sumps[:], scalar1=1.0 / D, scalar2=EPS,
                                                        op0=ALU.mult, op1=ALU.add), [sumps], [tmpn])
                    op('act', lambda e: e.activation(out=tmpn[:], in_=tmpn[:], func=AF.Sqrt), [tmpn], [tmpn])
                    op('dve', lambda e: e.reciprocal(out=rstd[:], in_=tmpn[:]), [tmpn], [rstd])

                with ExitStack() as ph:
                    hT = [tr.buf(ph, "hT%d" % i, [128, KC, 512], BF16) for i in range(2)]
                    xc = [tr.buf(ph, "xc%d" % i, [128, 2, 512], F32) for i in range(3)]
                    sq = [tr.buf(ph, "sq%d" % i, [128, 512], BF16) for i in range(2)]
                    tmpn = tr.buf(ph, "tmpn", [128, 512], F32)
                    rstd = tr.buf(ph, "rstd", [128, 512], F32)
                    tmpx = [tr.buf(ph, "tmpx%d" % i, [128, 512], F32) for i in range(2)]
                    wv = [tr.buf(ph, "wv%d" % i, [128, KC, 512], BF16) for i in range(2)]
                    qk = [tr.buf(ph, "qk%d" % i, [128, 512], BF16) for i in range(3)]
                    ropeA = tr.buf(ph, "ropeA", [32, 512], F32)
                    ropeB = tr.buf(ph, "ropeB", [32, 512], F32)
                    cosb = [tr.buf(ph, "cosb%d" % i, [32, 512], F32) for i in range(2)]
                    sinb = [tr.buf(ph, "sinb%d" % i, [32, 512], F32) for i in range(2)]
                    vout = [tr.buf(ph, "vout%d" % i, [128, 4, 512], BF16) for i in range(2)]
                    gsb = [tr.buf(ph, "gsb%d" % i, [12, 512], BF16) for i in range(2)]
                    fe = tr.buf(ph, "fe", [8, 512], F32)
                    fl = tr.buf(ph, "fl", [8, 512], F32)
                    Cb = [tr.buf(ph, "Cb%d" % i, [8, 512], F32) for i in range(2)]
                    rhob = [tr.buf(ph, "rhob%d" % i, [8, 512], BF16) for i in range(2)]
                    ones8 = tr.buf(ph, "ones8", [8, 512], F32)
                    kmf = tr.buf(ph, "kmf", [128, 4, 32], F32)
                    kmt = tr.buf(ph, "kmt", [128, 4, 32], F32)
                    op('pool', lambda e: e.memset(ones8[:], 1.0), [], [ones8])
                    op('pool', lambda e: e.memset(kmf[:], 0.0), [], [kmf])
                    xcn = [0]
                    qkn = [0]
                    pcn = [0]
                    vgn = [0]
                    roped = set(range(0, 13)) | {14, 15}
                    roped.discard(13)
                    vgroups = [(32, 4, 0), (36, 2, 512), (38, 4, 768), (42, 4, 1280)]
                    for b in range(NB):
                        t0 = b * 512
                        tsl = slice(t0, t0 + 512)
                        h = hT[b % 2]
                        pn = ps[6]
                        for g in range(8):
                            xb = xc[xcn[0] % 3]
                            xcn[0] += 1
                            tr.dma('sp', xb[:], xin[:, 2 * g:2 * g + 2, tsl], xb, True)
                            for c in range(2):
                                s_ = sq[(2 * g + c) % 2]
                                op('act', lambda e: e.activation(out=s_[:], in_=xb[:, c, :], func=AF.Square), [xb], [s_])
                                op('pe', lambda e: e.matmul(pn[:], lhsT=onesb[:], rhs=s_[:], start=(g == 0 and c == 0),
                                                            stop=(g == 7 and c == 1)), [s_, onesb], [pn])
                        rms_rstd((tmpn, rstd), pn)
                        for g in range(8):
                            xb = xc[xcn[0] % 3]
                            xcn[0] += 1
                            tr.dma('sp', xb[:], xin[:, 2 * g:2 * g + 2, tsl], xb, True)
                            for c in range(2):
                                k = 2 * g + c
                                tx = tmpx[k % 2]
                                op('dve', lambda e: e.tensor_tensor(out=tx[:], in0=xb[:, c, :], in1=rstd[:], op=ALU.mult),
                                   [xb, rstd], [tx])
                                op('pool', lambda e: e.tensor_scalar(out=h[:, k, :], in0=tx[:], scalar1=der[:, l, 0, k:k + 1],
                                                                     scalar2=der[:, l, 1, k:k + 1], op0=ALU.mult, op1=ALU.add),
                                   [tx, der], [h])
                        cb_ = cosb[b % 2]
                        sb_ = sinb[b % 2]
                        tr.dma('sp', cb_[:], COS[:, tsl], cb_, True)
                        tr.dma('sp', sb_[:], SIN[:, tsl], sb_, True)
                        for ci in range(32):
                            w = load_w(l, ci)
                            pp = ps[pcn[0] % 3]
                            pcn[0] += 1
                            for k in range(KC):
                                op('pe', lambda e: e.matmul(pp[:], lhsT=w[:, k, :], rhs=h[:, k, :], start=(k == 0),
                                                            stop=(k == KC - 1)), [w, h], [pp])
                            q_ = qk[qkn[0] % 3]
                            qkn[0] += 1
                            op('act', lambda e: e.copy(out=q_[:], in_=pp[:]), [pp], [q_])
                            if ci in roped:
                                pr = ps[3]
                                op('pe', lambda e: e.matmul(pr[0:32, :], lhsT=pmtb[:], rhs=q_[:], start=True, stop=True),
                                   [q_, pmtb], [pr])
                                op('dve', lambda e: e.tensor_tensor(out=ropeA[:], in0=pr[0:32, :], in1=sb_[:], op=ALU.mult),
                                   [pr, sb_], [ropeA])
                                op('dve', lambda e: e.tensor_tensor(out=ropeB[:], in0=q_[0:32, :], in1=cb_[:], op=ALU.mult),
                                   [q_, cb_], [ropeB])
                                op('dve', lambda e: e.tensor_tensor(out=q_[0:32, :], in0=ropeA[:], in1=ropeB[:], op=ALU.add),
                                   [ropeA, ropeB], [q_])
                            if 4 <= ci < 8:
                                op('dve', lambda e: e.tensor_reduce(out=kmf[:, ci - 4, 2 * b:2 * b + 2],
                                                                    in_=q_[:].rearrange("p (a c) -> p a c", a=2),
                                                                    axis=AX.X, op=ALU.add), [q_], [kmf])
                            tr.dma('pool', FM[ci, :, tsl], q_[:], q_, False)
                        for (c0, nchk, col0) in vgroups:
                            wvb = wv[vgn[0] % 2]
                            vo = vout[vgn[0] % 2]
                            vgn[0] += 1
                            ncols = nchk * 128
                            for i in range(nchk):
                                tr.dma('sp', wvb[:, :, i * 128:(i + 1) * 128], WC[l][c0 + i], wvb, True)
                            for tt in range(4):
                                pp = ps[pcn[0] % 3]
                                pcn[0] += 1
                                for k in range(KC):
                                    op('pe', lambda e: e.matmul(pp[:, 0:ncols], lhsT=h[:, k, tt * 128:(tt + 1) * 128],
                                                                rhs=wvb[:, k, 0:ncols], start=(k == 0), stop=(k == KC - 1)),
                                       [wvb, h], [pp])
                                if tt % 2 == 0:
                                    op('act', lambda e: e.copy(out=vo[:, tt, 0:ncols], in_=pp[:, 0:ncols]), [pp], [vo])
                                else:
                                    op('dve', lambda e: e.tensor_copy(out=vo[:, tt, 0:ncols], in_=pp[:, 0:ncols]), [pp], [vo])
                            for i in range(nchk):
                                tr.dma('pool', VT[col0 // 128 + i, :, 4 * b:4 * b + 4, :], vo[:, :, i * 128:(i + 1) * 128], vo, False)
                        w = load_w(l, 46)
                        pg = ps[4]
                        for k in range(KC):
                            op('pe', lambda e: e.matmul(pg[0:12, :], lhsT=w[:, k, 0:12], rhs=h[:, k, :], start=(k == 0),
                                                        stop=(k == KC - 1)), [w, h], [pg])
                        g_ = gsb[b % 2]
                        op('act', lambda e: e.activation(out=g_[:], in_=pg[0:12, :], func=AF.Sigmoid), [pg], [g_])
                        tr.dma('pool', GT[:, tsl], g_[:], g_, False)
                        pf = ps[5]
                        for k in range(KC):
                            op('pe', lambda e: e.matmul(pf[0:8, :], lhsT=w[:, k, 12:20], rhs=h[:, k, :], start=(k == 0),
                                                        stop=(k == KC - 1)), [w, h], [pf])
                        op('act', lambda e: e.activation(out=fe[:], in_=pf[0:8, :], func=AF.Exp, scale=-1.0,
                                                         bias=negb[:, l:l + 1]), [pf, negb], [fe])
                        op('act', lambda e: e.activation(out=fl[:], in_=fe[:], func=AF.Ln, bias=onecol[0:8, 0:1]),
                           [fe, onecol], [fl])
                        cbuf = Cb[b % 2]
                        cprev = Cb[(b - 1) % 2]
                        if b == 0:
                            op('dve', lambda e: e.tensor_tensor_scan(out=cbuf[:], data0=ones8[:], data1=fl[:], initial=0.0,
                                                                     op0=ALU.mult, op1=ALU.add), [ones8, fl], [cbuf])
                        else:
                            op('dve', lambda e: e.tensor_tensor_scan(out=cbuf[:], data0=ones8[:], data1=fl[:],
                                                                     initial=cprev[:, 511:512], op0=ALU.mult, op1=ALU.add),
                               [ones8, fl, cprev], [cbuf])
                        rb_ = rhob[b % 2]
                        op('dve', lambda e: e.tensor_scalar(out=rb_[:], in0=cbuf[:], scalar1=-1.0 / SCL, scalar2=None,
                                                            op0=ALU.mult), [cbuf], [rb_])
                        tr.dma('pool', RHO[:, tsl], rb_[:], rb_, False)
                        pt_ = ps[3]
                        for i in range(4):
                            op('pe', lambda e: e.transpose(out=pt_[:, 8 * i:8 * i + 8], in_=cbuf[0:8, 128 * i:128 * (i + 1)],
                                                           identity=identf[0:8, 0:8]), [cbuf, identf], [pt_])
                        op('dve', lambda e: e.tensor_copy(out=CK[:, 4 * b:4 * b + 4, :],
                                                          in_=pt_[:, 0:32].rearrange("p (a c) -> p a c", a=4)), [pt_], [CK])
                    op('dve', lambda e: e.tensor_scalar(out=kmf[:], in0=kmf[:], scalar1=1.0 / 256.0, scalar2=None,
                                                        op0=ALU.mult), [kmf], [kmf])
                    op('dve', lambda e: e.tensor_copy(out=kmh[:], in_=kmf[:]), [kmf], [kmh])
                    op('dve', lambda e: e.tensor_copy(out=kmt[:], in_=kmh[:]), [kmh], [kmt])
                    op('dve', lambda e: e.tensor_tensor(out=kmt[:], in0=kmf[:], in1=kmt[:], op=ALU.subtract), [kmf, kmt], [kmt])
                    op('dve', lambda e: e.tensor_copy(out=kml[:], in_=kmt[:]), [kmt], [kml])
                    tr.end_phase(ph)

                with ExitStack() as ph:
                    kcS = tr.buf(ph, "kcS", [128, S], BF16)
                    Z = tr.buf(ph, "Z", [128, 16, S // 16], BF16)
                    W1 = tr.buf(ph, "W1", [128, 32, 256], BF16)
                    W2 = tr.buf(ph, "W2", [128, 2, 128], BF16)
                    bcol = tr.buf(ph, "bcol", [128, 2], F32)
                    xg = tr.buf(ph, "xg", [128, 512], F32)
                    x2 = tr.buf(ph, "x2", [128, 512], F32)
                    sg = tr.buf(ph, "sg", [128, 512], F32)
                    GTt = tr.buf(ph, "GTt", [128, 2, 512], BF16)
                    for wi in range(2):
                        tr.dma('sp', kcS[:], FM[12 + wi], kcS, True)
                        op('pool', lambda e: e.tensor_copy(out=Z[:], in_=kcS[:].rearrange("p (m r) -> p r m", r=16)), [kcS], [Z])
                        for g in range(2):
                            for hc in range(2):
                                tr.dma('sp', W1[:, 16 * g:16 * g + 16, hc * 128:(hc + 1) * 128],
                                       WC[l][191 + wi * 4 + g * 2 + hc], W1, True)
                        tr.dma('sp', W2[:], WC[l][199 + wi, :, 0:2, :], W2, True)
                        op('pool', lambda e: e.memset(GTt[:], 0.0), [], [GTt])
                        for hc in range(2):
                            phd = ps[hc]
                            pbs = ps[2]
                            for tau in range(32):
                                a, r = divmod(tau, 16)
                                op('pe', lambda e: e.matmul(phd[:, 0:ncmp], lhsT=W1[:, tau, hc * 128:(hc + 1) * 128],
                                                            rhs=Z[:, r, a:a + ncmp], start=(tau == 0), stop=(tau == 31)),
                                   [W1, Z], [phd])
                            for tau in range(32):
                                op('pe', lambda e: e.matmul(pbs[:, hc:hc + 1], lhsT=W1[:, tau, hc * 128:(hc + 1) * 128],
                                                            rhs=peb[:, l, wi, tau:tau + 1], start=(tau == 0), stop=(tau == 31)),
                                   [W1, peb], [pbs])
                            op('dve', lambda e: e.tensor_copy(out=bcol[:, hc:hc + 1], in_=pbs[:, hc:hc + 1]), [pbs], [bcol])
                            op('act', lambda e: e.activation(out=xg[:, 0:ncmp], in_=phd[:, 0:ncmp], func=AF.Identity,
                                                             bias=bcol[:, hc:hc + 1]), [phd, bcol], [xg])
                            op('dve', lambda e: e.tensor_tensor(out=x2[:, 0:ncmp], in0=xg[:, 0:ncmp], in1=xg[:, 0:ncmp],
                                                                op=ALU.mult), [xg], [x2])
                            op('dve', lambda e: e.tensor_scalar(out=x2[:, 0:ncmp], in0=x2[:, 0:ncmp], scalar1=0.0713548163,
                                                                scalar2=1.5957691216, op0=ALU.mult, op1=ALU.add), [x2], [x2])
                            op('dve', lambda e: e.tensor_tensor(out=x2[:, 0:ncmp], in0=x2[:, 0:ncmp], in1=xg[:, 0:ncmp],
                                                                op=ALU.mult), [x2, xg], [x2])
                            op('act', lambda e: e.activation(out=sg[:, 0:ncmp], in_=x2[:, 0:ncmp], func=AF.Sigmoid), [x2], [sg])
                            op('dve', lambda e: e.tensor_tensor(out=GTt[:, hc, 0:ncmp], in0=xg[:, 0:ncmp], in1=sg[:, 0:ncmp],
                                                                op=ALU.mult), [xg, sg], [GTt])
                        po = ps[3]
                        if wi == 0:
                            for hc in range(2):
                                op('pe', lambda e: e.matmul(po[:, 0:NCP], lhsT=W2[:, hc, :], rhs=GTt[:, hc, 0:NCP],
                                                            start=(hc == 0), stop=(hc == 1)), [W2, GTt], [po])
                            op('act', lambda e: e.copy(out=KcmpT[:], in_=po[:, 0:NCP]), [po], [KcmpT])
                        else:
                            for ct in range(nct):
                                for hc in range(2):
                                    op('pe', lambda e: e.matmul(po[:, ct * 128:(ct + 1) * 128],
                                                                lhsT=GTt[:, hc, ct * 128:(ct + 1) * 128], rhs=W2[:, hc, :],
                                                                start=(hc == 0), stop=(hc == 1)), [W2, GTt], [po])
                            op('act', lambda e: e.copy(out=Vcmp[:], in_=po[:, 0:NCP].rearrange("p (a c) -> p a c", a=nct)),
                               [po], [Vcmp])
                    tr.end_phase(ph)

                with ExitStack() as ph:
                    QT = [tr.buf(ph, "QT%d" % i, [128, 512], BF16) for i in range(2)]
                    Kc = [tr.buf(ph, "Kc%d" % i, [128, 2048], BF16) for i in range(3)]
                    Vc = [tr.buf(ph, "Vc%d" % i, [128, 16, 128], BF16) for i in range(3)]
                    PT = [tr.buf(ph, "PT%d" % i, [128, 512], BF16) for i in range(6)]
                    ET = tr.buf(ph, "ET", [128, 4, nct, 512], BF16)
                    acc = [tr.buf(ph, "acc%d" % i, [128, 512], F32) for i in range(4)]
                    rec = [tr.buf(ph, "rec%d" % i, [128, 512], F32) for i in range(2)]
                    rgt = [tr.buf(ph, "rgt%d" % i, [128, 512], F32) for i in range(2)]
                    tmc = [tr.buf(ph, "tmc%d" % i, [128, 512], F32) for i in range(2)]
                    rho1 = [tr.buf(ph, "rho1_%d" % i, [128, 512], BF16) for i in range(2)]
                    for r_ in rho1:
                        op('pool', lambda e: e.memset(r_[:], 0.0), [], [r_])
                    tinycol = tr.buf(ph, "tinycol", [128, 1], F32)
                    op('pool', lambda e: e.memset(tinycol[:], TINY), [], [tinycol])
                    E0 = tr.buf(ph, "E0", [128, 128], BF16)
                    op('pool', lambda e: e.memset(E0[:], 0.0), [], [E0])
                    op('pool', lambda e: e.memset(E0[0:1, :], 1.0), [], [E0])
                    cstb = tr.buf(ph, "cstB", [128, ctot], BF16)
                    cst_holder[0] = cstb
                    tr.dma('sp', cstb[:], CSTB, cstb, True)
                    accD = [tr.buf(ph, "accD%d" % i, [128, 512], F32) for i in range(2)]
                    accG = [tr.buf(ph, "accG%d" % i, [128, 512], F32) for i in range(2)]
                    pending = []
                    pgB = iter(())
                    prep_per_job = max(1, -(-NCH // (NB * 28)))
                    gsbB = tr.buf(ph, "gsbB", [12, 512], BF16)
                    Gp = [tr.buf(ph, "Gp%d" % i, [128, 32], F32) for i in range(2)]
                    m8 = [tr.buf(ph, "m8_%d" % i, [128, 8], F32) for i in range(2)]
                    m8b = [tr.buf(ph, "m8b_%d" % i, [128, 8], F32) for i in range(2)]
                    Bq = [tr.buf(ph, "Bq%d" % i, [128, 32], BF16) for i in range(2)]
                    BtM = [tr.buf(ph, "BtM%d" % i, [128, 512], BF16) for i in range(2)]
                    for bt_ in BtM:
                        op('pool', lambda e: e.memset(bt_[:], 0.0), [], [bt_])
                    imp = [tr.buf(ph, "imp%d" % i, [128, n_sb], F32) for i in range(2)]
                    sc2 = tr.buf(ph, "sc2", [128, n_sb], F32)
                    BqN = [tr.buf(ph, "BqN%d" % i, [128, n_sb], BF16) for i in range(2)]
                    BtN = tr.buf(ph, "BtN", [128, 512], BF16)
                    op('pool', lambda e: e.memset(BtN[:], 0.0), [], [BtN])
                    rd = [tr.buf(ph, "rd%d" % i, [128, 1], F32) for i in range(2)]
                    Ost = [tr.buf(ph, "Ost%d" % i, [128, 512], BF16) for i in range(3)]
                    psL = [ps[0], ps[1], ps[4]]
                    psN = [ps[2], ps[3]]
                    psD = [ps[5], ps[5]]
                    psX = ps[6]
                    cnt = dict(q=0, kv=0, pt=0, job=0, rho=0, ost=0, u=0, x=0)

                    def load_q(ci, tsl):
                        q_ = QT[cnt['q'] % 2]
                        cnt['q'] += 1
                        tr.dma('sp', q_[:], FM[ci, :, tsl], q_, True)
                        return q_

                    def load_kv(kci, vcol, j0, j1):
                        res = {}
                        ck0, ck1 = j0 // 16, j1 // 16
                        for ck in range(ck0, ck1 + 1):
                            ja = max(j0, ck * 16)
                            jb = min(j1, ck * 16 + 15)
                            kb = Kc[cnt['kv'] % 3]
                            vb = Vc[cnt['kv'] % 3]
                            cnt['kv'] += 1
                            n = jb - ja + 1
                            tr.dma('sp', kb[:, 0:n * 128], FM[kci, :, ja * 128:(jb + 1) * 128], kb, True)
                            tr.dma('sp', vb[:, 0:n, :], VT[vcol // 128, :, ja:jb + 1, :], vb, True)
                            for j in range(ja, jb + 1):
                                res[j] = (kb[:, (j - ja) * 128:(j - ja + 1) * 128], kb, vb[:, j - ja, :], vb)
                        return res

                    def step_pending():
                        while pending:
                            try:
                                next(pending[0])
                                return
                            except StopIteration:
                                pending.pop(0)

                    def flush_pending():
                        while pending:
                            for _ in pending[0]:
                                pass
                            pending.pop(0)

                    def attn_job(q_, units, finalize):
                        jn = cnt['job']
                        cnt['job'] += 1
                        pN = psN[jn % 2]
                        pD = psD[jn % 2]
                        aD = accD[jn % 2]
                        aG = accG[jn % 2]
                        n = len(units)
                        pts = [None] * n
                        def full(i_):
                            return i_ < n and units[i_].get('f0', 0) == 0 and units[i_].get('f1', 512) == 512
                        copyD = full(0)
                        copyG = full(1)
                        if not copyD:
                            op('pool', lambda e: e.memset(aD[:], 0.0), [], [aD])
                        if not copyG:
                            op('pool', lambda e: e.memset(aG[:], 0.0), [], [aG])

                        def emit_S(i):
                            u = units[i]
                            f0, f1 = u.get('f0', 0), u.get('f1', 512)
                            pl = psL[cnt['pt'] % 3]
                            ex = u.get('extras', [])
                            op('pe', lambda e: e.matmul(pl[:, f0:f1], lhsT=u['k'], rhs=q_[:, f0:f1], start=True,
                                                        stop=(len(ex) == 0)), [u['kb'], q_], [pl])
                            for xi, (la, ra, bl) in enumerate(ex):
                                op('pe', lambda e: e.matmul(pl[:, f0:f1], lhsT=la, rhs=ra, start=False,
                                                            stop=(xi == len(ex) - 1)), bl, [pl])
                            if 'pt' in u:
                                pap, pbuf = u['pt']
                            else:
                                pbuf = PT[cnt['pt'] % 6]
                                pap = pbuf[:]
                            cnt['pt'] += 1
                            if u.get('bias') is not None:
                                bap, bbuf = u['bias']
                                op('act', lambda e: e.activation(out=pap[:, f0:f1], in_=pl[:, f0:f1], func=AF.Exp, scale=SCL,
                                                                 bias=bap), [pl, bbuf], [pbuf])
                            else:
                                op('act', lambda e: e.activation(out=pap[:, f0:f1], in_=pl[:, f0:f1], func=AF.Exp, scale=SCL),
                                   [pl], [pbuf])
                            pts[i] = (pap, pbuf)

                        def emit_PV(i):
                            u = units[i]
                            f0, f1 = u.get('f0', 0), u.get('f1', 512)
                            pap, pbuf = pts[i]
                            op('pe', lambda e: e.matmul(pN[:, f0:f1], lhsT=u['v'], rhs=pap[:, f0:f1], start=(i == 0),
                                                        stop=(i == n - 1)), [u['vb'], pbuf], [pN])
                            if i == 0 and copyD:
                                op('dve', lambda e: e.tensor_copy(out=aD[:], in_=pap[:]), [pbuf], [aD])
                            elif i == 1 and copyG:
                                op('pool', lambda e: e.tensor_copy(out=aG[:], in_=pap[:]), [pbuf], [aG])
                            elif i % 2 == 0:
                                op('dve', lambda e: e.tensor_tensor(out=aD[:, f0:f1], in0=aD[:, f0:f1], in1=pap[:, f0:f1],
                                                                    op=ALU.add), [aD, pbuf], [aD])
                            else:
                                op('pool', lambda e: e.tensor_tensor(out=aG[:, f0:f1], in0=aG[:, f0:f1], in1=pap[:, f0:f1],
                                                                     op=ALU.add), [aG, pbuf], [aG])

                        LAG = 2
                        for i in range(n + LAG):
                            if i < n:
                                emit_S(i)
                            if i >= LAG:
                                emit_PV(i - LAG)
                                if i >= LAG + 1:
                                    step_pending()
                        flush_pending()

                        def fin():
                            op('dve', lambda e: e.tensor_tensor(out=aD[:], in0=aD[:], in1=aG[:], op=ALU.add), [aD, aG], [aD])
                            yield
                            op('pe', lambda e: e.matmul(pD[:], lhsT=onesf[:], rhs=aD[:], start=True, stop=True),
                               [onesf, aD], [pD])
                            yield
                            for _ in finalize(pN, pD):
                                yield
                        pending.append(fin())
                        advance(pgB, prep_per_job)

                    def recip_den(pD):
                        r_ = rec[cnt['x'] % 2]
                        cnt['x'] += 1
                        op('act', lambda e: e.activation(out=r_[:], in_=pD[:], func=AF.Ln, bias=tinycol[:, 0:1]),
                           [pD, tinycol], [r_])
                        yield r_
                        op('act', lambda e: e.activation(out=r_[:], in_=r_[:], func=AF.Exp, scale=-1.0), [r_], [r_])
                        yield r_

                    def fin_plain(hc, tsl):
                        def f(pN, pD):
                            r_ = None
                            for r_ in recip_den(pD):
                                yield
                            o_ = Ost[cnt['ost'] % 3]
                            cnt['ost'] += 1
                            op('dve', lambda e: e.tensor_tensor(out=o_[:], in0=pN[:], in1=r_[:], op=ALU.mult), [pN, r_], [o_])
                            tr.dma('pool', OTD[hc, :, tsl], o_[:], o_, False)
                            yield
                        return f

                    def fin_nsa(hh, br, tsl):
                        def f(pN, pD):
                            r_ = None
                            for r_ in recip_den(pD):
                                yield
                            rr = 3 * hh + br
                            op('pe', lambda e: e.matmul(psX[:], lhsT=cv('eg', 12, (rr * 128, (rr + 1) * 128)), rhs=gsbB[:],
                                                        start=True, stop=True), [cstb, gsbB], [psX])
                            g_ = rgt[cnt['x'] % 2]
                            op('dve', lambda e: e.tensor_tensor(out=g_[:], in0=psX[:], in1=r_[:], op=ALU.mult), [psX, r_], [g_])
                            yield
                            a_ = acc[hh]
                            if br == 0:
                                op('dve', lambda e: e.tensor_tensor(out=a_[:], in0=pN[:], in1=g_[:], op=ALU.mult), [pN, g_], [a_])
                            else:
                                t_ = tmc[cnt['x'] % 2]
                                op('dve', lambda e: e.tensor_tensor(out=t_[:], in0=pN[:], in1=g_[:], op=ALU.mult), [pN, g_], [t_])
                                op('pool', lambda e: e.tensor_tensor(out=a_[:], in0=a_[:], in1=t_[:], op=ALU.add), [a_, t_], [a_])
                            yield
                            if br == 2:
                                o_ = Ost[cnt['ost'] % 3]
                                cnt['ost'] += 1
                                op('pool', lambda e: e.tensor_copy(out=o_[:], in_=a_[:]), [a_], [o_])
                                tr.dma('pool', OTD[4 + hh, :, tsl], o_[:], o_, False)
                                yield
                        return f

                    causal = cv('causal')

                    def diag_extra(jd):
                        f0 = 128 * jd
                        return (cv('i30k'), cv('causal', 128, (0, 512 - f0)), [cstb]), f0

                    for b in range(NB):
                        t0 = b * 512
                        tsl = slice(t0, t0 + 512)
                        jlast = 4 * b + 3
                        for hh in range(8):
                            q_ = load_q(16 + hh, tsl)
                            r1 = rho1[cnt['rho'] % 2]
                            cnt['rho'] += 1
                            tr.dma('sp', r1[0:1, :], RHO[hh:hh + 1, tsl], r1, True)
                            kv = load_kv(24 + hh, 768 + 128 * hh, 0, jlast)
                            units = []
                            for j in range(jlast + 1):
                                ka, kb, va, vb = kv[j]
                                u = dict(k=ka, kb=kb, v=va, vb=vb, bias=(CK[:, j, hh:hh + 1], CK))
                                f0 = 0
                                ex = []
                                if j >= 4 * b:
                                    dx, f0 = diag_extra(j - 4 * b)
                                    ex.append(dx)
                                ex.insert(0, (E0[:], r1[:, f0:512], [E0, r1]))
                                u['extras'] = ex
                                u['f0'] = f0
                                units.append(u)
                            attn_job(q_, units, fin_plain(8 + hh, tsl))
                        for hh in range(4):
                            q_ = load_q(hh, tsl)
                            bt = BtM[hh % 2]
                            for i in range(4):
                                own = 2 * b + i // 2
                                gp = Gp[i % 2]
                                mm = m8[i % 2]
                                bq = Bq[i % 2]
                                op('pe', lambda e: e.matmul(psX[:, 0:32], lhsT=q_[:, 128 * i:128 * (i + 1)], rhs=kmh[:, hh, :],
                                                            start=True, stop=False), [q_, kmh], [psX])
                                op('pe', lambda e: e.matmul(psX[:, 0:32], lhsT=q_[:, 128 * i:128 * (i + 1)], rhs=kml[:, hh, :],
                                                            start=False, stop=True), [q_, kml], [psX])
                                op('pool', lambda e: e.memset(gp[:], -1e30), [], [gp])
                                if own > 0:
                                    op('dve', lambda e: e.tensor_copy(out=gp[:, 0:own], in_=psX[:, 0:own]), [psX], [gp])
                                op('dve', lambda e: e.max(out=mm[:], in_=gp[:]), [gp], [mm])
                                op('dve', lambda e: e.tensor_scalar(out=mm[:, 2:3], in0=mm[:, 2:3], scalar1=-5e29, scalar2=None,
                                                                    op0=ALU.max), [mm], [mm])
                                op('dve', lambda e: e.tensor_scalar(out=bq[:], in0=gp[:], scalar1=mm[:, 2:3], scalar2=1.0,
                                                                    op0=ALU.is_ge, op1=ALU.subtract), [gp, mm], [bq])
                                op('pool', lambda e: e.memset(bq[:, own:own + 1], 0.0), [], [bq])
                                if own + 1 < 32:
                                    op('pool', lambda e: e.memset(bq[:, own + 1:32], -1.0), [], [bq])
                                op('pe', lambda e: e.transpose(out=psb[0:32, 128 * i:128 * (i + 1)], in_=bq[:],
                                                               identity=cv('ident')), [bq, cstb], [psb])
                            op('dve', lambda e: e.tensor_copy(out=bt[0:32, :], in_=psb[0:32, 0:512]), [psb], [bt])
                            kv = load_kv(4 + hh, 128 * hh, 0, jlast)
                            units = []
                            for j in range(jlast + 1):
                                ka, kb, va, vb = kv[j]
                                u = dict(k=ka, kb=kb, v=va, vb=vb)
                                f0 = 0
                                ex = []
                                if j >= 4 * b:
                                    dx, f0 = diag_extra(j - 4 * b)
                                    ex.append(dx)
                                nblk = j // 2
                                ex.insert(0, (cv('am', 128, (nblk * 128, (nblk + 1) * 128)), bt[:, f0:512], [cstb, bt]))
                                u['extras'] = ex
                                u['f0'] = f0
                                units.append(u)
                            attn_job(q_, units, fin_plain(hh, tsl))
                        tr.dma('sp', gsbB[:], GT[:, tsl], gsbB, True)
                        jcs = [jc for jc in range(nct) if b - 4 * jc >= 0]
                        for hh in range(4):
                            q_ = load_q(8 + hh, tsl)
                            units = []
                            for jc in jcs:
                                dlt = b - 4 * jc
                                u = dict(k=KcmpT[:, jc * 128:(jc + 1) * 128], kb=KcmpT, v=Vcmp[:, jc, :], vb=Vcmp,
                                         pt=(ET[:, hh, jc, :], ET))
                                if dlt <= 4:
                                    u['extras'] = [(cv('i30k'), cv('cmask', 128, (dlt * 512, (dlt + 1) * 512)), [cstb])]
                                units.append(u)
                            attn_job(q_, units, fin_nsa(hh, 0, tsl))
                        for i in range(4):
                            im = imp[i % 2]
                            for hh in range(4):
                                pu = psL[cnt['u'] % 3]
                                cnt['u'] += 1
                                for xi, jc in enumerate(jcs):
                                    op('pe', lambda e: e.matmul(pu[:, 0:n_sb + 1], lhsT=ET[:, hh, jc, 128 * i:128 * (i + 1)],
                                                                rhs=cv('ov', 128, (jc * (n_sb + 1), (jc + 1) * (n_sb + 1))),
                                                                start=(xi == 0), stop=(xi == len(jcs) - 1)), [ET, cstb], [pu])
                                rd_ = rd[hh % 2]
                                op('dve', lambda e: e.tensor_scalar(out=rd_[:], in0=pu[:, n_sb:n_sb + 1], scalar1=TINY,
                                                                    scalar2=None, op0=ALU.max), [pu], [rd_])
                                op('dve', lambda e: e.reciprocal(out=rd_[:], in_=rd_[:]), [rd_], [rd_])
                                if hh == 0:
                                    op('dve', lambda e: e.tensor_scalar(out=im[:], in0=pu[:, 0:n_sb], scalar1=rd_[:, 0:1],
                                                                        scalar2=None, op0=ALU.mult), [pu, rd_], [im])
                                else:
                                    op('dve', lambda e: e.scalar_tensor_tensor(out=im[:], in0=pu[:, 0:n_sb], scalar=rd_[:, 0:1],
                                                                               in1=im[:], op0=ALU.mult, op1=ALU.add),
                                       [pu, rd_, im], [im])
                            for hf in range(2):
                                cur = 8 * b + 2 * i + hf
                                rows = slice(64 * hf, 64 * hf + 64)
                                if cur - 1 >= 0:
                                    op('pool', lambda e: e.memset(im[rows, cur - 1:cur], 2e30), [], [im])
                                op('pool', lambda e: e.memset(im[rows, cur:cur + 1], 1e30), [], [im])
                                if cur + 1 < n_sb:
                                    op('pool', lambda e: e.memset(im[rows, cur + 1:n_sb], -2e30), [], [im])
                            op('pool', lambda e: e.memset(im[:, 0:1], 3e30), [], [im])
                            ma = m8[i % 2]
                            mb = m8b[i % 2]
                            bqn = BqN[i % 2]
                            op('dve', lambda e: e.max(out=ma[:], in_=im[:]), [im], [ma])
                            op('dve', lambda e: e.match_replace(out=sc2[:], in_to_replace=ma[:], in_values=im[:],
                                                                imm_value=-2e30), [ma, im], [sc2])
                            op('dve', lambda e: e.max(out=mb[:], in_=sc2[:]), [sc2], [mb])
                            op('dve', lambda e: e.tensor_scalar(out=mb[:, 7:8], in0=mb[:, 7:8], scalar1=-1e30, scalar2=None,
                                                                op0=ALU.max), [mb], [mb])
                            op('dve', lambda e: e.tensor_scalar(out=bqn[:], in0=im[:], scalar1=mb[:, 7:8], scalar2=1.0,
                                                                op0=ALU.is_ge, op1=ALU.subtract), [im, mb], [bqn])
                            op('pe', lambda e: e.transpose(out=psb[0:n_sb, 128 * i:128 * (i + 1)], in_=bqn[:],
                                                           identity=cv('ident')), [bqn, cstb], [psb])
                        op('dve', lambda e: e.tensor_copy(out=BtN[0:n_sb, :], in_=psb[0:n_sb, 0:512]), [psb], [BtN])
                        for hh in range(4):
                            q_ = load_q(8 + hh, tsl)
                            kv = load_kv(14, 512, 0, jlast)
                            units = []
                            for j in range(jlast + 1):
                                ka, kb, va, vb = kv[j]
                                u = dict(k=ka, kb=kb, v=va, vb=vb)
                                f0 = 0
                                ex = []
                                if j >= 4 * b:
                                    dx, f0 = diag_extra(j - 4 * b)
                                    ex.append(dx)
                                ex.insert(0, (cv('an', 128, (j * 128, (j + 1) * 128)), BtN[:, f0:512], [cstb, BtN]))
                                u['extras'] = ex
                                u['f0'] = f0
                                units.append(u)
                            attn_job(q_, units, fin_nsa(hh, 1, tsl))
                        for hh in range(4):
                            q_ = load_q(8 + hh, tsl)
                            jfirst = max(0, 4 * b - 4)
                            kv = load_kv(15, 640, jfirst, jlast)
                            units = []
                            for j in range(jfirst, jlast + 1):
                                ka, kb, va, vb = kv[j]
                                u = dict(k=ka, kb=kb, v=va, vb=vb)
                                if j >= 4 * b:
                                    dx, f0 = diag_extra(j - 4 * b)
                                    u['extras'] = [dx]
                                    u['f0'] = f0
                                else:
                                    jl = j - (4 * b - 4)
                                    u['f0'] = 0
                                    u['f1'] = 128 * (jl + 1)
                                    u['extras'] = [(cv('i30k'), cv('w2', 128, (384 - 128 * jl, 512)), [cstb])]
                                units.append(u)
                            attn_job(q_, units, fin_nsa(hh, 2, tsl))
                    flush_pending()
                    while advance(pgB, 8):
                        pass
                    tr.end_phase(ph)

                with ExitStack() as ph:
                    hT = [tr.buf(ph, "hT%d" % i, [128, KC, 512], BF16) for i in range(2)]
                    x1 = tr.buf(ph, "x1", [128, KC, 512], F32)
                    yT = tr.buf(ph, "yT", [128, KC, 512], F32)
                    OTs = tr.buf(ph, "OTs", [128, KC, 512], BF16)
                    sq = [tr.buf(ph, "sq%d" % i, [128, 512], BF16) for i in range(2)]
                    tmpn = tr.buf(ph, "tmpn", [128, 512], F32)
                    rstd = tr.buf(ph, "rstd", [128, 512], F32)
                    tmpx = [tr.buf(ph, "tmpx%d" % i, [128, 512], F32) for i in range(2)]
                    tmpu = [tr.buf(ph, "tmpu%d" % i, [128, 512], F32) for i in range(2)]
                    pcn = [0]
                    h2 = hT[0]
                    aT = hT[1]
                    if l + 1 < NL:
                        stgC = [tr.buf(ph, "stgC%d" % i, [128, 8, 128], F32) for i in range(2)]
                        stoC = [tr.buf(ph, "stoC%d" % i, [128, 8, 128], BF16) for i in range(2)]
                        pgC = prep_gen(l + 1, stgC, stoC, ('pool',), kh=8)
                    else:
                        pgC = iter(())
                    prep_per_gemm = -(-(2 * NCH) // (NB * 9))

                    def gemm16(l_, c0, src, outfn):
                        for oc in range(16):
                            w = load_w(l_, c0 + oc)
                            pp = ps[pcn[0] % 3]
                            pcn[0] += 1
                            for k in range(KC):
                                op('pe', lambda e: e.matmul(pp[:], lhsT=w[:, k, :], rhs=src[:, k, :], start=(k == 0),
                                                            stop=(k == KC - 1)), [w, src], [pp])
                            outfn(oc, pp)
                        advance(pgC, prep_per_gemm)

                    def sumsq(src, pn):
                        for k in range(KC):
                            s_ = sq[k % 2]
                            op('act', lambda e: e.activation(out=s_[:], in_=src[:, k, :], func=AF.Square), [src], [s_])
                            op('pe', lambda e: e.matmul(pn[:], lhsT=onesb[:], rhs=s_[:], start=(k == 0), stop=(k == KC - 1)),
                               [s_, onesb], [pn])

                    def resid(di):
                        for k in range(KC):
                            tx = tmpx[k % 2]
                            op('dve', lambda e: e.tensor_tensor(out=tx[:], in0=yT[:, k, :], in1=rstd[:], op=ALU.mult),
                               [yT, rstd], [tx])
                            op('dve', lambda e: e.scalar_tensor_tensor(out=x1[:, k, :], in0=tx[:], scalar=der[:, l, di, k:k + 1],
                                                                       in1=x1[:, k, :], op0=ALU.mult, op1=ALU.add),
                               [tx, der, x1], [x1])

                    for b in range(NB):
                        t0 = b * 512
                        tsl = slice(t0, t0 + 512)
                        tr.dma('sp', OTs[:], OTD[:, :, tsl].rearrange("c p t -> p c t"), OTs, True)
                        tr.dma('sp', x1[:], xin[:, :, tsl], x1, True)
                        pn = ps[6]

                        def out_c(oc, pp):
                            op('act', lambda e: e.copy(out=yT[:, oc, :], in_=pp[:]), [pp], [yT])
                        gemm16(l, 47, OTs, out_c)
                        sumsq(yT, pn)
                        rms_rstd((tmpn, rstd), pn)
                        resid(2)
                        sumsq(x1, pn)
                        rms_rstd((tmpn, rstd), pn)
                        for k in range(KC):
                            tx = tmpx[k % 2]
                            op('dve', lambda e: e.tensor_tensor(out=tx[:], in0=x1[:, k, :], in1=rstd[:], op=ALU.mult),
                               [x1, rstd], [tx])
                            op('pool', lambda e: e.tensor_scalar(out=h2[:, k, :], in0=tx[:], scalar1=der[:, l, 3, k:k + 1],
                                                                 scalar2=der[:, l, 4, k:k + 1], op0=ALU.mult, op1=ALU.add),
                               [tx, der], [h2])
                        for q in range(4):
                            def out_u(fc_, pp):
                                tu = tmpu[fc_ % 2]
                                op('act', lambda e: e.activation(out=tu[:], in_=pp[:], func=AF.Relu), [pp], [tu])
                                op('dve', lambda e: e.tensor_tensor(out=aT[:, fc_, :], in0=tu[:], in1=tu[:], op=ALU.mult),
                                   [tu], [aT])
                            gemm16(l, 63 + 16 * q, h2, out_u)

                            def out_d(dc, pp):
                                if q == 0:
                                    op('act', lambda e: e.copy(out=yT[:, dc, :], in_=pp[:]), [pp], [yT])
                                else:
                                    op('dve', lambda e: e.tensor_tensor(out=yT[:, dc, :], in0=pp[:], in1=yT[:, dc, :],
                                                                        op=ALU.add), [pp, yT], [yT])
                            gemm16(l, 127 + 16 * q, aT, out_d)
                        sumsq(yT, pn)
                        rms_rstd((tmpn, rstd), pn)
                        resid(5)
                        tr.dma('pool', xout[:, :, tsl], x1[:], x1, False)
                    while advance(pgC, 8):
                        pass
                    tr.end_phase(ph)
        tr.barrier()
        ninst = tr.ninst
    return nc, ninst


def fm_layout(v):
    v = np.asarray(v, np.float32)
    lead = v.shape[:-1]
    return np.ascontiguousarray(np.moveaxis(v.reshape(lead + (v.shape[-1] // 128, 128)), -1, 0))


def make_in_maps(inp, S, NL):
    f = lambda a: np.ascontiguousarray(np.asarray(a, np.float32))
    B = inp['x'].shape[0]
    cst, fcst = make_consts(S)
    gains = np.stack([np.asarray(inp[k], np.float32)[:NL] for k in ('g_pre_mix', 'g_post_mix', 'g_pre_mlp', 'g_post_mlp')], axis=1)
    gains = fm_layout(gains)
    bmodT = fm_layout(np.asarray(inp['b_mod'], np.float32)[:NL])
    negb = np.ascontiguousarray(np.asarray(inp['b_forget'], np.float32)[:NL].T)
    peT = np.stack([np.asarray(inp['cmp_pe_k'], np.float32)[:NL], np.asarray(inp['cmp_pe_v'], np.float32)[:NL]], axis=1)
    peT = np.ascontiguousarray(peT.transpose(3, 0, 1, 2))
    shared = dict(
        w_mod=f(inp['w_mod'][:NL]), bmodT=bmodT, gains=gains, w_in=f(inp['w_in'][:NL]), negb=negb, peT=peT,
        w1k=f(inp['cmp_w1_k'][:NL]), w2k=f(inp['cmp_w2_k'][:NL]), w1v=f(inp['cmp_w1_v'][:NL]), w2v=f(inp['cmp_w2_v'][:NL]),
        w_out=f(inp['w_out'][:NL]), w_up=f(inp['w_up'][:NL]), w_down=f(inp['w_down'][:NL]), cst=cst, fcst=fcst)
    maps = []
    for b in range(B):
        xb = np.asarray(inp['x'][b], np.float32)[:S]
        xT = np.ascontiguousarray(xb.T.reshape(KC, 128, S).transpose(1, 0, 2))
        m = dict(shared)
        m['xT'] = xT
        m['pos'] = np.ascontiguousarray(np.asarray(inp['positions'][b], np.int32)[None, :S])
        m['cT'] = fm_layout(np.asarray(inp['c'][b], np.float32))
        maps.append(m)
    return maps


def run(inp, S, NL):
    nc, ninst = build_program(S, NL)
    maps = make_in_maps(inp, S, NL)
    res = run_bass_kernel_spmd(nc, maps, core_ids=list(range(len(maps))))
    outs = []
    for r in res.results:
        yT = np.asarray(r['yT'], np.float32)
        outs.append(yT.transpose(2, 1, 0).reshape(S, D))
    return np.stack(outs, axis=0)


def kernel(**inputs):
    return run(inputs, 8192, 4)
```

```python
import numpy as np
from contextlib import ExitStack
import concourse.bass as bass
import concourse.mybir as mybir
from concourse.bass_utils import run_bass_kernel_spmd

F32 = mybir.dt.float32
BF16 = mybir.dt.bfloat16
I32 = mybir.dt.int32
ALU = mybir.AluOpType
AF = mybir.ActivationFunctionType
AX = mybir.AxisListType

D = 2048
KC = 16
HD = 128
PROJ = 5908
DFF = 8192
SCL = 128.0 ** -0.5
EPS = 1e-6
NCH = 201
TINY = 1e-30


class Buf:
    __slots__ = ('t', 'name', 'w', 'r', 'sem', 'sval')

    def __init__(self, t, name):
        self.t = t
        self.name = name
        self.w = None
        self.r = {}
        self.sem = None
        self.sval = 0

    def __getitem__(self, k):
        return self.t[k]


class TR:
    ENG = ('pe', 'act', 'dve', 'pool', 'sp')

    def __init__(self, nc, st):
        self.nc = nc
        self.st = st
        self.h = dict(pe=nc.tensor, act=nc.scalar, dve=nc.vector, pool=nc.gpsimd, sp=nc.sync)
        self.sem = {e: st.enter_context(nc.semaphore("c_" + e)) for e in self.ENG}
        self.cnt = {e: 0 for e in self.ENG}
        self.seen = {e: {} for e in self.ENG}
        self.dsems = []
        self.freesems = []
        self.by_stack = {}
        self.nbuf = 0
        self.ninst = 0

    def buf(self, st, name, shape, dtype, psum=False):
        self.nbuf += 1
        nm = "%s_%d" % (name, self.nbuf)
        if psum:
            t = st.enter_context(self.nc.psum_tensor(nm, shape, dtype))
        else:
            t = st.enter_context(self.nc.sbuf_tensor(nm, shape, dtype))
        b = Buf(t, nm)
        self.by_stack.setdefault(id(st), []).append(b)
        return b

    def _wait(self, eng, rec):
        kind, who, val = rec
        if kind == 'e':
            if who == eng and eng == 'pe':
                return
            key = ('e', who)
            semh = self.sem[who]
        else:
            key = ('d', who.name)
            semh = who.sem
            val = who.sval
        if self.seen[eng].get(key, 0) >= val:
            return
        self.h[eng].wait_ge(semh, val)
        self.ninst += 1
        self.seen[eng][key] = val

    def op(self, eng, fn, reads=(), writes=()):
        recs = []
        for b in reads:
            if b.w is not None:
                recs.append(b.w)
        for b in writes:
            if b.w is not None:
                recs.append(b.w)
            recs.extend(b.r.values())
        for rec in recs:
            self._wait(eng, rec)
        inst = fn(self.h[eng])
        self.cnt[eng] += 1
        self.ninst += 1
        inst.then_inc(self.sem[eng], 1)
        rec = ('e', eng, self.cnt[eng])
        for b in reads:
            b.r[('e', eng)] = rec
        for b in writes:
            b.w = rec
            b.r = {}
        return inst

    def dma(self, eng, out, in_, sb, load):
        recs = []
        if sb.w is not None:
            recs.append(sb.w)
        if load:
            recs.extend(sb.r.values())
        if sb.sem is None:
            if self.freesems:
                sb.sem, sb.sval = self.freesems.pop()
            else:
                sb.sem = self.st.enter_context(self.nc.semaphore("d_" + sb.name))
                sb.sval = 0
            self.dsems.append(sb)
        if sb.sval > 0:
            recs.append(('d', sb, sb.sval))
        for rec in recs:
            self._wait(eng, rec)
        inst = self.h[eng].dma_start(out=out, in_=in_)
        self.ninst += 1
        sb.sval += 16
        inst.then_inc(sb.sem, 16)
        rec = ('d', sb, sb.sval)
        if load:
            sb.w = rec
            sb.r = {}
        else:
            sb.r[('d', sb.name)] = rec
        return inst

    def barrier(self):
        for e in self.ENG:
            for e2 in self.ENG:
                if e2 != e and self.cnt[e2] > 0:
                    self._wait(e, ('e', e2, self.cnt[e2]))
            for b in self.dsems:
                if b.sval > 0:
                    self._wait(e, ('d', b, b.sval))

    def end_phase(self, st):
        self.barrier()
        names = set(b.name for b in self.by_stack.pop(id(st), []))
        keep = []
        for b in self.dsems:
            if b.name in names:
                self.freesems.append((b.sem, b.sval))
            else:
                keep.append(b)
        self.dsems = keep


def const_layout(S):
    n_sb = S // 64
    KW = min(64, n_sb)
    nct = max(1, S // 2048)
    off = {}
    o = 0
    for name, n in [('ident', 128), ('i30k', 128), ('causal', 512), ('w2', 512), ('cmask', 5 * 512),
                    ('am', 32 * 128), ('an', (S // 128) * 128), ('pmt', 32), ('ov', nct * (n_sb + 1)),
                    ('eg', 12 * 128)]:
        off[name] = (o, n)
        o += n
    return off, o


def make_consts(S):
    n_sb = S // 64
    KW = min(64, n_sb)
    nct = max(1, S // 2048)
    ncmp = S // 16 - 1
    off, tot = const_layout(S)
    c = np.zeros((128, tot), np.float32)
    p = np.arange(128)[:, None]

    def put(name, arr):
        o, n = off[name]
        assert arr.shape == (128, n), (name, arr.shape, n)
        c[:, o:o + n] = arr
    put('ident', np.eye(128, dtype=np.float32))
    put('i30k', 30000.0 * np.eye(128, dtype=np.float32))
    f = np.arange(512)[None, :]
    put('causal', np.where(f >= p, 0.0, -1.0).astype(np.float32))
    put('w2', np.where(f - 384 < p, 0.0, -1.0).astype(np.float32))
    cm = [np.where(16 * p - f <= 512 * d - 31, 0.0, -1.0) for d in range(5)]
    put('cmask', np.concatenate(cm, axis=1).astype(np.float32))
    am = np.zeros((128, 32, 128), np.float32)
    for n in range(32):
        am[n, n, :] = 30000.0
    put('am', am.reshape(128, -1))
    an = np.zeros((128, S // 128, 128), np.float32)
    for j in range(S // 128):
        an[2 * j, j, 0:64] = 30000.0
        an[2 * j + 1, j, 64:128] = 30000.0
    put('an', an.reshape(128, -1))
    pm = np.zeros((128, 32), np.float32)
    for i in range(16):
        pm[i + 16, i] = -1.0
        pm[i, i + 16] = 1.0
    put('pmt', pm)
    ov = np.zeros((128, nct, n_sb + 1), np.float32)
    for cc in range(ncmp):
        jc, pp = divmod(cc, 128)
        n0 = cc // 4
        ov[pp, jc, n0] = 1.0
        if cc % 4 == 3 and n0 + 1 < n_sb:
            ov[pp, jc, n0 + 1] = 1.0
        ov[pp, jc, n_sb] = 1.0
    put('ov', ov.reshape(128, -1))
    eg = np.zeros((128, 12, 128), np.float32)
    for r in range(12):
        eg[r, r, :] = 1.0
    put('eg', eg.reshape(128, -1))
    fc = np.zeros((128, 4), np.float32)
    invf = 500000.0 ** (-(np.arange(16, dtype=np.float64)) / 16.0)
    fc[0:32, 0] = np.tile(invf / (2 * np.pi), 2).astype(np.float32)
    return c, fc


def build_program(S, NL):
    assert S % 2048 == 0
    NB = S // 512
    NT = S // 128
    n_sb = S // 64
    KW = min(64, n_sb)
    nct = S // 2048
    ncmp = S // 16 - 1
    NCP = nct * 128
    assert NCP <= 512
    nmb = S // 256
    coff, ctot = const_layout(S)

    nc = bass.Bass("TRN2", target_bir_lowering=False)

    def din(name, shape, dt=F32):
        return nc.dram_tensor(name, shape, dt, kind="ExternalInput").ap()

    def dscr(name, shape, dt):
        return nc.dram_tensor(name, shape, dt, kind="Internal").ap()

    x_in = din("xT", [128, KC, S])
    pos_in = din("pos", [1, S], I32)
    cT_in = din("cT", [128, KC])
    w_mod = din("w_mod", [NL, D, 6 * D])
    bmodT = din("bmodT", [128, NL, 96])
    gT_in = din("gains", [128, NL, 4, KC])
    w_in = din("w_in", [NL, D, PROJ])
    negb_in = din("negb", [8, NL])
    peT_in = din("peT", [128, NL, 2, 32])
    w1k = din("w1k", [NL, 4096, 256])
    w2k = din("w2k", [NL, 256, 128])
    w1v = din("w1v", [NL, 4096, 256])
    w2v = din("w2v", [NL, 256, 128])
    w_out = din("w_out", [NL, D, D])
    w_up = din("w_up", [NL, D, DFF])
    w_down = din("w_down", [NL, DFF, D])
    cst_in = din("cst", [128, ctot])
    fcst_in = din("fcst", [128, 4])
    y_out = nc.dram_tensor("yT", [128, KC, S], F32, kind="ExternalOutput").ap()

    WC = [dscr("WC%d" % l_, [NCH, 128, KC, 128], BF16) for l_ in range(NL)]
    FM = dscr("FM", [32, 128, S], BF16)
    VT = dscr("VT", [14, 128, S // 128, 128], BF16)
    GT = dscr("GT", [12, S], BF16)
    RHO = dscr("RHO", [8, S], BF16)
    COS = dscr("COS", [32, S], F32)
    SIN = dscr("SIN", [32, S], F32)
    OTD = dscr("OTD", [16, 128, S], BF16)
    XS = [dscr("XS0", [128, KC, S], F32), dscr("XS1", [128, KC, S], F32)]
    CSTB = dscr("CSTB", [128, ctot], BF16)

    with ExitStack() as st:
        tr = TR(nc, st)
        op = tr.op

        onesb = tr.buf(st, "onesb", [128, 128], BF16)
        onesf = tr.buf(st, "onesf", [128, 128], F32)
        pmtb = tr.buf(st, "pmtb", [128, 32], BF16)
        identf = tr.buf(st, "identf", [128, 128], F32)
        fcst = tr.buf(st, "fcst", [128, 4], F32)
        onecol = tr.buf(st, "onecol", [128, 1], F32)
        der = tr.buf(st, "der", [128, NL, 6, KC], F32)
        negb = tr.buf(st, "negb", [8, NL], F32)
        peb = tr.buf(st, "peb", [128, NL, 2, 32], BF16)
        w4k = [tr.buf(st, "w4k%d" % i, [128, KC, 128], BF16) for i in range(4)]
        ps = [tr.buf(st, "ps%d" % i, [128, 512], F32, psum=True) for i in range(7)]
        psb = tr.buf(st, "psb", [128, 1024], BF16, psum=True)
        wslot = [0]

        cst_holder = [None]

        def cv(name, rows=128, sub=None):
            cstb = cst_holder[0]
            o, n = coff[name]
            if sub is None:
                return cstb[0:rows, o:o + n]
            return cstb[0:rows, o + sub[0]:o + sub[1]]

        def load_w(l, ci):
            b = w4k[wslot[0] % 4]
            wslot[0] += 1
            tr.dma('sp', b[:], WC[l][ci], b, True)
            return b

        fm_cols = ([i * 128 for i in range(4)] + [512 + i * 128 for i in range(4)] +
                   [1536 + i * 128 for i in range(4)] + [2048, 2176, 2304, 2560] +
                   [2828 + i * 128 for i in range(8)] + [3852 + i * 128 for i in range(8)])
        v_cols = ([1024 + i * 128 for i in range(4)] + [2432, 2688] + [4876 + i * 128 for i in range(8)])

        def prep_list(l):
            items = []
            for ci, c0 in enumerate(fm_cols):
                items.append((ci, [(w_in[l, :, c0:c0 + 128], 0)], KC))
            for ci, c0 in enumerate(v_cols):
                items.append((32 + ci, [(w_in[l, :, c0:c0 + 128], 0)], KC))
            items.append((46, [(w_in[l, :, 2816:2828], 0), (w_in[l, :, 5900:5908], 12)], KC))
            for oc in range(16):
                items.append((47 + oc, [(w_out[l, :, oc * 128:(oc + 1) * 128], 0)], KC))
            for fc_ in range(64):
                items.append((63 + fc_, [(w_up[l, :, fc_ * 128:(fc_ + 1) * 128], 0)], KC))
            for q in range(4):
                for dc in range(16):
                    items.append((127 + q * 16 + dc, [(w_down[l, q * 2048:(q + 1) * 2048, dc * 128:(dc + 1) * 128], 0)], KC))
            for wi, w1 in enumerate((w1k, w1v)):
                for g in range(2):
                    for hc in range(2):
                        items.append((191 + wi * 4 + g * 2 + hc,
                                      [(w1[l, g * 2048:(g + 1) * 2048, hc * 128:(hc + 1) * 128], 0)], KC))
            items.append((199, [(w2k[l, :, :], 0)], 2))
            items.append((200, [(w2v[l, :, :], 0)], 2))
            return items

        def prep_gen(l, stg, sto, cast_engs, kh=KC):
            nb_ = len(stg)
            steps = []
            for (ci, pieces, nk) in prep_list(l):
                for k0 in range(0, nk, kh):
                    steps.append((ci, pieces, k0, min(nk, k0 + kh)))

            def load(i):
                ci, pieces, k0, k1 = steps[i]
                sg = stg[i % nb_]
                for (src, co) in pieces:
                    ncols = src.shape[1]
                    tr.dma('sp', sg[:, 0:k1 - k0, co:co + ncols], src.rearrange("(k p) c -> p k c", p=128)[:, k0:k1, :], sg, True)

            load(0)
            for i, (ci, pieces, k0, k1) in enumerate(steps):
                if i + 1 < len(steps):
                    load(i + 1)
                sg = stg[i % nb_]
                so = sto[i % nb_]
                nk_ = k1 - k0
                wtot = max(co + src.shape[1] for (src, co) in pieces)
                ce = cast_engs[i % len(cast_engs)]
                if ce == 'act':
                    op('act', lambda e: e.copy(out=so[:, 0:nk_, 0:wtot], in_=sg[:, 0:nk_, 0:wtot]), [sg], [so])
                else:
                    op(ce, lambda e: e.tensor_copy(out=so[:, 0:nk_, 0:wtot], in_=sg[:, 0:nk_, 0:wtot]), [sg], [so])
                tr.dma('pool', WC[l][ci, :, k0:k1, 0:wtot], so[:, 0:nk_, 0:wtot], so, False)
                yield

        def advance(gen, n):
            for _ in range(n):
                try:
                    next(gen)
                except StopIteration:
                    return False
            return True

        with ExitStack() as ph:
            cstf = tr.buf(ph, "cstf", [128, ctot], F32)
            tr.dma('sp', cstf[:], cst_in, cstf, True)
            tr.dma('sp', fcst[:], fcst_in, fcst, True)
            bfg = tr.buf(ph, "bfg", [8, NL], F32)
            tr.dma('sp', bfg[:], negb_in, bfg, True)
            op('dve', lambda e: e.tensor_scalar(out=negb[:], in0=bfg[:], scalar1=-1.0, scalar2=None, op0=ALU.mult), [bfg], [negb])
            half = ctot // 2
            cstb = tr.buf(ph, "cstb0", [128, ctot], BF16)
            op('dve', lambda e: e.tensor_copy(out=cstb[:, 0:half], in_=cstf[:, 0:half]), [cstf], [cstb])
            op('pool', lambda e: e.tensor_copy(out=cstb[:, half:ctot], in_=cstf[:, half:ctot]), [cstf], [cstb])
            tr.dma('pool', CSTB, cstb[:], cstb, False)
            o_id = coff['ident'][0]
            op('dve', lambda e: e.tensor_copy(out=identf[:], in_=cstf[:, o_id:o_id + 128]), [cstf], [identf])
            o_pm = coff['pmt'][0]
            op('dve', lambda e: e.tensor_copy(out=pmtb[:], in_=cstf[:, o_pm:o_pm + 32]), [cstf], [pmtb])
            op('pool', lambda e: e.memset(onecol[:], 1.0), [], [onecol])
            op('pool', lambda e: e.memset(onesb[:], 1.0), [], [onesb])
            op('pool', lambda e: e.memset(onesf[:], 1.0), [], [onesf])
            pef = tr.buf(ph, "pef", [128, NL, 2, 32], F32)
            tr.dma('sp', pef[:], peT_in, pef, True)
            op('dve', lambda e: e.tensor_copy(out=peb[:], in_=pef[:]), [pef], [peb])
            tr.end_phase(ph)
        with ExitStack() as ph:
            CW = 2048
            posi = tr.buf(ph, "posi", [32, CW], I32)
            posf = tr.buf(ph, "posf", [32, CW], F32)
            ru = tr.buf(ph, "ru", [32, CW], F32)
            rni = tr.buf(ph, "rni", [32, CW], I32)
            rnf = tr.buf(ph, "rnf", [32, CW], F32)
            rtab = [tr.buf(ph, "rtab%d" % i, [32, CW], F32) for i in range(2)]
            for cidx in range(S // CW):
                sl = slice(cidx * CW, (cidx + 1) * CW)
                tr.dma('sp', posi[:], pos_in[0:1, sl].to_broadcast([32, CW]), posi, True)
                op('dve', lambda e: e.tensor_copy(out=posf[:], in_=posi[:]), [posi], [posf])
                for ti, (tab, addc) in enumerate(((SIN, 0.0), (COS, 0.25))):
                    op('dve', lambda e: e.tensor_scalar(out=ru[:], in0=posf[:], scalar1=fcst[0:32, 0:1], scalar2=addc,
                                                        op0=ALU.mult, op1=ALU.add), [posf, fcst], [ru])
                    op('dve', lambda e: e.tensor_copy(out=rni[:], in_=ru[:]), [ru], [rni])
                    op('dve', lambda e: e.tensor_copy(out=rnf[:], in_=rni[:]), [rni], [rnf])
                    op('dve', lambda e: e.tensor_tensor(out=ru[:], in0=ru[:], in1=rnf[:], op=ALU.subtract), [ru, rnf], [ru])
                    rt = rtab[ti]
                    op('act', lambda e: e.activation(out=rt[:], in_=ru[:], func=AF.Sin, scale=2.0 * np.pi), [ru], [rt])
                    tr.dma('pool', tab[:, sl], rt[:], rt, False)
            tr.end_phase(ph)
        with ExitStack() as ph:
            gains = tr.buf(ph, "gains", [128, NL, 4, KC], F32)
            tr.dma('sp', gains[:], gT_in, gains, True)
            bmod = tr.buf(ph, "bmod", [128, NL, 96], F32)
            tr.dma('sp', bmod[:], bmodT, bmod, True)
            cT = tr.buf(ph, "cT", [128, KC], F32)
            tr.dma('sp', cT[:], cT_in, cT, True)
            cs = tr.buf(ph, "cs", [128, KC], F32)
            op('act', lambda e: e.activation(out=cs[:], in_=cT[:], func=AF.Silu), [cT], [cs])
            modt = tr.buf(ph, "modt", [128, NL, 96], F32)
            wm = [tr.buf(ph, "wm%d" % i, [128, KC, 128], F32) for i in range(3)]
            stg0 = [tr.buf(ph, "stg%d" % i, [128, KC, 128], F32) for i in range(3)]
            sto0 = [tr.buf(ph, "sto%d" % i, [128, KC, 128], BF16) for i in range(3)]
            pg0 = prep_gen(0, stg0, sto0, ('dve', 'pool', 'act'))
            for l in range(NL):
                pm_ = ps[l % 2]
                for j in range(96):
                    b = wm[j % 3]
                    tr.dma('sp', b[:], w_mod[l, :, j * 128:(j + 1) * 128].rearrange("(k p) c -> p k c", p=128), b, True)
                    for k in range(KC):
                        op('pe', lambda e: e.matmul(pm_[:, j:j + 1], lhsT=b[:, k, :], rhs=cs[:, k:k + 1],
                                                    start=(k == 0), stop=(k == KC - 1)), [b, cs], [pm_])
                    advance(pg0, 1)
                op('dve', lambda e: e.tensor_tensor(out=modt[:, l, :], in0=pm_[:, 0:96], in1=bmod[:, l, :], op=ALU.add),
                   [pm_, bmod], [modt])
                for (di, si, gi) in ((0, 1, 0), (3, 4, 2)):
                    op('dve', lambda e: e.scalar_tensor_tensor(out=der[:, l, di, :], in0=modt[:, l, si * 16:(si + 1) * 16],
                                                               scalar=1.0, in1=gains[:, l, gi, :], op0=ALU.add, op1=ALU.mult),
                       [modt, gains], [der])
                for (di, si) in ((1, 0), (4, 3)):
                    op('dve', lambda e: e.tensor_copy(out=der[:, l, di, :], in_=modt[:, l, si * 16:(si + 1) * 16]), [modt], [der])
                for (di, si, gi) in ((2, 2, 1), (5, 5, 3)):
                    op('dve', lambda e: e.tensor_tensor(out=der[:, l, di, :], in0=modt[:, l, si * 16:(si + 1) * 16],
                                                        in1=gains[:, l, gi, :], op=ALU.mult), [modt, gains], [der])
            while advance(pg0, 8):
                pass
            tr.end_phase(ph)
        for l in range(NL):
            xin = x_in if l == 0 else XS[(l - 1) % 2]
            xout = y_out if l == NL - 1 else XS[l % 2]
            with ExitStack() as ly:
                kmh = tr.buf(ly, "kmh", [128, 4, 32], BF16)
                kml = tr.buf(ly, "kml", [128, 4, 32], BF16)
                CK = tr.buf(ly, "CK", [128, NT, 8], F32)
                KcmpT = tr.buf(ly, "KcmpT", [128, NCP], BF16)
                Vcmp = tr.buf(ly, "Vcmp", [128, nct, 128], BF16)

                def rms_rstd(ph_bufs, sumps):
                    tmpn, rstd = ph_bufs
                    op('dve', lambda e: e.tensor_scalar(out=tmpn[:], in0=sumps[:], scalar1=1.0 / D, scalar2=EPS,
                                                        op0=ALU.mult, op1=ALU.add), [sumps], [tmpn])
                    op('act', lambda e: e.activation(out=tmpn[:], in_=tmpn[:], func=AF.Sqrt), [tmpn], [tmpn])
                    op('dve', lambda e: e.reciprocal(out=rstd[:], in_=tmpn[:]), [tmpn], [rstd])

                with ExitStack() as ph:
                    hT = [tr.buf(ph, "hT%d" % i, [128, KC, 512], BF16) for i in range(2)]
                    xc = [tr.buf(ph, "xc%d" % i, [128, 2, 512], F32) for i in range(3)]
                    sq = [tr.buf(ph, "sq%d" % i, [128, 512], BF16) for i in range(2)]
                    tmpn = tr.buf(ph, "tmpn", [128, 512], F32)
                    rstd = tr.buf(ph, "rstd", [128, 512], F32)
                    tmpx = [tr.buf(ph, "tmpx%d" % i, [128, 512], F32) for i in range(2)]
                    wv = [tr.buf(ph, "wv%d" % i, [128, KC, 512], BF16) for i in range(2)]
                    qk = [tr.buf(ph, "qk%d" % i, [128, 512], BF16) for i in range(3)]
                    ropeA = tr.buf(ph, "ropeA", [32, 512], F32)
                    ropeB = tr.buf(ph, "ropeB", [32, 512], F32)
                    cosb = [tr.buf(ph, "cosb%d" % i, [32, 512], F32) for i in range(2)]
                    sinb = [tr.buf(ph, "sinb%d" % i, [32, 512], F32) for i in range(2)]
                    vout = [tr.buf(ph, "vout%d" % i, [128, 4, 512], BF16) for i in range(2)]
                    gsb = [tr.buf(ph, "gsb%d" % i, [12, 512], BF16) for i in range(2)]
                    fe = tr.buf(ph, "fe", [8, 512], F32)
                    fl = tr.buf(ph, "fl", [8, 512], F32)
                    Cb = [tr.buf(ph, "Cb%d" % i, [8, 512], F32) for i in range(2)]
                    rhob = [tr.buf(ph, "rhob%d" % i, [8, 512], BF16) for i in range(2)]
                    ones8 = tr.buf(ph, "ones8", [8, 512], F32)
                    kmf = tr.buf(ph, "kmf", [128, 4, 32], F32)
                    kmt = tr.buf(ph, "kmt", [128, 4, 32], F32)
                    op('pool', lambda e: e.memset(ones8[:], 1.0), [], [ones8])
                    op('pool', lambda e: e.memset(kmf[:], 0.0), [], [kmf])
                    xcn = [0]
                    qkn = [0]
                    pcn = [0]
                    vgn = [0]
                    roped = set(range(0, 13)) | {14, 15}
                    roped.discard(13)
                    vgroups = [(32, 4, 0), (36, 2, 512), (38, 4, 768), (42, 4, 1280)]
                    def norm_block(b):
                        t0 = b * 512
                        tsl = slice(t0, t0 + 512)
                        h = hT[b % 2]
                        pn = ps[6]
                        for g in range(8):
                            xb = xc[xcn[0] % 3]
                            xcn[0] += 1
                            tr.dma('sp', xb[:], xin[:, 2 * g:2 * g + 2, tsl], xb, True)
                            for c in range(2):
                                s_ = sq[(2 * g + c) % 2]
                                op('act', lambda e: e.activation(out=s_[:], in_=xb[:, c, :], func=AF.Square), [xb], [s_])
                                op('pe', lambda e: e.matmul(pn[:], lhsT=onesb[:], rhs=s_[:], start=(g == 0 and c == 0),
                                                            stop=(g == 7 and c == 1)), [s_, onesb], [pn])
                        rms_rstd((tmpn, rstd), pn)
                        for g in range(8):
                            xb = xc[xcn[0] % 3]
                            xcn[0] += 1
                            tr.dma('sp', xb[:], xin[:, 2 * g:2 * g + 2, tsl], xb, True)
                            for c in range(2):
                                k = 2 * g + c
                                tx = tmpx[k % 2]
                                op('dve', lambda e: e.tensor_tensor(out=tx[:], in0=xb[:, c, :], in1=rstd[:], op=ALU.mult),
                                   [xb, rstd], [tx])
                                op('pool', lambda e: e.tensor_scalar(out=h[:, k, :], in0=tx[:], scalar1=der[:, l, 0, k:k + 1],
                                                                     scalar2=der[:, l, 1, k:k + 1], op0=ALU.mult, op1=ALU.add),
                                   [tx, der], [h])

                    norm_block(0)
                    for b in range(NB):
                        t0 = b * 512
                        tsl = slice(t0, t0 + 512)
                        h = hT[b % 2]
                        if b + 1 < NB:
                            norm_block(b + 1)
                        cb_ = cosb[b % 2]
                        sb_ = sinb[b % 2]
                        tr.dma('sp', cb_[:], COS[:, tsl], cb_, True)
                        tr.dma('sp', sb_[:], SIN[:, tsl], sb_, True)
                        for ci in range(32):
                            w = load_w(l, ci)
                            pp = ps[pcn[0] % 3]
                            pcn[0] += 1
                            for k in range(KC):
                                op('pe', lambda e: e.matmul(pp[:], lhsT=w[:, k, :], rhs=h[:, k, :], start=(k == 0),
                                                            stop=(k == KC - 1)), [w, h], [pp])
                            q_ = qk[qkn[0] % 3]
                            qkn[0] += 1
                            op('act', lambda e: e.copy(out=q_[:], in_=pp[:]), [pp], [q_])
                            if ci in roped:
                                pr = ps[3]
                                op('pe', lambda e: e.matmul(pr[0:32, :], lhsT=pmtb[:], rhs=q_[:], start=True, stop=True),
                                   [q_, pmtb], [pr])
                                op('dve', lambda e: e.tensor_tensor(out=ropeA[:], in0=pr[0:32, :], in1=sb_[:], op=ALU.mult),
                                   [pr, sb_], [ropeA])
                                op('dve', lambda e: e.tensor_tensor(out=ropeB[:], in0=q_[0:32, :], in1=cb_[:], op=ALU.mult),
                                   [q_, cb_], [ropeB])
                                op('dve', lambda e: e.tensor_tensor(out=q_[0:32, :], in0=ropeA[:], in1=ropeB[:], op=ALU.add),
                                   [ropeA, ropeB], [q_])
                            if 4 <= ci < 8:
                                op('dve', lambda e: e.tensor_reduce(out=kmf[:, ci - 4, 2 * b:2 * b + 2],
                                                                    in_=q_[:].rearrange("p (a c) -> p a c", a=2),
                                                                    axis=AX.X, op=ALU.add), [q_], [kmf])
                            tr.dma('pool', FM[ci, :, tsl], q_[:], q_, False)
                        for (c0, nchk, col0) in vgroups:
                            wvb = wv[vgn[0] % 2]
                            vo = vout[vgn[0] % 2]
                            vgn[0] += 1
                            ncols = nchk * 128
                            for i in range(nchk):
                                tr.dma('sp', wvb[:, :, i * 128:(i + 1) * 128], WC[l][c0 + i], wvb, True)
                            for tt in range(4):
                                pp = ps[pcn[0] % 3]
                                pcn[0] += 1
                                for k in range(KC):
                                    op('pe', lambda e: e.matmul(pp[:, 0:ncols], lhsT=h[:, k, tt * 128:(tt + 1) * 128],
                                                                rhs=wvb[:, k, 0:ncols], start=(k == 0), stop=(k == KC - 1)),
                                       [wvb, h], [pp])
                                if tt % 2 == 0:
                                    op('act', lambda e: e.copy(out=vo[:, tt, 0:ncols], in_=pp[:, 0:ncols]), [pp], [vo])
                                else:
                                    op('dve', lambda e: e.tensor_copy(out=vo[:, tt, 0:ncols], in_=pp[:, 0:ncols]), [pp], [vo])
                            for i in range(nchk):
                                tr.dma('pool', VT[col0 // 128 + i, :, 4 * b:4 * b + 4, :], vo[:, :, i * 128:(i + 1) * 128], vo, False)
                        w = load_w(l, 46)
                        pg = ps[4]
                        for k in range(KC):
                            op('pe', lambda e: e.matmul(pg[0:12, :], lhsT=w[:, k, 0:12], rhs=h[:, k, :], start=(k == 0),
                                                        stop=(k == KC - 1)), [w, h], [pg])
                        g_ = gsb[b % 2]
                        op('act', lambda e: e.activation(out=g_[:], in_=pg[0:12, :], func=AF.Sigmoid), [pg], [g_])
                        tr.dma('pool', GT[:, tsl], g_[:], g_, False)
                        pf = ps[5]
                        for k in range(KC):
                            op('pe', lambda e: e.matmul(pf[0:8, :], lhsT=w[:, k, 12:20], rhs=h[:, k, :], start=(k == 0),
                                                        stop=(k == KC - 1)), [w, h], [pf])
                        op('act', lambda e: e.activation(out=fe[:], in_=pf[0:8, :], func=AF.Exp, scale=-1.0,
                                                         bias=negb[:, l:l + 1]), [pf, negb], [fe])
                        op('act', lambda e: e.activation(out=fl[:], in_=fe[:], func=AF.Ln, bias=onecol[0:8, 0:1]),
                           [fe, onecol], [fl])
                        cbuf = Cb[b % 2]
                        cprev = Cb[(b - 1) % 2]
                        if b == 0:
                            op('dve', lambda e: e.tensor_tensor_scan(out=cbuf[:], data0=ones8[:], data1=fl[:], initial=0.0,
                                                                     op0=ALU.mult, op1=ALU.add), [ones8, fl], [cbuf])
                        else:
                            op('dve', lambda e: e.tensor_tensor_scan(out=cbuf[:], data0=ones8[:], data1=fl[:],
                                                                     initial=cprev[:, 511:512], op0=ALU.mult, op1=ALU.add),
                               [ones8, fl, cprev], [cbuf])
                        rb_ = rhob[b % 2]
                        op('dve', lambda e: e.tensor_scalar(out=rb_[:], in0=cbuf[:], scalar1=-1.0 / SCL, scalar2=None,
                                                            op0=ALU.mult), [cbuf], [rb_])
                        tr.dma('pool', RHO[:, tsl], rb_[:], rb_, False)
                        pt_ = ps[3]
                        for i in range(4):
                            op('pe', lambda e: e.transpose(out=pt_[:, 8 * i:8 * i + 8], in_=cbuf[0:8, 128 * i:128 * (i + 1)],
                                                           identity=identf[0:8, 0:8]), [cbuf, identf], [pt_])
                        op('dve', lambda e: e.tensor_copy(out=CK[:, 4 * b:4 * b + 4, :],
                                                          in_=pt_[:, 0:32].rearrange("p (a c) -> p a c", a=4)), [pt_], [CK])
                    op('dve', lambda e: e.tensor_scalar(out=kmf[:], in0=kmf[:], scalar1=1.0 / 256.0, scalar2=None,
                                                        op0=ALU.mult), [kmf], [kmf])
                    op('dve', lambda e: e.tensor_copy(out=kmh[:], in_=kmf[:]), [kmf], [kmh])
                    op('dve', lambda e: e.tensor_copy(out=kmt[:], in_=kmh[:]), [kmh], [kmt])
                    op('dve', lambda e: e.tensor_tensor(out=kmt[:], in0=kmf[:], in1=kmt[:], op=ALU.subtract), [kmf, kmt], [kmt])
                    op('dve', lambda e: e.tensor_copy(out=kml[:], in_=kmt[:]), [kmt], [kml])
                    tr.end_phase(ph)

                with ExitStack() as ph:
                    kcS = tr.buf(ph, "kcS", [128, S], BF16)
                    Z = tr.buf(ph, "Z", [128, 16, S // 16], BF16)
                    W1 = tr.buf(ph, "W1", [128, 32, 256], BF16)
                    W2 = tr.buf(ph, "W2", [128, 2, 128], BF16)
                    bcol = tr.buf(ph, "bcol", [128, 2], F32)
                    xg = tr.buf(ph, "xg", [128, 512], F32)
                    x2 = tr.buf(ph, "x2", [128, 512], F32)
                    sg = tr.buf(ph, "sg", [128, 512], F32)
                    GTt = tr.buf(ph, "GTt", [128, 2, 512], BF16)
                    for wi in range(2):
                        tr.dma('sp', kcS[:], FM[12 + wi], kcS, True)
                        op('pool', lambda e: e.tensor_copy(out=Z[:], in_=kcS[:].rearrange("p (m r) -> p r m", r=16)), [kcS], [Z])
                        for g in range(2):
                            for hc in range(2):
                                tr.dma('sp', W1[:, 16 * g:16 * g + 16, hc * 128:(hc + 1) * 128],
                                       WC[l][191 + wi * 4 + g * 2 + hc], W1, True)
                        tr.dma('sp', W2[:], WC[l][199 + wi, :, 0:2, :], W2, True)
                        op('pool', lambda e: e.memset(GTt[:], 0.0), [], [GTt])
                        for hc in range(2):
                            phd = ps[hc]
                            pbs = ps[2]
                            for tau in range(32):
                                a, r = divmod(tau, 16)
                                op('pe', lambda e: e.matmul(phd[:, 0:ncmp], lhsT=W1[:, tau, hc * 128:(hc + 1) * 128],
                                                            rhs=Z[:, r, a:a + ncmp], start=(tau == 0), stop=(tau == 31)),
                                   [W1, Z], [phd])
                            for tau in range(32):
                                op('pe', lambda e: e.matmul(pbs[:, hc:hc + 1], lhsT=W1[:, tau, hc * 128:(hc + 1) * 128],
                                                            rhs=peb[:, l, wi, tau:tau + 1], start=(tau == 0), stop=(tau == 31)),
                                   [W1, peb], [pbs])
                            op('dve', lambda e: e.tensor_copy(out=bcol[:, hc:hc + 1], in_=pbs[:, hc:hc + 1]), [pbs], [bcol])
                            op('act', lambda e: e.activation(out=xg[:, 0:ncmp], in_=phd[:, 0:ncmp], func=AF.Identity,
                                                             bias=bcol[:, hc:hc + 1]), [phd, bcol], [xg])
                            op('dve', lambda e: e.tensor_tensor(out=x2[:, 0:ncmp], in0=xg[:, 0:ncmp], in1=xg[:, 0:ncmp],
                                                                op=ALU.mult), [xg], [x2])
                            op('dve', lambda e: e.tensor_scalar(out=x2[:, 0:ncmp], in0=x2[:, 0:ncmp], scalar1=0.0713548163,
                                                                scalar2=1.5957691216, op0=ALU.mult, op1=ALU.add), [x2], [x2])
                            op('dve', lambda e: e.tensor_tensor(out=x2[:, 0:ncmp], in0=x2[:, 0:ncmp], in1=xg[:, 0:ncmp],
                                                                op=ALU.mult), [x2, xg], [x2])
                            op('act', lambda e: e.activation(out=sg[:, 0:ncmp], in_=x2[:, 0:ncmp], func=AF.Sigmoid), [x2], [sg])
                            op('dve', lambda e: e.tensor_tensor(out=GTt[:, hc, 0:ncmp], in0=xg[:, 0:ncmp], in1=sg[:, 0:ncmp],
                                                                op=ALU.mult), [xg, sg], [GTt])
                        po = ps[3]
                        if wi == 0:
                            for hc in range(2):
                                op('pe', lambda e: e.matmul(po[:, 0:NCP], lhsT=W2[:, hc, :], rhs=GTt[:, hc, 0:NCP],
                                                            start=(hc == 0), stop=(hc == 1)), [W2, GTt], [po])
                            op('act', lambda e: e.copy(out=KcmpT[:], in_=po[:, 0:NCP]), [po], [KcmpT])
                        else:
                            for ct in range(nct):
                                for hc in range(2):
                                    op('pe', lambda e: e.matmul(po[:, ct * 128:(ct + 1) * 128],
                                                                lhsT=GTt[:, hc, ct * 128:(ct + 1) * 128], rhs=W2[:, hc, :],
                                                                start=(hc == 0), stop=(hc == 1)), [W2, GTt], [po])
                            op('act', lambda e: e.copy(out=Vcmp[:], in_=po[:, 0:NCP].rearrange("p (a c) -> p a c", a=nct)),
                               [po], [Vcmp])
                    tr.end_phase(ph)

                with ExitStack() as ph:
                    QT = [tr.buf(ph, "QT%d" % i, [128, 512], BF16) for i in range(4)]
                    Kc = [tr.buf(ph, "Kc%d" % i, [128, 2048], BF16) for i in range(6)]
                    Vc = [tr.buf(ph, "Vc%d" % i, [128, 16, 128], BF16) for i in range(6)]
                    PT = [tr.buf(ph, "PT%d" % i, [128, 512], BF16) for i in range(6)]
                    ET = tr.buf(ph, "ET", [128, 4, nct, 512], BF16)
                    acc = [tr.buf(ph, "acc%d" % i, [128, 512], F32) for i in range(4)]
                    rec = [tr.buf(ph, "rec%d" % i, [128, 512], F32) for i in range(2)]
                    rgt = [tr.buf(ph, "rgt%d" % i, [128, 512], F32) for i in range(2)]
                    tmc = [tr.buf(ph, "tmc%d" % i, [128, 512], F32) for i in range(2)]
                    rho1 = [tr.buf(ph, "rho1_%d" % i, [128, 512], BF16) for i in range(4)]
                    for r_ in rho1:
                        op('pool', lambda e: e.memset(r_[:], 0.0), [], [r_])
                    tinycol = tr.buf(ph, "tinycol", [128, 1], F32)
                    op('pool', lambda e: e.memset(tinycol[:], TINY), [], [tinycol])
                    E0 = tr.buf(ph, "E0", [128, 128], BF16)
                    op('pool', lambda e: e.memset(E0[:], 0.0), [], [E0])
                    op('pool', lambda e: e.memset(E0[0:1, :], 1.0), [], [E0])
                    cstb = tr.buf(ph, "cstB", [128, ctot], BF16)
                    cst_holder[0] = cstb
                    tr.dma('sp', cstb[:], CSTB, cstb, True)
                    accD = [tr.buf(ph, "accD%d" % i, [128, 512], F32) for i in range(2)]
                    accG = [tr.buf(ph, "accG%d" % i, [128, 512], F32) for i in range(2)]
                    pending = []
                    pgB = iter(())
                    prep_per_job = max(1, -(-NCH // (NB * 28)))
                    gsbB = tr.buf(ph, "gsbB", [12, 512], BF16)
                    Gp = [tr.buf(ph, "Gp%d" % i, [128, 32], F32) for i in range(2)]
                    m8 = [tr.buf(ph, "m8_%d" % i, [128, 8], F32) for i in range(2)]
                    m8b = [tr.buf(ph, "m8b_%d" % i, [128, 8], F32) for i in range(2)]
                    Bq = [tr.buf(ph, "Bq%d" % i, [128, 32], BF16) for i in range(2)]
                    BtM = [tr.buf(ph, "BtM%d" % i, [128, 512], BF16) for i in range(2)]
                    for bt_ in BtM:
                        op('pool', lambda e: e.memset(bt_[:], 0.0), [], [bt_])
                    imp = [tr.buf(ph, "imp%d" % i, [128, n_sb], F32) for i in range(2)]
                    sc2 = tr.buf(ph, "sc2", [128, n_sb], F32)
                    BqN = [tr.buf(ph, "BqN%d" % i, [128, n_sb], BF16) for i in range(2)]
                    BtN = tr.buf(ph, "BtN", [128, 512], BF16)
                    op('pool', lambda e: e.memset(BtN[:], 0.0), [], [BtN])
                    rd = [tr.buf(ph, "rd%d" % i, [128, 1], F32) for i in range(2)]
                    Ost = [tr.buf(ph, "Ost%d" % i, [128, 512], BF16) for i in range(3)]
                    psL = [ps[0], ps[1], ps[4]]
                    psN = [ps[2], ps[3]]
                    psD = [ps[5], ps[5]]
                    psX = ps[6]
                    cnt = dict(q=0, kv=0, pt=0, job=0, rho=0, ost=0, u=0, x=0)

                    def load_q(ci, tsl):
                        q_ = QT[cnt['q'] % 4]
                        cnt['q'] += 1
                        tr.dma('sp', q_[:], FM[ci, :, tsl], q_, True)
                        return q_

                    def load_kv(kci, vcol, j0, j1):
                        res = {}
                        ck0, ck1 = j0 // 16, j1 // 16
                        for ck in range(ck0, ck1 + 1):
                            ja = max(j0, ck * 16)
                            jb = min(j1, ck * 16 + 15)
                            kb = Kc[cnt['kv'] % 6]
                            vb = Vc[cnt['kv'] % 6]
                            cnt['kv'] += 1
                            n = jb - ja + 1
                            tr.dma('sp', kb[:, 0:n * 128], FM[kci, :, ja * 128:(jb + 1) * 128], kb, True)
                            tr.dma('sp', vb[:, 0:n, :], VT[vcol // 128, :, ja:jb + 1, :], vb, True)
                            for j in range(ja, jb + 1):
                                res[j] = (kb[:, (j - ja) * 128:(j - ja + 1) * 128], kb, vb[:, j - ja, :], vb)
                        return res

                    def step_pending():
                        while pending:
                            try:
                                next(pending[0])
                                return
                            except StopIteration:
                                pending.pop(0)

                    def flush_pending():
                        while pending:
                            for _ in pending[0]:
                                pass
                            pending.pop(0)

                    def attn_job(q_, units, finalize):
                        jn = cnt['job']
                        cnt['job'] += 1
                        pN = psN[jn % 2]
                        pD = psD[jn % 2]
                        aD = accD[jn % 2]
                        aG = accG[jn % 2]
                        n = len(units)
                        pts = [None] * n
                        def full(i_):
                            return i_ < n and units[i_].get('f0', 0) == 0 and units[i_].get('f1', 512) == 512
                        copyD = full(0)
                        copyG = full(1)
                        if not copyD:
                            op('pool', lambda e: e.memset(aD[:], 0.0), [], [aD])
                        if not copyG:
                            op('pool', lambda e: e.memset(aG[:], 0.0), [], [aG])

                        def emit_S(i):
                            u = units[i]
                            f0, f1 = u.get('f0', 0), u.get('f1', 512)
                            pl = psL[cnt['pt'] % 3]
                            ex = u.get('extras', [])
                            op('pe', lambda e: e.matmul(pl[:, f0:f1], lhsT=u['k'], rhs=q_[:, f0:f1], start=True,
                                                        stop=(len(ex) == 0)), [u['kb'], q_], [pl])
                            for xi, (la, ra, bl) in enumerate(ex):
                                op('pe', lambda e: e.matmul(pl[:, f0:f1], lhsT=la, rhs=ra, start=False,
                                                            stop=(xi == len(ex) - 1)), bl, [pl])
                            if 'pt' in u:
                                pap, pbuf = u['pt']
                            else:
                                pbuf = PT[cnt['pt'] % 6]
                                pap = pbuf[:]
                            cnt['pt'] += 1
                            if u.get('bias') is not None:
                                bap, bbuf = u['bias']
                                op('act', lambda e: e.activation(out=pap[:, f0:f1], in_=pl[:, f0:f1], func=AF.Exp, scale=SCL,
                                                                 bias=bap), [pl, bbuf], [pbuf])
                            else:
                                op('act', lambda e: e.activation(out=pap[:, f0:f1], in_=pl[:, f0:f1], func=AF.Exp, scale=SCL),
                                   [pl], [pbuf])
                            pts[i] = (pap, pbuf)

                        def emit_PV(i):
                            u = units[i]
                            f0, f1 = u.get('f0', 0), u.get('f1', 512)
                            pap, pbuf = pts[i]
                            op('pe', lambda e: e.matmul(pN[:, f0:f1], lhsT=u['v'], rhs=pap[:, f0:f1], start=(i == 0),
                                                        stop=(i == n - 1)), [u['vb'], pbuf], [pN])
                            if i == 0 and copyD:
                                op('dve', lambda e: e.tensor_copy(out=aD[:], in_=pap[:]), [pbuf], [aD])
                            elif i == 1 and copyG:
                                op('pool', lambda e: e.tensor_copy(out=aG[:], in_=pap[:]), [pbuf], [aG])
                            elif i % 2 == 0:
                                op('dve', lambda e: e.tensor_tensor(out=aD[:, f0:f1], in0=aD[:, f0:f1], in1=pap[:, f0:f1],
                                                                    op=ALU.add), [aD, pbuf], [aD])
                            else:
                                op('pool', lambda e: e.tensor_tensor(out=aG[:, f0:f1], in0=aG[:, f0:f1], in1=pap[:, f0:f1],
                                                                     op=ALU.add), [aG, pbuf], [aG])

                        LAG = 2
                        for i in range(n + LAG):
                            if i < n:
                                emit_S(i)
                            if i >= LAG:
                                emit_PV(i - LAG)
                                if i >= LAG + 1:
                                    step_pending()
                        flush_pending()

                        def fin():
                            op('dve', lambda e: e.tensor_tensor(out=aD[:], in0=aD[:], in1=aG[:], op=ALU.add), [aD, aG], [aD])
                            yield
                            op('pe', lambda e: e.matmul(pD[:], lhsT=onesf[:], rhs=aD[:], start=True, stop=True),
                               [onesf, aD], [pD])
                            yield
                            for _ in finalize(pN, pD):
                                yield
                        pending.append(fin())
                        advance(pgB, prep_per_job)

                    def recip_den(pD):
                        r_ = rec[cnt['x'] % 2]
                        cnt['x'] += 1
                        op('act', lambda e: e.activation(out=r_[:], in_=pD[:], func=AF.Ln, bias=tinycol[:, 0:1]),
                           [pD, tinycol], [r_])
                        yield r_
                        op('act', lambda e: e.activation(out=r_[:], in_=r_[:], func=AF.Exp, scale=-1.0), [r_], [r_])
                        yield r_

                    def fin_plain(hc, tsl):
                        def f(pN, pD):
                            r_ = None
                            for r_ in recip_den(pD):
                                yield
                            o_ = Ost[cnt['ost'] % 3]
                            cnt['ost'] += 1
                            op('dve', lambda e: e.tensor_tensor(out=o_[:], in0=pN[:], in1=r_[:], op=ALU.mult), [pN, r_], [o_])
                            tr.dma('pool', OTD[hc, :, tsl], o_[:], o_, False)
                            yield
                        return f

                    def fin_nsa(hh, br, tsl):
                        def f(pN, pD):
                            r_ = None
                            for r_ in recip_den(pD):
                                yield
                            rr = 3 * hh + br
                            op('pe', lambda e: e.matmul(psX[:], lhsT=cv('eg', 12, (rr * 128, (rr + 1) * 128)), rhs=gsbB[:],
                                                        start=True, stop=True), [cstb, gsbB], [psX])
                            g_ = rgt[cnt['x'] % 2]
                            op('dve', lambda e: e.tensor_tensor(out=g_[:], in0=psX[:], in1=r_[:], op=ALU.mult), [psX, r_], [g_])
                            yield
                            a_ = acc[hh]
                            if br == 0:
                                op('dve', lambda e: e.tensor_tensor(out=a_[:], in0=pN[:], in1=g_[:], op=ALU.mult), [pN, g_], [a_])
                            else:
                                t_ = tmc[cnt['x'] % 2]
                                op('dve', lambda e: e.tensor_tensor(out=t_[:], in0=pN[:], in1=g_[:], op=ALU.mult), [pN, g_], [t_])
                                op('pool', lambda e: e.tensor_tensor(out=a_[:], in0=a_[:], in1=t_[:], op=ALU.add), [a_, t_], [a_])
                            yield
                            if br == 2:
                                o_ = Ost[cnt['ost'] % 3]
                                cnt['ost'] += 1
                                op('pool', lambda e: e.tensor_copy(out=o_[:], in_=a_[:]), [a_], [o_])
                                tr.dma('pool', OTD[4 + hh, :, tsl], o_[:], o_, False)
                                yield
                        return f

                    causal = cv('causal')

                    def diag_extra(jd):
                        f0 = 128 * jd
                        return (cv('i30k'), cv('causal', 128, (0, 512 - f0)), [cstb]), f0

                    for b in range(NB):
                        t0 = b * 512
                        tsl = slice(t0, t0 + 512)
                        jlast = 4 * b + 3
                        for hh in range(8):
                            q_ = load_q(16 + hh, tsl)
                            r1 = rho1[cnt['rho'] % 4]
                            cnt['rho'] += 1
                            tr.dma('sp', r1[0:1, :], RHO[hh:hh + 1, tsl], r1, True)
                            kv = load_kv(24 + hh, 768 + 128 * hh, 0, jlast)
                            units = []
                            for j in range(jlast + 1):
                                ka, kb, va, vb = kv[j]
                                u = dict(k=ka, kb=kb, v=va, vb=vb, bias=(CK[:, j, hh:hh + 1], CK))
                                f0 = 0
                                ex = []
                                if j >= 4 * b:
                                    dx, f0 = diag_extra(j - 4 * b)
                                    ex.append(dx)
                                ex.insert(0, (E0[:], r1[:, f0:512], [E0, r1]))
                                u['extras'] = ex
                                u['f0'] = f0
                                units.append(u)
                            attn_job(q_, units, fin_plain(8 + hh, tsl))
                        for hh in range(4):
                            q_ = load_q(hh, tsl)
                            bt = BtM[hh % 2]
                            for i in range(4):
                                own = 2 * b + i // 2
                                gp = Gp[i % 2]
                                mm = m8[i % 2]
                                bq = Bq[i % 2]
                                op('pe', lambda e: e.matmul(psX[:, 0:32], lhsT=q_[:, 128 * i:128 * (i + 1)], rhs=kmh[:, hh, :],
                                                            start=True, stop=False), [q_, kmh], [psX])
                                op('pe', lambda e: e.matmul(psX[:, 0:32], lhsT=q_[:, 128 * i:128 * (i + 1)], rhs=kml[:, hh, :],
                                                            start=False, stop=True), [q_, kml], [psX])
                                op('pool', lambda e: e.memset(gp[:], -1e30), [], [gp])
                                if own > 0:
                                    op('dve', lambda e: e.tensor_copy(out=gp[:, 0:own], in_=psX[:, 0:own]), [psX], [gp])
                                op('dve', lambda e: e.max(out=mm[:], in_=gp[:]), [gp], [mm])
                                op('dve', lambda e: e.tensor_scalar(out=mm[:, 2:3], in0=mm[:, 2:3], scalar1=-5e29, scalar2=None,
                                                                    op0=ALU.max), [mm], [mm])
                                op('dve', lambda e: e.tensor_scalar(out=bq[:], in0=gp[:], scalar1=mm[:, 2:3], scalar2=1.0,
                                                                    op0=ALU.is_ge, op1=ALU.subtract), [gp, mm], [bq])
                                op('pool', lambda e: e.memset(bq[:, own:own + 1], 0.0), [], [bq])
                                if own + 1 < 32:
                                    op('pool', lambda e: e.memset(bq[:, own + 1:32], -1.0), [], [bq])
                                op('pe', lambda e: e.transpose(out=psb[0:32, 128 * i:128 * (i + 1)], in_=bq[:],
                                                               identity=cv('ident')), [bq, cstb], [psb])
                            op('dve', lambda e: e.tensor_copy(out=bt[0:32, :], in_=psb[0:32, 0:512]), [psb], [bt])
                            kv = load_kv(4 + hh, 128 * hh, 0, jlast)
                            units = []
                            for j in range(jlast + 1):
                                ka, kb, va, vb = kv[j]
                                u = dict(k=ka, kb=kb, v=va, vb=vb)
                                f0 = 0
                                ex = []
                                if j >= 4 * b:
                                    dx, f0 = diag_extra(j - 4 * b)
                                    ex.append(dx)
                                nblk = j // 2
                                ex.insert(0, (cv('am', 128, (nblk * 128, (nblk + 1) * 128)), bt[:, f0:512], [cstb, bt]))
                                u['extras'] = ex
                                u['f0'] = f0
                                units.append(u)
                            attn_job(q_, units, fin_plain(hh, tsl))
                        tr.dma('sp', gsbB[:], GT[:, tsl], gsbB, True)
                        jcs = [jc for jc in range(nct) if b - 4 * jc >= 0]
                        for hh in range(4):
                            q_ = load_q(8 + hh, tsl)
                            units = []
                            for jc in jcs:
                                dlt = b - 4 * jc
                                u = dict(k=KcmpT[:, jc * 128:(jc + 1) * 128], kb=KcmpT, v=Vcmp[:, jc, :], vb=Vcmp,
                                         pt=(ET[:, hh, jc, :], ET))
                                if dlt <= 4:
                                    u['extras'] = [(cv('i30k'), cv('cmask', 128, (dlt * 512, (dlt + 1) * 512)), [cstb])]
                                units.append(u)
                            attn_job(q_, units, fin_nsa(hh, 0, tsl))
                        for i in range(4):
                            im = imp[i % 2]
                            for hh in range(4):
                                pu = psL[cnt['u'] % 3]
                                cnt['u'] += 1
                                for xi, jc in enumerate(jcs):
                                    op('pe', lambda e: e.matmul(pu[:, 0:n_sb + 1], lhsT=ET[:, hh, jc, 128 * i:128 * (i + 1)],
                                                                rhs=cv('ov', 128, (jc * (n_sb + 1), (jc + 1) * (n_sb + 1))),
                                                                start=(xi == 0), stop=(xi == len(jcs) - 1)), [ET, cstb], [pu])
                                rd_ = rd[hh % 2]
                                op('dve', lambda e: e.tensor_scalar(out=rd_[:], in0=pu[:, n_sb:n_sb + 1], scalar1=TINY,
                                                                    scalar2=None, op0=ALU.max), [pu], [rd_])
                                op('dve', lambda e: e.reciprocal(out=rd_[:], in_=rd_[:]), [rd_], [rd_])
                                if hh == 0:
                                    op('dve', lambda e: e.tensor_scalar(out=im[:], in0=pu[:, 0:n_sb], scalar1=rd_[:, 0:1],
                                                                        scalar2=None, op0=ALU.mult), [pu, rd_], [im])
                                else:
                                    op('dve', lambda e: e.scalar_tensor_tensor(out=im[:], in0=pu[:, 0:n_sb], scalar=rd_[:, 0:1],
                                                                               in1=im[:], op0=ALU.mult, op1=ALU.add),
                                       [pu, rd_, im], [im])
                            for hf in range(2):
                                cur = 8 * b + 2 * i + hf
                                rows = slice(64 * hf, 64 * hf + 64)
                                if cur - 1 >= 0:
                                    op('pool', lambda e: e.memset(im[rows, cur - 1:cur], 2e30), [], [im])
                                op('pool', lambda e: e.memset(im[rows, cur:cur + 1], 1e30), [], [im])
                                if cur + 1 < n_sb:
                                    op('pool', lambda e: e.memset(im[rows, cur + 1:n_sb], -2e30), [], [im])
                            op('pool', lambda e: e.memset(im[:, 0:1], 3e30), [], [im])
                            ma = m8[i % 2]
                            mb = m8b[i % 2]
                            bqn = BqN[i % 2]
                            op('dve', lambda e: e.max(out=ma[:], in_=im[:]), [im], [ma])
                            op('dve', lambda e: e.match_replace(out=sc2[:], in_to_replace=ma[:], in_values=im[:],
                                                                imm_value=-2e30), [ma, im], [sc2])
                            op('dve', lambda e: e.max(out=mb[:], in_=sc2[:]), [sc2], [mb])
                            op('dve', lambda e: e.tensor_scalar(out=mb[:, 7:8], in0=mb[:, 7:8], scalar1=-1e30, scalar2=None,
                                                                op0=ALU.max), [mb], [mb])
                            op('dve', lambda e: e.tensor_scalar(out=bqn[:], in0=im[:], scalar1=mb[:, 7:8], scalar2=1.0,
                                                                op0=ALU.is_ge, op1=ALU.subtract), [im, mb], [bqn])
                            op('pe', lambda e: e.transpose(out=psb[0:n_sb, 128 * i:128 * (i + 1)], in_=bqn[:],
                                                           identity=cv('ident')), [bqn, cstb], [psb])
                        op('dve', lambda e: e.tensor_copy(out=BtN[0:n_sb, :], in_=psb[0:n_sb, 0:512]), [psb], [BtN])
                        for hh in range(4):
                            q_ = load_q(8 + hh, tsl)
                            kv = load_kv(14, 512, 0, jlast)
                            units = []
                            for j in range(jlast + 1):
                                ka, kb, va, vb = kv[j]
                                u = dict(k=ka, kb=kb, v=va, vb=vb)
                                f0 = 0
                                ex = []
                                if j >= 4 * b:
                                    dx, f0 = diag_extra(j - 4 * b)
                                    ex.append(dx)
                                ex.insert(0, (cv('an', 128, (j * 128, (j + 1) * 128)), BtN[:, f0:512], [cstb, BtN]))
                                u['extras'] = ex
                                u['f0'] = f0
                                units.append(u)
                            attn_job(q_, units, fin_nsa(hh, 1, tsl))
                        for hh in range(4):
                            q_ = load_q(8 + hh, tsl)
                            jfirst = max(0, 4 * b - 4)
                            kv = load_kv(15, 640, jfirst, jlast)
                            units = []
                            for j in range(jfirst, jlast + 1):
                                ka, kb, va, vb = kv[j]
                                u = dict(k=ka, kb=kb, v=va, vb=vb)
                                if j >= 4 * b:
                                    dx, f0 = diag_extra(j - 4 * b)
                                    u['extras'] = [dx]
                                    u['f0'] = f0
                                else:
                                    jl = j - (4 * b - 4)
                                    u['f0'] = 0
                                    u['f1'] = 128 * (jl + 1)
                                    u['extras'] = [(cv('i30k'), cv('w2', 128, (384 - 128 * jl, 512)), [cstb])]
                                units.append(u)
                            attn_job(q_, units, fin_nsa(hh, 2, tsl))
                    flush_pending()
                    while advance(pgB, 8):
                        pass
                    tr.end_phase(ph)

                with ExitStack() as ph:
                    hT = [tr.buf(ph, "hT%d" % i, [128, KC, 512], BF16) for i in range(2)]
                    x1 = tr.buf(ph, "x1", [128, KC, 512], F32)
                    yT = tr.buf(ph, "yT", [128, KC, 512], F32)
                    OTs = tr.buf(ph, "OTs", [128, KC, 512], BF16)
                    sq = [tr.buf(ph, "sq%d" % i, [128, 512], BF16) for i in range(2)]
                    tmpn = tr.buf(ph, "tmpn", [128, 512], F32)
                    rstd = tr.buf(ph, "rstd", [128, 512], F32)
                    tmpx = [tr.buf(ph, "tmpx%d" % i, [128, 512], F32) for i in range(2)]
                    tmpu = [tr.buf(ph, "tmpu%d" % i, [128, 512], F32) for i in range(2)]
                    pcn = [0]
                    h2 = hT[0]
                    aT = hT[1]
                    if l + 1 < NL:
                        stgC = [tr.buf(ph, "stgC%d" % i, [128, 8, 128], F32) for i in range(2)]
                        stoC = [tr.buf(ph, "stoC%d" % i, [128, 8, 128], BF16) for i in range(2)]
                        pgC = prep_gen(l + 1, stgC, stoC, ('pool',), kh=8)
                    else:
                        pgC = iter(())
                    prep_per_gemm = -(-(2 * NCH) // (NB * 9))

                    def gemm16(l_, c0, src, outfn):
                        for oc in range(16):
                            w = load_w(l_, c0 + oc)
                            pp = ps[pcn[0] % 3]
                            pcn[0] += 1
                            for k in range(KC):
                                op('pe', lambda e: e.matmul(pp[:], lhsT=w[:, k, :], rhs=src[:, k, :], start=(k == 0),
                                                            stop=(k == KC - 1)), [w, src], [pp])
                            outfn(oc, pp)
                        advance(pgC, prep_per_gemm)

                    def sumsq(src, pn):
                        for k in range(KC):
                            s_ = sq[k % 2]
                            op('act', lambda e: e.activation(out=s_[:], in_=src[:, k, :], func=AF.Square), [src], [s_])
                            op('pe', lambda e: e.matmul(pn[:], lhsT=onesb[:], rhs=s_[:], start=(k == 0), stop=(k == KC - 1)),
                               [s_, onesb], [pn])

                    def resid(di):
                        for k in range(KC):
                            tx = tmpx[k % 2]
                            op('dve', lambda e: e.tensor_tensor(out=tx[:], in0=yT[:, k, :], in1=rstd[:], op=ALU.mult),
                               [yT, rstd], [tx])
                            op('dve', lambda e: e.scalar_tensor_tensor(out=x1[:, k, :], in0=tx[:], scalar=der[:, l, di, k:k + 1],
                                                                       in1=x1[:, k, :], op0=ALU.mult, op1=ALU.add),
                               [tx, der, x1], [x1])

                    for b in range(NB):
                        t0 = b * 512
                        tsl = slice(t0, t0 + 512)
                        tr.dma('sp', OTs[:], OTD[:, :, tsl].rearrange("c p t -> p c t"), OTs, True)
                        tr.dma('sp', x1[:], xin[:, :, tsl], x1, True)
                        pn = ps[6]

                        def out_c(oc, pp):
                            op('act', lambda e: e.copy(out=yT[:, oc, :], in_=pp[:]), [pp], [yT])
                        gemm16(l, 47, OTs, out_c)
                        sumsq(yT, pn)
                        rms_rstd((tmpn, rstd), pn)
                        resid(2)
                        sumsq(x1, pn)
                        rms_rstd((tmpn, rstd), pn)
                        for k in range(KC):
                            tx = tmpx[k % 2]
                            op('dve', lambda e: e.tensor_tensor(out=tx[:], in0=x1[:, k, :], in1=rstd[:], op=ALU.mult),
                               [x1, rstd], [tx])
                            op('pool', lambda e: e.tensor_scalar(out=h2[:, k, :], in0=tx[:], scalar1=der[:, l, 3, k:k + 1],
                                                                 scalar2=der[:, l, 4, k:k + 1], op0=ALU.mult, op1=ALU.add),
                               [tx, der], [h2])
                        for q in range(4):
                            def out_u(fc_, pp):
                                tu = tmpu[fc_ % 2]
                                op('act', lambda e: e.activation(out=tu[:], in_=pp[:], func=AF.Relu), [pp], [tu])
                                op('dve', lambda e: e.tensor_tensor(out=aT[:, fc_, :], in0=tu[:], in1=tu[:], op=ALU.mult),
                                   [tu], [aT])
                            gemm16(l, 63 + 16 * q, h2, out_u)

                            def out_d(dc, pp):
                                if q == 0:
                                    op('act', lambda e: e.copy(out=yT[:, dc, :], in_=pp[:]), [pp], [yT])
                                else:
                                    op('dve', lambda e: e.tensor_tensor(out=yT[:, dc, :], in0=pp[:], in1=yT[:, dc, :],
                                                                        op=ALU.add), [pp, yT], [yT])
                            gemm16(l, 127 + 16 * q, aT, out_d)
                        sumsq(yT, pn)
                        rms_rstd((tmpn, rstd), pn)
                        resid(5)
                        tr.dma('pool', xout[:, :, tsl], x1[:], x1, False)
                    while advance(pgC, 8):
                        pass
                    tr.end_phase(ph)
        tr.barrier()
        ninst = tr.ninst
    return nc, ninst


def fm_layout(v):
    v = np.asarray(v, np.float32)
    lead = v.shape[:-1]
    return np.ascontiguousarray(np.moveaxis(v.reshape(lead + (v.shape[-1] // 128, 128)), -1, 0))


def make_in_maps(inp, S, NL):
    f = lambda a: np.ascontiguousarray(np.asarray(a, np.float32))
    B = inp['x'].shape[0]
    cst, fcst = make_consts(S)
    gains = np.stack([np.asarray(inp[k], np.float32)[:NL] for k in ('g_pre_mix', 'g_post_mix', 'g_pre_mlp', 'g_post_mlp')], axis=1)
    gains = fm_layout(gains)
    bmodT = fm_layout(np.asarray(inp['b_mod'], np.float32)[:NL])
    negb = np.ascontiguousarray(np.asarray(inp['b_forget'], np.float32)[:NL].T)
    peT = np.stack([np.asarray(inp['cmp_pe_k'], np.float32)[:NL], np.asarray(inp['cmp_pe_v'], np.float32)[:NL]], axis=1)
    peT = np.ascontiguousarray(peT.transpose(3, 0, 1, 2))
    shared = dict(
        w_mod=f(inp['w_mod'][:NL]), bmodT=bmodT, gains=gains, w_in=f(inp['w_in'][:NL]), negb=negb, peT=peT,
        w1k=f(inp['cmp_w1_k'][:NL]), w2k=f(inp['cmp_w2_k'][:NL]), w1v=f(inp['cmp_w1_v'][:NL]), w2v=f(inp['cmp_w2_v'][:NL]),
        w_out=f(inp['w_out'][:NL]), w_up=f(inp['w_up'][:NL]), w_down=f(inp['w_down'][:NL]), cst=cst, fcst=fcst)
    maps = []
    for b in range(B):
        xb = np.asarray(inp['x'][b], np.float32)[:S]
        xT = np.ascontiguousarray(xb.T.reshape(KC, 128, S).transpose(1, 0, 2))
        m = dict(shared)
        m['xT'] = xT
        m['pos'] = np.ascontiguousarray(np.asarray(inp['positions'][b], np.int32)[None, :S])
        m['cT'] = fm_layout(np.asarray(inp['c'][b], np.float32))
        maps.append(m)
    return maps


def run(inp, S, NL):
    nc, ninst = build_program(S, NL)
    maps = make_in_maps(inp, S, NL)
    res = run_bass_kernel_spmd(nc, maps, core_ids=list(range(len(maps))))
    outs = []
    for r in res.results:
        yT = np.asarray(r['yT'], np.float32)
        outs.append(yT.transpose(2, 1, 0).reshape(S, D))
    return np.stack(outs, axis=0)


def kernel(**inputs):
    return run(inputs, 8192, 4)
```
